# Optimizing a Trainium2 kernel written in Bass

```python
import jax, jax.numpy as jnp
from jax import lax
import numpy as np

D_MODEL = 1024
BATCH = 1
SEQ = 16384
DEPTH = 1

RET_HEADS = 8
RET_DK = 64
RET_DV = 128
RET_CHUNK = 128
ROPE_BASE = 10000.0
SB_HEADS = 8
SB_DH = 64
SB_BLOCK = 128
PEER_HEADS = 8
N_KEYS = 128
N_EXPERTS = N_KEYS * N_KEYS
PEER_TOPK = 16
PEER_DKEY = 256
PEER_CHUNK = 128
EPS = 1e-6

RET_QK = RET_HEADS * RET_DK
RET_V = RET_HEADS * RET_DV
SB_W = SB_HEADS * SB_DH
IN_SIZES = (RET_QK, RET_QK, RET_V, RET_V, SB_W, SB_W, SB_W, D_MODEL, D_MODEL)
IN_WIDTH = sum(IN_SIZES)

kernel_name = 'hybrid_retention_stickbreak_peer'


def rmsnorm(x, g):
    xf = x.astype(jnp.float32)
    y = xf * lax.rsqrt(jnp.mean(xf * xf, axis=-1, keepdims=True) + EPS)
    return (y * g.astype(jnp.float32)).astype(x.dtype)


def group_norm(y, g):
    yf = y.astype(jnp.float32)
    mu = jnp.mean(yf, axis=-1, keepdims=True)
    var = jnp.mean(jnp.square(yf - mu), axis=-1, keepdims=True)
    return (yf - mu) * lax.rsqrt(var + EPS) * g.astype(jnp.float32)


def rope(x, pos):
    half = x.shape[-1] // 2
    freqs = ROPE_BASE ** (-jnp.arange(half, dtype=jnp.float32) / half)
    ang = pos[:, None] * freqs[None, :]
    cos = jnp.cos(ang)[:, None, :]
    sin = jnp.sin(ang)[:, None, :]
    xf = x.astype(jnp.float32)
    x1, x2 = xf[..., :half], xf[..., half:]
    return jnp.concatenate([x1 * cos - x2 * sin, x1 * sin + x2 * cos], axis=-1)


def retention(q, k, v):
    B, S, H, dk = q.shape
    dv = v.shape[-1]
    C = RET_CHUNK
    n = S // C
    log_g = jnp.log1p(-jnp.exp2(-5.0 - jnp.arange(H, dtype=jnp.float32)))
    i = jnp.arange(C, dtype=jnp.float32)
    diff = i[:, None] - i[None, :]
    dmask = jnp.where(diff >= 0, jnp.exp(log_g[:, None, None] * jnp.maximum(diff, 0.0)), 0.0)
    q_dec = jnp.exp(log_g[:, None] * (i + 1.0)).T[None, :, :, None]
    k_dec = jnp.exp(log_g[:, None] * (C - 1.0 - i)).T[None, :, :, None]
    c_dec = jnp.exp(log_g * C)[None, :, None, None]

    def chunks(t):
        return jnp.moveaxis(t.reshape(B, n, C, H, t.shape[-1]), 1, 0)

    def step(state, xs):
        qc, kc, vc = xs
        scores = jnp.einsum('bihd,bjhd->bhij', qc, kc) * dmask[None]
        y = jnp.einsum('bhij,bjhe->bihe', scores, vc)
        y = y + jnp.einsum('bihd,bhde->bihe', qc, state) * q_dec
        state = state * c_dec + jnp.einsum('bjhd,bjhe->bhde', kc * k_dec, vc)
        return state, y

    state0 = jnp.zeros((B, H, dk, dv), jnp.float32)
    _, ys = lax.scan(step, state0, (chunks(q), chunks(k), chunks(v)))
    return jnp.moveaxis(ys, 0, 1).reshape(B, S, H, dv)


def stick_breaking(q, k, v):
    B, S, H, d = q.shape
    n = S // SB_BLOCK
    scale = SB_DH ** -0.5
    qh = jnp.transpose(q, (0, 2, 1, 3))
    kh = jnp.transpose(k, (0, 2, 1, 3))
    vh = jnp.transpose(v, (0, 2, 1, 3))
    qb = jnp.moveaxis(qh.reshape(B, H, n, SB_BLOCK, d), 2, 0)
    kpos = jnp.arange(S)

    def block(args):
        qi, bi = args
        z = jnp.einsum('bhtd,bhsd->bhts', qi, kh).astype(jnp.float32) * scale
        tpos = bi * SB_BLOCK + jnp.arange(SB_BLOCK)
        mask = kpos[None, :] < tpos[:, None]
        log_stay = jnp.where(mask, jax.nn.log_sigmoid(-z), 0.0)
        later = lax.cumsum(log_stay, axis=3, reverse=True) - log_stay
        w = jnp.where(mask, jnp.exp(jax.nn.log_sigmoid(z) + later), 0.0)
        return jnp.einsum('bhts,bhsd->bhtd', w.astype(vh.dtype), vh)

    out = lax.map(block, (qb, jnp.arange(n)))
    out = jnp.moveaxis(out, 0, 2).reshape(B, H, S, d)
    return jnp.transpose(out, (0, 2, 1, 3)).reshape(B, S, H * d)


def peer(x, w_q, sub_keys_1, sub_keys_2, u_tab, v_tab):
    B, S, D = x.shape
    T = PEER_CHUNK
    n = S // T
    xc = jnp.moveaxis(x.reshape(B, n, T, D), 1, 0)
    k1 = sub_keys_1.astype(jnp.float32)
    k2 = sub_keys_2.astype(jnp.float32)

    def block(xb):
        q = (xb @ w_q).reshape(B, T, PEER_HEADS, 2, PEER_DKEY // 2).astype(jnp.float32)
        s1 = jnp.einsum('bthd,nd->bthn', q[..., 0, :], k1)
        s2 = jnp.einsum('bthd,nd->bthn', q[..., 1, :], k2)
        v1, i1 = lax.top_k(s1, PEER_TOPK)
        v2, i2 = lax.top_k(s2, PEER_TOPK)
        cand_s = (v1[..., :, None] + v2[..., None, :]).reshape(B, T, PEER_HEADS, PEER_TOPK * PEER_TOPK)
        cand_i = (i1[..., :, None] * N_KEYS + i2[..., None, :]).reshape(B, T, PEER_HEADS, PEER_TOPK * PEER_TOPK)
        top_s, pos = lax.top_k(cand_s, PEER_TOPK)
        idx = jnp.take_along_axis(cand_i, pos, axis=-1)
        g = jax.nn.softmax(top_s, axis=-1)
        u_sel = jnp.take(u_tab, idx, axis=0)
        h = jnp.einsum('bthkd,btd->bthk', u_sel, xb)
        a = (jax.nn.gelu(h.astype(jnp.float32)) * g).astype(xb.dtype)
        return jnp.einsum('bthk,bthkd->btd', a, jnp.take(v_tab, idx, axis=0))

    y = lax.map(block, xc)
    return jnp.moveaxis(y, 0, 1).reshape(B, S, D)


def setup_inputs(seed: int = 0) -> dict:
    key = jax.random.key(seed)
    ks = jax.random.split(key, 17)
    f = jnp.float32
    nrm = lambda k, shape, s: jax.random.normal(k, shape, f) * s
    gain = lambda k, shape: 1.0 + 0.02 * jax.random.normal(k, shape, f)
    return {
        'x': jax.random.normal(ks[0], (BATCH, SEQ, D_MODEL), f),
        'norm_attn': gain(ks[1], (DEPTH, D_MODEL)),
        'w_in': nrm(ks[2], (DEPTH, D_MODEL, IN_WIDTH), D_MODEL ** -0.5),
        'ret_q_norm': gain(ks[3], (DEPTH, RET_DK)),
        'ret_k_norm': gain(ks[4], (DEPTH, RET_DK)),
        'ret_group_norm': gain(ks[5], (DEPTH, RET_V)),
        'sb_q_norm': gain(ks[6], (DEPTH, SB_DH)),
        'sb_k_norm': gain(ks[7], (DEPTH, SB_DH)),
        'w_branch_ret': nrm(ks[8], (DEPTH, RET_V, D_MODEL), RET_V ** -0.5),
        'w_branch_sb': nrm(ks[9], (DEPTH, SB_W, D_MODEL), SB_W ** -0.5),
        'w_out': nrm(ks[10], (DEPTH, D_MODEL, D_MODEL), D_MODEL ** -0.5),
        'norm_ffn': gain(ks[11], (DEPTH, D_MODEL)),
        'peer_w_q': nrm(ks[12], (DEPTH, D_MODEL, PEER_HEADS * PEER_DKEY), D_MODEL ** -0.5),
        'peer_sub_keys_1': nrm(ks[13], (DEPTH, N_KEYS, PEER_DKEY // 2), (PEER_DKEY // 2) ** -0.5),
        'peer_sub_keys_2': nrm(ks[14], (DEPTH, N_KEYS, PEER_DKEY // 2), (PEER_DKEY // 2) ** -0.5),
        'peer_u': nrm(ks[15], (DEPTH, N_EXPERTS, D_MODEL), D_MODEL ** -0.5),
        'peer_v': nrm(ks[16], (DEPTH, N_EXPERTS, D_MODEL), PEER_HEADS ** -0.5),
    }


def reference(x, norm_attn, w_in, ret_q_norm, ret_k_norm, ret_group_norm, sb_q_norm, sb_k_norm,
              w_branch_ret, w_branch_sb, w_out, norm_ffn, peer_w_q, peer_sub_keys_1,
              peer_sub_keys_2, peer_u, peer_v):
    B, S, D = x.shape
    pos = jnp.arange(S, dtype=jnp.float32)
    split_points = np.cumsum(IN_SIZES)[:-1].tolist()
    for l in range(DEPTH):
        xn = rmsnorm(x, norm_attn[l])
        proj = xn @ w_in[l]
        rq, rk, rv, rg, sq, sk, sv, ga, gb = jnp.split(proj, split_points, axis=-1)

        rq = rope(rmsnorm(rq.reshape(B, S, RET_HEADS, RET_DK), ret_q_norm[l]), pos)
        rk = rope(rmsnorm(rk.reshape(B, S, RET_HEADS, RET_DK), ret_k_norm[l]), pos) * (RET_DK ** -0.5)
        ret = retention(rq, rk, rv.reshape(B, S, RET_HEADS, RET_DV).astype(jnp.float32))
        ret = group_norm(ret, ret_group_norm[l].reshape(RET_HEADS, RET_DV)).reshape(B, S, RET_V)
        ret = ret.astype(x.dtype) * jax.nn.silu(rg)
        y_a = ret @ w_branch_ret[l]

        sq = rmsnorm(sq.reshape(B, S, SB_HEADS, SB_DH), sb_q_norm[l])
        sk = rmsnorm(sk.reshape(B, S, SB_HEADS, SB_DH), sb_k_norm[l])
        sb = stick_breaking(sq, sk, sv.reshape(B, S, SB_HEADS, SB_DH))
        y_b = sb @ w_branch_sb[l]

        mixed = jax.nn.sigmoid(ga) * y_a + jax.nn.sigmoid(gb) * y_b
        x = x + mixed @ w_out[l]

        hn = rmsnorm(x, norm_ffn[l])
        x = x + peer(hn, peer_w_q[l], peer_sub_keys_1[l], peer_sub_keys_2[l], peer_u[l], peer_v[l])
    return x
```

```python
import numpy as np
from contextlib import ExitStack
import concourse.bass as bass
import concourse.mybir as mybir
from concourse.bass_utils import run_bass_kernel_spmd

F32 = mybir.dt.float32
BF16 = mybir.dt.bfloat16
U32 = mybir.dt.uint32
I32 = mybir.dt.int32
AF = mybir.ActivationFunctionType
ALU = mybir.AluOpType
AX = mybir.AxisListType

NCORES = 8
D = 1024
P = 128
H = 8
EPS = 1e-6
NEXP = 16384
INW = 6656
NEG = -1.0e30


class Buf:
    def __init__(self, name):
        self.name = name
        self.w = None
        self.r = {}
        self.dsem = None
        self.dcnt = 0


class Sched:
    def __init__(self, nc, es):
        self.nc = nc
        self.es = es
        self.E = {'pe': nc.tensor, 'act': nc.scalar, 'dve': nc.vector, 'pool': nc.gpsimd, 'sp': nc.sync}
        self.sem = {e: es.enter_context(nc.semaphore('sem_' + e)) for e in self.E}
        self.cnt = {e: 0 for e in self.E}
        self.known = {e: {} for e in self.E}
        self.nsem = 0

    def buf(self, name, dma=False):
        b = Buf(name)
        if dma:
            b.dsem = self.es.enter_context(self.nc.semaphore('d_' + name))
        return b

    def _wait(self, e, ev):
        if ev is None:
            return
        sem, val, src = ev
        if src == e and e == 'pe':
            return
        k = self.known[e]
        if k.get(id(sem), 0) >= val:
            return
        self.E[e].wait_ge(sem, val)
        k[id(sem)] = val

    @staticmethod
    def _flat(bs):
        out = []
        for b in bs:
            if isinstance(b, (list, tuple)):
                out.extend(Sched._flat(b))
            else:
                out.append(b)
        return out

    def _deps(self, e, reads, writes):
        reads = self._flat(reads); writes = self._flat(writes)
        for b in reads:
            self._wait(e, b.w)
        for b in writes:
            self._wait(e, b.w)
            for ev in list(b.r.values()):
                self._wait(e, ev)

    def _post(self, ev, reads, writes):
        reads = self._flat(reads); writes = self._flat(writes)
        for b in reads:
            old = b.r.get(id(ev[0]))
            if old is None or old[1] < ev[1]:
                b.r[id(ev[0])] = ev
        for b in writes:
            b.w = ev
            b.r = {}

    def op(self, e, fn, reads=(), writes=()):
        self._deps(e, reads, writes)
        ins = fn(self.E[e])
        self.cnt[e] += 1
        ins.then_inc(self.sem[e], 1)
        self._post((self.sem[e], self.cnt[e], e), reads, writes)

    def dma(self, q, fn, dbuf, reads=(), writes=()):
        self._deps(q, reads, writes)
        ins = fn(self.E[q])
        dbuf.dcnt += 16
        ins.then_inc(dbuf.dsem, 16)
        self._post((dbuf.dsem, dbuf.dcnt, 'dma'), reads, writes)

    def wait_all(self, e, bufs):
        for b in self._flat(bufs):
            self._wait(e, b.w)
            for ev in list(b.r.values()):
                self._wait(e, ev)


class _Stop(Exception):
    pass


_H = {}
STAGE = 99
RUN_KW = {}
SKIP_GATHER = False
FORCE_NPRE = None


def build_program(NT, NPRE, dbg=False):
    nc = bass.Bass("TRN2", target_bir_lowering=False)
    es = ExitStack()
    S = Sched(nc, es)
    _H['nc'] = nc; _H['es'] = es

    def din(name, shape, dt=F32):
        return nc.dram_tensor(name, list(shape), dt, kind="ExternalInput").ap()

    x_own = din("x_own", [NT * P, D])
    x_halo = din("x_halo", [P, D])
    x_pre = din("x_pre", [max(NPRE, 1) * P, D])
    cos_own = din("cos_own", [P, NT, 32]); sin_own = din("sin_own", [P, NT, 32])
    cos_pre = din("cos_pre", [P, max(NPRE, 1), 32]); sin_pre = din("sin_pre", [P, max(NPRE, 1), 32])
    ksc_pre = din("ksc_pre", [P, max(NPRE, 1), H])
    g_attn = din("g_attn", [P, D]); g_ffn = din("g_ffn", [P, D]); g_gn = din("g_gn", [P, D])
    g_rq = din("g_rq", [P, 512]); g_rk = din("g_rk", [P, 512]); g_sq = din("g_sq", [P, 512]); g_sk = din("g_sk", [P, 512])
    c_ident = din("c_ident", [P, P]); c_tri = din("c_tri", [P, P]); c_ones = din("c_ones", [P, P])
    c_mpos = din("c_mpos", [P, 512]); c_mstay = din("c_mstay", [P, 1024])
    c_maskT = din("c_maskT", [P, 1024]); c_qdec = din("c_qdec", [P, H]); c_kdec = din("c_kdec", [P, H])
    c_cdec = din("c_cdec", [64, D])
    w_in = din("w_in", [D, INW]); w_bra = din("w_bra", [D, D]); w_brb = din("w_brb", [512, D]); w_out = din("w_out", [D, D])
    w_q = din("w_q", [D, 2048]); k1T = din("k1T", [P, P]); k2T = din("k2T", [P, P])
    u_tab = din("u_tab", [NEXP, D]); v_tab = din("v_tab", [NEXP, D])
    y_out = nc.dram_tensor("y_out", [NT * P, D], F32, kind="ExternalOutput").ap()
    uv_bf = nc.dram_tensor("uv_bf", [NEXP, 2 * D], BF16, kind="Internal").ap()
    def dump(stage, src_ap, bsrc, ncols=D):
        if STAGE != stage:
            return
        b_d = S.buf("dump", dma=True)
        npart = src_ap.shape[0]
        S.dma('sp', lambda e: e.dma_start(out=y_out[0:npart, 0:ncols], in_=src_ap), b_d, reads=bsrc, writes=[b_d])
        S.wait_all('sp', [b_d])
        raise _Stop()

    tot = [0]

    def sb(name, shape, dt=F32):
        n = int(np.prod(shape[1:])) * (4 if dt in (F32, U32, I32) else 2)
        tot[0] += n
        if dbg:
            print("SB", name, n, tot[0])
        return es.enter_context(nc.sbuf_tensor(name, list(shape), dt))

    def ps(name, shape, dt=F32):
        return es.enter_context(nc.psum_tensor(name, list(shape), dt))

    psA = ps("psA", [P, 1024]); bA = S.buf("psA")
    psB = ps("psB", [P, 1024]); bB = S.buf("psB")
    psC = ps("psC", [P, 512]); bC = S.buf("psC")
    psD = ps("psD", [P, 512]); bD = S.buf("psD")
    psT = ps("psT", [P, 1024], BF16); bT = S.buf("psT")
    psS = ps("psS", [P, 512]); bS = S.buf("psS")

    consts = []

    def load_const(name, src, shape, dt=F32, q='sp'):
        t = sb(name, shape, dt)
        b = S.buf(name, dma=True)
        S.dma(q, lambda e: e.dma_start(out=t[:], in_=src), b, writes=[b])
        consts.append(b)
        return t, b

    def load_cast(name, src, shape):
        return load_const(name, src, shape, BF16, q='pool')

    ident, b_ident = load_cast("ident", c_ident, [P, P])
    tri, b_tri = load_cast("tri", c_tri, [P, P])
    ones, b_ones = load_cast("ones", c_ones, [P, P])
    mpos, b_mpos = load_cast("mpos", c_mpos, [P, 512])
    mstay, b_mstay = load_cast("mstay", c_mstay, [P, 1024])
    maskT, b_maskT = load_const("maskT", c_maskT, [P, 1024])
    qdec, b_qdec = load_const("qdec", c_qdec, [P, H])
    kdec, b_kdec = load_const("kdec", c_kdec, [P, H])
    cdec, b_cdec = load_const("cdec", c_cdec, [64, D])
    gattn, b_gattn = load_const("gattn", g_attn, [P, D])
    gffn, b_gffn = load_const("gffn", g_ffn, [P, D])
    ggn, b_ggn = load_const("ggn", g_gn, [P, D])
    grq, b_grq = load_const("grq", g_rq, [P, 512])
    grk, b_grk = load_const("grk", g_rk, [P, 512])
    gsq, b_gsq = load_const("gsq", g_sq, [P, 512])
    gsk, b_gsk = load_const("gsk", g_sk, [P, 512])
    cso, b_cso = load_const("cso", cos_own, [P, NT, 32])
    sno, b_sno = load_const("sno", sin_own, [P, NT, 32])

    def wview(w, c0, n):
        return w[:, c0:c0 + n].rearrange("(c p) n -> p c n", p=P)

    TPbig = sb("TPbig", [P, 10, D])
    TP = [TPbig[:, i, :] for i in range(10)]
    b_TP = [S.buf("TP%d" % i, dma=True) for i in range(10)]
    xt = [TP[0], TP[1]]
    b_xt = [b_TP[0], b_TP[1]]
    junk = sb("junk", [P, D], BF16); b_junk = S.buf("junk")
    st4 = sb("st4", [P, 4]); b_st4 = S.buf("st4")

    def rmsnorm_T(src, b_src, gain, b_gain, keep_f32=None, b_keep=None):
        S.op('act', lambda e: e.activation(out=junk[:], in_=src, func=AF.Square, accum_out=st4[:, 0:1]),
             reads=[b_src], writes=[b_junk, b_st4])
        S.op('act', lambda e: e.activation(out=st4[:, 1:2], in_=st4[:, 0:1], func=AF.Sqrt, bias=eps_t[:, 0:1], scale=1.0 / D),
             reads=[b_st4, b_eps], writes=[b_st4])
        S.op('dve', lambda e: e.reciprocal(out=st4[:, 2:3], in_=st4[:, 1:2]), reads=[b_st4], writes=[b_st4])
        if keep_f32 is not None:
            S.op('dve', lambda e: e.scalar_tensor_tensor(out=keep_f32, in0=src, scalar=st4[:, 2:3], in1=gain[:],
                                                         op0=ALU.mult, op1=ALU.mult),
                 reads=[b_src, b_st4, b_gain], writes=[b_keep])
            S.op('act', lambda e: e.activation(out=xn[:], in_=keep_f32, func=AF.Copy), reads=[b_keep], writes=[b_xn])
        else:
            S.op('dve', lambda e: e.scalar_tensor_tensor(out=xn[:], in0=src, scalar=st4[:, 2:3], in1=gain[:],
                                                         op0=ALU.mult, op1=ALU.mult),
                 reads=[b_src, b_st4, b_gain], writes=[b_xn])
        transpose8(xn, b_xn, xnT, b_xnT)

    def transpose8(src, b_src, dst, b_dst, nchunk=8):
        for half in range((nchunk + 3) // 4):
            n = min(4, nchunk - half * 4)
            for k in range(n):
                c = half * 4 + k
                S.op('pe', lambda e, c=c, k=k: e.transpose(out=psT[:, k * P:(k + 1) * P], in_=src[:, c * P:(c + 1) * P],
                                                           identity=ident[:]),
                     reads=[b_src, b_ident], writes=[bT])
            eng = 'act' if half % 2 == 0 else 'dve'
            if eng == 'act':
                S.op('act', lambda e, half=half, n=n: e.activation(
                    out=dst[:, half * 4:half * 4 + n, :].rearrange("p c t -> p (c t)"), in_=psT[:, 0:n * P], func=AF.Copy),
                    reads=[bT], writes=[b_dst])
            else:
                S.op('dve', lambda e, half=half, n=n: e.tensor_copy(
                    out=dst[:, half * 4:half * 4 + n, :].rearrange("p c t -> p (c t)"), in_=psT[:, 0:n * P]),
                    reads=[bT], writes=[b_dst])

    def transposeH(src3, b_src, dst, b_dst):
        for h in range(H):
            S.op('pe', lambda e, h=h: e.transpose(out=psT[0:64, h * P:(h + 1) * P], in_=src3[:, h, :], identity=ident[:]),
                 reads=[b_src, b_ident], writes=[bT])
        S.op('act', lambda e: e.activation(out=dst[:].rearrange("p h t -> p (h t)"), in_=psT[0:64, :], func=AF.Copy),
             reads=[bT], writes=[b_dst])

    def proj512(wb, b_wb, pst, b_pst):
        for c in range(8):
            S.op('pe', lambda e, c=c: e.matmul(out=pst, lhsT=xnT[:, c, :], rhs=wb[:, c, :], start=(c == 0), stop=(c == 7)),
                 reads=[b_xnT, b_wb], writes=[b_pst])

    def qknorm(pst, b_pst, gain, b_gain, slot, scale_ap=None, b_scale=None, cos=None, sin=None, b_cs=(), out_bf=None, b_out=None):
        S.op('act', lambda e: e.activation(out=qf[:], in_=pst, func=AF.Copy), reads=[b_pst], writes=[b_qf])
        S.op('act', lambda e: e.activation(out=sq_s[:], in_=pst, func=AF.Square), reads=[b_pst], writes=[b_sq])
        S.op('dve', lambda e: e.tensor_reduce(out=st8[:, 0, :], in_=sq_s[:].rearrange("p (h d) -> p h d", d=64), axis=AX.X, op=ALU.add),
             reads=[b_sq], writes=[b_st8])
        S.op('act', lambda e: e.activation(out=st8[:, 1, :], in_=st8[:, 0, :], func=AF.Sqrt, bias=eps_t[:, 0:1], scale=1.0 / 64),
             reads=[b_st8, b_eps], writes=[b_st8])
        S.op('dve', lambda e: e.reciprocal(out=st8[:, 2, :], in_=st8[:, 1, :]), reads=[b_st8], writes=[b_st8])
        rs = st8[:, 2, :]
        if scale_ap is not None:
            S.op('dve', lambda e: e.tensor_tensor(out=st8[:, 3, :], in0=st8[:, 2, :], in1=scale_ap, op=ALU.mult),
                 reads=[b_st8, b_scale], writes=[b_st8])
            rs = st8[:, 3, :]
        S.op('dve', lambda e: e.tensor_tensor(out=qn[:].rearrange("p (h d) -> p h d", d=64), in0=qf[:].rearrange("p (h d) -> p h d", d=64),
                                              in1=rs.unsqueeze(2).to_broadcast([P, H, 64]), op=ALU.mult),
             reads=[b_qf, b_st8], writes=[b_qn])
        if cos is None:
            S.op('dve', lambda e: e.tensor_tensor(out=out_bf[:].rearrange("p h d -> p (h d)"), in0=qn[:], in1=gain[:], op=ALU.mult),
                 reads=[b_qn, b_gain], writes=[b_out])
            return
        S.op('pool', lambda e: e.tensor_tensor(out=qn[:], in0=qn[:], in1=gain[:], op=ALU.mult), reads=[b_qn, b_gain], writes=[b_qn])
        q3 = qn[:].rearrange("p (h d) -> p h d", d=64)
        x1 = q3[:, :, 0:32]; x2 = q3[:, :, 32:64]
        cb = cos.unsqueeze(1).to_broadcast([P, H, 32]); sbb = sin.unsqueeze(1).to_broadcast([P, H, 32])
        r = [rt[:, i, :].rearrange("p (h d) -> p h d", d=32) for i in range(4)]
        S.op('dve', lambda e: e.tensor_tensor(out=r[0], in0=x1, in1=cb, op=ALU.mult), reads=[b_qn] + list(b_cs), writes=[b_rt])
        S.op('pool', lambda e: e.tensor_tensor(out=r[1], in0=x2, in1=sbb, op=ALU.mult), reads=[b_qn] + list(b_cs), writes=[b_rt])
        S.op('dve', lambda e: e.tensor_tensor(out=r[2], in0=x1, in1=sbb, op=ALU.mult), reads=[b_qn] + list(b_cs), writes=[b_rt])
        S.op('pool', lambda e: e.tensor_tensor(out=r[3], in0=x2, in1=cb, op=ALU.mult), reads=[b_qn] + list(b_cs), writes=[b_rt])
        S.op('dve', lambda e: e.tensor_tensor(out=out_bf[:, :, 0:32], in0=r[0], in1=r[1], op=ALU.subtract), reads=[b_rt], writes=[b_out])
        S.op('dve', lambda e: e.tensor_tensor(out=out_bf[:, :, 32:64], in0=r[2], in1=r[3], op=ALU.add), reads=[b_rt], writes=[b_out])

    b_ubf = S.buf("uv_bf", dma=True); b_vbf = b_ubf
    identF, b_identF = load_const("identF", c_ident, [P, P])
    eps_t = sb("eps_t", [P, 1]); b_eps = S.buf("eps")
    S.op('dve', lambda e: e.memset(eps_t[:], EPS), writes=[b_eps])
    gk_t = sb("gk_t", [P, 1]); b_gk = S.buf("gk")
    S.op('dve', lambda e: e.memset(gk_t[:], 1.5957691216057308), writes=[b_gk])
    one_t = sb("one_t", [P, 1]); b_one = S.buf("one")
    S.op('dve', lambda e: e.memset(one_t[:], 1.0), writes=[b_one])

    state = sb("state", [64, D]); b_state = S.buf("state")
    state_bf = sb("state_bf", [64, D], BF16); b_state_bf = S.buf("state_bf")

    with ExitStack() as es0:
        NBLK = NEXP // 128
        NCB = 4
        cb = [es0.enter_context(nc.sbuf_tensor("cb%d" % i, [P, 1, D], BF16)) for i in range(NCB)]
        b_cb = [S.buf("cb%d" % i, dma=True) for i in range(NCB)]
        b_cin = b_cb; b_cout = []
        pc_jobs = [(tab, dst, bd, blk) for blk in range(NBLK) for (tab, dst, bd) in ((u_tab, uv_bf[:, 0:D], b_ubf), (v_tab, uv_bf[:, D:2 * D], b_vbf))]
        pc_state = [0, 0]

        def _pc_store(k):
            tab, dst, bd, blk = pc_jobs[k]
            i = k % NCB
            S.dma('pool', lambda e: e.dma_start(out=dst[blk * 128:(blk + 1) * 128, :].rearrange("(p r) d -> p r d", r=1), in_=cb[i][:]),
                  bd, reads=[b_cb[i]], writes=[bd])

        def precast(nblocks):
            for _ in range(nblocks):
                k = pc_state[0]
                if k >= len(pc_jobs):
                    break
                pc_state[0] += 1
                tab, dst, bd, blk = pc_jobs[k]
                i = k % NCB
                S.dma('pool', lambda e, tab=tab, blk=blk, i=i: e.dma_start(out=cb[i][:], in_=tab[blk * 128:(blk + 1) * 128, :].rearrange("(p r) d -> p r d", r=1)),
                      b_cb[i], writes=[b_cb[i]])
                if k - 2 >= 0:
                    _pc_store(k - 2)
                    pc_state[1] = k - 1
            if pc_state[0] >= len(pc_jobs):
                while pc_state[1] < len(pc_jobs):
                    _pc_store(pc_state[1])
                    pc_state[1] += 1

        pc_per_tile = -(-len(pc_jobs) // max(NPRE, 1))
        if NPRE > 0:
            wk = es0.enter_context(nc.sbuf_tensor("wk_pre", [P, 8, 512], BF16)); b_wk = S.buf("wk_pre", dma=True)
            wv = es0.enter_context(nc.sbuf_tensor("wv_pre", [P, 8, 1024], BF16)); b_wv = S.buf("wv_pre", dma=True)
            csp = es0.enter_context(nc.sbuf_tensor("csp", [P, NPRE, 32], F32)); b_csp = S.buf("csp", dma=True)
            snp = es0.enter_context(nc.sbuf_tensor("snp", [P, NPRE, 32], F32)); b_snp = S.buf("snp", dma=True)
            ksp = es0.enter_context(nc.sbuf_tensor("ksp", [P, NPRE, H], F32)); b_ksp = S.buf("ksp", dma=True)
            S.dma('pool', lambda e: e.dma_start(out=wk[:], in_=wview(w_in, 512, 512)), b_wk, writes=[b_wk])
            for hh in range(2):
                S.dma('pool', lambda e, hh=hh: e.dma_start(out=wv[:, :, hh * 512:(hh + 1) * 512], in_=wview(w_in, 1024 + hh * 512, 512)),
                      b_wv, writes=[b_wv])
            S.dma('sp', lambda e: e.dma_start(out=csp[:], in_=cos_pre), b_csp, writes=[b_csp])
            S.dma('sp', lambda e: e.dma_start(out=snp[:], in_=sin_pre), b_snp, writes=[b_snp])
            S.dma('sp', lambda e: e.dma_start(out=ksp[:], in_=ksc_pre), b_ksp, writes=[b_ksp])
            B0 = 4
            def _al(name, shape, dt):
                return es0.enter_context(nc.sbuf_tensor(name, list(shape), dt))
            xnb = _al("xnb", [P, 2, D], BF16); xnTb = _al("xnTb", [P, 2, 8, P], BF16); st4b = _al("st4b", [P, B0, 4], F32)
            b_xnb = [S.buf("xnb%d" % (i % 2)) for i in range(2)] * 2; b_xnTb = [S.buf("xnTb%d" % (i % 2)) for i in range(2)] * 2; b_st4b = [S.buf("st4b%d" % i) for i in range(B0)]
            sets = []
            for si in range(2):
                d_ = dict(kf=TPbig[:, 2 + 4 * si:4 + 4 * si, :].rearrange("p a d -> p (a d)").rearrange("p (b n) -> p b n", b=B0),
                          sq=TPbig[:, 4 + 4 * si:6 + 4 * si, :].rearrange("p a d -> p (a d)").rearrange("p (b n) -> p b n", b=B0),
                          s8=_al("s8b%d" % si, [P, 4, B0 * H], F32),
                          r1=_al("r1b%d" % si, [P, B0 * 256], F32),
                          kd=_al("kdb%d" % si, [P, B0, H, 64], BF16), v=_al("vb%d" % si, [P, B0, D], BF16))
                d_.update(b_kf=S.buf("kfb%d" % si), b_sq=S.buf("sqb%d" % si), b_s8=S.buf("s8b%d" % si), b_r0=S.buf("r0b%d" % si),
                          b_r1=S.buf("r1b%d" % si), b_kd=S.buf("kdb%d" % si), b_v=S.buf("vb%d" % si))
                d_["r0"] = d_["kf"].rearrange("p b n -> p (b n)")[:, 0:B0 * 256]; d_["b_r0"] = d_["b_kf"]
                sets.append(d_)
            all_pre_bufs = b_xnb + b_xnTb + b_st4b + [sets[i][k] for i in range(2) for k in ("b_kf", "b_sq", "b_s8", "b_r0", "b_r1", "b_kd", "b_v")]

            def front_a(m, b, st):
                xb = xt[m % 2]; bx = b_xt[m % 2]
                S.dma('sp', lambda e: e.dma_start(out=xb[:], in_=x_pre[m * P:(m + 1) * P, :]), bx, writes=[bx])
                S.op('act', lambda e: e.activation(out=junk[:], in_=xb[:], func=AF.Square, accum_out=st4b[:, b, 0:1]), reads=[bx], writes=[b_junk, b_st4b[b]])
                S.op('act', lambda e: e.activation(out=st4b[:, b, 1:2], in_=st4b[:, b, 0:1], func=AF.Sqrt, bias=eps_t[:, 0:1], scale=1.0 / D),
                     reads=[b_st4b[b], b_eps], writes=[b_st4b[b]])
                S.op('dve', lambda e: e.reciprocal(out=st4b[:, b, 2:3], in_=st4b[:, b, 1:2]), reads=[b_st4b[b]], writes=[b_st4b[b]])
                S.op('dve', lambda e: e.scalar_tensor_tensor(out=xnb[:, b % 2, :], in0=xb[:], scalar=st4b[:, b, 2:3], in1=gattn[:], op0=ALU.mult, op1=ALU.mult),
                     reads=[bx, b_st4b[b], b_gattn], writes=[b_xnb[b]])

            def front_b(m, b, st):
                precast(pc_per_tile)
                for half in range(2):
                    for k in range(4):
                        c = half * 4 + k
                        S.op('pe', lambda e, c=c, k=k: e.transpose(out=psT[:, k * P:(k + 1) * P], in_=xnb[:, b % 2, c * P:(c + 1) * P], identity=ident[:]),
                             reads=[b_xnb[b], b_ident], writes=[bT])
                    dst = xnTb[:, b % 2, half * 4:half * 4 + 4, :].rearrange("p c t -> p (c t)")
                    if half == 0:
                        S.op('act', lambda e, dst=dst: e.activation(out=dst, in_=psT[:, 0:512], func=AF.Copy), reads=[bT], writes=[b_xnTb[b]])
                    else:
                        S.op('dve', lambda e, dst=dst: e.tensor_copy(out=dst, in_=psT[:, 0:512]), reads=[bT], writes=[b_xnTb[b]])
                for c in range(8):
                    S.op('pe', lambda e, c=c: e.matmul(out=psC[:], lhsT=xnTb[:, b % 2, c, :], rhs=wk[:, c, :], start=(c == 0), stop=(c == 7)),
                         reads=[b_xnTb[b], b_wk], writes=[bC])
                S.op('act', lambda e: e.activation(out=st["kf"][:, b, :], in_=psC[:], func=AF.Copy), reads=[bC], writes=[st["b_kf"]])
                S.op('act', lambda e: e.activation(out=st["sq"][:, b, :], in_=psC[:], func=AF.Square), reads=[bC], writes=[st["b_sq"]])
                psV, bV = (psA, bA) if m % 2 == 0 else (psB, bB)
                for hh in range(2):
                    for c in range(8):
                        S.op('pe', lambda e, c=c, hh=hh: e.matmul(out=psV[:, hh * 512:(hh + 1) * 512], lhsT=xnTb[:, b % 2, c, :], rhs=wv[:, c, hh * 512:(hh + 1) * 512],
                                                                  start=(c == 0), stop=(c == 7)), reads=[b_xnTb[b], b_wv], writes=[bV])
                S.op('act', lambda e: e.activation(out=st["v"][:, b, :], in_=psV[:], func=AF.Copy), reads=[bV], writes=[st["b_v"]])

            def chain_gen(m0, nb, st):
                n8 = nb * H
                kf, sq, s8, r0, r1, kdb = st["kf"], st["sq"], st["s8"], st["r0"], st["r1"], st["kd"]
                S.op('dve', lambda e: e.tensor_reduce(out=s8[:, 0, 0:n8], in_=sq[:, 0:nb, :].rearrange("p b (h d) -> p (b h) d", d=64), axis=AX.X, op=ALU.add),
                     reads=[st["b_sq"]], writes=[st["b_s8"]]); yield
                S.op('act', lambda e: e.activation(out=s8[:, 1, 0:n8], in_=s8[:, 0, 0:n8], func=AF.Sqrt, bias=eps_t[:, 0:1], scale=1.0 / 64),
                     reads=[st["b_s8"], b_eps], writes=[st["b_s8"]]); yield
                S.op('dve', lambda e: e.reciprocal(out=s8[:, 2, 0:n8], in_=s8[:, 1, 0:n8]), reads=[st["b_s8"]], writes=[st["b_s8"]]); yield
                S.op('dve', lambda e: e.tensor_tensor(out=s8[:, 3, 0:n8], in0=s8[:, 2, 0:n8], in1=ksp[:, m0:m0 + nb, :].rearrange("p m h -> p (m h)"), op=ALU.mult),
                     reads=[st["b_s8"], b_ksp], writes=[st["b_s8"]]); yield
                q3 = sq[:, 0:nb, :].rearrange("p b (h d) -> p (b h) d", d=64)
                S.op('dve', lambda e: e.tensor_tensor(out=q3, in0=kf[:, 0:nb, :].rearrange("p b (h d) -> p (b h) d", d=64),
                                                      in1=s8[:, 3, 0:n8].unsqueeze(2).to_broadcast([P, n8, 64]), op=ALU.mult),
                     reads=[st["b_kf"], st["b_s8"]], writes=[st["b_sq"]]); yield
                S.op('pool', lambda e: e.tensor_tensor(out=sq[:, 0:nb, :], in0=sq[:, 0:nb, :], in1=grk[:].unsqueeze(1).to_broadcast([P, nb, 512]), op=ALU.mult),
                     reads=[st["b_sq"], b_grk], writes=[st["b_sq"]]); yield
                q4 = sq[:, 0:nb, :].rearrange("p b (h d) -> p b h d", d=64)
                x1_ = q4[:, :, :, 0:32]; x2_ = q4[:, :, :, 32:64]
                cb = csp[:, m0:m0 + nb, :].unsqueeze(2).to_broadcast([P, nb, H, 32]); sb_ = snp[:, m0:m0 + nb, :].unsqueeze(2).to_broadcast([P, nb, H, 32])
                r0v = r0[:, 0:nb * 256].rearrange("p (b h d) -> p b h d", b=nb, h=H); r1v = r1[:, 0:nb * 256].rearrange("p (b h d) -> p b h d", b=nb, h=H)
                S.op('dve', lambda e: e.tensor_tensor(out=r0v, in0=x1_, in1=cb, op=ALU.mult), reads=[st["b_sq"], b_csp], writes=[st["b_r0"]]); yield
                S.op('pool', lambda e: e.tensor_tensor(out=r1v, in0=x2_, in1=sb_, op=ALU.mult), reads=[st["b_sq"], b_snp], writes=[st["b_r1"]]); yield
                S.op('dve', lambda e: e.tensor_tensor(out=kdb[:, 0:nb, :, 0:32], in0=r0v, in1=r1v, op=ALU.subtract), reads=[st["b_r0"], st["b_r1"]], writes=[st["b_kd"]]); yield
                S.op('dve', lambda e: e.tensor_tensor(out=r0v, in0=x1_, in1=sb_, op=ALU.mult), reads=[st["b_sq"], b_snp], writes=[st["b_r0"]]); yield
                S.op('pool', lambda e: e.tensor_tensor(out=r1v, in0=x2_, in1=cb, op=ALU.mult), reads=[st["b_sq"], b_csp], writes=[st["b_r1"]]); yield
                S.op('dve', lambda e: e.tensor_tensor(out=kdb[:, 0:nb, :, 32:64], in0=r0v, in1=r1v, op=ALU.add), reads=[st["b_r0"], st["b_r1"]], writes=[st["b_kd"]]); yield

            def state_mm(m0, nb, st):
                for b in range(nb):
                    m = m0 + b
                    for h in range(H):
                        acc_ps, b_acc_ps = (psS, bS) if h < 4 else (psD, bD)
                        S.op('pe', lambda e, h=h, m=m, b=b, acc_ps=acc_ps: e.matmul(
                            out=acc_ps[0:64, (h % 4) * P:(h % 4 + 1) * P], lhsT=st["kd"][:, b, h, :], rhs=st["v"][:, b, h * P:(h + 1) * P],
                            start=(m == 0 and h % 4 == 0), stop=(m == NPRE - 1), skip_group_check=True),
                            reads=[st["b_kd"], st["b_v"]], writes=[b_acc_ps])

            batches = [(m0, min(B0, NPRE - m0)) for m0 in range(0, NPRE, B0)]
            tiles = [(m0 + b, b, sets[bi % 2]) for bi, (m0, nb) in enumerate(batches) for b in range(nb)]
            pend = None
            front_a(*tiles[0])
            ti_ = 0
            for bi, (m0, nb) in enumerate(batches):
                st = sets[bi % 2]
                gen = chain_gen(*pend) if pend is not None else None
                for b in range(nb):
                    if ti_ + 1 < len(tiles):
                        front_a(*tiles[ti_ + 1])
                    front_b(m0 + b, b, st)
                    ti_ += 1
                    if gen is not None:
                        for _ in range(4):
                            next(gen, None)
                if gen is not None:
                    for _ in gen:
                        pass
                    state_mm(*pend)
                pend = (m0, nb, st)
            for _ in chain_gen(*pend):
                pass
            state_mm(*pend)
            for e_ in ('pe', 'act', 'dve', 'pool', 'sp'):
                S.wait_all(e_, all_pre_bufs)
            S.op('act', lambda e: e.activation(out=state[:, 0:512], in_=psS[0:64, :], func=AF.Copy), reads=[bS], writes=[b_state])
            S.op('act', lambda e: e.activation(out=state[:, 512:1024], in_=psD[0:64, :], func=AF.Copy), reads=[bD], writes=[b_state])
        else:
            S.op('dve', lambda e: e.memset(state[:], 0.0), writes=[b_state])
        S.op('act', lambda e: e.activation(out=state_bf[:], in_=state[:], func=AF.Copy), reads=[b_state], writes=[b_state_bf])
        precast(len(pc_jobs))
        for e_ in ('pe', 'act', 'dve', 'pool', 'sp'):
            S.wait_all(e_, b_cin + b_cout + [b_ubf])
        if NPRE > 0:
            for e_ in ('pe', 'act', 'dve', 'pool'):
                S.wait_all(e_, [b_wk, b_wv, b_csp, b_snp, b_ksp])

    dump(1, state[:], [b_state], D)
    xn = sb("xn", [P, D], BF16); b_xn = S.buf("xn")
    xnT = sb("xnT", [P, 8, P], BF16); b_xnT = S.buf("xnT")
    qf = sb("qf", [P, 512]); b_qf = S.buf("qf")
    sq_s = sb("sq_s", [P, 512]); b_sq = S.buf("sq_s")
    st8 = sb("st8", [P, 4, H]); b_st8 = S.buf("st8")
    qn = sb("qn", [P, 512]); b_qn = S.buf("qn")
    rt = sb("rt", [P, 4, 256]); b_rt = S.buf("rt")
    kd = sb("kd", [P, H, 64], BF16); b_kd = S.buf("kd")
    v_r = sb("v_r", [P, D], BF16); b_vr = S.buf("v_r")
    wbra = sb("wbra", [P, 8, D], BF16); b_wbra = S.buf("wbra", dma=True)
    wbrb = sb("wbrb", [P, 4, D], BF16); b_wbrb = S.buf("wbrb", dma=True)
    wout = sb("wout", [P, 8, D], BF16); b_wout = S.buf("wout", dma=True)
    k1 = sb("k1", [P, P], BF16); b_k1 = S.buf("k1", dma=True)
    k2 = sb("k2", [P, P], BF16); b_k2 = S.buf("k2", dma=True)
    for hh in range(2):
        S.dma('pool', lambda e, hh=hh: e.dma_start(out=wbra[:, :, hh * 512:(hh + 1) * 512], in_=wview(w_bra, hh * 512, 512)), b_wbra, writes=[b_wbra])
        S.dma('pool', lambda e, hh=hh: e.dma_start(out=wbrb[:, :, hh * 512:(hh + 1) * 512], in_=wview(w_brb, hh * 512, 512)), b_wbrb, writes=[b_wbrb])
        S.dma('pool', lambda e, hh=hh: e.dma_start(out=wout[:, :, hh * 512:(hh + 1) * 512], in_=wview(w_out, hh * 512, 512)), b_wout, writes=[b_wout])
    S.dma('pool', lambda e: e.dma_start(out=k1[:], in_=k1T), b_k1, writes=[b_k1])
    S.dma('pool', lambda e: e.dma_start(out=k2[:], in_=k2T), b_k2, writes=[b_k2])

    wbuf = [sb("wbuf%d" % i, [P, 8, 512], BF16) for i in range(2)]
    b_wbuf = [S.buf("wbuf%d" % i, dma=True) for i in range(2)]
    wcount = [0]

    def stream_w(c0, src=None):
        src = w_in if src is None else src
        i = wcount[0] % 2
        wcount[0] += 1
        S.dma('pool', lambda e: e.dma_start(out=wbuf[i][:], in_=wview(src, c0, 512)), b_wbuf[i], writes=[b_wbuf[i]])
        return wbuf[i], b_wbuf[i]

    xres = TP[2]; b_xres = b_TP[2]
    qd = sb("qd", [P, H, 64], BF16); b_qd = S.buf("qd")
    qdT = sb("qdT", [64, H, P], BF16); b_qdT = S.buf("qdT")
    kdT = sb("kdT", [64, H, P], BF16); b_kdT = S.buf("kdT")
    rg = sb("rg", [P, D], BF16); b_rg = S.buf("rg")
    sqn = sb("sqn", [P, H, 64], BF16); b_sqn = S.buf("sqn")
    skn = sb("skn", [P, H, 64], BF16); b_skn = S.buf("skn")
    sqT = sb("sqT", [64, H, P], BF16); b_sqT = S.buf("sqT")
    skT = [sb("skT%d" % i, [64, H, P], BF16) for i in range(2)]; b_skT = [S.buf("skT%d" % i) for i in range(2)]
    sv = [sb("sv%d" % i, [P, 512], BF16) for i in range(2)]; b_sv = [S.buf("sv%d" % i) for i in range(2)]
    siga = TP[7]; b_siga = b_TP[7]
    sigb = TP[8]; b_sigb = b_TP[8]
    pm = sb("pm", [P, D], BF16); b_pm = S.buf("pm")
    yf = TP[3]; b_yf = b_TP[3]
    ysq = TP[4]; b_ysq = b_TP[4]
    gst = sb("gst", [P, 6, H]); b_gst = S.buf("gst")
    ret = sb("ret", [P, D], BF16); b_ret = S.buf("ret")
    retT = sb("retT", [P, 8, P], BF16); b_retT = S.buf("retT")
    e_s = TP[3]; b_es = b_TP[3]
    sp_s = TP[4]; b_sp = b_TP[4]
    spm = sb("spm", [P, D], BF16); b_spm = S.buf("spm")
    u_s = TP[0]; b_us = b_TP[0]
    w_s = sb("w_s", [P, D], BF16); b_ws = S.buf("w_s")
    sbT = sb("sbT", [P, 4, P], BF16); b_sbT = S.buf("sbT")
    m1 = TP[5]; b_m1 = b_TP[5]
    m2 = TP[6]; b_m2 = b_TP[6]
    mixed = sb("mixed", [P, D], BF16); b_mixed = S.buf("mixed")
    mixT = sb("mixT", [P, 8, P], BF16); b_mixT = S.buf("mixT")
    x1 = TP[1]; b_x1 = b_TP[1]
    hn = TP[9]; b_hn = b_TP[9]
    qT = sb("qT", [P, 16, P], BF16); b_qT = S.buf("qT")
    sc = TPbig[:, 3:5, :].rearrange("p a d -> p (a d)").rearrange("p (g n) -> p g n", g=16); b_sc = [b_TP[3], b_TP[4]]
    sc2 = TPbig[:, 5:7, :].rearrange("p a d -> p (a d)").rearrange("p (g n) -> p g n", g=16); b_sc2 = [b_TP[5], b_TP[6]]
    tv = sb("tv", [P, 16, 16]); b_tv = S.buf("tv")
    ti = sb("ti", [P, 16, 16], U32); b_ti = S.buf("ti")
    tif = sb("tif", [P, 16, 16]); b_tif = S.buf("tif")
    cand = TPbig[:, 7:9, :].rearrange("p a d -> p (a d)").rearrange("p (h c) -> p h c", h=H); b_cand = [b_TP[7], b_TP[8]]
    cand2 = sc.rearrange("p g n -> p (g n)").rearrange("p (h c) -> p h c", h=H); b_cand2 = b_sc
    tsv = sb("tsv", [P, H, 16]); b_tsv = S.buf("tsv")
    tpos = sb("tpos", [P, H, 16], U32); b_tpos = S.buf("tpos")
    tposf = sb("tposf", [P, H, 16]); b_tposf = S.buf("tposf")
    ta = sb("ta", [P, H, 16]); b_ta = S.buf("ta")
    tb = sb("tb", [P, H, 16]); b_tb = S.buf("tb")
    iota16 = sb("iota16", [P, 16]); b_iota = S.buf("iota16")
    oh = sc2.rearrange("p g n -> p (g n)").rearrange("p (h a b) -> p h a b", h=H, a=16); b_oh = b_sc2
    idx1 = sb("idx1", [P, H, 16]); b_idx1 = S.buf("idx1")
    idx2 = sb("idx2", [P, H, 16]); b_idx2 = S.buf("idx2")
    eidx = sb("eidx", [P, 128], U32); b_eidx = S.buf("eidx")
    gw = sb("gw", [P, H, 16]); b_gw = S.buf("gw")
    gs = sb("gs", [P, 2, H]); b_gs = S.buf("gs")
    hv = sb("hv", [P, 128]); b_hv = S.buf("hv")
    ga_ = sb("ga_", [P, 6, 128]); b_ga = S.buf("ga_"); b_ga2 = S.buf("ga2"); b_ga3 = S.buf("ga3")
    aw = sb("aw", [P, 128]); b_aw = S.buf("aw")
    NG = 8
    dg = [sb("dg%d" % i, [P, P], BF16) for i in range(4)]; b_dg = [S.buf("dg%d" % i) for i in range(4)]
    gbuf = None; b_gbuf = None
    acc = TP[0]; b_acc = b_TP[0]
    b_yout = S.buf("yout", dma=True)

    _gi = [3, 4, 5, 6, 7, 8, 2, 0]
    gbuf = [TPbig[:, i, :].bitcast(BF16) for i in _gi]
    b_gbuf = [b_TP[i] for i in _gi]
    gsem = [S.buf("gsem%d" % i, dma=True) for i in range(len(_gi))]
    S.op('pool', lambda e: e.iota(iota16[:], pattern=[[1, 16]], base=0, channel_multiplier=0, allow_small_or_imprecise_dtypes=True),
         writes=[b_iota])
    thr16 = sb("thr16", [P, 16])
    S.op('pool', lambda e: e.iota(thr16[:], pattern=[[16, 16]], base=0, channel_multiplier=0, allow_small_or_imprecise_dtypes=True),
         reads=[b_iota], writes=[b_iota])

    def sb_kv(cur):
        wb, bw = stream_w(3584)
        proj512(wb, bw, psC[:], bC)
        qknorm(psC[:], bC, gsk, b_gsk, 0, out_bf=skn, b_out=b_skn)
        transposeH(skn, b_skn, skT[cur], b_skT[cur])
        wb, bw = stream_w(4096)
        proj512(wb, bw, psD[:], bD)
        S.op('act', lambda e: e.activation(out=sv[cur][:], in_=psD[:], func=AF.Copy), reads=[bD], writes=[b_sv[cur]])

    S.dma('sp', lambda e: e.dma_start(out=xt[0][:], in_=x_halo), b_xt[0], writes=[b_xt[0]])
    rmsnorm_T(xt[0][:], b_xt[0], gattn, b_gattn)
    sb_kv(1)
    dump(2, xt[0][:], [b_xt[0], b_sv[1], b_skT[1]])

    for n in range(NT):
        cur = n % 2; prv = 1 - cur
        S.dma('sp', lambda e, n=n: e.dma_start(out=xres[:], in_=x_own[n * P:(n + 1) * P, :]), b_xres, writes=[b_xres])
        rmsnorm_T(xres[:], b_xres, gattn, b_gattn)
        wb, bw = stream_w(0)
        proj512(wb, bw, psC[:], bC)
        qknorm(psC[:], bC, grq, b_grq, 0, scale_ap=qdec[:], b_scale=b_qdec, cos=cso[:, n, :], sin=sno[:, n, :],
               b_cs=[b_cso, b_sno], out_bf=qd, b_out=b_qd)
        transposeH(qd, b_qd, qdT, b_qdT)
        wb, bw = stream_w(512)
        proj512(wb, bw, psD[:], bD)
        qknorm(psD[:], bD, grk, b_grk, 0, scale_ap=kdec[:], b_scale=b_kdec, cos=cso[:, n, :], sin=sno[:, n, :],
               b_cs=[b_cso, b_sno], out_bf=kd, b_out=b_kd)
        transposeH(kd, b_kd, kdT, b_kdT)
        for hh in range(2):
            wb, bw = stream_w(1024 + hh * 512)
            proj512(wb, bw, psA[:, hh * 512:(hh + 1) * 512], bA)
        S.op('act', lambda e: e.activation(out=v_r[:], in_=psA[:], func=AF.Copy), reads=[bA], writes=[b_vr])
        for hh in range(2):
            wb, bw = stream_w(2048 + hh * 512)
            proj512(wb, bw, psB[:, hh * 512:(hh + 1) * 512], bB)
        S.op('act', lambda e: e.activation(out=m1[:], in_=psB[:], func=AF.Silu), reads=[bB], writes=[b_m1])
        S.op('pool', lambda e: e.tensor_tensor(out=rg[:], in0=m1[:], in1=ggn[:], op=ALU.mult), reads=[b_m1, b_ggn], writes=[b_rg])
        for h in range(H):
            S.op('pe', lambda e, h=h: e.matmul(out=psA[:, h * P:(h + 1) * P], lhsT=kdT[:, h, :], rhs=qdT[:, h, :], start=True, stop=True),
                 reads=[b_kdT, b_qdT], writes=[bA])
        S.op('dve', lambda e: e.tensor_tensor(out=pm[:], in0=psA[:], in1=maskT[:], op=ALU.mult), reads=[bA, b_maskT], writes=[b_pm])
        for h in range(H):
            S.op('pe', lambda e, h=h: e.matmul(out=psB[:, h * P:(h + 1) * P], lhsT=pm[:, h * P:(h + 1) * P], rhs=v_r[:, h * P:(h + 1) * P],
                                               start=True, stop=False, skip_group_check=True),
                 reads=[b_pm, b_vr], writes=[bB])
            S.op('pe', lambda e, h=h: e.matmul(out=psB[:, h * P:(h + 1) * P], lhsT=qdT[:, h, :], rhs=state_bf[:, h * P:(h + 1) * P],
                                               start=False, stop=True, skip_group_check=True),
                 reads=[b_qdT, b_state_bf], writes=[bB])
        S.op('dve', lambda e: e.tensor_tensor(out=state[:], in0=state[:], in1=cdec[:], op=ALU.mult), reads=[b_state, b_cdec], writes=[b_state])
        for rnd in range(2):
            for hl in range(4):
                h = rnd * 4 + hl
                S.op('pe', lambda e, h=h, hl=hl: e.matmul(out=psS[0:64, hl * P:(hl + 1) * P], lhsT=kd[:, h, :], rhs=v_r[:, h * P:(h + 1) * P],
                                                   start=True, stop=True, skip_group_check=True),
                     reads=[b_kd, b_vr], writes=[bS])
            S.op('dve', lambda e, rnd=rnd: e.tensor_tensor(out=state[:, rnd * 512:(rnd + 1) * 512], in0=state[:, rnd * 512:(rnd + 1) * 512],
                                                         in1=psS[0:64, :], op=ALU.add), reads=[b_state, bS], writes=[b_state])
        S.op('act', lambda e: e.activation(out=state_bf[:], in_=state[:], func=AF.Copy), reads=[b_state], writes=[b_state_bf])
        S.op('act', lambda e: e.activation(out=yf[:], in_=psB[:], func=AF.Copy), reads=[bB], writes=[b_yf])
        S.op('act', lambda e: e.activation(out=ysq[:], in_=psB[:], func=AF.Square), reads=[bB], writes=[b_ysq])
        S.op('dve', lambda e: e.tensor_reduce(out=gst[:, 0, :], in_=yf[:].rearrange("p (h d) -> p h d", d=P), axis=AX.X, op=ALU.add),
             reads=[b_yf], writes=[b_gst])
        S.op('dve', lambda e: e.tensor_reduce(out=gst[:, 1, :], in_=ysq[:].rearrange("p (h d) -> p h d", d=P), axis=AX.X, op=ALU.add),
             reads=[b_ysq], writes=[b_gst])
        S.op('dve', lambda e: e.tensor_scalar(out=gst[:, 2, :], in0=gst[:, 0, :], scalar1=1.0 / P, scalar2=None, op0=ALU.mult), reads=[b_gst], writes=[b_gst])
        S.op('dve', lambda e: e.tensor_tensor(out=gst[:, 3, :], in0=gst[:, 2, :], in1=gst[:, 2, :], op=ALU.mult), reads=[b_gst], writes=[b_gst])
        S.op('dve', lambda e: e.scalar_tensor_tensor(out=gst[:, 4, :], in0=gst[:, 1, :], scalar=1.0 / P, in1=gst[:, 3, :], op0=ALU.mult, op1=ALU.subtract),
             reads=[b_gst], writes=[b_gst])
        S.op('act', lambda e: e.activation(out=gst[:, 5, :], in_=gst[:, 4, :], func=AF.Sqrt, bias=eps_t[:, 0:1], scale=1.0), reads=[b_gst, b_eps], writes=[b_gst])
        S.op('dve', lambda e: e.reciprocal(out=gst[:, 3, :], in_=gst[:, 5, :]), reads=[b_gst], writes=[b_gst])
        y3 = yf[:].rearrange("p (h d) -> p h d", d=P)
        S.op('dve', lambda e: e.tensor_tensor(out=y3, in0=y3, in1=gst[:, 2, :].unsqueeze(2).to_broadcast([P, H, P]), op=ALU.subtract),
             reads=[b_yf, b_gst], writes=[b_yf])
        S.op('dve', lambda e: e.tensor_tensor(out=y3, in0=y3, in1=gst[:, 3, :].unsqueeze(2).to_broadcast([P, H, P]), op=ALU.mult),
             reads=[b_yf, b_gst], writes=[b_yf])
        S.op('pool', lambda e: e.tensor_tensor(out=ret[:], in0=yf[:], in1=rg[:], op=ALU.mult), reads=[b_yf, b_rg], writes=[b_ret])
        transpose8(ret, b_ret, retT, b_retT)
        dump(3, yf[:], [b_yf, b_retT])
        wb, bw = stream_w(3072)
        proj512(wb, bw, psC[:], bC)
        qknorm(psC[:], bC, gsq, b_gsq, 0, out_bf=sqn, b_out=b_sqn)
        transposeH(sqn, b_sqn, sqT, b_sqT)
        sb_kv(cur)
        for half in range(2):
            for blk, kT_ in ((0, skT[prv]), (1, skT[cur])):
                for hl in range(4):
                    h = half * 4 + hl
                    S.op('pe', lambda e, blk=blk, hl=hl, h=h, kT_=kT_: e.matmul(
                        out=psA[:, blk * 512 + hl * P: blk * 512 + (hl + 1) * P], lhsT=kT_[:, h, :],
                        rhs=sqT[:, h, :], start=True, stop=True),
                        reads=[b_skT[prv], b_skT[cur], b_sqT], writes=[bA])
            S.op('act', lambda e: e.activation(out=e_s[:], in_=psA[:], func=AF.Exp, scale=0.125), reads=[bA], writes=[b_es])
            S.op('act', lambda e: e.activation(out=sp_s[:], in_=e_s[:], func=AF.Ln, bias=one_t[:, 0:1], scale=1.0), reads=[b_es, b_one], writes=[b_sp])
            S.op('dve', lambda e: e.tensor_tensor(
                out=spm[:].rearrange("p (b h t) -> p b h t", b=2, h=4), in0=sp_s[:].rearrange("p (b h t) -> p b h t", b=2, h=4),
                in1=mstay[:].rearrange("p (b h t) -> p b h t", b=2, h=4), op=ALU.mult), reads=[b_sp, b_mstay], writes=[b_spm])
            S.op('pe', lambda e: e.matmul(out=psB[:, 0:512], lhsT=tri[:], rhs=spm[:, 0:512], start=True, stop=False), reads=[b_tri, b_spm], writes=[bB])
            S.op('pe', lambda e: e.matmul(out=psB[:, 0:512], lhsT=ones[:], rhs=spm[:, 512:1024], start=False, stop=True), reads=[b_ones, b_spm], writes=[bB])
            S.op('pe', lambda e: e.matmul(out=psB[:, 512:1024], lhsT=tri[:], rhs=spm[:, 512:1024], start=True, stop=False), reads=[b_tri, b_spm], writes=[bB])
            S.op('pe', lambda e: e.matmul(out=psB[:, 512:1024], lhsT=ident[:], rhs=mpos[:], start=False, stop=True), reads=[b_ident, b_mpos], writes=[bB])
            S.op('dve', lambda e: e.scalar_tensor_tensor(out=u_s[:], in0=psA[:], scalar=0.125, in1=sp_s[:], op0=ALU.mult, op1=ALU.subtract),
                 reads=[bA, b_sp], writes=[b_us])
            S.op('dve', lambda e: e.tensor_tensor(out=u_s[:], in0=u_s[:], in1=psB[:], op=ALU.subtract), reads=[b_us, bB], writes=[b_us])
            S.op('act', lambda e: e.activation(out=w_s[:], in_=u_s[:], func=AF.Exp), reads=[b_us], writes=[b_ws])
            for hl in range(4):
                h = half * 4 + hl
                po = (h % 2) * 64
                for blk, svb, bsv in ((0, sv[prv], b_sv[prv]), (1, sv[cur], b_sv[cur])):
                    S.op('pe', lambda e, h=h, hl=hl, po=po, blk=blk, svb=svb: e.matmul(
                        out=psS[po:po + 64, (h // 2) * P:(h // 2 + 1) * P], lhsT=svb[:, h * 64:(h + 1) * 64],
                        rhs=w_s[:, blk * 512 + hl * P: blk * 512 + (hl + 1) * P], start=(blk == 0), stop=(blk == 1), skip_group_check=True),
                        reads=[bsv, b_ws], writes=[bS])
        S.op('act', lambda e: e.activation(out=sbT[:].rearrange("p c t -> p (c t)"), in_=psS[:], func=AF.Copy), reads=[bS], writes=[b_sbT])
        if STAGE == 4:
            S.op('act', lambda e: e.activation(out=m2[:, 0:512], in_=psS[:], func=AF.Copy), reads=[bS], writes=[b_m2])
            dump(4, m2[:, 0:512], [b_m2], 512)
        for hh in range(2):
            for c in range(8):
                S.op('pe', lambda e, c=c, hh=hh: e.matmul(out=psA[:, hh * 512:(hh + 1) * 512], lhsT=retT[:, c, :], rhs=wbra[:, c, hh * 512:(hh + 1) * 512],
                                                          start=(c == 0), stop=(c == 7)), reads=[b_retT, b_wbra], writes=[bA])
            for c in range(4):
                S.op('pe', lambda e, c=c, hh=hh: e.matmul(out=psB[:, hh * 512:(hh + 1) * 512], lhsT=sbT[:, c, :], rhs=wbrb[:, c, hh * 512:(hh + 1) * 512],
                                                          start=(c == 0), stop=(c == 3)), reads=[b_sbT, b_wbrb], writes=[bB])
        for gi, (gt, bg) in enumerate(((siga, b_siga), (sigb, b_sigb))):
            for hh in range(2):
                wb, bw = stream_w(4608 + gi * 1024 + hh * 512)
                pst, bp = (psC, bC) if hh == 0 else (psD, bD)
                proj512(wb, bw, pst[:], bp)
                S.op('act', lambda e, gt=gt, hh=hh, pst=pst: e.activation(out=gt[:, hh * 512:(hh + 1) * 512], in_=pst[:], func=AF.Sigmoid),
                     reads=[bp], writes=[bg])
        S.op('dve', lambda e: e.tensor_tensor(out=m1[:], in0=psA[:], in1=siga[:], op=ALU.mult), reads=[bA, b_siga], writes=[b_m1])
        S.op('dve', lambda e: e.tensor_tensor(out=m2[:], in0=psB[:], in1=sigb[:], op=ALU.mult), reads=[bB, b_sigb], writes=[b_m2])
        S.op('pool', lambda e: e.tensor_tensor(out=mixed[:], in0=m1[:], in1=m2[:], op=ALU.add), reads=[b_m1, b_m2], writes=[b_mixed])
        transpose8(mixed, b_mixed, mixT, b_mixT)
        for hh in range(2):
            for c in range(8):
                S.op('pe', lambda e, c=c, hh=hh: e.matmul(out=psA[:, hh * 512:(hh + 1) * 512], lhsT=mixT[:, c, :], rhs=wout[:, c, hh * 512:(hh + 1) * 512],
                                                          start=(c == 0), stop=(c == 7)), reads=[b_mixT, b_wout], writes=[bA])
        S.op('dve', lambda e: e.tensor_tensor(out=x1[:], in0=psA[:], in1=xres[:], op=ALU.add), reads=[bA, b_xres], writes=[b_x1])
        dump(5, x1[:], [b_x1])
        rmsnorm_T(x1[:], b_x1, gffn, b_gffn, keep_f32=hn[:], b_keep=b_hn)
        for g4 in range(4):
            wqb, b_wq = stream_w(g4 * 512, w_q)
            for gl in range(4):
                g = g4 * 4 + gl
                pst = psA[:, gl * P:(gl + 1) * P] if g4 % 2 == 0 else psB[:, gl * P:(gl + 1) * P]
                bp = bA if g4 % 2 == 0 else bB
                for c in range(8):
                    S.op('pe', lambda e, c=c, gl=gl, pst=pst, wqb=wqb: e.matmul(out=pst, lhsT=wqb[:, c, gl * P:(gl + 1) * P], rhs=xnT[:, c, :],
                                                                     start=(c == 0), stop=(c == 7)), reads=[b_wq, b_xnT], writes=[bp])
            src = psA if g4 % 2 == 0 else psB
            bp = bA if g4 % 2 == 0 else bB
            S.op('act', lambda e, g4=g4, src=src: e.activation(out=qT[:, g4 * 4:(g4 + 1) * 4, :].rearrange("p g t -> p (g t)"), in_=src[:, 0:512], func=AF.Copy),
                 reads=[bp], writes=[b_qT])
        for g4 in range(4):
            pst, bp = (psA, bA) if g4 % 2 == 0 else (psB, bB)
            for gl in range(4):
                g = g4 * 4 + gl
                kk, bk = (k1, b_k1) if g % 2 == 0 else (k2, b_k2)
                S.op('pe', lambda e, g=g, gl=gl, pst=pst, kk=kk: e.matmul(out=pst[:, gl * P:(gl + 1) * P], lhsT=qT[:, g, :], rhs=kk[:], start=True, stop=True),
                     reads=[b_qT, bk], writes=[bp])
            S.op('act', lambda e, g4=g4, pst=pst: e.activation(out=sc[:, g4 * 4:(g4 + 1) * 4, :].rearrange("p g n -> p (g n)"), in_=pst[:, 0:512], func=AF.Copy),
                 reads=[bp], writes=[b_sc])
        bg_tv = [S.buf("tv%d" % g) for g in range(16)]; bg_ti = [S.buf("ti%d" % g) for g in range(16)]; bg_s2 = [S.buf("s2%d" % g) for g in range(16)]
        for g in range(16):
            S.op('dve', lambda e, g=g: e.max(out=tv[:, g, 0:8], in_=sc[:, g, :]), reads=[b_sc], writes=[bg_tv[g]])
        for g in range(16):
            S.op('dve', lambda e, g=g: e.match_replace(out=sc2[:, g, :], in_to_replace=tv[:, g, 0:8], in_values=sc[:, g, :], imm_value=NEG),
                 reads=[b_sc, bg_tv[g]], writes=[bg_s2[g]])
        for g in range(16):
            S.op('dve', lambda e, g=g: e.max_index(out=ti[:, g, 0:8], in_max=tv[:, g, 0:8], in_values=sc[:, g, :]), reads=[b_sc, bg_tv[g]], writes=[bg_ti[g]])
        for g in range(16):
            S.op('dve', lambda e, g=g: e.max(out=tv[:, g, 8:16], in_=sc2[:, g, :]), reads=[bg_s2[g]], writes=[bg_tv[g]])
        for g in range(16):
            S.op('dve', lambda e, g=g: e.max_index(out=ti[:, g, 8:16], in_max=tv[:, g, 8:16], in_values=sc2[:, g, :]), reads=[bg_s2[g], bg_tv[g]], writes=[bg_ti[g]])
        b_tv.w = None; b_ti.w = None
        S.op('dve', lambda e: e.tensor_copy(out=tif[:], in_=ti[:]), reads=bg_ti + bg_tv + bg_s2 + [b_sc2], writes=[b_tif, b_tv, b_ti, b_sc2])
        tv4 = tv[:].rearrange("p (h s) k -> p h s k", s=2)
        tif4 = tif[:].rearrange("p (h s) k -> p h s k", s=2)
        S.op('dve', lambda e: e.tensor_tensor(out=cand.rearrange("p h (a b) -> p h a b", a=16),
                                              in0=tv4[:, :, 0, :].unsqueeze(3).to_broadcast([P, H, 16, 16]),
                                              in1=tv4[:, :, 1, :].unsqueeze(2).to_broadcast([P, H, 16, 16]), op=ALU.add),
             reads=[b_tv], writes=[b_cand])
        bh_ts = [S.buf("ts%d" % h) for h in range(H)]; bh_tp = [S.buf("tp%d" % h) for h in range(H)]; bh_c2 = [S.buf("c2%d" % h) for h in range(H)]
        for h in range(H):
            S.op('dve', lambda e, h=h: e.max(out=tsv[:, h, 0:8], in_=cand[:, h, :]), reads=[b_cand], writes=[bh_ts[h]])
        for h in range(H):
            S.op('dve', lambda e, h=h: e.match_replace(out=cand2[:, h, :], in_to_replace=tsv[:, h, 0:8], in_values=cand[:, h, :], imm_value=NEG),
                 reads=[b_cand, bh_ts[h], b_cand2], writes=[bh_c2[h]])
        for h in range(H):
            S.op('dve', lambda e, h=h: e.max_index(out=tpos[:, h, 0:8], in_max=tsv[:, h, 0:8], in_values=cand[:, h, :]), reads=[b_cand, bh_ts[h]], writes=[bh_tp[h]])
        for h in range(H):
            S.op('dve', lambda e, h=h: e.max(out=tsv[:, h, 8:16], in_=cand2[:, h, :]), reads=[bh_c2[h]], writes=[bh_ts[h]])
        for h in range(H):
            S.op('dve', lambda e, h=h: e.max_index(out=tpos[:, h, 8:16], in_max=tsv[:, h, 8:16], in_values=cand2[:, h, :]), reads=[bh_c2[h], bh_ts[h]], writes=[bh_tp[h]])
        b_tsv.w = None; b_tpos.w = None
        S.op('dve', lambda e: e.tensor_copy(out=tposf[:], in_=tpos[:]), reads=bh_tp + bh_ts + bh_c2, writes=[b_tposf, b_tsv, b_tpos, b_cand2])
        S.op('dve', lambda e: e.tensor_tensor(out=oh, in0=tposf[:].unsqueeze(3).to_broadcast([P, H, 16, 16]),
                                              in1=thr16[:].unsqueeze(1).unsqueeze(1).to_broadcast([P, H, 16, 16]), op=ALU.is_ge),
             reads=[b_tposf, b_iota], writes=[b_oh])
        S.op('dve', lambda e: e.tensor_reduce(out=ta[:], in_=oh, axis=AX.X, op=ALU.add), reads=[b_oh], writes=[b_ta])
        S.op('dve', lambda e: e.tensor_scalar(out=ta[:], in0=ta[:], scalar1=-1.0, scalar2=None, op0=ALU.add), reads=[b_ta], writes=[b_ta])
        S.op('dve', lambda e: e.scalar_tensor_tensor(out=tb[:], in0=ta[:], scalar=-16.0, in1=tposf[:], op0=ALU.mult, op1=ALU.add),
             reads=[b_ta, b_tposf], writes=[b_tb])
        io_b = iota16[:].unsqueeze(1).unsqueeze(1).to_broadcast([P, H, 16, 16])
        for sel, half, dst, bd in ((ta, 0, idx1, b_idx1), (tb, 1, idx2, b_idx2)):
            bsel = b_ta if half == 0 else b_tb
            S.op('dve', lambda e, sel=sel: e.tensor_tensor(out=oh, in0=sel[:].unsqueeze(3).to_broadcast([P, H, 16, 16]), in1=io_b, op=ALU.is_equal),
                 reads=[bsel, b_iota], writes=[b_oh])
            S.op('dve', lambda e, half=half: e.tensor_tensor(out=oh, in0=oh, in1=tif4[:, :, half, :].unsqueeze(2).to_broadcast([P, H, 16, 16]), op=ALU.mult),
                 reads=[b_oh, b_tif], writes=[b_oh])
            S.op('dve', lambda e, dst=dst: e.tensor_reduce(out=dst[:], in_=oh, axis=AX.X, op=ALU.add), reads=[b_oh], writes=[bd])
        S.op('dve', lambda e: e.scalar_tensor_tensor(out=idx1[:], in0=idx1[:], scalar=128.0, in1=idx2[:], op0=ALU.mult, op1=ALU.add),
             reads=[b_idx1, b_idx2], writes=[b_idx1])
        S.op('dve', lambda e: e.tensor_copy(out=eidx[:], in_=idx1[:].rearrange("p h k -> p (h k)")), reads=[b_idx1], writes=[b_eidx])
        S.op('dve', lambda e: e.tensor_tensor(out=gw[:], in0=tsv[:], in1=tsv[:, :, 0:1].to_broadcast([P, H, 16]), op=ALU.subtract), reads=[b_tsv], writes=[b_gw])
        S.op('act', lambda e: e.activation(out=gw[:], in_=gw[:], func=AF.Exp), reads=[b_gw], writes=[b_gw])
        S.op('dve', lambda e: e.tensor_reduce(out=gs[:, 0, :], in_=gw[:], axis=AX.X, op=ALU.add), reads=[b_gw], writes=[b_gs])
        S.op('dve', lambda e: e.reciprocal(out=gs[:, 1, :], in_=gs[:, 0, :]), reads=[b_gs], writes=[b_gs])
        S.op('dve', lambda e: e.tensor_tensor(out=gw[:], in0=gw[:], in1=gs[:, 1, :].unsqueeze(2).to_broadcast([P, H, 16]), op=ALU.mult), reads=[b_gw, b_gs], writes=[b_gw])
        if STAGE == 6:
            S.op('dve', lambda e: e.tensor_copy(out=m2[:, 0:128], in_=idx1[:].rearrange("p h k -> p (h k)")), reads=[b_idx1], writes=[b_m2])
            S.op('dve', lambda e: e.tensor_copy(out=m2[:, 128:256], in_=gw[:].rearrange("p h k -> p (h k)")), reads=[b_gw], writes=[b_m2])
            dump(6, m2[:, 0:256], [b_m2], 256)
        GS = 2
        NGRP = 128 // GS
        gwf = gw[:].rearrange("p h k -> p (h k)")

        def emit_gather(g):
            for k in range(GS):
                j = g * GS + k
                gb_, bgb = gbuf[j % NG], b_gbuf[j % NG]
                S.dma('pool', lambda e, j=j, gb_=gb_: e.indirect_dma_start(out=gb_[:], out_offset=None, in_=uv_bf,
                                                                          in_offset=bass.IndirectOffsetOnAxis(ap=eidx[:, j:j + 1], axis=0)),
                      gsem[j % NG], reads=[b_eidx, b_ubf], writes=[bgb])

        def emit_dots(g):
            for k in range(GS):
                j = g * GS + k
                gb_, bgb = gbuf[j % NG], b_gbuf[j % NG]
                S.op('dve', lambda e, j=j, gb_=gb_: e.scalar_tensor_tensor(out=junk[:], in0=gb_[:, 0:D], scalar=1.0, in1=hn[:],
                                                                          op0=ALU.mult, op1=ALU.mult, accum_out=hv[:, j:j + 1]),
                     reads=[bgb, b_hn], writes=[b_junk, b_hv])

        def emit_pre(g):
            c = slice(g * GS, (g + 1) * GS)
            S.op('act', lambda e: e.activation(out=ga_[:, 0, c], in_=hv[:, c], func=AF.Square), reads=[b_hv], writes=[b_ga])
            S.op('act', lambda e: e.activation(out=ga_[:, 1, c], in_=ga_[:, 0, c], func=AF.Identity, scale=0.0713548162726, bias=gk_t[:, 0:1]),
                 reads=[b_ga, b_gk], writes=[b_ga])
            for k in range(GS):
                j = g * GS + k
                S.op('act', lambda e, j=j: e.activation(out=ga_[:, 3, j:j + 1], in_=ga_[:, 1, j:j + 1], func=AF.Sigmoid, scale=hv[:, j:j + 1]),
                     reads=[b_ga, b_hv], writes=[b_ga2])
            S.op('dve', lambda e: e.tensor_tensor(out=ga_[:, 4, c], in0=hv[:, c], in1=gwf[:, c], op=ALU.mult), reads=[b_hv, b_gw], writes=[b_ga3])

        def emit_post(g):
            for k in range(GS):
                j = g * GS + k
                S.op('act', lambda e, j=j: e.activation(out=aw[:, j:j + 1], in_=ga_[:, 3, j:j + 1], func=AF.Copy, scale=ga_[:, 4, j:j + 1]),
                     reads=[b_ga2, b_ga3], writes=[b_aw])

        def emit_axpy(g):
            for k in range(GS):
                j = g * GS + k
                gb_, bgb = gbuf[j % NG], b_gbuf[j % NG]
                dgj, bdg = dg[j % 4], b_dg[j % 4]
                S.op('act', lambda e, j=j, dgj=dgj: e.activation(out=dgj[:], in_=identF[:], func=AF.Copy, scale=aw[:, j:j + 1]),
                     reads=[b_identF, b_aw], writes=[bdg])
                for hh in range(2):
                    S.op('pe', lambda e, j=j, hh=hh, dgj=dgj, gb_=gb_: e.matmul(out=psA[:, hh * 512:(hh + 1) * 512], lhsT=dgj[:],
                                                                            rhs=gb_[:, D + hh * 512:D + (hh + 1) * 512],
                                                                            start=(j == 0), stop=(j == 127)), reads=[bdg, bgb], writes=[bA])

        for g0 in range(3):
            emit_gather(g0)
        for st_ in range(NGRP + 1):
            if 0 <= st_ - 1 < NGRP:
                emit_pre(st_ - 1)
            if st_ < NGRP:
                emit_dots(st_)
            if 0 <= st_ - 1 < NGRP:
                emit_post(st_ - 1)
                emit_axpy(st_ - 1)
            if st_ + 3 < NGRP:
                emit_gather(st_ + 3)
        S.op('dve', lambda e: e.tensor_tensor(out=acc[:], in0=psA[:], in1=x1[:], op=ALU.add), reads=[bA, b_x1], writes=[b_acc])
        S.dma('sp', lambda e, n=n: e.dma_start(out=y_out[n * P:(n + 1) * P, :], in_=acc[:]), b_yout, reads=[b_acc], writes=[b_yout])

    S.wait_all('sp', [b_yout])
    es.close()
    return nc, None


def _consts():
    hs = np.arange(H, dtype=np.float64)
    gam = 1.0 - 2.0 ** (-5.0 - hs)
    i = np.arange(P, dtype=np.float64)
    c = {}
    c["c_ident"] = np.eye(P, dtype=np.float32)
    c["c_tri"] = (i[:, None] > i[None, :]).astype(np.float32)
    c["c_ones"] = np.ones((P, P), np.float32)
    mp = 1.0e4 * (i[:, None] >= i[None, :]).astype(np.float32)
    c["c_mpos"] = np.tile(mp, (1, 4)).astype(np.float32)
    ms = np.ones((P, 2, 4, P), np.float32)
    ms[:, 1, :, :] = (i[:, None] < i[None, :]).astype(np.float32)[:, None, :]
    c["c_mstay"] = ms.reshape(P, 1024)
    mk = np.zeros((P, H, P), np.float64)
    for h in range(H):
        mk[:, h, :] = (i[None, :] >= i[:, None]) * gam[h] ** (-128.0)
    c["c_maskT"] = mk.reshape(P, 1024).astype(np.float32)
    c["c_qdec"] = (gam[None, :] ** (i[:, None] + 1.0)).astype(np.float32)
    c["c_kdec"] = (0.125 * gam[None, :] ** (127.0 - i[:, None])).astype(np.float32)
    cd = np.zeros((64, D), np.float64)
    for h in range(H):
        cd[:, h * P:(h + 1) * P] = gam[h] ** 128.0
    c["c_cdec"] = cd.astype(np.float32)
    return c, gam


def _rope_tabs(pos):
    half = 32
    freqs = (np.float32(10000.0) ** (-np.arange(half, dtype=np.float32) / np.float32(half))).astype(np.float32)
    ang = (pos.astype(np.float32)[:, :, None] * freqs[None, None, :]).astype(np.float32).astype(np.float64)
    return np.cos(ang).astype(np.float32), np.sin(ang).astype(np.float32)


_CACHE = {}


def kernel(x, norm_attn, w_in, ret_q_norm, ret_k_norm, ret_group_norm, sb_q_norm, sb_k_norm,
           w_branch_ret, w_branch_sb, w_out, norm_ffn, peer_w_q, peer_sub_keys_1,
           peer_sub_keys_2, peer_u, peer_v):
    f = np.float32
    x = np.asarray(x, f)
    B, SEQ, _ = x.shape
    assert B == 1
    NT = SEQ // (NCORES * P)
    NPRE = NT * (NCORES - 1) if FORCE_NPRE is None else FORCE_NPRE
    x2 = x[0]
    key = (NT, NPRE)
    if key not in _CACHE:
        try:
            _CACHE[key] = build_program(NT, NPRE)
        except _Stop:
            _H['es'].close()
            _CACHE[key] = (_H['nc'], None)
    nc, _es = _CACHE[key]
    cst, gam = _consts()
    rep = lambda v, n: np.ascontiguousarray(np.broadcast_to(np.tile(np.asarray(v, f).reshape(-1), n)[None, :], (P, np.asarray(v).size * n)))
    shared = dict(cst)
    shared.update({
        "g_attn": rep(norm_attn[0], 1), "g_ffn": rep(norm_ffn[0], 1), "g_gn": rep(ret_group_norm[0], 1),
        "g_rq": rep(ret_q_norm[0], 8), "g_rk": rep(ret_k_norm[0], 8), "g_sq": rep(sb_q_norm[0], 8), "g_sk": rep(sb_k_norm[0], 8),
        "w_in": np.ascontiguousarray(w_in[0], f), "w_bra": np.ascontiguousarray(w_branch_ret[0], f),
        "w_brb": np.ascontiguousarray(w_branch_sb[0], f), "w_out": np.ascontiguousarray(w_out[0], f),
        "w_q": np.ascontiguousarray(peer_w_q[0], f),
        "k1T": np.ascontiguousarray(np.asarray(peer_sub_keys_1[0], f).T), "k2T": np.ascontiguousarray(np.asarray(peer_sub_keys_2[0], f).T),
        "u_tab": np.ascontiguousarray(peer_u[0], f), "v_tab": np.ascontiguousarray(peer_v[0], f),
    })
    in_maps = []
    pp = np.arange(P, dtype=np.float64)
    for c in range(NCORES):
        t0 = c * NT
        m = dict(shared)
        m["x_own"] = np.ascontiguousarray(x2[t0 * P:(t0 + NT) * P])
        m["x_halo"] = np.ascontiguousarray(x2[(t0 - 1) * P:t0 * P]) if c > 0 else np.zeros((P, D), f)
        npre = max(NPRE, 1)
        xp = np.zeros((npre * P, D), f)
        gt = np.arange(npre) + (t0 - NPRE)
        nvalid = min(t0, NPRE)
        if nvalid > 0:
            xp[(NPRE - nvalid) * P:NPRE * P] = x2[(t0 - nvalid) * P:t0 * P]
        m["x_pre"] = xp
        pos_own = (np.arange(NT)[None, :] + t0) * P + pp[:, None]
        m["cos_own"], m["sin_own"] = _rope_tabs(pos_own)
        pos_pre = np.maximum(gt, 0)[None, :] * P + pp[:, None]
        m["cos_pre"], m["sin_pre"] = _rope_tabs(pos_pre)
        ks = np.zeros((P, npre, H), np.float64)
        for mm in range(npre):
            ks[:, mm, :] = 0.125 * gam[None, :] ** (127.0 - pp[:, None]) * gam[None, :] ** (128.0 * (NPRE - 1 - mm))
        m["ksc_pre"] = ks.astype(f)
        in_maps.append(m)
    res = run_bass_kernel_spmd(nc, in_maps, core_ids=list(range(NCORES)), **RUN_KW)
    _H['res'] = res
    out = np.concatenate([np.asarray(r["y_out"], f) for r in res.results], axis=0)
    return out.reshape(1, SEQ, D)
```

```python
import numpy as np
from contextlib import ExitStack
import concourse.bass as bass
import concourse.mybir as mybir
from concourse.bass_utils import run_bass_kernel_spmd

F32 = mybir.dt.float32
BF16 = mybir.dt.bfloat16
U32 = mybir.dt.uint32
I32 = mybir.dt.int32
AF = mybir.ActivationFunctionType
ALU = mybir.AluOpType
AX = mybir.AxisListType

NCORES = 8
D = 1024
P = 128
H = 8
EPS = 1e-6
NEXP = 16384
INW = 6656
NEG = -1.0e30


class Buf:
    def __init__(self, name):
        self.name = name
        self.w = None
        self.r = {}
        self.dsem = None
        self.dcnt = 0


class Sched:
    def __init__(self, nc, es):
        self.nc = nc
        self.es = es
        self.E = {'pe': nc.tensor, 'act': nc.scalar, 'dve': nc.vector, 'pool': nc.gpsimd, 'sp': nc.sync}
        self.sem = {e: es.enter_context(nc.semaphore('sem_' + e)) for e in self.E}
        self.cnt = {e: 0 for e in self.E}
        self.known = {e: {} for e in self.E}
        self.nsem = 0

    def buf(self, name, dma=False):
        b = Buf(name)
        if dma:
            b.dsem = self.es.enter_context(self.nc.semaphore('d_' + name))
        return b

    def _wait(self, e, ev):
        if ev is None:
            return
        sem, val, src = ev
        if src == e and e == 'pe':
            return
        k = self.known[e]
        if k.get(id(sem), 0) >= val:
            return
        self.E[e].wait_ge(sem, val)
        k[id(sem)] = val

    @staticmethod
    def _flat(bs):
        out = []
        for b in bs:
            if isinstance(b, (list, tuple)):
                out.extend(Sched._flat(b))
            else:
                out.append(b)
        return out

    def _deps(self, e, reads, writes):
        reads = self._flat(reads); writes = self._flat(writes)
        for b in reads:
            self._wait(e, b.w)
        for b in writes:
            self._wait(e, b.w)
            for ev in list(b.r.values()):
                self._wait(e, ev)

    def _post(self, ev, reads, writes):
        reads = self._flat(reads); writes = self._flat(writes)
        for b in reads:
            old = b.r.get(id(ev[0]))
            if old is None or old[1] < ev[1]:
                b.r[id(ev[0])] = ev
        for b in writes:
            b.w = ev
            b.r = {}

    cap = None

    def replay(self, cap, k=None):
        n = len(cap) if k is None else min(k, len(cap))
        for _ in range(n):
            kind, a = cap.pop(0)
            if kind == 'op':
                self.op(*a)
            else:
                self.dma(*a)

    def op(self, e, fn, reads=(), writes=()):
        if self.cap is not None:
            self.cap.append(('op', (e, fn, tuple(reads), tuple(writes))))
            return
        self._deps(e, reads, writes)
        ins = fn(self.E[e])
        self.cnt[e] += 1
        ins.then_inc(self.sem[e], 1)
        self._post((self.sem[e], self.cnt[e], e), reads, writes)

    def dma(self, q, fn, dbuf, reads=(), writes=()):
        if self.cap is not None:
            self.cap.append(('dma', (q, fn, dbuf, tuple(reads), tuple(writes))))
            return
        self._deps(q, reads, writes)
        ins = fn(self.E[q])
        dbuf.dcnt += 16
        ins.then_inc(dbuf.dsem, 16)
        self._post((dbuf.dsem, dbuf.dcnt, 'dma'), reads, writes)

    def wait_all(self, e, bufs):
        for b in self._flat(bufs):
            self._wait(e, b.w)
            for ev in list(b.r.values()):
                self._wait(e, ev)


class _Stop(Exception):
    pass


_H = {}
STAGE = 99
RUN_KW = {}
SKIP_GATHER = False
FORCE_NPRE = None


def build_program(NT, NPRE, dbg=False):
    nc = bass.Bass("TRN2", target_bir_lowering=False)
    es = ExitStack()
    S = Sched(nc, es)
    _H['nc'] = nc; _H['es'] = es

    def din(name, shape, dt=F32):
        return nc.dram_tensor(name, list(shape), dt, kind="ExternalInput").ap()

    x_own = din("x_own", [NT * P, D])
    x_halo = din("x_halo", [P, D])
    x_pre = din("x_pre", [max(NPRE, 1) * P, D])
    cos_own = din("cos_own", [P, NT, 32]); sin_own = din("sin_own", [P, NT, 32])
    cos_pre = din("cos_pre", [P, max(NPRE, 1), 32]); sin_pre = din("sin_pre", [P, max(NPRE, 1), 32])
    ksc_pre = din("ksc_pre", [P, max(NPRE, 1), H])
    g_attn = din("g_attn", [P, D]); g_ffn = din("g_ffn", [P, D]); g_gn = din("g_gn", [P, D])
    g_rq = din("g_rq", [P, 512]); g_rk = din("g_rk", [P, 512]); g_sq = din("g_sq", [P, 512]); g_sk = din("g_sk", [P, 512])
    c_ident = din("c_ident", [P, P]); c_tri = din("c_tri", [P, P]); c_ones = din("c_ones", [P, P])
    c_mpos = din("c_mpos", [P, 512]); c_mstay = din("c_mstay", [P, 1024])
    c_maskT = din("c_maskT", [P, 1024]); c_qdec = din("c_qdec", [P, H]); c_kdec = din("c_kdec", [P, H])
    c_cdec = din("c_cdec", [64, D])
    w_in = din("w_in", [D, INW]); w_bra = din("w_bra", [D, D]); w_brb = din("w_brb", [512, D]); w_out = din("w_out", [D, D])
    w_q = din("w_q", [D, 2048]); k1T = din("k1T", [P, P]); k2T = din("k2T", [P, P])
    u_tab = din("u_tab", [NEXP, D]); v_tab = din("v_tab", [NEXP, D])
    y_out = nc.dram_tensor("y_out", [NT * P, D], F32, kind="ExternalOutput").ap()
    uv_bf = nc.dram_tensor("uv_bf", [NEXP, 2 * D], BF16, kind="Internal").ap()
    def dump(stage, src_ap, bsrc, ncols=D):
        if STAGE != stage:
            return
        b_d = S.buf("dump", dma=True)
        npart = src_ap.shape[0]
        S.dma('sp', lambda e: e.dma_start(out=y_out[0:npart, 0:ncols], in_=src_ap), b_d, reads=bsrc, writes=[b_d])
        S.wait_all('sp', [b_d])
        raise _Stop()

    tot = [0]

    def sb(name, shape, dt=F32):
        n = int(np.prod(shape[1:])) * (4 if dt in (F32, U32, I32) else 2)
        tot[0] += n
        if dbg:
            print("SB", name, n, tot[0])
        return es.enter_context(nc.sbuf_tensor(name, list(shape), dt))

    def ps(name, shape, dt=F32):
        return es.enter_context(nc.psum_tensor(name, list(shape), dt))

    psA = ps("psA", [P, 1024]); bA = S.buf("psA")
    psB = ps("psB", [P, 1024]); bB = S.buf("psB")
    psC = ps("psC", [P, 512]); bC = S.buf("psC")
    psD = ps("psD", [P, 512]); bD = S.buf("psD")
    psT = ps("psT", [P, 1024], BF16); bT = S.buf("psT")
    psS = ps("psS", [P, 512]); bS = S.buf("psS")

    consts = []

    def load_const(name, src, shape, dt=F32, q='sp'):
        t = sb(name, shape, dt)
        b = S.buf(name, dma=True)
        S.dma(q, lambda e: e.dma_start(out=t[:], in_=src), b, writes=[b])
        consts.append(b)
        return t, b

    def load_cast(name, src, shape):
        return load_const(name, src, shape, BF16, q='pool')

    ident, b_ident = load_cast("ident", c_ident, [P, P])
    tri, b_tri = load_cast("tri", c_tri, [P, P])
    ones, b_ones = load_cast("ones", c_ones, [P, P])
    mpos, b_mpos = load_cast("mpos", c_mpos, [P, 512])
    mstay, b_mstay = load_cast("mstay", c_mstay, [P, 1024])
    maskT, b_maskT = load_const("maskT", c_maskT, [P, 1024])
    qdec, b_qdec = load_const("qdec", c_qdec, [P, H])
    kdec, b_kdec = load_const("kdec", c_kdec, [P, H])
    cdec, b_cdec = load_const("cdec", c_cdec, [64, D])
    gattn, b_gattn = load_const("gattn", g_attn, [P, D])
    gffn, b_gffn = load_const("gffn", g_ffn, [P, D])
    ggn, b_ggn = load_const("ggn", g_gn, [P, D])
    grq, b_grq = load_const("grq", g_rq, [P, 512])
    grk, b_grk = load_const("grk", g_rk, [P, 512])
    gsq, b_gsq = load_const("gsq", g_sq, [P, 512])
    gsk, b_gsk = load_const("gsk", g_sk, [P, 512])
    cso, b_cso = load_const("cso", cos_own, [P, NT, 32])
    sno, b_sno = load_const("sno", sin_own, [P, NT, 32])

    def wview(w, c0, n):
        return w[:, c0:c0 + n].rearrange("(c p) n -> p c n", p=P)

    TPbig = sb("TPbig", [P, 10, D])
    TP = [TPbig[:, i, :] for i in range(10)]
    b_TP = [S.buf("TP%d" % i, dma=True) for i in range(10)]
    xt = [TP[0], TP[1]]
    b_xt = [b_TP[0], b_TP[1]]
    junk = sb("junk", [P, D], BF16); b_junk = S.buf("junk")
    st4 = sb("st4", [P, 4]); b_st4 = S.buf("st4")

    def rmsnorm_T(src, b_src, gain, b_gain, keep_f32=None, b_keep=None):
        S.op('act', lambda e: e.activation(out=junk[:], in_=src, func=AF.Square, accum_out=st4[:, 0:1]),
             reads=[b_src], writes=[b_junk, b_st4])
        S.op('act', lambda e: e.activation(out=st4[:, 1:2], in_=st4[:, 0:1], func=AF.Sqrt, bias=eps_t[:, 0:1], scale=1.0 / D),
             reads=[b_st4, b_eps], writes=[b_st4])
        S.op('dve', lambda e: e.reciprocal(out=st4[:, 2:3], in_=st4[:, 1:2]), reads=[b_st4], writes=[b_st4])
        if keep_f32 is not None:
            S.op('dve', lambda e: e.scalar_tensor_tensor(out=keep_f32, in0=src, scalar=st4[:, 2:3], in1=gain[:],
                                                         op0=ALU.mult, op1=ALU.mult),
                 reads=[b_src, b_st4, b_gain], writes=[b_keep])
            S.op('act', lambda e: e.activation(out=xn[:], in_=keep_f32, func=AF.Copy), reads=[b_keep], writes=[b_xn])
        else:
            S.op('dve', lambda e: e.scalar_tensor_tensor(out=xn[:], in0=src, scalar=st4[:, 2:3], in1=gain[:],
                                                         op0=ALU.mult, op1=ALU.mult),
                 reads=[b_src, b_st4, b_gain], writes=[b_xn])
        transpose8(xn, b_xn, xnT, b_xnT)

    def transpose8(src, b_src, dst, b_dst, nchunk=8):
        for half in range((nchunk + 3) // 4):
            n = min(4, nchunk - half * 4)
            for k in range(n):
                c = half * 4 + k
                S.op('pe', lambda e, c=c, k=k: e.transpose(out=psT[:, k * P:(k + 1) * P], in_=src[:, c * P:(c + 1) * P],
                                                           identity=ident[:]),
                     reads=[b_src, b_ident], writes=[bT])
            eng = 'act' if half % 2 == 0 else 'dve'
            if eng == 'act':
                S.op('act', lambda e, half=half, n=n: e.activation(
                    out=dst[:, half * 4:half * 4 + n, :].rearrange("p c t -> p (c t)"), in_=psT[:, 0:n * P], func=AF.Copy),
                    reads=[bT], writes=[b_dst])
            else:
                S.op('dve', lambda e, half=half, n=n: e.tensor_copy(
                    out=dst[:, half * 4:half * 4 + n, :].rearrange("p c t -> p (c t)"), in_=psT[:, 0:n * P]),
                    reads=[bT], writes=[b_dst])

    def transposeH(src3, b_src, dst, b_dst):
        for h in range(H):
            S.op('pe', lambda e, h=h: e.transpose(out=psT[0:64, h * P:(h + 1) * P], in_=src3[:, h, :], identity=ident[:]),
                 reads=[b_src, b_ident], writes=[bT])
        S.op('act', lambda e: e.activation(out=dst[:].rearrange("p h t -> p (h t)"), in_=psT[0:64, :], func=AF.Copy),
             reads=[bT], writes=[b_dst])

    def proj512(wb, b_wb, pst, b_pst):
        for c in range(8):
            S.op('pe', lambda e, c=c: e.matmul(out=pst, lhsT=xnT[:, c, :], rhs=wb[:, c, :], start=(c == 0), stop=(c == 7)),
                 reads=[b_xnT, b_wb], writes=[b_pst])

    def qknorm(pst, b_pst, gain, b_gain, slot, scale_ap=None, b_scale=None, cos=None, sin=None, b_cs=(), out_bf=None, b_out=None):
        S.op('act', lambda e: e.activation(out=qf[:], in_=pst, func=AF.Copy), reads=[b_pst], writes=[b_qf])
        S.op('act', lambda e: e.activation(out=sq_s[:], in_=pst, func=AF.Square), reads=[b_pst], writes=[b_sq])
        S.op('dve', lambda e: e.tensor_reduce(out=st8[:, 0, :], in_=sq_s[:].rearrange("p (h d) -> p h d", d=64), axis=AX.X, op=ALU.add),
             reads=[b_sq], writes=[b_st8])
        S.op('act', lambda e: e.activation(out=st8[:, 1, :], in_=st8[:, 0, :], func=AF.Sqrt, bias=eps_t[:, 0:1], scale=1.0 / 64),
             reads=[b_st8, b_eps], writes=[b_st8])
        S.op('dve', lambda e: e.reciprocal(out=st8[:, 2, :], in_=st8[:, 1, :]), reads=[b_st8], writes=[b_st8])
        rs = st8[:, 2, :]
        if scale_ap is not None:
            S.op('dve', lambda e: e.tensor_tensor(out=st8[:, 3, :], in0=st8[:, 2, :], in1=scale_ap, op=ALU.mult),
                 reads=[b_st8, b_scale], writes=[b_st8])
            rs = st8[:, 3, :]
        S.op('dve', lambda e: e.tensor_tensor(out=qn[:].rearrange("p (h d) -> p h d", d=64), in0=qf[:].rearrange("p (h d) -> p h d", d=64),
                                              in1=rs.unsqueeze(2).to_broadcast([P, H, 64]), op=ALU.mult),
             reads=[b_qf, b_st8], writes=[b_qn])
        if cos is None:
            S.op('dve', lambda e: e.tensor_tensor(out=out_bf[:].rearrange("p h d -> p (h d)"), in0=qn[:], in1=gain[:], op=ALU.mult),
                 reads=[b_qn, b_gain], writes=[b_out])
            return
        S.op('pool', lambda e: e.tensor_tensor(out=qn[:], in0=qn[:], in1=gain[:], op=ALU.mult), reads=[b_qn, b_gain], writes=[b_qn])
        q3 = qn[:].rearrange("p (h d) -> p h d", d=64)
        x1 = q3[:, :, 0:32]; x2 = q3[:, :, 32:64]
        cb = cos.unsqueeze(1).to_broadcast([P, H, 32]); sbb = sin.unsqueeze(1).to_broadcast([P, H, 32])
        r = [rt[:, i, :].rearrange("p (h d) -> p h d", d=32) for i in range(4)]
        S.op('dve', lambda e: e.tensor_tensor(out=r[0], in0=x1, in1=cb, op=ALU.mult), reads=[b_qn] + list(b_cs), writes=[b_rt])
        S.op('pool', lambda e: e.tensor_tensor(out=r[1], in0=x2, in1=sbb, op=ALU.mult), reads=[b_qn] + list(b_cs), writes=[b_rt])
        S.op('dve', lambda e: e.tensor_tensor(out=r[2], in0=x1, in1=sbb, op=ALU.mult), reads=[b_qn] + list(b_cs), writes=[b_rt])
        S.op('pool', lambda e: e.tensor_tensor(out=r[3], in0=x2, in1=cb, op=ALU.mult), reads=[b_qn] + list(b_cs), writes=[b_rt])
        S.op('dve', lambda e: e.tensor_tensor(out=out_bf[:, :, 0:32], in0=r[0], in1=r[1], op=ALU.subtract), reads=[b_rt], writes=[b_out])
        S.op('dve', lambda e: e.tensor_tensor(out=out_bf[:, :, 32:64], in0=r[2], in1=r[3], op=ALU.add), reads=[b_rt], writes=[b_out])

    b_ubf = S.buf("uv_bf", dma=True); b_vbf = b_ubf
    identF, b_identF = load_const("identF", c_ident, [P, P])
    eps_t = sb("eps_t", [P, 1]); b_eps = S.buf("eps")
    S.op('dve', lambda e: e.memset(eps_t[:], EPS), writes=[b_eps])
    gk_t = sb("gk_t", [P, 1]); b_gk = S.buf("gk")
    S.op('dve', lambda e: e.memset(gk_t[:], 1.5957691216057308), writes=[b_gk])
    one_t = sb("one_t", [P, 1]); b_one = S.buf("one")
    S.op('dve', lambda e: e.memset(one_t[:], 1.0), writes=[b_one])

    state = sb("state", [64, D]); b_state = S.buf("state")
    state_bf = sb("state_bf", [64, D], BF16); b_state_bf = S.buf("state_bf")

    with ExitStack() as es0:
        NBLK = NEXP // 128
        NCB = 4
        cb = [es0.enter_context(nc.sbuf_tensor("cb%d" % i, [P, 1, D], BF16)) for i in range(NCB)]
        b_cb = [S.buf("cb%d" % i, dma=True) for i in range(NCB)]
        b_cin = b_cb; b_cout = []
        pc_jobs = [(tab, dst, bd, blk) for blk in range(NBLK) for (tab, dst, bd) in ((u_tab, uv_bf[:, 0:D], b_ubf), (v_tab, uv_bf[:, D:2 * D], b_vbf))]
        pc_state = [0, 0]

        def _pc_store(k):
            tab, dst, bd, blk = pc_jobs[k]
            i = k % NCB
            S.dma('pool', lambda e: e.dma_start(out=dst[blk * 128:(blk + 1) * 128, :].rearrange("(p r) d -> p r d", r=1), in_=cb[i][:]),
                  bd, reads=[b_cb[i]], writes=[bd])

        def precast(nblocks):
            for _ in range(nblocks):
                k = pc_state[0]
                if k >= len(pc_jobs):
                    break
                pc_state[0] += 1
                tab, dst, bd, blk = pc_jobs[k]
                i = k % NCB
                S.dma('pool', lambda e, tab=tab, blk=blk, i=i: e.dma_start(out=cb[i][:], in_=tab[blk * 128:(blk + 1) * 128, :].rearrange("(p r) d -> p r d", r=1)),
                      b_cb[i], writes=[b_cb[i]])
                if k - 2 >= 0:
                    _pc_store(k - 2)
                    pc_state[1] = k - 1
            if pc_state[0] >= len(pc_jobs):
                while pc_state[1] < len(pc_jobs):
                    _pc_store(pc_state[1])
                    pc_state[1] += 1

        pc_per_tile = -(-len(pc_jobs) // max(NPRE, 1))
        if NPRE > 0:
            wk = es0.enter_context(nc.sbuf_tensor("wk_pre", [P, 8, 512], BF16)); b_wk = S.buf("wk_pre", dma=True)
            wv = es0.enter_context(nc.sbuf_tensor("wv_pre", [P, 8, 1024], BF16)); b_wv = S.buf("wv_pre", dma=True)
            csp = es0.enter_context(nc.sbuf_tensor("csp", [P, NPRE, 32], F32)); b_csp = S.buf("csp", dma=True)
            snp = es0.enter_context(nc.sbuf_tensor("snp", [P, NPRE, 32], F32)); b_snp = S.buf("snp", dma=True)
            ksp = es0.enter_context(nc.sbuf_tensor("ksp", [P, NPRE, H], F32)); b_ksp = S.buf("ksp", dma=True)
            S.dma('pool', lambda e: e.dma_start(out=wk[:], in_=wview(w_in, 512, 512)), b_wk, writes=[b_wk])
            for hh in range(2):
                S.dma('pool', lambda e, hh=hh: e.dma_start(out=wv[:, :, hh * 512:(hh + 1) * 512], in_=wview(w_in, 1024 + hh * 512, 512)),
                      b_wv, writes=[b_wv])
            S.dma('sp', lambda e: e.dma_start(out=csp[:], in_=cos_pre), b_csp, writes=[b_csp])
            S.dma('sp', lambda e: e.dma_start(out=snp[:], in_=sin_pre), b_snp, writes=[b_snp])
            S.dma('sp', lambda e: e.dma_start(out=ksp[:], in_=ksc_pre), b_ksp, writes=[b_ksp])
            B0 = 4
            def _al(name, shape, dt):
                return es0.enter_context(nc.sbuf_tensor(name, list(shape), dt))
            xnb = _al("xnb", [P, 2, D], BF16); xnTb = _al("xnTb", [P, 2, 8, P], BF16); st4b = _al("st4b", [P, B0, 4], F32)
            b_xnb = [S.buf("xnb%d" % (i % 2)) for i in range(2)] * 2; b_xnTb = [S.buf("xnTb%d" % (i % 2)) for i in range(2)] * 2; b_st4b = [S.buf("st4b%d" % i) for i in range(B0)]
            sets = []
            for si in range(2):
                d_ = dict(kf=TPbig[:, 2 + 4 * si:4 + 4 * si, :].rearrange("p a d -> p (a d)").rearrange("p (b n) -> p b n", b=B0),
                          sq=TPbig[:, 4 + 4 * si:6 + 4 * si, :].rearrange("p a d -> p (a d)").rearrange("p (b n) -> p b n", b=B0),
                          s8=_al("s8b%d" % si, [P, 4, B0 * H], F32),
                          r1=_al("r1b%d" % si, [P, B0 * 256], F32),
                          kd=_al("kdb%d" % si, [P, B0, H, 64], BF16), v=_al("vb%d" % si, [P, B0, D], BF16))
                d_.update(b_kf=S.buf("kfb%d" % si), b_sq=S.buf("sqb%d" % si), b_s8=S.buf("s8b%d" % si), b_r0=S.buf("r0b%d" % si),
                          b_r1=S.buf("r1b%d" % si), b_kd=S.buf("kdb%d" % si), b_v=S.buf("vb%d" % si))
                d_["r0"] = d_["kf"].rearrange("p b n -> p (b n)")[:, 0:B0 * 256]; d_["b_r0"] = d_["b_kf"]
                sets.append(d_)
            all_pre_bufs = b_xnb + b_xnTb + b_st4b + [sets[i][k] for i in range(2) for k in ("b_kf", "b_sq", "b_s8", "b_r0", "b_r1", "b_kd", "b_v")]

            def front_a(m, b, st):
                xb = xt[m % 2]; bx = b_xt[m % 2]
                S.dma('sp', lambda e: e.dma_start(out=xb[:], in_=x_pre[m * P:(m + 1) * P, :]), bx, writes=[bx])
                S.op('act', lambda e: e.activation(out=junk[:], in_=xb[:], func=AF.Square, accum_out=st4b[:, b, 0:1]), reads=[bx], writes=[b_junk, b_st4b[b]])
                S.op('act', lambda e: e.activation(out=st4b[:, b, 1:2], in_=st4b[:, b, 0:1], func=AF.Sqrt, bias=eps_t[:, 0:1], scale=1.0 / D),
                     reads=[b_st4b[b], b_eps], writes=[b_st4b[b]])
                S.op('dve', lambda e: e.reciprocal(out=st4b[:, b, 2:3], in_=st4b[:, b, 1:2]), reads=[b_st4b[b]], writes=[b_st4b[b]])
                S.op('dve', lambda e: e.scalar_tensor_tensor(out=xnb[:, b % 2, :], in0=xb[:], scalar=st4b[:, b, 2:3], in1=gattn[:], op0=ALU.mult, op1=ALU.mult),
                     reads=[bx, b_st4b[b], b_gattn], writes=[b_xnb[b]])

            def front_b(m, b, st):
                precast(pc_per_tile)
                for half in range(2):
                    for k in range(4):
                        c = half * 4 + k
                        S.op('pe', lambda e, c=c, k=k: e.transpose(out=psT[:, k * P:(k + 1) * P], in_=xnb[:, b % 2, c * P:(c + 1) * P], identity=ident[:]),
                             reads=[b_xnb[b], b_ident], writes=[bT])
                    dst = xnTb[:, b % 2, half * 4:half * 4 + 4, :].rearrange("p c t -> p (c t)")
                    if half == 0:
                        S.op('act', lambda e, dst=dst: e.activation(out=dst, in_=psT[:, 0:512], func=AF.Copy), reads=[bT], writes=[b_xnTb[b]])
                    else:
                        S.op('dve', lambda e, dst=dst: e.tensor_copy(out=dst, in_=psT[:, 0:512]), reads=[bT], writes=[b_xnTb[b]])
                for c in range(8):
                    S.op('pe', lambda e, c=c: e.matmul(out=psC[:], lhsT=xnTb[:, b % 2, c, :], rhs=wk[:, c, :], start=(c == 0), stop=(c == 7)),
                         reads=[b_xnTb[b], b_wk], writes=[bC])
                S.op('act', lambda e: e.activation(out=st["kf"][:, b, :], in_=psC[:], func=AF.Copy), reads=[bC], writes=[st["b_kf"]])
                S.op('act', lambda e: e.activation(out=st["sq"][:, b, :], in_=psC[:], func=AF.Square), reads=[bC], writes=[st["b_sq"]])
                psV, bV = (psA, bA) if m % 2 == 0 else (psB, bB)
                for hh in range(2):
                    for c in range(8):
                        S.op('pe', lambda e, c=c, hh=hh: e.matmul(out=psV[:, hh * 512:(hh + 1) * 512], lhsT=xnTb[:, b % 2, c, :], rhs=wv[:, c, hh * 512:(hh + 1) * 512],
                                                                  start=(c == 0), stop=(c == 7)), reads=[b_xnTb[b], b_wv], writes=[bV])
                S.op('act', lambda e: e.activation(out=st["v"][:, b, :], in_=psV[:], func=AF.Copy), reads=[bV], writes=[st["b_v"]])

            def chain_gen(m0, nb, st):
                n8 = nb * H
                kf, sq, s8, r0, r1, kdb = st["kf"], st["sq"], st["s8"], st["r0"], st["r1"], st["kd"]
                S.op('dve', lambda e: e.tensor_reduce(out=s8[:, 0, 0:n8], in_=sq[:, 0:nb, :].rearrange("p b (h d) -> p (b h) d", d=64), axis=AX.X, op=ALU.add),
                     reads=[st["b_sq"]], writes=[st["b_s8"]]); yield
                S.op('act', lambda e: e.activation(out=s8[:, 1, 0:n8], in_=s8[:, 0, 0:n8], func=AF.Sqrt, bias=eps_t[:, 0:1], scale=1.0 / 64),
                     reads=[st["b_s8"], b_eps], writes=[st["b_s8"]]); yield
                S.op('dve', lambda e: e.reciprocal(out=s8[:, 2, 0:n8], in_=s8[:, 1, 0:n8]), reads=[st["b_s8"]], writes=[st["b_s8"]]); yield
                S.op('dve', lambda e: e.tensor_tensor(out=s8[:, 3, 0:n8], in0=s8[:, 2, 0:n8], in1=ksp[:, m0:m0 + nb, :].rearrange("p m h -> p (m h)"), op=ALU.mult),
                     reads=[st["b_s8"], b_ksp], writes=[st["b_s8"]]); yield
                q3 = sq[:, 0:nb, :].rearrange("p b (h d) -> p (b h) d", d=64)
                S.op('dve', lambda e: e.tensor_tensor(out=q3, in0=kf[:, 0:nb, :].rearrange("p b (h d) -> p (b h) d", d=64),
                                                      in1=s8[:, 3, 0:n8].unsqueeze(2).to_broadcast([P, n8, 64]), op=ALU.mult),
                     reads=[st["b_kf"], st["b_s8"]], writes=[st["b_sq"]]); yield
                S.op('pool', lambda e: e.tensor_tensor(out=sq[:, 0:nb, :], in0=sq[:, 0:nb, :], in1=grk[:].unsqueeze(1).to_broadcast([P, nb, 512]), op=ALU.mult),
                     reads=[st["b_sq"], b_grk], writes=[st["b_sq"]]); yield
                q4 = sq[:, 0:nb, :].rearrange("p b (h d) -> p b h d", d=64)
                x1_ = q4[:, :, :, 0:32]; x2_ = q4[:, :, :, 32:64]
                cb = csp[:, m0:m0 + nb, :].unsqueeze(2).to_broadcast([P, nb, H, 32]); sb_ = snp[:, m0:m0 + nb, :].unsqueeze(2).to_broadcast([P, nb, H, 32])
                r0v = r0[:, 0:nb * 256].rearrange("p (b h d) -> p b h d", b=nb, h=H); r1v = r1[:, 0:nb * 256].rearrange("p (b h d) -> p b h d", b=nb, h=H)
                S.op('dve', lambda e: e.tensor_tensor(out=r0v, in0=x1_, in1=cb, op=ALU.mult), reads=[st["b_sq"], b_csp], writes=[st["b_r0"]]); yield
                S.op('pool', lambda e: e.tensor_tensor(out=r1v, in0=x2_, in1=sb_, op=ALU.mult), reads=[st["b_sq"], b_snp], writes=[st["b_r1"]]); yield
                S.op('dve', lambda e: e.tensor_tensor(out=kdb[:, 0:nb, :, 0:32], in0=r0v, in1=r1v, op=ALU.subtract), reads=[st["b_r0"], st["b_r1"]], writes=[st["b_kd"]]); yield
                S.op('dve', lambda e: e.tensor_tensor(out=r0v, in0=x1_, in1=sb_, op=ALU.mult), reads=[st["b_sq"], b_snp], writes=[st["b_r0"]]); yield
                S.op('pool', lambda e: e.tensor_tensor(out=r1v, in0=x2_, in1=cb, op=ALU.mult), reads=[st["b_sq"], b_csp], writes=[st["b_r1"]]); yield
                S.op('dve', lambda e: e.tensor_tensor(out=kdb[:, 0:nb, :, 32:64], in0=r0v, in1=r1v, op=ALU.add), reads=[st["b_r0"], st["b_r1"]], writes=[st["b_kd"]]); yield

            def state_mm(m0, nb, st):
                for b in range(nb):
                    m = m0 + b
                    for h in range(H):
                        acc_ps, b_acc_ps = (psS, bS) if h < 4 else (psD, bD)
                        S.op('pe', lambda e, h=h, m=m, b=b, acc_ps=acc_ps: e.matmul(
                            out=acc_ps[0:64, (h % 4) * P:(h % 4 + 1) * P], lhsT=st["kd"][:, b, h, :], rhs=st["v"][:, b, h * P:(h + 1) * P],
                            start=(m == 0 and h % 4 == 0), stop=(m == NPRE - 1), skip_group_check=True),
                            reads=[st["b_kd"], st["b_v"]], writes=[b_acc_ps])

            batches = [(m0, min(B0, NPRE - m0)) for m0 in range(0, NPRE, B0)]
            tiles = [(m0 + b, b, sets[bi % 2]) for bi, (m0, nb) in enumerate(batches) for b in range(nb)]
            pend = None
            front_a(*tiles[0])
            ti_ = 0
            for bi, (m0, nb) in enumerate(batches):
                st = sets[bi % 2]
                gen = chain_gen(*pend) if pend is not None else None
                for b in range(nb):
                    if ti_ + 1 < len(tiles):
                        front_a(*tiles[ti_ + 1])
                    front_b(m0 + b, b, st)
                    ti_ += 1
                    if gen is not None:
                        for _ in range(4):
                            next(gen, None)
                if gen is not None:
                    for _ in gen:
                        pass
                    state_mm(*pend)
                pend = (m0, nb, st)
            for _ in chain_gen(*pend):
                pass
            state_mm(*pend)
            for e_ in ('pe', 'act', 'dve', 'pool', 'sp'):
                S.wait_all(e_, all_pre_bufs)
            S.op('act', lambda e: e.activation(out=state[:, 0:512], in_=psS[0:64, :], func=AF.Copy), reads=[bS], writes=[b_state])
            S.op('act', lambda e: e.activation(out=state[:, 512:1024], in_=psD[0:64, :], func=AF.Copy), reads=[bD], writes=[b_state])
        else:
            S.op('dve', lambda e: e.memset(state[:], 0.0), writes=[b_state])
        S.op('act', lambda e: e.activation(out=state_bf[:], in_=state[:], func=AF.Copy), reads=[b_state], writes=[b_state_bf])
        precast(len(pc_jobs))
        for e_ in ('pe', 'act', 'dve', 'pool', 'sp'):
            S.wait_all(e_, b_cin + b_cout + [b_ubf])
        if NPRE > 0:
            for e_ in ('pe', 'act', 'dve', 'pool'):
                S.wait_all(e_, [b_wk, b_wv, b_csp, b_snp, b_ksp])

    dump(1, state[:], [b_state], D)
    xn = sb("xn", [P, D], BF16); b_xn = S.buf("xn")
    xnT = sb("xnT", [P, 8, P], BF16); b_xnT = S.buf("xnT")
    qf = sb("qf", [P, 512]); b_qf = S.buf("qf")
    sq_s = sb("sq_s", [P, 512]); b_sq = S.buf("sq_s")
    st8 = sb("st8", [P, 4, H]); b_st8 = S.buf("st8")
    qn = sb("qn", [P, 512]); b_qn = S.buf("qn")
    rt = sb("rt", [P, 4, 256]); b_rt = S.buf("rt")
    kd = sb("kd", [P, H, 64], BF16); b_kd = S.buf("kd")
    v_r = sb("v_r", [P, D], BF16); b_vr = S.buf("v_r")
    wbra = sb("wbra", [P, 8, D], BF16); b_wbra = S.buf("wbra", dma=True)
    wbrb = sb("wbrb", [P, 4, D], BF16); b_wbrb = S.buf("wbrb", dma=True)
    wout = sb("wout", [P, 8, D], BF16); b_wout = S.buf("wout", dma=True)
    k1 = sb("k1", [P, P], BF16); b_k1 = S.buf("k1", dma=True)
    k2 = sb("k2", [P, P], BF16); b_k2 = S.buf("k2", dma=True)
    for hh in range(2):
        S.dma('pool', lambda e, hh=hh: e.dma_start(out=wbra[:, :, hh * 512:(hh + 1) * 512], in_=wview(w_bra, hh * 512, 512)), b_wbra, writes=[b_wbra])
        S.dma('pool', lambda e, hh=hh: e.dma_start(out=wbrb[:, :, hh * 512:(hh + 1) * 512], in_=wview(w_brb, hh * 512, 512)), b_wbrb, writes=[b_wbrb])
        S.dma('pool', lambda e, hh=hh: e.dma_start(out=wout[:, :, hh * 512:(hh + 1) * 512], in_=wview(w_out, hh * 512, 512)), b_wout, writes=[b_wout])
    S.dma('pool', lambda e: e.dma_start(out=k1[:], in_=k1T), b_k1, writes=[b_k1])
    S.dma('pool', lambda e: e.dma_start(out=k2[:], in_=k2T), b_k2, writes=[b_k2])

    wbuf = [sb("wbuf%d" % i, [P, 8, 512], BF16) for i in range(2)]
    b_wbuf = [S.buf("wbuf%d" % i, dma=True) for i in range(2)]
    wcount = [0]

    def stream_w(c0, src=None):
        src = w_in if src is None else src
        i = wcount[0] % 2
        wcount[0] += 1
        S.dma('pool', lambda e: e.dma_start(out=wbuf[i][:], in_=wview(src, c0, 512)), b_wbuf[i], writes=[b_wbuf[i]])
        return wbuf[i], b_wbuf[i]

    xres = sb("xres", [P, D]); b_xres = S.buf("xres", dma=True)
    qd = sb("qd", [P, H, 64], BF16); b_qd = S.buf("qd")
    qdT = sb("qdT", [64, H, P], BF16); b_qdT = S.buf("qdT")
    kdT = sb("kdT", [64, H, P], BF16); b_kdT = S.buf("kdT")
    rg = sb("rg", [P, D], BF16); b_rg = S.buf("rg")
    sqn = sb("sqn", [P, H, 64], BF16); b_sqn = S.buf("sqn")
    skn = sb("skn", [P, H, 64], BF16); b_skn = S.buf("skn")
    sqT = sb("sqT", [64, H, P], BF16); b_sqT = S.buf("sqT")
    skT = [sb("skT%d" % i, [64, H, P], BF16) for i in range(2)]; b_skT = [S.buf("skT%d" % i) for i in range(2)]
    sv = [sb("sv%d" % i, [P, 512], BF16) for i in range(2)]; b_sv = [S.buf("sv%d" % i) for i in range(2)]
    siga = TP[7]; b_siga = b_TP[7]
    sigb = TP[8]; b_sigb = b_TP[8]
    pm = sb("pm", [P, D], BF16); b_pm = S.buf("pm")
    yf = TP[3]; b_yf = b_TP[3]
    ysq = TP[4]; b_ysq = b_TP[4]
    gst = sb("gst", [P, 6, H]); b_gst = S.buf("gst")
    ret = sb("ret", [P, D], BF16); b_ret = S.buf("ret")
    retT = sb("retT", [P, 8, P], BF16); b_retT = S.buf("retT")
    e_s = TP[3]; b_es = b_TP[3]
    sp_s = TP[4]; b_sp = b_TP[4]
    spm = sb("spm", [P, D], BF16); b_spm = S.buf("spm")
    u_s = TP[0]; b_us = b_TP[0]
    w_s = sb("w_s", [P, D], BF16); b_ws = S.buf("w_s")
    sbT = sb("sbT", [P, 4, P], BF16); b_sbT = S.buf("sbT")
    m1 = TP[5]; b_m1 = b_TP[5]
    m2 = TP[6]; b_m2 = b_TP[6]
    mixed = ret; b_mixed = b_ret
    mixT = retT; b_mixT = b_retT
    x1 = TP[1]; b_x1 = b_TP[1]
    hn = TP[9]; b_hn = b_TP[9]
    qT = sb("qT", [P, 16, P], BF16); b_qT = S.buf("qT")
    sc = TPbig[:, 3:5, :].rearrange("p a d -> p (a d)").rearrange("p (g n) -> p g n", g=16); b_sc = [b_TP[3], b_TP[4]]
    sc2 = TPbig[:, 5:7, :].rearrange("p a d -> p (a d)").rearrange("p (g n) -> p g n", g=16); b_sc2 = [b_TP[5], b_TP[6]]
    tv = sb("tv", [P, 16, 16]); b_tv = S.buf("tv")
    ti = sb("ti", [P, 16, 16], U32); b_ti = S.buf("ti")
    tif = sb("tif", [P, 16, 16]); b_tif = S.buf("tif")
    cand = TPbig[:, 7:9, :].rearrange("p a d -> p (a d)").rearrange("p (h c) -> p h c", h=H); b_cand = [b_TP[7], b_TP[8]]
    cand2 = sc.rearrange("p g n -> p (g n)").rearrange("p (h c) -> p h c", h=H); b_cand2 = b_sc
    tsv = sb("tsv", [P, H, 16]); b_tsv = S.buf("tsv")
    tpos = sb("tpos", [P, H, 16], U32); b_tpos = S.buf("tpos")
    tposf = sb("tposf", [P, H, 16]); b_tposf = S.buf("tposf")
    ta = sb("ta", [P, H, 16]); b_ta = S.buf("ta")
    tb = sb("tb", [P, H, 16]); b_tb = S.buf("tb")
    iota16 = sb("iota16", [P, 16]); b_iota = S.buf("iota16")
    oh = sc2.rearrange("p g n -> p (g n)").rearrange("p (h a b) -> p h a b", h=H, a=16); b_oh = b_sc2
    idx1 = sb("idx1", [P, H, 16]); b_idx1 = S.buf("idx1")
    idx2 = sb("idx2", [P, H, 16]); b_idx2 = S.buf("idx2")
    eidx = sb("eidx", [P, 128], U32); b_eidx = S.buf("eidx")
    gw = sb("gw", [P, H, 16]); b_gw = S.buf("gw")
    gs = sb("gs", [P, 2, H]); b_gs = S.buf("gs")
    hv = sb("hv", [P, 128]); b_hv = S.buf("hv")
    ga_ = sb("ga_", [P, 6, 128]); b_ga = S.buf("ga_"); b_ga2 = S.buf("ga2"); b_ga3 = S.buf("ga3")
    aw = sb("aw", [P, 128]); b_aw = S.buf("aw")
    NG = 8
    dg = [sb("dg%d" % i, [P, P], BF16) for i in range(4)]; b_dg = [S.buf("dg%d" % i) for i in range(4)]
    gbuf = None; b_gbuf = None
    acc = TP[0]; b_acc = b_TP[0]
    b_yout = S.buf("yout", dma=True)

    _gi = [3, 4, 5, 6, 7, 8, 2, 0]
    gbuf = [TPbig[:, i, :].bitcast(BF16) for i in _gi]
    b_gbuf = [b_TP[i] for i in _gi]
    gsem = [S.buf("gsem%d" % i, dma=True) for i in range(len(_gi))]
    S.op('pool', lambda e: e.iota(iota16[:], pattern=[[1, 16]], base=0, channel_multiplier=0, allow_small_or_imprecise_dtypes=True),
         writes=[b_iota])
    thr16 = sb("thr16", [P, 16])
    S.op('pool', lambda e: e.iota(thr16[:], pattern=[[16, 16]], base=0, channel_multiplier=0, allow_small_or_imprecise_dtypes=True),
         reads=[b_iota], writes=[b_iota])

    def sb_kv(cur):
        wb, bw = stream_w(3584)
        proj512(wb, bw, psC[:], bC)
        qknorm(psC[:], bC, gsk, b_gsk, 0, out_bf=skn, b_out=b_skn)
        transposeH(skn, b_skn, skT[cur], b_skT[cur])
        wb, bw = stream_w(4096)
        proj512(wb, bw, psD[:], bD)
        S.op('act', lambda e: e.activation(out=sv[cur][:], in_=psD[:], func=AF.Copy), reads=[bD], writes=[b_sv[cur]])

    S.dma('sp', lambda e: e.dma_start(out=xt[0][:], in_=x_halo), b_xt[0], writes=[b_xt[0]])
    rmsnorm_T(xt[0][:], b_xt[0], gattn, b_gattn)
    sb_kv(1)
    dump(2, xt[0][:], [b_xt[0], b_sv[1], b_skT[1]])

    def m_head(n):
        cur = n % 2
        S.dma('sp', lambda e, n=n: e.dma_start(out=xres[:], in_=x_own[n * P:(n + 1) * P, :]), b_xres, writes=[b_xres])
        rmsnorm_T(xres[:], b_xres, gattn, b_gattn)
        wb, bw = stream_w(0)
        proj512(wb, bw, psC[:], bC)
        qknorm(psC[:], bC, grq, b_grq, 0, scale_ap=qdec[:], b_scale=b_qdec, cos=cso[:, n, :], sin=sno[:, n, :],
               b_cs=[b_cso, b_sno], out_bf=qd, b_out=b_qd)
        transposeH(qd, b_qd, qdT, b_qdT)
        wb, bw = stream_w(512)
        proj512(wb, bw, psD[:], bD)
        qknorm(psD[:], bD, grk, b_grk, 0, scale_ap=kdec[:], b_scale=b_kdec, cos=cso[:, n, :], sin=sno[:, n, :],
               b_cs=[b_cso, b_sno], out_bf=kd, b_out=b_kd)
        transposeH(kd, b_kd, kdT, b_kdT)
        wb, bw = stream_w(3072)
        proj512(wb, bw, psC[:], bC)
        qknorm(psC[:], bC, gsq, b_gsq, 0, out_bf=sqn, b_out=b_sqn)
        transposeH(sqn, b_sqn, sqT, b_sqT)
        sb_kv(cur)

    m_head(0)
    for n in range(NT):
        cur = n % 2; prv = 1 - cur
        for hh in range(2):
            wb, bw = stream_w(1024 + hh * 512)
            proj512(wb, bw, psA[:, hh * 512:(hh + 1) * 512], bA)
        S.op('act', lambda e: e.activation(out=v_r[:], in_=psA[:], func=AF.Copy), reads=[bA], writes=[b_vr])
        for hh in range(2):
            wb, bw = stream_w(2048 + hh * 512)
            proj512(wb, bw, psB[:, hh * 512:(hh + 1) * 512], bB)
        S.op('act', lambda e: e.activation(out=m1[:], in_=psB[:], func=AF.Silu), reads=[bB], writes=[b_m1])
        S.op('pool', lambda e: e.tensor_tensor(out=rg[:], in0=m1[:], in1=ggn[:], op=ALU.mult), reads=[b_m1, b_ggn], writes=[b_rg])
        for h in range(H):
            S.op('pe', lambda e, h=h: e.matmul(out=psA[:, h * P:(h + 1) * P], lhsT=kdT[:, h, :], rhs=qdT[:, h, :], start=True, stop=True),
                 reads=[b_kdT, b_qdT], writes=[bA])
        S.op('dve', lambda e: e.tensor_tensor(out=pm[:], in0=psA[:], in1=maskT[:], op=ALU.mult), reads=[bA, b_maskT], writes=[b_pm])
        for h in range(H):
            S.op('pe', lambda e, h=h: e.matmul(out=psB[:, h * P:(h + 1) * P], lhsT=pm[:, h * P:(h + 1) * P], rhs=v_r[:, h * P:(h + 1) * P],
                                               start=True, stop=False, skip_group_check=True),
                 reads=[b_pm, b_vr], writes=[bB])
            S.op('pe', lambda e, h=h: e.matmul(out=psB[:, h * P:(h + 1) * P], lhsT=qdT[:, h, :], rhs=state_bf[:, h * P:(h + 1) * P],
                                               start=False, stop=True, skip_group_check=True),
                 reads=[b_qdT, b_state_bf], writes=[bB])
        S.op('dve', lambda e: e.tensor_tensor(out=state[:], in0=state[:], in1=cdec[:], op=ALU.mult), reads=[b_state, b_cdec], writes=[b_state])
        for rnd in range(2):
            for hl in range(4):
                h = rnd * 4 + hl
                S.op('pe', lambda e, h=h, hl=hl: e.matmul(out=psS[0:64, hl * P:(hl + 1) * P], lhsT=kd[:, h, :], rhs=v_r[:, h * P:(h + 1) * P],
                                                   start=True, stop=True, skip_group_check=True),
                     reads=[b_kd, b_vr], writes=[bS])
            S.op('dve', lambda e, rnd=rnd: e.tensor_tensor(out=state[:, rnd * 512:(rnd + 1) * 512], in0=state[:, rnd * 512:(rnd + 1) * 512],
                                                         in1=psS[0:64, :], op=ALU.add), reads=[b_state, bS], writes=[b_state])
        S.op('act', lambda e: e.activation(out=state_bf[:], in_=state[:], func=AF.Copy), reads=[b_state], writes=[b_state_bf])
        S.op('act', lambda e: e.activation(out=yf[:], in_=psB[:], func=AF.Copy), reads=[bB], writes=[b_yf])
        S.op('act', lambda e: e.activation(out=ysq[:], in_=psB[:], func=AF.Square), reads=[bB], writes=[b_ysq])
        S.op('dve', lambda e: e.tensor_reduce(out=gst[:, 0, :], in_=yf[:].rearrange("p (h d) -> p h d", d=P), axis=AX.X, op=ALU.add),
             reads=[b_yf], writes=[b_gst])
        S.op('dve', lambda e: e.tensor_reduce(out=gst[:, 1, :], in_=ysq[:].rearrange("p (h d) -> p h d", d=P), axis=AX.X, op=ALU.add),
             reads=[b_ysq], writes=[b_gst])
        S.op('dve', lambda e: e.tensor_scalar(out=gst[:, 2, :], in0=gst[:, 0, :], scalar1=1.0 / P, scalar2=None, op0=ALU.mult), reads=[b_gst], writes=[b_gst])
        S.op('dve', lambda e: e.tensor_tensor(out=gst[:, 3, :], in0=gst[:, 2, :], in1=gst[:, 2, :], op=ALU.mult), reads=[b_gst], writes=[b_gst])
        S.op('dve', lambda e: e.scalar_tensor_tensor(out=gst[:, 4, :], in0=gst[:, 1, :], scalar=1.0 / P, in1=gst[:, 3, :], op0=ALU.mult, op1=ALU.subtract),
             reads=[b_gst], writes=[b_gst])
        S.op('act', lambda e: e.activation(out=gst[:, 5, :], in_=gst[:, 4, :], func=AF.Sqrt, bias=eps_t[:, 0:1], scale=1.0), reads=[b_gst, b_eps], writes=[b_gst])
        S.op('dve', lambda e: e.reciprocal(out=gst[:, 3, :], in_=gst[:, 5, :]), reads=[b_gst], writes=[b_gst])
        y3 = yf[:].rearrange("p (h d) -> p h d", d=P)
        S.op('dve', lambda e: e.tensor_tensor(out=y3, in0=y3, in1=gst[:, 2, :].unsqueeze(2).to_broadcast([P, H, P]), op=ALU.subtract),
             reads=[b_yf, b_gst], writes=[b_yf])
        S.op('dve', lambda e: e.tensor_tensor(out=y3, in0=y3, in1=gst[:, 3, :].unsqueeze(2).to_broadcast([P, H, P]), op=ALU.mult),
             reads=[b_yf, b_gst], writes=[b_yf])
        S.op('pool', lambda e: e.tensor_tensor(out=ret[:], in0=yf[:], in1=rg[:], op=ALU.mult), reads=[b_yf, b_rg], writes=[b_ret])
        transpose8(ret, b_ret, retT, b_retT)
        dump(3, yf[:], [b_yf, b_retT])
        for half in range(2):
            for blk, kT_ in ((0, skT[prv]), (1, skT[cur])):
                for hl in range(4):
                    h = half * 4 + hl
                    S.op('pe', lambda e, blk=blk, hl=hl, h=h, kT_=kT_: e.matmul(
                        out=psA[:, blk * 512 + hl * P: blk * 512 + (hl + 1) * P], lhsT=kT_[:, h, :],
                        rhs=sqT[:, h, :], start=True, stop=True),
                        reads=[b_skT[prv], b_skT[cur], b_sqT], writes=[bA])
            S.op('act', lambda e: e.activation(out=e_s[:], in_=psA[:], func=AF.Exp, scale=0.125), reads=[bA], writes=[b_es])
            S.op('act', lambda e: e.activation(out=sp_s[:], in_=e_s[:], func=AF.Ln, bias=one_t[:, 0:1], scale=1.0), reads=[b_es, b_one], writes=[b_sp])
            S.op('dve', lambda e: e.tensor_tensor(
                out=spm[:].rearrange("p (b h t) -> p b h t", b=2, h=4), in0=sp_s[:].rearrange("p (b h t) -> p b h t", b=2, h=4),
                in1=mstay[:].rearrange("p (b h t) -> p b h t", b=2, h=4), op=ALU.mult), reads=[b_sp, b_mstay], writes=[b_spm])
            S.op('pe', lambda e: e.matmul(out=psB[:, 0:512], lhsT=tri[:], rhs=spm[:, 0:512], start=True, stop=False), reads=[b_tri, b_spm], writes=[bB])
            S.op('pe', lambda e: e.matmul(out=psB[:, 0:512], lhsT=ones[:], rhs=spm[:, 512:1024], start=False, stop=True), reads=[b_ones, b_spm], writes=[bB])
            S.op('pe', lambda e: e.matmul(out=psB[:, 512:1024], lhsT=tri[:], rhs=spm[:, 512:1024], start=True, stop=False), reads=[b_tri, b_spm], writes=[bB])
            S.op('pe', lambda e: e.matmul(out=psB[:, 512:1024], lhsT=ident[:], rhs=mpos[:], start=False, stop=True), reads=[b_ident, b_mpos], writes=[bB])
            S.op('dve', lambda e: e.scalar_tensor_tensor(out=u_s[:], in0=psA[:], scalar=0.125, in1=sp_s[:], op0=ALU.mult, op1=ALU.subtract),
                 reads=[bA, b_sp], writes=[b_us])
            S.op('dve', lambda e: e.tensor_tensor(out=u_s[:], in0=u_s[:], in1=psB[:], op=ALU.subtract), reads=[b_us, bB], writes=[b_us])
            S.op('act', lambda e: e.activation(out=w_s[:], in_=u_s[:], func=AF.Exp), reads=[b_us], writes=[b_ws])
            for hl in range(4):
                h = half * 4 + hl
                po = (h % 2) * 64
                for blk, svb, bsv in ((0, sv[prv], b_sv[prv]), (1, sv[cur], b_sv[cur])):
                    S.op('pe', lambda e, h=h, hl=hl, po=po, blk=blk, svb=svb: e.matmul(
                        out=psS[po:po + 64, (h // 2) * P:(h // 2 + 1) * P], lhsT=svb[:, h * 64:(h + 1) * 64],
                        rhs=w_s[:, blk * 512 + hl * P: blk * 512 + (hl + 1) * P], start=(blk == 0), stop=(blk == 1), skip_group_check=True),
                        reads=[bsv, b_ws], writes=[bS])
        S.op('act', lambda e: e.activation(out=sbT[:].rearrange("p c t -> p (c t)"), in_=psS[:], func=AF.Copy), reads=[bS], writes=[b_sbT])
        if STAGE == 4:
            S.op('act', lambda e: e.activation(out=m2[:, 0:512], in_=psS[:], func=AF.Copy), reads=[bS], writes=[b_m2])
            dump(4, m2[:, 0:512], [b_m2], 512)
        for hh in range(2):
            for c in range(8):
                S.op('pe', lambda e, c=c, hh=hh: e.matmul(out=psA[:, hh * 512:(hh + 1) * 512], lhsT=retT[:, c, :], rhs=wbra[:, c, hh * 512:(hh + 1) * 512],
                                                          start=(c == 0), stop=(c == 7)), reads=[b_retT, b_wbra], writes=[bA])
            for c in range(4):
                S.op('pe', lambda e, c=c, hh=hh: e.matmul(out=psB[:, hh * 512:(hh + 1) * 512], lhsT=sbT[:, c, :], rhs=wbrb[:, c, hh * 512:(hh + 1) * 512],
                                                          start=(c == 0), stop=(c == 3)), reads=[b_sbT, b_wbrb], writes=[bB])
        for gi, (gt, bg) in enumerate(((siga, b_siga), (sigb, b_sigb))):
            for hh in range(2):
                wb, bw = stream_w(4608 + gi * 1024 + hh * 512)
                pst, bp = (psC, bC) if hh == 0 else (psD, bD)
                proj512(wb, bw, pst[:], bp)
                S.op('act', lambda e, gt=gt, hh=hh, pst=pst: e.activation(out=gt[:, hh * 512:(hh + 1) * 512], in_=pst[:], func=AF.Sigmoid),
                     reads=[bp], writes=[bg])
        S.op('dve', lambda e: e.tensor_tensor(out=m1[:], in0=psA[:], in1=siga[:], op=ALU.mult), reads=[bA, b_siga], writes=[b_m1])
        S.op('dve', lambda e: e.tensor_tensor(out=m2[:], in0=psB[:], in1=sigb[:], op=ALU.mult), reads=[bB, b_sigb], writes=[b_m2])
        S.op('pool', lambda e: e.tensor_tensor(out=mixed[:], in0=m1[:], in1=m2[:], op=ALU.add), reads=[b_m1, b_m2], writes=[b_mixed])
        transpose8(mixed, b_mixed, mixT, b_mixT)
        for hh in range(2):
            for c in range(8):
                S.op('pe', lambda e, c=c, hh=hh: e.matmul(out=psA[:, hh * 512:(hh + 1) * 512], lhsT=mixT[:, c, :], rhs=wout[:, c, hh * 512:(hh + 1) * 512],
                                                          start=(c == 0), stop=(c == 7)), reads=[b_mixT, b_wout], writes=[bA])
        S.op('dve', lambda e: e.tensor_tensor(out=x1[:], in0=psA[:], in1=xres[:], op=ALU.add), reads=[bA, b_xres], writes=[b_x1])
        dump(5, x1[:], [b_x1])
        rmsnorm_T(x1[:], b_x1, gffn, b_gffn, keep_f32=hn[:], b_keep=b_hn)
        for g4 in range(4):
            wqb, b_wq = stream_w(g4 * 512, w_q)
            for gl in range(4):
                g = g4 * 4 + gl
                pst = psA[:, gl * P:(gl + 1) * P] if g4 % 2 == 0 else psB[:, gl * P:(gl + 1) * P]
                bp = bA if g4 % 2 == 0 else bB
                for c in range(8):
                    S.op('pe', lambda e, c=c, gl=gl, pst=pst, wqb=wqb: e.matmul(out=pst, lhsT=wqb[:, c, gl * P:(gl + 1) * P], rhs=xnT[:, c, :],
                                                                     start=(c == 0), stop=(c == 7)), reads=[b_wq, b_xnT], writes=[bp])
            src = psA if g4 % 2 == 0 else psB
            bp = bA if g4 % 2 == 0 else bB
            S.op('act', lambda e, g4=g4, src=src: e.activation(out=qT[:, g4 * 4:(g4 + 1) * 4, :].rearrange("p g t -> p (g t)"), in_=src[:, 0:512], func=AF.Copy),
                 reads=[bp], writes=[b_qT])
        for g4 in range(4):
            pst, bp = (psA, bA) if g4 % 2 == 0 else (psB, bB)
            for gl in range(4):
                g = g4 * 4 + gl
                kk, bk = (k1, b_k1) if g % 2 == 0 else (k2, b_k2)
                S.op('pe', lambda e, g=g, gl=gl, pst=pst, kk=kk: e.matmul(out=pst[:, gl * P:(gl + 1) * P], lhsT=qT[:, g, :], rhs=kk[:], start=True, stop=True),
                     reads=[b_qT, bk], writes=[bp])
            S.op('act', lambda e, g4=g4, pst=pst: e.activation(out=sc[:, g4 * 4:(g4 + 1) * 4, :].rearrange("p g n -> p (g n)"), in_=pst[:, 0:512], func=AF.Copy),
                 reads=[bp], writes=[b_sc])
        bg_tv = [S.buf("tv%d" % g) for g in range(16)]; bg_ti = [S.buf("ti%d" % g) for g in range(16)]; bg_s2 = [S.buf("s2%d" % g) for g in range(16)]
        for g in range(16):
            S.op('dve', lambda e, g=g: e.max(out=tv[:, g, 0:8], in_=sc[:, g, :]), reads=[b_sc], writes=[bg_tv[g]])
        for g in range(16):
            S.op('dve', lambda e, g=g: e.match_replace(out=sc2[:, g, :], in_to_replace=tv[:, g, 0:8], in_values=sc[:, g, :], imm_value=NEG),
                 reads=[b_sc, bg_tv[g]], writes=[bg_s2[g]])
        for g in range(16):
            S.op('dve', lambda e, g=g: e.max_index(out=ti[:, g, 0:8], in_max=tv[:, g, 0:8], in_values=sc[:, g, :]), reads=[b_sc, bg_tv[g]], writes=[bg_ti[g]])
        for g in range(16):
            S.op('dve', lambda e, g=g: e.max(out=tv[:, g, 8:16], in_=sc2[:, g, :]), reads=[bg_s2[g]], writes=[bg_tv[g]])
        for g in range(16):
            S.op('dve', lambda e, g=g: e.max_index(out=ti[:, g, 8:16], in_max=tv[:, g, 8:16], in_values=sc2[:, g, :]), reads=[bg_s2[g], bg_tv[g]], writes=[bg_ti[g]])
        b_tv.w = None; b_ti.w = None
        S.op('dve', lambda e: e.tensor_copy(out=tif[:], in_=ti[:]), reads=bg_ti + bg_tv + bg_s2 + [b_sc2], writes=[b_tif, b_tv, b_ti, b_sc2])
        tv4 = tv[:].rearrange("p (h s) k -> p h s k", s=2)
        tif4 = tif[:].rearrange("p (h s) k -> p h s k", s=2)
        S.op('dve', lambda e: e.tensor_tensor(out=cand.rearrange("p h (a b) -> p h a b", a=16),
                                              in0=tv4[:, :, 0, :].unsqueeze(3).to_broadcast([P, H, 16, 16]),
                                              in1=tv4[:, :, 1, :].unsqueeze(2).to_broadcast([P, H, 16, 16]), op=ALU.add),
             reads=[b_tv], writes=[b_cand])
        bh_ts = [S.buf("ts%d" % h) for h in range(H)]; bh_tp = [S.buf("tp%d" % h) for h in range(H)]; bh_c2 = [S.buf("c2%d" % h) for h in range(H)]
        for h in range(H):
            S.op('dve', lambda e, h=h: e.max(out=tsv[:, h, 0:8], in_=cand[:, h, :]), reads=[b_cand], writes=[bh_ts[h]])
        for h in range(H):
            S.op('dve', lambda e, h=h: e.match_replace(out=cand2[:, h, :], in_to_replace=tsv[:, h, 0:8], in_values=cand[:, h, :], imm_value=NEG),
                 reads=[b_cand, bh_ts[h], b_cand2], writes=[bh_c2[h]])
        for h in range(H):
            S.op('dve', lambda e, h=h: e.max_index(out=tpos[:, h, 0:8], in_max=tsv[:, h, 0:8], in_values=cand[:, h, :]), reads=[b_cand, bh_ts[h]], writes=[bh_tp[h]])
        for h in range(H):
            S.op('dve', lambda e, h=h: e.max(out=tsv[:, h, 8:16], in_=cand2[:, h, :]), reads=[bh_c2[h]], writes=[bh_ts[h]])
        for h in range(H):
            S.op('dve', lambda e, h=h: e.max_index(out=tpos[:, h, 8:16], in_max=tsv[:, h, 8:16], in_values=cand2[:, h, :]), reads=[bh_c2[h], bh_ts[h]], writes=[bh_tp[h]])
        b_tsv.w = None; b_tpos.w = None
        S.op('dve', lambda e: e.tensor_copy(out=tposf[:], in_=tpos[:]), reads=bh_tp + bh_ts + bh_c2, writes=[b_tposf, b_tsv, b_tpos, b_cand2])
        S.op('dve', lambda e: e.tensor_tensor(out=oh, in0=tposf[:].unsqueeze(3).to_broadcast([P, H, 16, 16]),
                                              in1=thr16[:].unsqueeze(1).unsqueeze(1).to_broadcast([P, H, 16, 16]), op=ALU.is_ge),
             reads=[b_tposf, b_iota], writes=[b_oh])
        S.op('dve', lambda e: e.tensor_reduce(out=ta[:], in_=oh, axis=AX.X, op=ALU.add), reads=[b_oh], writes=[b_ta])
        S.op('dve', lambda e: e.tensor_scalar(out=ta[:], in0=ta[:], scalar1=-1.0, scalar2=None, op0=ALU.add), reads=[b_ta], writes=[b_ta])
        S.op('dve', lambda e: e.scalar_tensor_tensor(out=tb[:], in0=ta[:], scalar=-16.0, in1=tposf[:], op0=ALU.mult, op1=ALU.add),
             reads=[b_ta, b_tposf], writes=[b_tb])
        io_b = iota16[:].unsqueeze(1).unsqueeze(1).to_broadcast([P, H, 16, 16])
        for sel, half, dst, bd in ((ta, 0, idx1, b_idx1), (tb, 1, idx2, b_idx2)):
            bsel = b_ta if half == 0 else b_tb
            S.op('dve', lambda e, sel=sel: e.tensor_tensor(out=oh, in0=sel[:].unsqueeze(3).to_broadcast([P, H, 16, 16]), in1=io_b, op=ALU.is_equal),
                 reads=[bsel, b_iota], writes=[b_oh])
            S.op('dve', lambda e, half=half: e.tensor_tensor(out=oh, in0=oh, in1=tif4[:, :, half, :].unsqueeze(2).to_broadcast([P, H, 16, 16]), op=ALU.mult),
                 reads=[b_oh, b_tif], writes=[b_oh])
            S.op('dve', lambda e, dst=dst: e.tensor_reduce(out=dst[:], in_=oh, axis=AX.X, op=ALU.add), reads=[b_oh], writes=[bd])
        S.op('dve', lambda e: e.scalar_tensor_tensor(out=idx1[:], in0=idx1[:], scalar=128.0, in1=idx2[:], op0=ALU.mult, op1=ALU.add),
             reads=[b_idx1, b_idx2], writes=[b_idx1])
        S.op('dve', lambda e: e.tensor_copy(out=eidx[:], in_=idx1[:].rearrange("p h k -> p (h k)")), reads=[b_idx1], writes=[b_eidx])
        S.op('dve', lambda e: e.tensor_tensor(out=gw[:], in0=tsv[:], in1=tsv[:, :, 0:1].to_broadcast([P, H, 16]), op=ALU.subtract), reads=[b_tsv], writes=[b_gw])
        S.op('act', lambda e: e.activation(out=gw[:], in_=gw[:], func=AF.Exp), reads=[b_gw], writes=[b_gw])
        S.op('dve', lambda e: e.tensor_reduce(out=gs[:, 0, :], in_=gw[:], axis=AX.X, op=ALU.add), reads=[b_gw], writes=[b_gs])
        S.op('dve', lambda e: e.reciprocal(out=gs[:, 1, :], in_=gs[:, 0, :]), reads=[b_gs], writes=[b_gs])
        S.op('dve', lambda e: e.tensor_tensor(out=gw[:], in0=gw[:], in1=gs[:, 1, :].unsqueeze(2).to_broadcast([P, H, 16]), op=ALU.mult), reads=[b_gw, b_gs], writes=[b_gw])
        if STAGE == 6:
            S.op('dve', lambda e: e.tensor_copy(out=m2[:, 0:128], in_=idx1[:].rearrange("p h k -> p (h k)")), reads=[b_idx1], writes=[b_m2])
            S.op('dve', lambda e: e.tensor_copy(out=m2[:, 128:256], in_=gw[:].rearrange("p h k -> p (h k)")), reads=[b_gw], writes=[b_m2])
            dump(6, m2[:, 0:256], [b_m2], 256)
        GS = 2
        NGRP = 128 // GS
        gwf = gw[:].rearrange("p h k -> p (h k)")

        def emit_gather(g):
            for k in range(GS):
                j = g * GS + k
                gb_, bgb = gbuf[j % NG], b_gbuf[j % NG]
                S.dma('pool', lambda e, j=j, gb_=gb_: e.indirect_dma_start(out=gb_[:], out_offset=None, in_=uv_bf,
                                                                          in_offset=bass.IndirectOffsetOnAxis(ap=eidx[:, j:j + 1], axis=0)),
                      gsem[j % NG], reads=[b_eidx, b_ubf], writes=[bgb])

        def emit_dots(g):
            for k in range(GS):
                j = g * GS + k
                gb_, bgb = gbuf[j % NG], b_gbuf[j % NG]
                S.op('dve', lambda e, j=j, gb_=gb_: e.scalar_tensor_tensor(out=junk[:], in0=gb_[:, 0:D], scalar=1.0, in1=hn[:],
                                                                          op0=ALU.mult, op1=ALU.mult, accum_out=hv[:, j:j + 1]),
                     reads=[bgb, b_hn], writes=[b_junk, b_hv])

        def emit_pre(g):
            c = slice(g * GS, (g + 1) * GS)
            S.op('act', lambda e: e.activation(out=ga_[:, 0, c], in_=hv[:, c], func=AF.Square), reads=[b_hv], writes=[b_ga])
            S.op('act', lambda e: e.activation(out=ga_[:, 1, c], in_=ga_[:, 0, c], func=AF.Identity, scale=0.0713548162726, bias=gk_t[:, 0:1]),
                 reads=[b_ga, b_gk], writes=[b_ga])
            for k in range(GS):
                j = g * GS + k
                S.op('act', lambda e, j=j: e.activation(out=ga_[:, 3, j:j + 1], in_=ga_[:, 1, j:j + 1], func=AF.Sigmoid, scale=hv[:, j:j + 1]),
                     reads=[b_ga, b_hv], writes=[b_ga2])
            S.op('dve', lambda e: e.tensor_tensor(out=ga_[:, 4, c], in0=hv[:, c], in1=gwf[:, c], op=ALU.mult), reads=[b_hv, b_gw], writes=[b_ga3])

        def emit_post(g):
            for k in range(GS):
                j = g * GS + k
                S.op('act', lambda e, j=j: e.activation(out=aw[:, j:j + 1], in_=ga_[:, 3, j:j + 1], func=AF.Copy, scale=ga_[:, 4, j:j + 1]),
                     reads=[b_ga2, b_ga3], writes=[b_aw])

        def emit_axpy(g):
            for k in range(GS):
                j = g * GS + k
                gb_, bgb = gbuf[j % NG], b_gbuf[j % NG]
                dgj, bdg = dg[j % 4], b_dg[j % 4]
                S.op('act', lambda e, j=j, dgj=dgj: e.activation(out=dgj[:], in_=identF[:], func=AF.Copy, scale=aw[:, j:j + 1]),
                     reads=[b_identF, b_aw], writes=[bdg])
                for hh in range(2):
                    S.op('pe', lambda e, j=j, hh=hh, dgj=dgj, gb_=gb_: e.matmul(out=psA[:, hh * 512:(hh + 1) * 512], lhsT=dgj[:],
                                                                            rhs=gb_[:, D + hh * 512:D + (hh + 1) * 512],
                                                                            start=(j == 0), stop=(j == 127)), reads=[bdg, bgb], writes=[bA])

        cap = []
        if n + 1 < NT:
            S.cap = cap
            m_head(n + 1)
            S.cap = None
        for g0 in range(3):
            emit_gather(g0)
        for st_ in range(NGRP + 1):
            S.replay(cap, 3)
            if 0 <= st_ - 1 < NGRP:
                emit_pre(st_ - 1)
            if st_ < NGRP:
                emit_dots(st_)
            if 0 <= st_ - 1 < NGRP:
                emit_post(st_ - 1)
                emit_axpy(st_ - 1)
            if st_ + 3 < NGRP:
                emit_gather(st_ + 3)
        S.replay(cap)
        S.op('dve', lambda e: e.tensor_tensor(out=acc[:], in0=psA[:], in1=x1[:], op=ALU.add), reads=[bA, b_x1], writes=[b_acc])
        S.dma('sp', lambda e, n=n: e.dma_start(out=y_out[n * P:(n + 1) * P, :], in_=acc[:]), b_yout, reads=[b_acc], writes=[b_yout])

    S.wait_all('sp', [b_yout])
    es.close()
    return nc, None


def _consts():
    hs = np.arange(H, dtype=np.float64)
    gam = 1.0 - 2.0 ** (-5.0 - hs)
    i = np.arange(P, dtype=np.float64)
    c = {}
    c["c_ident"] = np.eye(P, dtype=np.float32)
    c["c_tri"] = (i[:, None] > i[None, :]).astype(np.float32)
    c["c_ones"] = np.ones((P, P), np.float32)
    mp = 1.0e4 * (i[:, None] >= i[None, :]).astype(np.float32)
    c["c_mpos"] = np.tile(mp, (1, 4)).astype(np.float32)
    ms = np.ones((P, 2, 4, P), np.float32)
    ms[:, 1, :, :] = (i[:, None] < i[None, :]).astype(np.float32)[:, None, :]
    c["c_mstay"] = ms.reshape(P, 1024)
    mk = np.zeros((P, H, P), np.float64)
    for h in range(H):
        mk[:, h, :] = (i[None, :] >= i[:, None]) * gam[h] ** (-128.0)
    c["c_maskT"] = mk.reshape(P, 1024).astype(np.float32)
    c["c_qdec"] = (gam[None, :] ** (i[:, None] + 1.0)).astype(np.float32)
    c["c_kdec"] = (0.125 * gam[None, :] ** (127.0 - i[:, None])).astype(np.float32)
    cd = np.zeros((64, D), np.float64)
    for h in range(H):
        cd[:, h * P:(h + 1) * P] = gam[h] ** 128.0
    c["c_cdec"] = cd.astype(np.float32)
    return c, gam


def _rope_tabs(pos):
    half = 32
    freqs = (np.float32(10000.0) ** (-np.arange(half, dtype=np.float32) / np.float32(half))).astype(np.float32)
    ang = (pos.astype(np.float32)[:, :, None] * freqs[None, None, :]).astype(np.float32).astype(np.float64)
    return np.cos(ang).astype(np.float32), np.sin(ang).astype(np.float32)


_CACHE = {}


def kernel(x, norm_attn, w_in, ret_q_norm, ret_k_norm, ret_group_norm, sb_q_norm, sb_k_norm,
           w_branch_ret, w_branch_sb, w_out, norm_ffn, peer_w_q, peer_sub_keys_1,
           peer_sub_keys_2, peer_u, peer_v):
    f = np.float32
    x = np.asarray(x, f)
    B, SEQ, _ = x.shape
    assert B == 1
    NT = SEQ // (NCORES * P)
    NPRE = NT * (NCORES - 1) if FORCE_NPRE is None else FORCE_NPRE
    x2 = x[0]
    key = (NT, NPRE)
    if key not in _CACHE:
        try:
            _CACHE[key] = build_program(NT, NPRE)
        except _Stop:
            _H['es'].close()
            _CACHE[key] = (_H['nc'], None)
    nc, _es = _CACHE[key]
    cst, gam = _consts()
    rep = lambda v, n: np.ascontiguousarray(np.broadcast_to(np.tile(np.asarray(v, f).reshape(-1), n)[None, :], (P, np.asarray(v).size * n)))
    shared = dict(cst)
    shared.update({
        "g_attn": rep(norm_attn[0], 1), "g_ffn": rep(norm_ffn[0], 1), "g_gn": rep(ret_group_norm[0], 1),
        "g_rq": rep(ret_q_norm[0], 8), "g_rk": rep(ret_k_norm[0], 8), "g_sq": rep(sb_q_norm[0], 8), "g_sk": rep(sb_k_norm[0], 8),
        "w_in": np.ascontiguousarray(w_in[0], f), "w_bra": np.ascontiguousarray(w_branch_ret[0], f),
        "w_brb": np.ascontiguousarray(w_branch_sb[0], f), "w_out": np.ascontiguousarray(w_out[0], f),
        "w_q": np.ascontiguousarray(peer_w_q[0], f),
        "k1T": np.ascontiguousarray(np.asarray(peer_sub_keys_1[0], f).T), "k2T": np.ascontiguousarray(np.asarray(peer_sub_keys_2[0], f).T),
        "u_tab": np.ascontiguousarray(peer_u[0], f), "v_tab": np.ascontiguousarray(peer_v[0], f),
    })
    in_maps = []
    pp = np.arange(P, dtype=np.float64)
    for c in range(NCORES):
        t0 = c * NT
        m = dict(shared)
        m["x_own"] = np.ascontiguousarray(x2[t0 * P:(t0 + NT) * P])
        m["x_halo"] = np.ascontiguousarray(x2[(t0 - 1) * P:t0 * P]) if c > 0 else np.zeros((P, D), f)
        npre = max(NPRE, 1)
        xp = np.zeros((npre * P, D), f)
        gt = np.arange(npre) + (t0 - NPRE)
        nvalid = min(t0, NPRE)
        if nvalid > 0:
            xp[(NPRE - nvalid) * P:NPRE * P] = x2[(t0 - nvalid) * P:t0 * P]
        m["x_pre"] = xp
        pos_own = (np.arange(NT)[None, :] + t0) * P + pp[:, None]
        m["cos_own"], m["sin_own"] = _rope_tabs(pos_own)
        pos_pre = np.maximum(gt, 0)[None, :] * P + pp[:, None]
        m["cos_pre"], m["sin_pre"] = _rope_tabs(pos_pre)
        ks = np.zeros((P, npre, H), np.float64)
        for mm in range(npre):
            ks[:, mm, :] = 0.125 * gam[None, :] ** (127.0 - pp[:, None]) * gam[None, :] ** (128.0 * (NPRE - 1 - mm))
        m["ksc_pre"] = ks.astype(f)
        in_maps.append(m)
    res = run_bass_kernel_spmd(nc, in_maps, core_ids=list(range(NCORES)), **RUN_KW)
    _H['res'] = res
    out = np.concatenate([np.asarray(r["y_out"], f) for r in res.results], axis=0)
    return out.reshape(1, SEQ, D)
```

```python
import numpy as np
from contextlib import ExitStack
import concourse.bass as bass
import concourse.mybir as mybir
from concourse.bass_utils import run_bass_kernel_spmd

F32 = mybir.dt.float32
BF16 = mybir.dt.bfloat16
U32 = mybir.dt.uint32
I32 = mybir.dt.int32
AF = mybir.ActivationFunctionType
ALU = mybir.AluOpType
AX = mybir.AxisListType

NCORES = 8
D = 1024
P = 128
H = 8
EPS = 1e-6
NEXP = 16384
INW = 6656
NEG = -1.0e30


class Buf:
    def __init__(self, name):
        self.name = name
        self.w = None
        self.r = {}
        self.dsem = None
        self.dcnt = 0


class Sched:
    def __init__(self, nc, es):
        self.nc = nc
        self.es = es
        self.E = {'pe': nc.tensor, 'act': nc.scalar, 'dve': nc.vector, 'pool': nc.gpsimd, 'sp': nc.sync}
        self.sem = {e: es.enter_context(nc.semaphore('sem_' + e)) for e in self.E}
        self.cnt = {e: 0 for e in self.E}
        self.known = {e: {} for e in self.E}
        self.nsem = 0

    def buf(self, name, dma=False):
        b = Buf(name)
        if dma:
            b.dsem = self.es.enter_context(self.nc.semaphore('d_' + name))
        return b

    def _wait(self, e, ev):
        if ev is None:
            return
        sem, val, src = ev
        if src == e and e == 'pe':
            return
        k = self.known[e]
        if k.get(id(sem), 0) >= val:
            return
        self.E[e].wait_ge(sem, val)
        k[id(sem)] = val

    @staticmethod
    def _flat(bs):
        out = []
        for b in bs:
            if isinstance(b, (list, tuple)):
                out.extend(Sched._flat(b))
            else:
                out.append(b)
        return out

    def _deps(self, e, reads, writes):
        reads = self._flat(reads); writes = self._flat(writes)
        for b in reads:
            self._wait(e, b.w)
        for b in writes:
            self._wait(e, b.w)
            for ev in list(b.r.values()):
                self._wait(e, ev)

    def _post(self, ev, reads, writes):
        reads = self._flat(reads); writes = self._flat(writes)
        for b in reads:
            old = b.r.get(id(ev[0]))
            if old is None or old[1] < ev[1]:
                b.r[id(ev[0])] = ev
        for b in writes:
            b.w = ev
            b.r = {}

    cap = None

    def replay(self, cap, k=None):
        n = len(cap) if k is None else min(k, len(cap))
        for _ in range(n):
            kind, a = cap.pop(0)
            if kind == 'op':
                self.op(*a)
            else:
                self.dma(*a)

    def op(self, e, fn, reads=(), writes=()):
        if self.cap is not None:
            self.cap.append(('op', (e, fn, tuple(reads), tuple(writes))))
            return
        self._deps(e, reads, writes)
        ins = fn(self.E[e])
        self.cnt[e] += 1
        ins.then_inc(self.sem[e], 1)
        self._post((self.sem[e], self.cnt[e], e), reads, writes)

    def dma(self, q, fn, dbuf, reads=(), writes=()):
        if self.cap is not None:
            self.cap.append(('dma', (q, fn, dbuf, tuple(reads), tuple(writes))))
            return
        self._deps(q, reads, writes)
        ins = fn(self.E[q])
        dbuf.dcnt += 16
        ins.then_inc(dbuf.dsem, 16)
        self._post((dbuf.dsem, dbuf.dcnt, 'dma'), reads, writes)

    def wait_all(self, e, bufs):
        for b in self._flat(bufs):
            self._wait(e, b.w)
            for ev in list(b.r.values()):
                self._wait(e, ev)


class _Stop(Exception):
    pass


_H = {}
STAGE = 99
RUN_KW = {}
SKIP_GATHER = False
FORCE_NPRE = None


def build_program(NT, NPRE, dbg=False):
    nc = bass.Bass("TRN2", target_bir_lowering=False)
    es = ExitStack()
    S = Sched(nc, es)
    _H['nc'] = nc; _H['es'] = es

    def din(name, shape, dt=F32):
        return nc.dram_tensor(name, list(shape), dt, kind="ExternalInput").ap()

    x_own = din("x_own", [NT * P, D])
    x_halo = din("x_halo", [P, D])
    x_pre = din("x_pre", [max(NPRE, 1) * P, D])
    cos_own = din("cos_own", [P, NT, 32]); sin_own = din("sin_own", [P, NT, 32])
    cos_pre = din("cos_pre", [P, max(NPRE, 1), 32]); sin_pre = din("sin_pre", [P, max(NPRE, 1), 32])
    ksc_pre = din("ksc_pre", [P, max(NPRE, 1), H])
    g_attn = din("g_attn", [P, D]); g_ffn = din("g_ffn", [P, D]); g_gn = din("g_gn", [P, D])
    g_rq = din("g_rq", [P, 512]); g_rk = din("g_rk", [P, 512]); g_sq = din("g_sq", [P, 512]); g_sk = din("g_sk", [P, 512])
    c_ident = din("c_ident", [P, P]); c_tri = din("c_tri", [P, P]); c_ones = din("c_ones", [P, P])
    c_mpos = din("c_mpos", [P, 512]); c_mstay = din("c_mstay", [P, 1024])
    c_maskT = din("c_maskT", [P, 1024]); c_qdec = din("c_qdec", [P, H]); c_kdec = din("c_kdec", [P, H])
    c_cdec = din("c_cdec", [64, D])
    w_in = din("w_in", [D, INW]); w_bra = din("w_bra", [D, D]); w_brb = din("w_brb", [512, D]); w_out = din("w_out", [D, D])
    w_q = din("w_q", [D, 2048]); k1T = din("k1T", [P, P]); k2T = din("k2T", [P, P])
    u_tab = din("u_tab", [NEXP, D]); v_tab = din("v_tab", [NEXP, D])
    y_out = nc.dram_tensor("y_out", [NT * P, D], F32, kind="ExternalOutput").ap()
    uv_bf = nc.dram_tensor("uv_bf", [NEXP, 2 * D], BF16, kind="Internal").ap()
    NWB = 17
    wsc = nc.dram_tensor("wsc", [NWB * P, 4096], BF16, kind="Internal").ap()
    def dump(stage, src_ap, bsrc, ncols=D):
        if STAGE != stage:
            return
        b_d = S.buf("dump", dma=True)
        npart = src_ap.shape[0]
        S.dma('sp', lambda e: e.dma_start(out=y_out[0:npart, 0:ncols], in_=src_ap), b_d, reads=bsrc, writes=[b_d])
        S.wait_all('sp', [b_d])
        raise _Stop()

    tot = [0]

    def sb(name, shape, dt=F32):
        n = int(np.prod(shape[1:])) * (4 if dt in (F32, U32, I32) else 2)
        tot[0] += n
        if dbg:
            print("SB", name, n, tot[0])
        return es.enter_context(nc.sbuf_tensor(name, list(shape), dt))

    def ps(name, shape, dt=F32):
        return es.enter_context(nc.psum_tensor(name, list(shape), dt))

    psA = ps("psA", [P, 1024]); bA = S.buf("psA")
    psB = ps("psB", [P, 1024]); bB = S.buf("psB")
    psC = ps("psC", [P, 512]); bC = S.buf("psC")
    psD = ps("psD", [P, 512]); bD = S.buf("psD")
    psT = ps("psT", [P, 1024], BF16); bT = S.buf("psT")
    psS = ps("psS", [P, 512]); bS = S.buf("psS")

    consts = []

    def load_const(name, src, shape, dt=F32, q='sp'):
        t = sb(name, shape, dt)
        b = S.buf(name, dma=True)
        S.dma(q, lambda e: e.dma_start(out=t[:], in_=src), b, writes=[b])
        consts.append(b)
        return t, b

    def load_cast(name, src, shape):
        return load_const(name, src, shape, BF16, q='pool')

    ident, b_ident = load_cast("ident", c_ident, [P, P])
    tri, b_tri = load_cast("tri", c_tri, [P, P])
    ones, b_ones = load_cast("ones", c_ones, [P, P])
    mpos, b_mpos = load_cast("mpos", c_mpos, [P, 512])
    mstay, b_mstay = load_cast("mstay", c_mstay, [P, 1024])
    maskT, b_maskT = load_const("maskT", c_maskT, [P, 1024])
    qdec, b_qdec = load_const("qdec", c_qdec, [P, H])
    kdec, b_kdec = load_const("kdec", c_kdec, [P, H])
    cdec, b_cdec = load_const("cdec", c_cdec, [64, D])
    gattn, b_gattn = load_const("gattn", g_attn, [P, D])
    gffn, b_gffn = load_const("gffn", g_ffn, [P, D])
    ggn, b_ggn = load_const("ggn", g_gn, [P, D])
    grq, b_grq = load_const("grq", g_rq, [P, 512])
    grk, b_grk = load_const("grk", g_rk, [P, 512])
    gsq, b_gsq = load_const("gsq", g_sq, [P, 512])
    gsk, b_gsk = load_const("gsk", g_sk, [P, 512])
    cso, b_cso = load_const("cso", cos_own, [P, NT, 32])
    sno, b_sno = load_const("sno", sin_own, [P, NT, 32])

    def wview(w, c0, n):
        return w[:, c0:c0 + n].rearrange("(c p) n -> p c n", p=P)

    TPbig = sb("TPbig", [P, 10, D])
    TP = [TPbig[:, i, :] for i in range(10)]
    b_TP = [S.buf("TP%d" % i, dma=True) for i in range(10)]
    xt = [TP[0], TP[1]]
    b_xt = [b_TP[0], b_TP[1]]
    junk = sb("junk", [P, D], BF16); b_junk = S.buf("junk")
    st4 = sb("st4", [P, 4]); b_st4 = S.buf("st4")

    def rmsnorm_T(src, b_src, gain, b_gain, keep_f32=None, b_keep=None):
        S.op('act', lambda e: e.activation(out=junk[:], in_=src, func=AF.Square, accum_out=st4[:, 0:1]),
             reads=[b_src], writes=[b_junk, b_st4])
        S.op('act', lambda e: e.activation(out=st4[:, 1:2], in_=st4[:, 0:1], func=AF.Sqrt, bias=eps_t[:, 0:1], scale=1.0 / D),
             reads=[b_st4, b_eps], writes=[b_st4])
        S.op('dve', lambda e: e.reciprocal(out=st4[:, 2:3], in_=st4[:, 1:2]), reads=[b_st4], writes=[b_st4])
        if keep_f32 is not None:
            S.op('dve', lambda e: e.scalar_tensor_tensor(out=keep_f32, in0=src, scalar=st4[:, 2:3], in1=gain[:],
                                                         op0=ALU.mult, op1=ALU.mult),
                 reads=[b_src, b_st4, b_gain], writes=[b_keep])
            S.op('act', lambda e: e.activation(out=xn[:], in_=keep_f32, func=AF.Copy), reads=[b_keep], writes=[b_xn])
        else:
            S.op('dve', lambda e: e.scalar_tensor_tensor(out=xn[:], in0=src, scalar=st4[:, 2:3], in1=gain[:],
                                                         op0=ALU.mult, op1=ALU.mult),
                 reads=[b_src, b_st4, b_gain], writes=[b_xn])
        transpose8(xn, b_xn, xnT, b_xnT)

    def transpose8(src, b_src, dst, b_dst, nchunk=8):
        for half in range((nchunk + 3) // 4):
            n = min(4, nchunk - half * 4)
            for k in range(n):
                c = half * 4 + k
                S.op('pe', lambda e, c=c, k=k: e.transpose(out=psT[:, k * P:(k + 1) * P], in_=src[:, c * P:(c + 1) * P],
                                                           identity=ident[:]),
                     reads=[b_src, b_ident], writes=[bT])
            eng = 'act' if half % 2 == 0 else 'dve'
            if eng == 'act':
                S.op('act', lambda e, half=half, n=n: e.activation(
                    out=dst[:, half * 4:half * 4 + n, :].rearrange("p c t -> p (c t)"), in_=psT[:, 0:n * P], func=AF.Copy),
                    reads=[bT], writes=[b_dst])
            else:
                S.op('dve', lambda e, half=half, n=n: e.tensor_copy(
                    out=dst[:, half * 4:half * 4 + n, :].rearrange("p c t -> p (c t)"), in_=psT[:, 0:n * P]),
                    reads=[bT], writes=[b_dst])

    def transposeH(src3, b_src, dst, b_dst):
        for h in range(H):
            S.op('pe', lambda e, h=h: e.transpose(out=psT[0:64, h * P:(h + 1) * P], in_=src3[:, h, :], identity=ident[:]),
                 reads=[b_src, b_ident], writes=[bT])
        S.op('act', lambda e: e.activation(out=dst[:].rearrange("p h t -> p (h t)"), in_=psT[0:64, :], func=AF.Copy),
             reads=[bT], writes=[b_dst])

    def proj512(wb, b_wb, pst, b_pst):
        for c in range(8):
            S.op('pe', lambda e, c=c: e.matmul(out=pst, lhsT=xnT[:, c, :], rhs=wb[:, c, :], start=(c == 0), stop=(c == 7)),
                 reads=[b_xnT, b_wb], writes=[b_pst])

    def qknorm(pst, b_pst, gain, b_gain, slot, scale_ap=None, b_scale=None, cos=None, sin=None, b_cs=(), out_bf=None, b_out=None):
        S.op('act', lambda e: e.activation(out=qf[:], in_=pst, func=AF.Copy), reads=[b_pst], writes=[b_qf])
        S.op('act', lambda e: e.activation(out=sq_s[:], in_=pst, func=AF.Square), reads=[b_pst], writes=[b_sq])
        S.op('dve', lambda e: e.tensor_reduce(out=st8[:, 0, :], in_=sq_s[:].rearrange("p (h d) -> p h d", d=64), axis=AX.X, op=ALU.add),
             reads=[b_sq], writes=[b_st8])
        S.op('act', lambda e: e.activation(out=st8[:, 1, :], in_=st8[:, 0, :], func=AF.Sqrt, bias=eps_t[:, 0:1], scale=1.0 / 64),
             reads=[b_st8, b_eps], writes=[b_st8])
        S.op('dve', lambda e: e.reciprocal(out=st8[:, 2, :], in_=st8[:, 1, :]), reads=[b_st8], writes=[b_st8])
        rs = st8[:, 2, :]
        if scale_ap is not None:
            S.op('dve', lambda e: e.tensor_tensor(out=st8[:, 3, :], in0=st8[:, 2, :], in1=scale_ap, op=ALU.mult),
                 reads=[b_st8, b_scale], writes=[b_st8])
            rs = st8[:, 3, :]
        S.op('dve', lambda e: e.tensor_tensor(out=qn[:].rearrange("p (h d) -> p h d", d=64), in0=qf[:].rearrange("p (h d) -> p h d", d=64),
                                              in1=rs.unsqueeze(2).to_broadcast([P, H, 64]), op=ALU.mult),
             reads=[b_qf, b_st8], writes=[b_qn])
        if cos is None:
            S.op('dve', lambda e: e.tensor_tensor(out=out_bf[:].rearrange("p h d -> p (h d)"), in0=qn[:], in1=gain[:], op=ALU.mult),
                 reads=[b_qn, b_gain], writes=[b_out])
            return
        S.op('pool', lambda e: e.tensor_tensor(out=qn[:], in0=qn[:], in1=gain[:], op=ALU.mult), reads=[b_qn, b_gain], writes=[b_qn])
        q3 = qn[:].rearrange("p (h d) -> p h d", d=64)
        x1 = q3[:, :, 0:32]; x2 = q3[:, :, 32:64]
        cb = cos.unsqueeze(1).to_broadcast([P, H, 32]); sbb = sin.unsqueeze(1).to_broadcast([P, H, 32])
        r = [rt[:, i, :].rearrange("p (h d) -> p h d", d=32) for i in range(4)]
        S.op('dve', lambda e: e.tensor_tensor(out=r[0], in0=x1, in1=cb, op=ALU.mult), reads=[b_qn] + list(b_cs), writes=[b_rt])
        S.op('pool', lambda e: e.tensor_tensor(out=r[1], in0=x2, in1=sbb, op=ALU.mult), reads=[b_qn] + list(b_cs), writes=[b_rt])
        S.op('dve', lambda e: e.tensor_tensor(out=r[2], in0=x1, in1=sbb, op=ALU.mult), reads=[b_qn] + list(b_cs), writes=[b_rt])
        S.op('pool', lambda e: e.tensor_tensor(out=r[3], in0=x2, in1=cb, op=ALU.mult), reads=[b_qn] + list(b_cs), writes=[b_rt])
        S.op('dve', lambda e: e.tensor_tensor(out=out_bf[:, :, 0:32], in0=r[0], in1=r[1], op=ALU.subtract), reads=[b_rt], writes=[b_out])
        S.op('dve', lambda e: e.tensor_tensor(out=out_bf[:, :, 32:64], in0=r[2], in1=r[3], op=ALU.add), reads=[b_rt], writes=[b_out])

    b_ubf = S.buf("uv_bf", dma=True); b_vbf = b_ubf
    b_wsc = S.buf("wsc", dma=True)
    identF, b_identF = load_const("identF", c_ident, [P, P])
    eps_t = sb("eps_t", [P, 1]); b_eps = S.buf("eps")
    S.op('dve', lambda e: e.memset(eps_t[:], EPS), writes=[b_eps])
    gk_t = sb("gk_t", [P, 1]); b_gk = S.buf("gk")
    S.op('dve', lambda e: e.memset(gk_t[:], 1.5957691216057308), writes=[b_gk])
    one_t = sb("one_t", [P, 1]); b_one = S.buf("one")
    S.op('dve', lambda e: e.memset(one_t[:], 1.0), writes=[b_one])

    state = sb("state", [64, D]); b_state = S.buf("state")
    state_bf = sb("state_bf", [64, D], BF16); b_state_bf = S.buf("state_bf")

    with ExitStack() as es0:
        NBLK = NEXP // 128
        NCB = 4
        cb = [es0.enter_context(nc.sbuf_tensor("cb%d" % i, [P, 1, D], BF16)) for i in range(NCB)]
        b_cb = [S.buf("cb%d" % i, dma=True) for i in range(NCB)]
        b_cin = b_cb; b_cout = []
        pc_jobs = [(tab, dst, bd, blk) for blk in range(NBLK) for (tab, dst, bd) in ((u_tab, uv_bf[:, 0:D], b_ubf), (v_tab, uv_bf[:, D:2 * D], b_vbf))]
        pc_state = [0, 0]

        def _pc_store(k):
            tab, dst, bd, blk = pc_jobs[k]
            i = k % NCB
            S.dma('pool', lambda e: e.dma_start(out=dst[blk * 128:(blk + 1) * 128, :].rearrange("(p r) d -> p r d", r=1), in_=cb[i][:]),
                  bd, reads=[b_cb[i]], writes=[bd])

        def precast(nblocks):
            for _ in range(nblocks):
                k = pc_state[0]
                if k >= len(pc_jobs):
                    break
                pc_state[0] += 1
                tab, dst, bd, blk = pc_jobs[k]
                i = k % NCB
                S.dma('pool', lambda e, tab=tab, blk=blk, i=i: e.dma_start(out=cb[i][:], in_=tab[blk * 128:(blk + 1) * 128, :].rearrange("(p r) d -> p r d", r=1)),
                      b_cb[i], writes=[b_cb[i]])
                if k - 2 >= 0:
                    _pc_store(k - 2)
                    pc_state[1] = k - 1
            if pc_state[0] >= len(pc_jobs):
                while pc_state[1] < len(pc_jobs):
                    _pc_store(pc_state[1])
                    pc_state[1] += 1

        wjobs = [(w_in if blk < 13 else w_q, (blk if blk < 13 else blk - 13) * 512, blk, cc) for blk in range(NWB) for cc in range(4)]

        def _w_store(k):
            src, c0, blk, cc = wjobs[k]
            i = k % NCB
            dstv = wsc[blk * P:(blk + 1) * P, :].rearrange("p (c n) -> p c n", c=8)[:, 2 * cc:2 * cc + 2, :]
            S.dma('pool', lambda e: e.dma_start(out=dstv, in_=cb[i][:, 0, :].rearrange("p (c n) -> p c n", c=2)), b_wsc, reads=[b_cb[i]], writes=[b_wsc])

        for k, (src, c0, blk, cc) in enumerate(wjobs):
            i = k % NCB
            S.dma('pool', lambda e, src=src, c0=c0, cc=cc, i=i: e.dma_start(out=cb[i][:, 0, :].rearrange("p (c n) -> p c n", c=2),
                                                                          in_=wview(src, c0, 512)[:, 2 * cc:2 * cc + 2, :]),
                  b_cb[i], writes=[b_cb[i]])
            if k - 2 >= 0:
                _w_store(k - 2)
        _w_store(len(wjobs) - 2); _w_store(len(wjobs) - 1)
        pc_per_tile = -(-len(pc_jobs) // max(NPRE, 1))
        if NPRE > 0:
            wk = es0.enter_context(nc.sbuf_tensor("wk_pre", [P, 8, 512], BF16)); b_wk = S.buf("wk_pre", dma=True)
            wv = es0.enter_context(nc.sbuf_tensor("wv_pre", [P, 8, 1024], BF16)); b_wv = S.buf("wv_pre", dma=True)
            csp = es0.enter_context(nc.sbuf_tensor("csp", [P, NPRE, 32], F32)); b_csp = S.buf("csp", dma=True)
            snp = es0.enter_context(nc.sbuf_tensor("snp", [P, NPRE, 32], F32)); b_snp = S.buf("snp", dma=True)
            ksp = es0.enter_context(nc.sbuf_tensor("ksp", [P, NPRE, H], F32)); b_ksp = S.buf("ksp", dma=True)
            S.dma('pool', lambda e: e.dma_start(out=wk[:], in_=wview(w_in, 512, 512)), b_wk, writes=[b_wk])
            for hh in range(2):
                S.dma('pool', lambda e, hh=hh: e.dma_start(out=wv[:, :, hh * 512:(hh + 1) * 512], in_=wview(w_in, 1024 + hh * 512, 512)),
                      b_wv, writes=[b_wv])
            S.dma('sp', lambda e: e.dma_start(out=csp[:], in_=cos_pre), b_csp, writes=[b_csp])
            S.dma('sp', lambda e: e.dma_start(out=snp[:], in_=sin_pre), b_snp, writes=[b_snp])
            S.dma('sp', lambda e: e.dma_start(out=ksp[:], in_=ksc_pre), b_ksp, writes=[b_ksp])
            B0 = 4
            def _al(name, shape, dt):
                return es0.enter_context(nc.sbuf_tensor(name, list(shape), dt))
            xnb = _al("xnb", [P, 2, D], BF16); xnTb = _al("xnTb", [P, 2, 8, P], BF16); st4b = _al("st4b", [P, B0, 4], F32)
            b_xnb = [S.buf("xnb%d" % (i % 2)) for i in range(2)] * 2; b_xnTb = [S.buf("xnTb%d" % (i % 2)) for i in range(2)] * 2; b_st4b = [S.buf("st4b%d" % i) for i in range(B0)]
            sets = []
            for si in range(2):
                d_ = dict(kf=TPbig[:, 2 + 4 * si:4 + 4 * si, :].rearrange("p a d -> p (a d)").rearrange("p (b n) -> p b n", b=B0),
                          sq=TPbig[:, 4 + 4 * si:6 + 4 * si, :].rearrange("p a d -> p (a d)").rearrange("p (b n) -> p b n", b=B0),
                          s8=_al("s8b%d" % si, [P, 4, B0 * H], F32),
                          r1=_al("r1b%d" % si, [P, B0 * 256], F32),
                          kd=_al("kdb%d" % si, [P, B0, H, 64], BF16), v=_al("vb%d" % si, [P, B0, D], BF16))
                d_.update(b_kf=S.buf("kfb%d" % si), b_sq=S.buf("sqb%d" % si), b_s8=S.buf("s8b%d" % si), b_r0=S.buf("r0b%d" % si),
                          b_r1=S.buf("r1b%d" % si), b_kd=S.buf("kdb%d" % si), b_v=S.buf("vb%d" % si))
                d_["r0"] = d_["kf"].rearrange("p b n -> p (b n)")[:, 0:B0 * 256]; d_["b_r0"] = d_["b_kf"]
                sets.append(d_)
            all_pre_bufs = b_xnb + b_xnTb + b_st4b + [sets[i][k] for i in range(2) for k in ("b_kf", "b_sq", "b_s8", "b_r0", "b_r1", "b_kd", "b_v")]

            def front_a(m, b, st):
                xb = xt[m % 2]; bx = b_xt[m % 2]
                S.dma('sp', lambda e: e.dma_start(out=xb[:], in_=x_pre[m * P:(m + 1) * P, :]), bx, writes=[bx])
                S.op('act', lambda e: e.activation(out=junk[:], in_=xb[:], func=AF.Square, accum_out=st4b[:, b, 0:1]), reads=[bx], writes=[b_junk, b_st4b[b]])
                S.op('act', lambda e: e.activation(out=st4b[:, b, 1:2], in_=st4b[:, b, 0:1], func=AF.Sqrt, bias=eps_t[:, 0:1], scale=1.0 / D),
                     reads=[b_st4b[b], b_eps], writes=[b_st4b[b]])
                S.op('dve', lambda e: e.reciprocal(out=st4b[:, b, 2:3], in_=st4b[:, b, 1:2]), reads=[b_st4b[b]], writes=[b_st4b[b]])
                S.op('dve', lambda e: e.scalar_tensor_tensor(out=xnb[:, b % 2, :], in0=xb[:], scalar=st4b[:, b, 2:3], in1=gattn[:], op0=ALU.mult, op1=ALU.mult),
                     reads=[bx, b_st4b[b], b_gattn], writes=[b_xnb[b]])

            def front_b(m, b, st):
                precast(pc_per_tile)
                for half in range(2):
                    for k in range(4):
                        c = half * 4 + k
                        S.op('pe', lambda e, c=c, k=k: e.transpose(out=psT[:, k * P:(k + 1) * P], in_=xnb[:, b % 2, c * P:(c + 1) * P], identity=ident[:]),
                             reads=[b_xnb[b], b_ident], writes=[bT])
                    dst = xnTb[:, b % 2, half * 4:half * 4 + 4, :].rearrange("p c t -> p (c t)")
                    if half == 0:
                        S.op('act', lambda e, dst=dst: e.activation(out=dst, in_=psT[:, 0:512], func=AF.Copy), reads=[bT], writes=[b_xnTb[b]])
                    else:
                        S.op('dve', lambda e, dst=dst: e.tensor_copy(out=dst, in_=psT[:, 0:512]), reads=[bT], writes=[b_xnTb[b]])
                for c in range(8):
                    S.op('pe', lambda e, c=c: e.matmul(out=psC[:], lhsT=xnTb[:, b % 2, c, :], rhs=wk[:, c, :], start=(c == 0), stop=(c == 7)),
                         reads=[b_xnTb[b], b_wk], writes=[bC])
                S.op('act', lambda e: e.activation(out=st["kf"][:, b, :], in_=psC[:], func=AF.Copy), reads=[bC], writes=[st["b_kf"]])
                S.op('act', lambda e: e.activation(out=st["sq"][:, b, :], in_=psC[:], func=AF.Square), reads=[bC], writes=[st["b_sq"]])
                psV, bV = (psA, bA) if m % 2 == 0 else (psB, bB)
                for hh in range(2):
                    for c in range(8):
                        S.op('pe', lambda e, c=c, hh=hh: e.matmul(out=psV[:, hh * 512:(hh + 1) * 512], lhsT=xnTb[:, b % 2, c, :], rhs=wv[:, c, hh * 512:(hh + 1) * 512],
                                                                  start=(c == 0), stop=(c == 7)), reads=[b_xnTb[b], b_wv], writes=[bV])
                S.op('act', lambda e: e.activation(out=st["v"][:, b, :], in_=psV[:], func=AF.Copy), reads=[bV], writes=[st["b_v"]])

            def chain_gen(m0, nb, st):
                n8 = nb * H
                kf, sq, s8, r0, r1, kdb = st["kf"], st["sq"], st["s8"], st["r0"], st["r1"], st["kd"]
                S.op('dve', lambda e: e.tensor_reduce(out=s8[:, 0, 0:n8], in_=sq[:, 0:nb, :].rearrange("p b (h d) -> p (b h) d", d=64), axis=AX.X, op=ALU.add),
                     reads=[st["b_sq"]], writes=[st["b_s8"]]); yield
                S.op('act', lambda e: e.activation(out=s8[:, 1, 0:n8], in_=s8[:, 0, 0:n8], func=AF.Sqrt, bias=eps_t[:, 0:1], scale=1.0 / 64),
                     reads=[st["b_s8"], b_eps], writes=[st["b_s8"]]); yield
                S.op('dve', lambda e: e.reciprocal(out=s8[:, 2, 0:n8], in_=s8[:, 1, 0:n8]), reads=[st["b_s8"]], writes=[st["b_s8"]]); yield
                S.op('dve', lambda e: e.tensor_tensor(out=s8[:, 3, 0:n8], in0=s8[:, 2, 0:n8], in1=ksp[:, m0:m0 + nb, :].rearrange("p m h -> p (m h)"), op=ALU.mult),
                     reads=[st["b_s8"], b_ksp], writes=[st["b_s8"]]); yield
                q3 = sq[:, 0:nb, :].rearrange("p b (h d) -> p (b h) d", d=64)
                S.op('dve', lambda e: e.tensor_tensor(out=q3, in0=kf[:, 0:nb, :].rearrange("p b (h d) -> p (b h) d", d=64),
                                                      in1=s8[:, 3, 0:n8].unsqueeze(2).to_broadcast([P, n8, 64]), op=ALU.mult),
                     reads=[st["b_kf"], st["b_s8"]], writes=[st["b_sq"]]); yield
                S.op('pool', lambda e: e.tensor_tensor(out=sq[:, 0:nb, :], in0=sq[:, 0:nb, :], in1=grk[:].unsqueeze(1).to_broadcast([P, nb, 512]), op=ALU.mult),
                     reads=[st["b_sq"], b_grk], writes=[st["b_sq"]]); yield
                q4 = sq[:, 0:nb, :].rearrange("p b (h d) -> p b h d", d=64)
                x1_ = q4[:, :, :, 0:32]; x2_ = q4[:, :, :, 32:64]
                cb = csp[:, m0:m0 + nb, :].unsqueeze(2).to_broadcast([P, nb, H, 32]); sb_ = snp[:, m0:m0 + nb, :].unsqueeze(2).to_broadcast([P, nb, H, 32])
                r0v = r0[:, 0:nb * 256].rearrange("p (b h d) -> p b h d", b=nb, h=H); r1v = r1[:, 0:nb * 256].rearrange("p (b h d) -> p b h d", b=nb, h=H)
                S.op('dve', lambda e: e.tensor_tensor(out=r0v, in0=x1_, in1=cb, op=ALU.mult), reads=[st["b_sq"], b_csp], writes=[st["b_r0"]]); yield
                S.op('pool', lambda e: e.tensor_tensor(out=r1v, in0=x2_, in1=sb_, op=ALU.mult), reads=[st["b_sq"], b_snp], writes=[st["b_r1"]]); yield
                S.op('dve', lambda e: e.tensor_tensor(out=kdb[:, 0:nb, :, 0:32], in0=r0v, in1=r1v, op=ALU.subtract), reads=[st["b_r0"], st["b_r1"]], writes=[st["b_kd"]]); yield
                S.op('dve', lambda e: e.tensor_tensor(out=r0v, in0=x1_, in1=sb_, op=ALU.mult), reads=[st["b_sq"], b_snp], writes=[st["b_r0"]]); yield
                S.op('pool', lambda e: e.tensor_tensor(out=r1v, in0=x2_, in1=cb, op=ALU.mult), reads=[st["b_sq"], b_csp], writes=[st["b_r1"]]); yield
                S.op('dve', lambda e: e.tensor_tensor(out=kdb[:, 0:nb, :, 32:64], in0=r0v, in1=r1v, op=ALU.add), reads=[st["b_r0"], st["b_r1"]], writes=[st["b_kd"]]); yield

            def state_mm(m0, nb, st):
                for b in range(nb):
                    m = m0 + b
                    for h in range(H):
                        acc_ps, b_acc_ps = (psS, bS) if h < 4 else (psD, bD)
                        S.op('pe', lambda e, h=h, m=m, b=b, acc_ps=acc_ps: e.matmul(
                            out=acc_ps[0:64, (h % 4) * P:(h % 4 + 1) * P], lhsT=st["kd"][:, b, h, :], rhs=st["v"][:, b, h * P:(h + 1) * P],
                            start=(m == 0 and h % 4 == 0), stop=(m == NPRE - 1), skip_group_check=True),
                            reads=[st["b_kd"], st["b_v"]], writes=[b_acc_ps])

            batches = [(m0, min(B0, NPRE - m0)) for m0 in range(0, NPRE, B0)]
            tiles = [(m0 + b, b, sets[bi % 2]) for bi, (m0, nb) in enumerate(batches) for b in range(nb)]
            pend = None
            front_a(*tiles[0])
            ti_ = 0
            for bi, (m0, nb) in enumerate(batches):
                st = sets[bi % 2]
                gen = chain_gen(*pend) if pend is not None else None
                for b in range(nb):
                    if ti_ + 1 < len(tiles):
                        front_a(*tiles[ti_ + 1])
                    front_b(m0 + b, b, st)
                    ti_ += 1
                    if gen is not None:
                        for _ in range(4):
                            next(gen, None)
                if gen is not None:
                    for _ in gen:
                        pass
                    state_mm(*pend)
                pend = (m0, nb, st)
            for _ in chain_gen(*pend):
                pass
            state_mm(*pend)
            for e_ in ('pe', 'act', 'dve', 'pool', 'sp'):
                S.wait_all(e_, all_pre_bufs)
            S.op('act', lambda e: e.activation(out=state[:, 0:512], in_=psS[0:64, :], func=AF.Copy), reads=[bS], writes=[b_state])
            S.op('act', lambda e: e.activation(out=state[:, 512:1024], in_=psD[0:64, :], func=AF.Copy), reads=[bD], writes=[b_state])
        else:
            S.op('dve', lambda e: e.memset(state[:], 0.0), writes=[b_state])
        S.op('act', lambda e: e.activation(out=state_bf[:], in_=state[:], func=AF.Copy), reads=[b_state], writes=[b_state_bf])
        precast(len(pc_jobs))
        for e_ in ('pe', 'act', 'dve', 'pool', 'sp'):
            S.wait_all(e_, b_cin + b_cout + [b_ubf, b_wsc])
        if NPRE > 0:
            for e_ in ('pe', 'act', 'dve', 'pool'):
                S.wait_all(e_, [b_wk, b_wv, b_csp, b_snp, b_ksp])

    dump(1, state[:], [b_state], D)
    xn = sb("xn", [P, D], BF16); b_xn = S.buf("xn")
    xnT = sb("xnT", [P, 8, P], BF16); b_xnT = S.buf("xnT")
    qf = sb("qf", [P, 512]); b_qf = S.buf("qf")
    sq_s = sb("sq_s", [P, 512]); b_sq = S.buf("sq_s")
    st8 = sb("st8", [P, 4, H]); b_st8 = S.buf("st8")
    qn = sb("qn", [P, 512]); b_qn = S.buf("qn")
    rt = sb("rt", [P, 4, 256]); b_rt = S.buf("rt")
    kd = sb("kd", [P, H, 64], BF16); b_kd = S.buf("kd")
    v_r = sb("v_r", [P, D], BF16); b_vr = S.buf("v_r")
    wbra = sb("wbra", [P, 8, D], BF16); b_wbra = S.buf("wbra", dma=True)
    wbrb = sb("wbrb", [P, 4, D], BF16); b_wbrb = S.buf("wbrb", dma=True)
    wout = sb("wout", [P, 8, D], BF16); b_wout = S.buf("wout", dma=True)
    k1 = sb("k1", [P, P], BF16); b_k1 = S.buf("k1", dma=True)
    k2 = sb("k2", [P, P], BF16); b_k2 = S.buf("k2", dma=True)
    for hh in range(2):
        S.dma('pool', lambda e, hh=hh: e.dma_start(out=wbra[:, :, hh * 512:(hh + 1) * 512], in_=wview(w_bra, hh * 512, 512)), b_wbra, writes=[b_wbra])
        S.dma('pool', lambda e, hh=hh: e.dma_start(out=wbrb[:, :, hh * 512:(hh + 1) * 512], in_=wview(w_brb, hh * 512, 512)), b_wbrb, writes=[b_wbrb])
        S.dma('pool', lambda e, hh=hh: e.dma_start(out=wout[:, :, hh * 512:(hh + 1) * 512], in_=wview(w_out, hh * 512, 512)), b_wout, writes=[b_wout])
    S.dma('pool', lambda e: e.dma_start(out=k1[:], in_=k1T), b_k1, writes=[b_k1])
    S.dma('pool', lambda e: e.dma_start(out=k2[:], in_=k2T), b_k2, writes=[b_k2])

    wbuf = [sb("wbuf%d" % i, [P, 8, 512], BF16) for i in range(2)]
    b_wbuf = [S.buf("wbuf%d" % i, dma=True) for i in range(2)]
    wcount = [0]

    def stream_w(c0, src=None):
        blk = c0 // 512 if src is None else 13 + c0 // 512
        i = wcount[0] % 2
        wcount[0] += 1
        S.dma('sp', lambda e: e.dma_start(out=wbuf[i][:], in_=wsc[blk * P:(blk + 1) * P, :].rearrange("p (c n) -> p c n", c=8)),
              b_wbuf[i], reads=[b_wsc], writes=[b_wbuf[i]])
        return wbuf[i], b_wbuf[i]

    xres = sb("xres", [P, D]); b_xres = S.buf("xres", dma=True)
    qd = sb("qd", [P, H, 64], BF16); b_qd = S.buf("qd")
    qdT = sb("qdT", [64, H, P], BF16); b_qdT = S.buf("qdT")
    kdT = sb("kdT", [64, H, P], BF16); b_kdT = S.buf("kdT")
    rg = sb("rg", [P, D], BF16); b_rg = S.buf("rg")
    sqn = sb("sqn", [P, H, 64], BF16); b_sqn = S.buf("sqn")
    skn = sb("skn", [P, H, 64], BF16); b_skn = S.buf("skn")
    sqT = sb("sqT", [64, H, P], BF16); b_sqT = S.buf("sqT")
    skT = [sb("skT%d" % i, [64, H, P], BF16) for i in range(2)]; b_skT = [S.buf("skT%d" % i) for i in range(2)]
    sv = [sb("sv%d" % i, [P, 512], BF16) for i in range(2)]; b_sv = [S.buf("sv%d" % i) for i in range(2)]
    siga = TP[7]; b_siga = b_TP[7]
    sigb = TP[8]; b_sigb = b_TP[8]
    pm = sb("pm", [P, D], BF16); b_pm = S.buf("pm")
    yf = TP[3]; b_yf = b_TP[3]
    ysq = TP[4]; b_ysq = b_TP[4]
    gst = sb("gst", [P, 6, H]); b_gst = S.buf("gst")
    ret = sb("ret", [P, D], BF16); b_ret = S.buf("ret")
    retT = sb("retT", [P, 8, P], BF16); b_retT = S.buf("retT")
    e_s = TP[3]; b_es = b_TP[3]
    sp_s = TP[4]; b_sp = b_TP[4]
    spm = sb("spm", [P, D], BF16); b_spm = S.buf("spm")
    u_s = TP[0]; b_us = b_TP[0]
    w_s = sb("w_s", [P, D], BF16); b_ws = S.buf("w_s")
    sbT = sb("sbT", [P, 4, P], BF16); b_sbT = S.buf("sbT")
    m1 = TP[5]; b_m1 = b_TP[5]
    m2 = TP[6]; b_m2 = b_TP[6]
    mixed = ret; b_mixed = b_ret
    mixT = retT; b_mixT = b_retT
    x1 = TP[1]; b_x1 = b_TP[1]
    hn = TP[9]; b_hn = b_TP[9]
    qT = sb("qT", [P, 16, P], BF16); b_qT = S.buf("qT")
    sc = TPbig[:, 3:5, :].rearrange("p a d -> p (a d)").rearrange("p (g n) -> p g n", g=16); b_sc = [b_TP[3], b_TP[4]]
    sc2 = TPbig[:, 5:7, :].rearrange("p a d -> p (a d)").rearrange("p (g n) -> p g n", g=16); b_sc2 = [b_TP[5], b_TP[6]]
    tv = sb("tv", [P, 16, 16]); b_tv = S.buf("tv")
    ti = sb("ti", [P, 16, 16], U32); b_ti = S.buf("ti")
    tif = sb("tif", [P, 16, 16]); b_tif = S.buf("tif")
    cand = TPbig[:, 7:9, :].rearrange("p a d -> p (a d)").rearrange("p (h c) -> p h c", h=H); b_cand = [b_TP[7], b_TP[8]]
    cand2 = sc.rearrange("p g n -> p (g n)").rearrange("p (h c) -> p h c", h=H); b_cand2 = b_sc
    tsv = sb("tsv", [P, H, 16]); b_tsv = S.buf("tsv")
    tpos = sb("tpos", [P, H, 16], U32); b_tpos = S.buf("tpos")
    tposf = sb("tposf", [P, H, 16]); b_tposf = S.buf("tposf")
    ta = sb("ta", [P, H, 16]); b_ta = S.buf("ta")
    tb = sb("tb", [P, H, 16]); b_tb = S.buf("tb")
    iota16 = sb("iota16", [P, 16]); b_iota = S.buf("iota16")
    oh = sc2.rearrange("p g n -> p (g n)").rearrange("p (h a b) -> p h a b", h=H, a=16); b_oh = b_sc2
    idx1 = sb("idx1", [P, H, 16]); b_idx1 = S.buf("idx1")
    idx2 = sb("idx2", [P, H, 16]); b_idx2 = S.buf("idx2")
    eidx = sb("eidx", [P, 128], U32); b_eidx = S.buf("eidx")
    gw = sb("gw", [P, H, 16]); b_gw = S.buf("gw")
    gs = sb("gs", [P, 2, H]); b_gs = S.buf("gs")
    hv = sb("hv", [P, 128]); b_hv = S.buf("hv")
    ga_ = sb("ga_", [P, 6, 128]); b_ga = S.buf("ga_"); b_ga2 = S.buf("ga2"); b_ga3 = S.buf("ga3")
    aw = sb("aw", [P, 128]); b_aw = S.buf("aw")
    NG = 8
    dg = [sb("dg%d" % i, [P, P], BF16) for i in range(4)]; b_dg = [S.buf("dg%d" % i) for i in range(4)]
    gbuf = None; b_gbuf = None
    acc = TP[0]; b_acc = b_TP[0]
    b_yout = S.buf("yout", dma=True)

    _gi = [3, 4, 5, 6, 7, 8, 2, 0]
    gbuf = [TPbig[:, i, :].bitcast(BF16) for i in _gi]
    b_gbuf = [b_TP[i] for i in _gi]
    gsem = [S.buf("gsem%d" % i, dma=True) for i in range(len(_gi))]
    S.op('pool', lambda e: e.iota(iota16[:], pattern=[[1, 16]], base=0, channel_multiplier=0, allow_small_or_imprecise_dtypes=True),
         writes=[b_iota])
    thr16 = sb("thr16", [P, 16])
    S.op('pool', lambda e: e.iota(thr16[:], pattern=[[16, 16]], base=0, channel_multiplier=0, allow_small_or_imprecise_dtypes=True),
         reads=[b_iota], writes=[b_iota])

    def sb_kv(cur):
        wb, bw = stream_w(3584)
        proj512(wb, bw, psC[:], bC)
        qknorm(psC[:], bC, gsk, b_gsk, 0, out_bf=skn, b_out=b_skn)
        transposeH(skn, b_skn, skT[cur], b_skT[cur])
        wb, bw = stream_w(4096)
        proj512(wb, bw, psD[:], bD)
        S.op('act', lambda e: e.activation(out=sv[cur][:], in_=psD[:], func=AF.Copy), reads=[bD], writes=[b_sv[cur]])

    S.dma('sp', lambda e: e.dma_start(out=xt[0][:], in_=x_halo), b_xt[0], writes=[b_xt[0]])
    rmsnorm_T(xt[0][:], b_xt[0], gattn, b_gattn)
    sb_kv(1)
    dump(2, xt[0][:], [b_xt[0], b_sv[1], b_skT[1]])

    def m_head(n):
        cur = n % 2
        S.dma('sp', lambda e, n=n: e.dma_start(out=xres[:], in_=x_own[n * P:(n + 1) * P, :]), b_xres, writes=[b_xres])
        rmsnorm_T(xres[:], b_xres, gattn, b_gattn)
        wb, bw = stream_w(0)
        proj512(wb, bw, psC[:], bC)
        qknorm(psC[:], bC, grq, b_grq, 0, scale_ap=qdec[:], b_scale=b_qdec, cos=cso[:, n, :], sin=sno[:, n, :],
               b_cs=[b_cso, b_sno], out_bf=qd, b_out=b_qd)
        transposeH(qd, b_qd, qdT, b_qdT)
        wb, bw = stream_w(512)
        proj512(wb, bw, psD[:], bD)
        qknorm(psD[:], bD, grk, b_grk, 0, scale_ap=kdec[:], b_scale=b_kdec, cos=cso[:, n, :], sin=sno[:, n, :],
               b_cs=[b_cso, b_sno], out_bf=kd, b_out=b_kd)
        transposeH(kd, b_kd, kdT, b_kdT)
        wb, bw = stream_w(3072)
        proj512(wb, bw, psC[:], bC)
        qknorm(psC[:], bC, gsq, b_gsq, 0, out_bf=sqn, b_out=b_sqn)
        transposeH(sqn, b_sqn, sqT, b_sqT)
        sb_kv(cur)

    m_head(0)
    for n in range(NT):
        cur = n % 2; prv = 1 - cur
        for hh in range(2):
            wb, bw = stream_w(1024 + hh * 512)
            proj512(wb, bw, psA[:, hh * 512:(hh + 1) * 512], bA)
        S.op('act', lambda e: e.activation(out=v_r[:], in_=psA[:], func=AF.Copy), reads=[bA], writes=[b_vr])
        for hh in range(2):
            wb, bw = stream_w(2048 + hh * 512)
            proj512(wb, bw, psB[:, hh * 512:(hh + 1) * 512], bB)
        S.op('act', lambda e: e.activation(out=m1[:], in_=psB[:], func=AF.Silu), reads=[bB], writes=[b_m1])
        S.op('pool', lambda e: e.tensor_tensor(out=rg[:], in0=m1[:], in1=ggn[:], op=ALU.mult), reads=[b_m1, b_ggn], writes=[b_rg])
        for h in range(H):
            S.op('pe', lambda e, h=h: e.matmul(out=psA[:, h * P:(h + 1) * P], lhsT=kdT[:, h, :], rhs=qdT[:, h, :], start=True, stop=True),
                 reads=[b_kdT, b_qdT], writes=[bA])
        S.op('dve', lambda e: e.tensor_tensor(out=pm[:], in0=psA[:], in1=maskT[:], op=ALU.mult), reads=[bA, b_maskT], writes=[b_pm])
        for h in range(H):
            S.op('pe', lambda e, h=h: e.matmul(out=psB[:, h * P:(h + 1) * P], lhsT=pm[:, h * P:(h + 1) * P], rhs=v_r[:, h * P:(h + 1) * P],
                                               start=True, stop=False, skip_group_check=True),
                 reads=[b_pm, b_vr], writes=[bB])
            S.op('pe', lambda e, h=h: e.matmul(out=psB[:, h * P:(h + 1) * P], lhsT=qdT[:, h, :], rhs=state_bf[:, h * P:(h + 1) * P],
                                               start=False, stop=True, skip_group_check=True),
                 reads=[b_qdT, b_state_bf], writes=[bB])
        S.op('dve', lambda e: e.tensor_tensor(out=state[:], in0=state[:], in1=cdec[:], op=ALU.mult), reads=[b_state, b_cdec], writes=[b_state])
        for rnd in range(2):
            for hl in range(4):
                h = rnd * 4 + hl
                S.op('pe', lambda e, h=h, hl=hl: e.matmul(out=psS[0:64, hl * P:(hl + 1) * P], lhsT=kd[:, h, :], rhs=v_r[:, h * P:(h + 1) * P],
                                                   start=True, stop=True, skip_group_check=True),
                     reads=[b_kd, b_vr], writes=[bS])
            S.op('dve', lambda e, rnd=rnd: e.tensor_tensor(out=state[:, rnd * 512:(rnd + 1) * 512], in0=state[:, rnd * 512:(rnd + 1) * 512],
                                                         in1=psS[0:64, :], op=ALU.add), reads=[b_state, bS], writes=[b_state])
        S.op('act', lambda e: e.activation(out=state_bf[:], in_=state[:], func=AF.Copy), reads=[b_state], writes=[b_state_bf])
        S.op('act', lambda e: e.activation(out=yf[:], in_=psB[:], func=AF.Copy), reads=[bB], writes=[b_yf])
        S.op('act', lambda e: e.activation(out=ysq[:], in_=psB[:], func=AF.Square), reads=[bB], writes=[b_ysq])
        S.op('dve', lambda e: e.tensor_reduce(out=gst[:, 0, :], in_=yf[:].rearrange("p (h d) -> p h d", d=P), axis=AX.X, op=ALU.add),
             reads=[b_yf], writes=[b_gst])
        S.op('dve', lambda e: e.tensor_reduce(out=gst[:, 1, :], in_=ysq[:].rearrange("p (h d) -> p h d", d=P), axis=AX.X, op=ALU.add),
             reads=[b_ysq], writes=[b_gst])
        S.op('dve', lambda e: e.tensor_scalar(out=gst[:, 2, :], in0=gst[:, 0, :], scalar1=1.0 / P, scalar2=None, op0=ALU.mult), reads=[b_gst], writes=[b_gst])
        S.op('dve', lambda e: e.tensor_tensor(out=gst[:, 3, :], in0=gst[:, 2, :], in1=gst[:, 2, :], op=ALU.mult), reads=[b_gst], writes=[b_gst])
        S.op('dve', lambda e: e.scalar_tensor_tensor(out=gst[:, 4, :], in0=gst[:, 1, :], scalar=1.0 / P, in1=gst[:, 3, :], op0=ALU.mult, op1=ALU.subtract),
             reads=[b_gst], writes=[b_gst])
        S.op('act', lambda e: e.activation(out=gst[:, 5, :], in_=gst[:, 4, :], func=AF.Sqrt, bias=eps_t[:, 0:1], scale=1.0), reads=[b_gst, b_eps], writes=[b_gst])
        S.op('dve', lambda e: e.reciprocal(out=gst[:, 3, :], in_=gst[:, 5, :]), reads=[b_gst], writes=[b_gst])
        y3 = yf[:].rearrange("p (h d) -> p h d", d=P)
        S.op('dve', lambda e: e.tensor_tensor(out=y3, in0=y3, in1=gst[:, 2, :].unsqueeze(2).to_broadcast([P, H, P]), op=ALU.subtract),
             reads=[b_yf, b_gst], writes=[b_yf])
        S.op('dve', lambda e: e.tensor_tensor(out=y3, in0=y3, in1=gst[:, 3, :].unsqueeze(2).to_broadcast([P, H, P]), op=ALU.mult),
             reads=[b_yf, b_gst], writes=[b_yf])
        S.op('pool', lambda e: e.tensor_tensor(out=ret[:], in0=yf[:], in1=rg[:], op=ALU.mult), reads=[b_yf, b_rg], writes=[b_ret])
        transpose8(ret, b_ret, retT, b_retT)
        dump(3, yf[:], [b_yf, b_retT])
        for half in range(2):
            for blk, kT_ in ((0, skT[prv]), (1, skT[cur])):
                for hl in range(4):
                    h = half * 4 + hl
                    S.op('pe', lambda e, blk=blk, hl=hl, h=h, kT_=kT_: e.matmul(
                        out=psA[:, blk * 512 + hl * P: blk * 512 + (hl + 1) * P], lhsT=kT_[:, h, :],
                        rhs=sqT[:, h, :], start=True, stop=True),
                        reads=[b_skT[prv], b_skT[cur], b_sqT], writes=[bA])
            S.op('act', lambda e: e.activation(out=e_s[:], in_=psA[:], func=AF.Exp, scale=0.125), reads=[bA], writes=[b_es])
            S.op('act', lambda e: e.activation(out=sp_s[:], in_=e_s[:], func=AF.Ln, bias=one_t[:, 0:1], scale=1.0), reads=[b_es, b_one], writes=[b_sp])
            S.op('dve', lambda e: e.tensor_tensor(
                out=spm[:].rearrange("p (b h t) -> p b h t", b=2, h=4), in0=sp_s[:].rearrange("p (b h t) -> p b h t", b=2, h=4),
                in1=mstay[:].rearrange("p (b h t) -> p b h t", b=2, h=4), op=ALU.mult), reads=[b_sp, b_mstay], writes=[b_spm])
            S.op('pe', lambda e: e.matmul(out=psB[:, 0:512], lhsT=tri[:], rhs=spm[:, 0:512], start=True, stop=False), reads=[b_tri, b_spm], writes=[bB])
            S.op('pe', lambda e: e.matmul(out=psB[:, 0:512], lhsT=ones[:], rhs=spm[:, 512:1024], start=False, stop=True), reads=[b_ones, b_spm], writes=[bB])
            S.op('pe', lambda e: e.matmul(out=psB[:, 512:1024], lhsT=tri[:], rhs=spm[:, 512:1024], start=True, stop=False), reads=[b_tri, b_spm], writes=[bB])
            S.op('pe', lambda e: e.matmul(out=psB[:, 512:1024], lhsT=ident[:], rhs=mpos[:], start=False, stop=True), reads=[b_ident, b_mpos], writes=[bB])
            S.op('dve', lambda e: e.scalar_tensor_tensor(out=u_s[:], in0=psA[:], scalar=0.125, in1=sp_s[:], op0=ALU.mult, op1=ALU.subtract),
                 reads=[bA, b_sp], writes=[b_us])
            S.op('dve', lambda e: e.tensor_tensor(out=u_s[:], in0=u_s[:], in1=psB[:], op=ALU.subtract), reads=[b_us, bB], writes=[b_us])
            S.op('act', lambda e: e.activation(out=w_s[:], in_=u_s[:], func=AF.Exp), reads=[b_us], writes=[b_ws])
            for hl in range(4):
                h = half * 4 + hl
                po = (h % 2) * 64
                for blk, svb, bsv in ((0, sv[prv], b_sv[prv]), (1, sv[cur], b_sv[cur])):
                    S.op('pe', lambda e, h=h, hl=hl, po=po, blk=blk, svb=svb: e.matmul(
                        out=psS[po:po + 64, (h // 2) * P:(h // 2 + 1) * P], lhsT=svb[:, h * 64:(h + 1) * 64],
                        rhs=w_s[:, blk * 512 + hl * P: blk * 512 + (hl + 1) * P], start=(blk == 0), stop=(blk == 1), skip_group_check=True),
                        reads=[bsv, b_ws], writes=[bS])
        S.op('act', lambda e: e.activation(out=sbT[:].rearrange("p c t -> p (c t)"), in_=psS[:], func=AF.Copy), reads=[bS], writes=[b_sbT])
        if STAGE == 4:
            S.op('act', lambda e: e.activation(out=m2[:, 0:512], in_=psS[:], func=AF.Copy), reads=[bS], writes=[b_m2])
            dump(4, m2[:, 0:512], [b_m2], 512)
        for hh in range(2):
            for c in range(8):
                S.op('pe', lambda e, c=c, hh=hh: e.matmul(out=psA[:, hh * 512:(hh + 1) * 512], lhsT=retT[:, c, :], rhs=wbra[:, c, hh * 512:(hh + 1) * 512],
                                                          start=(c == 0), stop=(c == 7)), reads=[b_retT, b_wbra], writes=[bA])
            for c in range(4):
                S.op('pe', lambda e, c=c, hh=hh: e.matmul(out=psB[:, hh * 512:(hh + 1) * 512], lhsT=sbT[:, c, :], rhs=wbrb[:, c, hh * 512:(hh + 1) * 512],
                                                          start=(c == 0), stop=(c == 3)), reads=[b_sbT, b_wbrb], writes=[bB])
        for gi, (gt, bg) in enumerate(((siga, b_siga), (sigb, b_sigb))):
            for hh in range(2):
                wb, bw = stream_w(4608 + gi * 1024 + hh * 512)
                pst, bp = (psC, bC) if hh == 0 else (psD, bD)
                proj512(wb, bw, pst[:], bp)
                S.op('act', lambda e, gt=gt, hh=hh, pst=pst: e.activation(out=gt[:, hh * 512:(hh + 1) * 512], in_=pst[:], func=AF.Sigmoid),
                     reads=[bp], writes=[bg])
        S.op('dve', lambda e: e.tensor_tensor(out=m1[:], in0=psA[:], in1=siga[:], op=ALU.mult), reads=[bA, b_siga], writes=[b_m1])
        S.op('dve', lambda e: e.tensor_tensor(out=m2[:], in0=psB[:], in1=sigb[:], op=ALU.mult), reads=[bB, b_sigb], writes=[b_m2])
        S.op('pool', lambda e: e.tensor_tensor(out=mixed[:], in0=m1[:], in1=m2[:], op=ALU.add), reads=[b_m1, b_m2], writes=[b_mixed])
        transpose8(mixed, b_mixed, mixT, b_mixT)
        for hh in range(2):
            for c in range(8):
                S.op('pe', lambda e, c=c, hh=hh: e.matmul(out=psA[:, hh * 512:(hh + 1) * 512], lhsT=mixT[:, c, :], rhs=wout[:, c, hh * 512:(hh + 1) * 512],
                                                          start=(c == 0), stop=(c == 7)), reads=[b_mixT, b_wout], writes=[bA])
        S.op('dve', lambda e: e.tensor_tensor(out=x1[:], in0=psA[:], in1=xres[:], op=ALU.add), reads=[bA, b_xres], writes=[b_x1])
        dump(5, x1[:], [b_x1])
        rmsnorm_T(x1[:], b_x1, gffn, b_gffn, keep_f32=hn[:], b_keep=b_hn)
        for g4 in range(4):
            wqb, b_wq = stream_w(g4 * 512, w_q)
            for gl in range(4):
                g = g4 * 4 + gl
                pst = psA[:, gl * P:(gl + 1) * P] if g4 % 2 == 0 else psB[:, gl * P:(gl + 1) * P]
                bp = bA if g4 % 2 == 0 else bB
                for c in range(8):
                    S.op('pe', lambda e, c=c, gl=gl, pst=pst, wqb=wqb: e.matmul(out=pst, lhsT=wqb[:, c, gl * P:(gl + 1) * P], rhs=xnT[:, c, :],
                                                                     start=(c == 0), stop=(c == 7)), reads=[b_wq, b_xnT], writes=[bp])
            src = psA if g4 % 2 == 0 else psB
            bp = bA if g4 % 2 == 0 else bB
            S.op('act', lambda e, g4=g4, src=src: e.activation(out=qT[:, g4 * 4:(g4 + 1) * 4, :].rearrange("p g t -> p (g t)"), in_=src[:, 0:512], func=AF.Copy),
                 reads=[bp], writes=[b_qT])
        for g4 in range(4):
            pst, bp = (psA, bA) if g4 % 2 == 0 else (psB, bB)
            for gl in range(4):
                g = g4 * 4 + gl
                kk, bk = (k1, b_k1) if g % 2 == 0 else (k2, b_k2)
                S.op('pe', lambda e, g=g, gl=gl, pst=pst, kk=kk: e.matmul(out=pst[:, gl * P:(gl + 1) * P], lhsT=qT[:, g, :], rhs=kk[:], start=True, stop=True),
                     reads=[b_qT, bk], writes=[bp])
            S.op('act', lambda e, g4=g4, pst=pst: e.activation(out=sc[:, g4 * 4:(g4 + 1) * 4, :].rearrange("p g n -> p (g n)"), in_=pst[:, 0:512], func=AF.Copy),
                 reads=[bp], writes=[b_sc])
        bg_tv = [S.buf("tv%d" % g) for g in range(16)]; bg_ti = [S.buf("ti%d" % g) for g in range(16)]; bg_s2 = [S.buf("s2%d" % g) for g in range(16)]
        for g in range(16):
            S.op('dve', lambda e, g=g: e.max(out=tv[:, g, 0:8], in_=sc[:, g, :]), reads=[b_sc], writes=[bg_tv[g]])
        for g in range(16):
            S.op('dve', lambda e, g=g: e.match_replace(out=sc2[:, g, :], in_to_replace=tv[:, g, 0:8], in_values=sc[:, g, :], imm_value=NEG),
                 reads=[b_sc, bg_tv[g]], writes=[bg_s2[g]])
        for g in range(16):
            S.op('dve', lambda e, g=g: e.max_index(out=ti[:, g, 0:8], in_max=tv[:, g, 0:8], in_values=sc[:, g, :]), reads=[b_sc, bg_tv[g]], writes=[bg_ti[g]])
        for g in range(16):
            S.op('dve', lambda e, g=g: e.max(out=tv[:, g, 8:16], in_=sc2[:, g, :]), reads=[bg_s2[g]], writes=[bg_tv[g]])
        for g in range(16):
            S.op('dve', lambda e, g=g: e.max_index(out=ti[:, g, 8:16], in_max=tv[:, g, 8:16], in_values=sc2[:, g, :]), reads=[bg_s2[g], bg_tv[g]], writes=[bg_ti[g]])
        b_tv.w = None; b_ti.w = None
        S.op('dve', lambda e: e.tensor_copy(out=tif[:], in_=ti[:]), reads=bg_ti + bg_tv + bg_s2 + [b_sc2], writes=[b_tif, b_tv, b_ti, b_sc2])
        tv4 = tv[:].rearrange("p (h s) k -> p h s k", s=2)
        tif4 = tif[:].rearrange("p (h s) k -> p h s k", s=2)
        S.op('dve', lambda e: e.tensor_tensor(out=cand.rearrange("p h (a b) -> p h a b", a=16),
                                              in0=tv4[:, :, 0, :].unsqueeze(3).to_broadcast([P, H, 16, 16]),
                                              in1=tv4[:, :, 1, :].unsqueeze(2).to_broadcast([P, H, 16, 16]), op=ALU.add),
             reads=[b_tv], writes=[b_cand])
        bh_ts = [S.buf("ts%d" % h) for h in range(H)]; bh_tp = [S.buf("tp%d" % h) for h in range(H)]; bh_c2 = [S.buf("c2%d" % h) for h in range(H)]
        for h in range(H):
            S.op('dve', lambda e, h=h: e.max(out=tsv[:, h, 0:8], in_=cand[:, h, :]), reads=[b_cand], writes=[bh_ts[h]])
        for h in range(H):
            S.op('dve', lambda e, h=h: e.match_replace(out=cand2[:, h, :], in_to_replace=tsv[:, h, 0:8], in_values=cand[:, h, :], imm_value=NEG),
                 reads=[b_cand, bh_ts[h], b_cand2], writes=[bh_c2[h]])
        for h in range(H):
            S.op('dve', lambda e, h=h: e.max_index(out=tpos[:, h, 0:8], in_max=tsv[:, h, 0:8], in_values=cand[:, h, :]), reads=[b_cand, bh_ts[h]], writes=[bh_tp[h]])
        for h in range(H):
            S.op('dve', lambda e, h=h: e.max(out=tsv[:, h, 8:16], in_=cand2[:, h, :]), reads=[bh_c2[h]], writes=[bh_ts[h]])
        for h in range(H):
            S.op('dve', lambda e, h=h: e.max_index(out=tpos[:, h, 8:16], in_max=tsv[:, h, 8:16], in_values=cand2[:, h, :]), reads=[bh_c2[h], bh_ts[h]], writes=[bh_tp[h]])
        b_tsv.w = None; b_tpos.w = None
        S.op('dve', lambda e: e.tensor_copy(out=tposf[:], in_=tpos[:]), reads=bh_tp + bh_ts + bh_c2, writes=[b_tposf, b_tsv, b_tpos, b_cand2])
        S.op('dve', lambda e: e.tensor_tensor(out=oh, in0=tposf[:].unsqueeze(3).to_broadcast([P, H, 16, 16]),
                                              in1=thr16[:].unsqueeze(1).unsqueeze(1).to_broadcast([P, H, 16, 16]), op=ALU.is_ge),
             reads=[b_tposf, b_iota], writes=[b_oh])
        S.op('dve', lambda e: e.tensor_reduce(out=ta[:], in_=oh, axis=AX.X, op=ALU.add), reads=[b_oh], writes=[b_ta])
        S.op('dve', lambda e: e.tensor_scalar(out=ta[:], in0=ta[:], scalar1=-1.0, scalar2=None, op0=ALU.add), reads=[b_ta], writes=[b_ta])
        S.op('dve', lambda e: e.scalar_tensor_tensor(out=tb[:], in0=ta[:], scalar=-16.0, in1=tposf[:], op0=ALU.mult, op1=ALU.add),
             reads=[b_ta, b_tposf], writes=[b_tb])
        io_b = iota16[:].unsqueeze(1).unsqueeze(1).to_broadcast([P, H, 16, 16])
        for sel, half, dst, bd in ((ta, 0, idx1, b_idx1), (tb, 1, idx2, b_idx2)):
            bsel = b_ta if half == 0 else b_tb
            S.op('dve', lambda e, sel=sel: e.tensor_tensor(out=oh, in0=sel[:].unsqueeze(3).to_broadcast([P, H, 16, 16]), in1=io_b, op=ALU.is_equal),
                 reads=[bsel, b_iota], writes=[b_oh])
            S.op('dve', lambda e, half=half: e.tensor_tensor(out=oh, in0=oh, in1=tif4[:, :, half, :].unsqueeze(2).to_broadcast([P, H, 16, 16]), op=ALU.mult),
                 reads=[b_oh, b_tif], writes=[b_oh])
            S.op('dve', lambda e, dst=dst: e.tensor_reduce(out=dst[:], in_=oh, axis=AX.X, op=ALU.add), reads=[b_oh], writes=[bd])
        S.op('dve', lambda e: e.scalar_tensor_tensor(out=idx1[:], in0=idx1[:], scalar=128.0, in1=idx2[:], op0=ALU.mult, op1=ALU.add),
             reads=[b_idx1, b_idx2], writes=[b_idx1])
        S.op('dve', lambda e: e.tensor_copy(out=eidx[:], in_=idx1[:].rearrange("p h k -> p (h k)")), reads=[b_idx1], writes=[b_eidx])
        S.op('dve', lambda e: e.tensor_tensor(out=gw[:], in0=tsv[:], in1=tsv[:, :, 0:1].to_broadcast([P, H, 16]), op=ALU.subtract), reads=[b_tsv], writes=[b_gw])
        S.op('act', lambda e: e.activation(out=gw[:], in_=gw[:], func=AF.Exp), reads=[b_gw], writes=[b_gw])
        S.op('dve', lambda e: e.tensor_reduce(out=gs[:, 0, :], in_=gw[:], axis=AX.X, op=ALU.add), reads=[b_gw], writes=[b_gs])
        S.op('dve', lambda e: e.reciprocal(out=gs[:, 1, :], in_=gs[:, 0, :]), reads=[b_gs], writes=[b_gs])
        S.op('dve', lambda e: e.tensor_tensor(out=gw[:], in0=gw[:], in1=gs[:, 1, :].unsqueeze(2).to_broadcast([P, H, 16]), op=ALU.mult), reads=[b_gw, b_gs], writes=[b_gw])
        if STAGE == 6:
            S.op('dve', lambda e: e.tensor_copy(out=m2[:, 0:128], in_=idx1[:].rearrange("p h k -> p (h k)")), reads=[b_idx1], writes=[b_m2])
            S.op('dve', lambda e: e.tensor_copy(out=m2[:, 128:256], in_=gw[:].rearrange("p h k -> p (h k)")), reads=[b_gw], writes=[b_m2])
            dump(6, m2[:, 0:256], [b_m2], 256)
        GS = 2
        NGRP = 128 // GS
        gwf = gw[:].rearrange("p h k -> p (h k)")

        def emit_gather(g):
            for k in range(GS):
                j = g * GS + k
                gb_, bgb = gbuf[j % NG], b_gbuf[j % NG]
                S.dma('pool', lambda e, j=j, gb_=gb_: e.indirect_dma_start(out=gb_[:], out_offset=None, in_=uv_bf,
                                                                          in_offset=bass.IndirectOffsetOnAxis(ap=eidx[:, j:j + 1], axis=0)),
                      gsem[j % NG], reads=[b_eidx, b_ubf], writes=[bgb])

        def emit_dots(g):
            for k in range(GS):
                j = g * GS + k
                gb_, bgb = gbuf[j % NG], b_gbuf[j % NG]
                S.op('dve', lambda e, j=j, gb_=gb_: e.scalar_tensor_tensor(out=junk[:], in0=gb_[:, 0:D], scalar=1.0, in1=hn[:],
                                                                          op0=ALU.mult, op1=ALU.mult, accum_out=hv[:, j:j + 1]),
                     reads=[bgb, b_hn], writes=[b_junk, b_hv])

        def emit_pre(g):
            c = slice(g * GS, (g + 1) * GS)
            S.op('act', lambda e: e.activation(out=ga_[:, 0, c], in_=hv[:, c], func=AF.Square), reads=[b_hv], writes=[b_ga])
            S.op('act', lambda e: e.activation(out=ga_[:, 1, c], in_=ga_[:, 0, c], func=AF.Identity, scale=0.0713548162726, bias=gk_t[:, 0:1]),
                 reads=[b_ga, b_gk], writes=[b_ga])
            for k in range(GS):
                j = g * GS + k
                S.op('act', lambda e, j=j: e.activation(out=ga_[:, 3, j:j + 1], in_=ga_[:, 1, j:j + 1], func=AF.Sigmoid, scale=hv[:, j:j + 1]),
                     reads=[b_ga, b_hv], writes=[b_ga2])
            S.op('dve', lambda e: e.tensor_tensor(out=ga_[:, 4, c], in0=hv[:, c], in1=gwf[:, c], op=ALU.mult), reads=[b_hv, b_gw], writes=[b_ga3])

        def emit_post(g):
            for k in range(GS):
                j = g * GS + k
                S.op('act', lambda e, j=j: e.activation(out=aw[:, j:j + 1], in_=ga_[:, 3, j:j + 1], func=AF.Copy, scale=ga_[:, 4, j:j + 1]),
                     reads=[b_ga2, b_ga3], writes=[b_aw])

        def emit_axpy(g):
            for k in range(GS):
                j = g * GS + k
                gb_, bgb = gbuf[j % NG], b_gbuf[j % NG]
                dgj, bdg = dg[j % 4], b_dg[j % 4]
                S.op('act', lambda e, j=j, dgj=dgj: e.activation(out=dgj[:], in_=identF[:], func=AF.Copy, scale=aw[:, j:j + 1]),
                     reads=[b_identF, b_aw], writes=[bdg])
                for hh in range(2):
                    S.op('pe', lambda e, j=j, hh=hh, dgj=dgj, gb_=gb_: e.matmul(out=psA[:, hh * 512:(hh + 1) * 512], lhsT=dgj[:],
                                                                            rhs=gb_[:, D + hh * 512:D + (hh + 1) * 512],
                                                                            start=(j == 0), stop=(j == 127)), reads=[bdg, bgb], writes=[bA])

        cap = []
        if n + 1 < NT:
            S.cap = cap
            m_head(n + 1)
            S.cap = None
        for g0 in range(3):
            emit_gather(g0)
        for st_ in range(NGRP + 1):
            S.replay(cap, 3)
            if 0 <= st_ - 1 < NGRP:
                emit_pre(st_ - 1)
            if st_ < NGRP:
                emit_dots(st_)
            if 0 <= st_ - 1 < NGRP:
                emit_post(st_ - 1)
                emit_axpy(st_ - 1)
            if st_ + 3 < NGRP:
                emit_gather(st_ + 3)
        S.replay(cap)
        S.op('dve', lambda e: e.tensor_tensor(out=acc[:], in0=psA[:], in1=x1[:], op=ALU.add), reads=[bA, b_x1], writes=[b_acc])
        S.dma('sp', lambda e, n=n: e.dma_start(out=y_out[n * P:(n + 1) * P, :], in_=acc[:]), b_yout, reads=[b_acc], writes=[b_yout])

    S.wait_all('sp', [b_yout])
    es.close()
    return nc, None


def _consts():
    hs = np.arange(H, dtype=np.float64)
    gam = 1.0 - 2.0 ** (-5.0 - hs)
    i = np.arange(P, dtype=np.float64)
    c = {}
    c["c_ident"] = np.eye(P, dtype=np.float32)
    c["c_tri"] = (i[:, None] > i[None, :]).astype(np.float32)
    c["c_ones"] = np.ones((P, P), np.float32)
    mp = 1.0e4 * (i[:, None] >= i[None, :]).astype(np.float32)
    c["c_mpos"] = np.tile(mp, (1, 4)).astype(np.float32)
    ms = np.ones((P, 2, 4, P), np.float32)
    ms[:, 1, :, :] = (i[:, None] < i[None, :]).astype(np.float32)[:, None, :]
    c["c_mstay"] = ms.reshape(P, 1024)
    mk = np.zeros((P, H, P), np.float64)
    for h in range(H):
        mk[:, h, :] = (i[None, :] >= i[:, None]) * gam[h] ** (-128.0)
    c["c_maskT"] = mk.reshape(P, 1024).astype(np.float32)
    c["c_qdec"] = (gam[None, :] ** (i[:, None] + 1.0)).astype(np.float32)
    c["c_kdec"] = (0.125 * gam[None, :] ** (127.0 - i[:, None])).astype(np.float32)
    cd = np.zeros((64, D), np.float64)
    for h in range(H):
        cd[:, h * P:(h + 1) * P] = gam[h] ** 128.0
    c["c_cdec"] = cd.astype(np.float32)
    return c, gam


def _rope_tabs(pos):
    half = 32
    freqs = (np.float32(10000.0) ** (-np.arange(half, dtype=np.float32) / np.float32(half))).astype(np.float32)
    ang = (pos.astype(np.float32)[:, :, None] * freqs[None, None, :]).astype(np.float32).astype(np.float64)
    return np.cos(ang).astype(np.float32), np.sin(ang).astype(np.float32)


_CACHE = {}


def kernel(x, norm_attn, w_in, ret_q_norm, ret_k_norm, ret_group_norm, sb_q_norm, sb_k_norm,
           w_branch_ret, w_branch_sb, w_out, norm_ffn, peer_w_q, peer_sub_keys_1,
           peer_sub_keys_2, peer_u, peer_v):
    f = np.float32
    x = np.asarray(x, f)
    B, SEQ, _ = x.shape
    assert B == 1
    NT = SEQ // (NCORES * P)
    NPRE = NT * (NCORES - 1) if FORCE_NPRE is None else FORCE_NPRE
    x2 = x[0]
    key = (NT, NPRE)
    if key not in _CACHE:
        try:
            _CACHE[key] = build_program(NT, NPRE)
        except _Stop:
            _H['es'].close()
            _CACHE[key] = (_H['nc'], None)
    nc, _es = _CACHE[key]
    cst, gam = _consts()
    rep = lambda v, n: np.ascontiguousarray(np.broadcast_to(np.tile(np.asarray(v, f).reshape(-1), n)[None, :], (P, np.asarray(v).size * n)))
    shared = dict(cst)
    shared.update({
        "g_attn": rep(norm_attn[0], 1), "g_ffn": rep(norm_ffn[0], 1), "g_gn": rep(ret_group_norm[0], 1),
        "g_rq": rep(ret_q_norm[0], 8), "g_rk": rep(ret_k_norm[0], 8), "g_sq": rep(sb_q_norm[0], 8), "g_sk": rep(sb_k_norm[0], 8),
        "w_in": np.ascontiguousarray(w_in[0], f), "w_bra": np.ascontiguousarray(w_branch_ret[0], f),
        "w_brb": np.ascontiguousarray(w_branch_sb[0], f), "w_out": np.ascontiguousarray(w_out[0], f),
        "w_q": np.ascontiguousarray(peer_w_q[0], f),
        "k1T": np.ascontiguousarray(np.asarray(peer_sub_keys_1[0], f).T), "k2T": np.ascontiguousarray(np.asarray(peer_sub_keys_2[0], f).T),
        "u_tab": np.ascontiguousarray(peer_u[0], f), "v_tab": np.ascontiguousarray(peer_v[0], f),
    })
    in_maps = []
    pp = np.arange(P, dtype=np.float64)
    for c in range(NCORES):
        t0 = c * NT
        m = dict(shared)
        m["x_own"] = np.ascontiguousarray(x2[t0 * P:(t0 + NT) * P])
        m["x_halo"] = np.ascontiguousarray(x2[(t0 - 1) * P:t0 * P]) if c > 0 else np.zeros((P, D), f)
        npre = max(NPRE, 1)
        xp = np.zeros((npre * P, D), f)
        gt = np.arange(npre) + (t0 - NPRE)
        nvalid = min(t0, NPRE)
        if nvalid > 0:
            xp[(NPRE - nvalid) * P:NPRE * P] = x2[(t0 - nvalid) * P:t0 * P]
        m["x_pre"] = xp
        pos_own = (np.arange(NT)[None, :] + t0) * P + pp[:, None]
        m["cos_own"], m["sin_own"] = _rope_tabs(pos_own)
        pos_pre = np.maximum(gt, 0)[None, :] * P + pp[:, None]
        m["cos_pre"], m["sin_pre"] = _rope_tabs(pos_pre)
        ks = np.zeros((P, npre, H), np.float64)
        for mm in range(npre):
            ks[:, mm, :] = 0.125 * gam[None, :] ** (127.0 - pp[:, None]) * gam[None, :] ** (128.0 * (NPRE - 1 - mm))
        m["ksc_pre"] = ks.astype(f)
        in_maps.append(m)
    res = run_bass_kernel_spmd(nc, in_maps, core_ids=list(range(NCORES)), **RUN_KW)
    _H['res'] = res
    out = np.concatenate([np.asarray(r["y_out"], f) for r in res.results], axis=0)
    return out.reshape(1, SEQ, D)
```

```python
import numpy as np
from contextlib import ExitStack
import concourse.bass as bass
import concourse.mybir as mybir
from concourse.bass_utils import run_bass_kernel_spmd

F32 = mybir.dt.float32
BF16 = mybir.dt.bfloat16
U32 = mybir.dt.uint32
I32 = mybir.dt.int32
AF = mybir.ActivationFunctionType
ALU = mybir.AluOpType
AX = mybir.AxisListType

NCORES = 8
D = 1024
P = 128
H = 8
EPS = 1e-6
NEXP = 16384
INW = 6656
NEG = -1.0e30


class Buf:
    def __init__(self, name):
        self.name = name
        self.w = None
        self.r = {}
        self.dsem = None
        self.dcnt = 0


class Sched:
    def __init__(self, nc, es):
        self.nc = nc
        self.es = es
        self.E = {'pe': nc.tensor, 'act': nc.scalar, 'dve': nc.vector, 'pool': nc.gpsimd, 'sp': nc.sync}
        self.sem = {e: es.enter_context(nc.semaphore('sem_' + e)) for e in self.E}
        self.cnt = {e: 0 for e in self.E}
        self.known = {e: {} for e in self.E}
        self.nsem = 0

    def buf(self, name, dma=False):
        b = Buf(name)
        if dma:
            b.dsem = self.es.enter_context(self.nc.semaphore('d_' + name))
        return b

    def _wait(self, e, ev):
        if ev is None:
            return
        sem, val, src = ev
        if src == e and e == 'pe':
            return
        k = self.known[e]
        if k.get(id(sem), 0) >= val:
            return
        self.E[e].wait_ge(sem, val)
        k[id(sem)] = val

    @staticmethod
    def _flat(bs):
        out = []
        for b in bs:
            if isinstance(b, (list, tuple)):
                out.extend(Sched._flat(b))
            else:
                out.append(b)
        return out

    def _deps(self, e, reads, writes):
        reads = self._flat(reads); writes = self._flat(writes)
        for b in reads:
            self._wait(e, b.w)
        for b in writes:
            self._wait(e, b.w)
            for ev in list(b.r.values()):
                self._wait(e, ev)

    def _post(self, ev, reads, writes):
        reads = self._flat(reads); writes = self._flat(writes)
        for b in reads:
            old = b.r.get(id(ev[0]))
            if old is None or old[1] < ev[1]:
                b.r[id(ev[0])] = ev
        for b in writes:
            b.w = ev
            b.r = {}

    cap = None
    after_op = None

    def replay(self, cap, k=None):
        n = len(cap) if k is None else min(k, len(cap))
        for _ in range(n):
            kind, a = cap.pop(0)
            if kind == 'op':
                self.op(*a)
            else:
                self.dma(*a)

    def op(self, e, fn, reads=(), writes=()):
        if self.cap is not None:
            self.cap.append(('op', (e, fn, tuple(reads), tuple(writes))))
            return
        self._deps(e, reads, writes)
        ins = fn(self.E[e])
        self.cnt[e] += 1
        ins.then_inc(self.sem[e], 1)
        self._post((self.sem[e], self.cnt[e], e), reads, writes)
        if self.after_op is not None:
            h, self.after_op = self.after_op, None
            h()
            self.after_op = h

    def dma(self, q, fn, dbuf, reads=(), writes=()):
        if self.cap is not None:
            self.cap.append(('dma', (q, fn, dbuf, tuple(reads), tuple(writes))))
            return
        self._deps(q, reads, writes)
        ins = fn(self.E[q])
        dbuf.dcnt += 16
        ins.then_inc(dbuf.dsem, 16)
        self._post((dbuf.dsem, dbuf.dcnt, 'dma'), reads, writes)

    def wait_all(self, e, bufs):
        for b in self._flat(bufs):
            self._wait(e, b.w)
            for ev in list(b.r.values()):
                self._wait(e, ev)


class _Stop(Exception):
    pass


_H = {}
STAGE = 99
RUN_KW = {}
SKIP_GATHER = False
FORCE_NPRE = None


def build_program(NT, NPRE, dbg=False):
    nc = bass.Bass("TRN2", target_bir_lowering=False)
    es = ExitStack()
    S = Sched(nc, es)
    _H['nc'] = nc; _H['es'] = es

    def din(name, shape, dt=F32):
        return nc.dram_tensor(name, list(shape), dt, kind="ExternalInput").ap()

    x_own = din("x_own", [NT * P, D])
    x_halo = din("x_halo", [P, D])
    x_pre = din("x_pre", [max(NPRE, 1) * P, D])
    cos_own = din("cos_own", [P, NT, 32]); sin_own = din("sin_own", [P, NT, 32])
    cos_pre = din("cos_pre", [P, max(NPRE, 1), 32]); sin_pre = din("sin_pre", [P, max(NPRE, 1), 32])
    ksc_pre = din("ksc_pre", [P, max(NPRE, 1), H])
    g_attn = din("g_attn", [P, D]); g_ffn = din("g_ffn", [P, D]); g_gn = din("g_gn", [P, D])
    g_rq = din("g_rq", [P, 512]); g_rk = din("g_rk", [P, 512]); g_sq = din("g_sq", [P, 512]); g_sk = din("g_sk", [P, 512])
    c_ident = din("c_ident", [P, P]); c_tri = din("c_tri", [P, P]); c_ones = din("c_ones", [P, P])
    c_mpos = din("c_mpos", [P, 512]); c_mstay = din("c_mstay", [P, 1024])
    c_maskT = din("c_maskT", [P, 1024]); c_qdec = din("c_qdec", [P, H]); c_kdec = din("c_kdec", [P, H])
    c_cdec = din("c_cdec", [64, D])
    w_in = din("w_in", [D, INW]); w_bra = din("w_bra", [D, D]); w_brb = din("w_brb", [512, D]); w_out = din("w_out", [D, D])
    w_q = din("w_q", [D, 2048]); k1T = din("k1T", [P, P]); k2T = din("k2T", [P, P])
    u_tab = din("u_tab", [NEXP, D]); v_tab = din("v_tab", [NEXP, D])
    y_out = nc.dram_tensor("y_out", [NT * P, D], F32, kind="ExternalOutput").ap()
    uv_bf = nc.dram_tensor("uv_bf", [NEXP, 2 * D], BF16, kind="Internal").ap()
    NWB = 17
    wsc = nc.dram_tensor("wsc", [NWB * P, 4096], BF16, kind="Internal").ap()
    def dump(stage, src_ap, bsrc, ncols=D):
        if STAGE != stage:
            return
        b_d = S.buf("dump", dma=True)
        npart = src_ap.shape[0]
        S.dma('sp', lambda e: e.dma_start(out=y_out[0:npart, 0:ncols], in_=src_ap), b_d, reads=bsrc, writes=[b_d])
        S.wait_all('sp', [b_d])
        raise _Stop()

    tot = [0]

    def sb(name, shape, dt=F32):
        n = int(np.prod(shape[1:])) * (4 if dt in (F32, U32, I32) else 2)
        tot[0] += n
        if dbg:
            print("SB", name, n, tot[0])
        return es.enter_context(nc.sbuf_tensor(name, list(shape), dt))

    def ps(name, shape, dt=F32):
        return es.enter_context(nc.psum_tensor(name, list(shape), dt))

    psA = ps("psA", [P, 1024]); bA = S.buf("psA")
    psB = ps("psB", [P, 1024]); bB = S.buf("psB")
    psC = ps("psC", [P, 512]); bC = S.buf("psC")
    psD = ps("psD", [P, 512]); bD = S.buf("psD")
    psT = ps("psT", [P, 1024], BF16); bT = S.buf("psT")
    psS = ps("psS", [P, 512]); bS = S.buf("psS")

    consts = []

    def load_const(name, src, shape, dt=F32, q='sp'):
        t = sb(name, shape, dt)
        b = S.buf(name, dma=True)
        S.dma(q, lambda e: e.dma_start(out=t[:], in_=src), b, writes=[b])
        consts.append(b)
        return t, b

    def load_cast(name, src, shape):
        return load_const(name, src, shape, BF16, q='pool')

    ident, b_ident = load_cast("ident", c_ident, [P, P])
    tri, b_tri = load_cast("tri", c_tri, [P, P])
    ones, b_ones = load_cast("ones", c_ones, [P, P])
    mpos, b_mpos = load_cast("mpos", c_mpos, [P, 512])
    mstay, b_mstay = load_cast("mstay", c_mstay, [P, 1024])
    maskT, b_maskT = load_const("maskT", c_maskT, [P, 1024])
    qdec, b_qdec = load_const("qdec", c_qdec, [P, H])
    kdec, b_kdec = load_const("kdec", c_kdec, [P, H])
    cdec, b_cdec = load_const("cdec", c_cdec, [64, D])
    gattn, b_gattn = load_const("gattn", g_attn, [P, D])
    gffn, b_gffn = load_const("gffn", g_ffn, [P, D])
    ggn, b_ggn = load_const("ggn", g_gn, [P, D])
    grq, b_grq = load_const("grq", g_rq, [P, 512])
    grk, b_grk = load_const("grk", g_rk, [P, 512])
    gsq, b_gsq = load_const("gsq", g_sq, [P, 512])
    gsk, b_gsk = load_const("gsk", g_sk, [P, 512])
    cso, b_cso = load_const("cso", cos_own, [P, NT, 32])
    sno, b_sno = load_const("sno", sin_own, [P, NT, 32])

    def wview(w, c0, n):
        return w[:, c0:c0 + n].rearrange("(c p) n -> p c n", p=P)

    TPbig = sb("TPbig", [P, 10, D])
    TP = [TPbig[:, i, :] for i in range(10)]
    b_TP = [S.buf("TP%d" % i, dma=True) for i in range(10)]
    xt = [TP[0], TP[1]]
    b_xt = [b_TP[0], b_TP[1]]
    junk = sb("junk", [P, D], BF16); b_junk = S.buf("junk")
    st4 = sb("st4", [P, 4]); b_st4 = S.buf("st4")

    def rmsnorm_T(src, b_src, gain, b_gain, keep_f32=None, b_keep=None):
        S.op('act', lambda e: e.activation(out=junk[:], in_=src, func=AF.Square, accum_out=st4[:, 0:1]),
             reads=[b_src], writes=[b_junk, b_st4])
        S.op('act', lambda e: e.activation(out=st4[:, 1:2], in_=st4[:, 0:1], func=AF.Sqrt, bias=eps_t[:, 0:1], scale=1.0 / D),
             reads=[b_st4, b_eps], writes=[b_st4])
        S.op('dve', lambda e: e.reciprocal(out=st4[:, 2:3], in_=st4[:, 1:2]), reads=[b_st4], writes=[b_st4])
        if keep_f32 is not None:
            S.op('dve', lambda e: e.scalar_tensor_tensor(out=keep_f32, in0=src, scalar=st4[:, 2:3], in1=gain[:],
                                                         op0=ALU.mult, op1=ALU.mult),
                 reads=[b_src, b_st4, b_gain], writes=[b_keep])
            S.op('act', lambda e: e.activation(out=xn[:], in_=keep_f32, func=AF.Copy), reads=[b_keep], writes=[b_xn])
        else:
            S.op('dve', lambda e: e.scalar_tensor_tensor(out=xn[:], in0=src, scalar=st4[:, 2:3], in1=gain[:],
                                                         op0=ALU.mult, op1=ALU.mult),
                 reads=[b_src, b_st4, b_gain], writes=[b_xn])
        transpose8(xn, b_xn, xnT, b_xnT)

    def transpose8(src, b_src, dst, b_dst, nchunk=8):
        for half in range((nchunk + 3) // 4):
            n = min(4, nchunk - half * 4)
            for k in range(n):
                c = half * 4 + k
                S.op('pe', lambda e, c=c, k=k: e.transpose(out=psT[:, k * P:(k + 1) * P], in_=src[:, c * P:(c + 1) * P],
                                                           identity=ident[:]),
                     reads=[b_src, b_ident], writes=[bT])
            eng = 'act' if half % 2 == 0 else 'dve'
            if eng == 'act':
                S.op('act', lambda e, half=half, n=n: e.activation(
                    out=dst[:, half * 4:half * 4 + n, :].rearrange("p c t -> p (c t)"), in_=psT[:, 0:n * P], func=AF.Copy),
                    reads=[bT], writes=[b_dst])
            else:
                S.op('dve', lambda e, half=half, n=n: e.tensor_copy(
                    out=dst[:, half * 4:half * 4 + n, :].rearrange("p c t -> p (c t)"), in_=psT[:, 0:n * P]),
                    reads=[bT], writes=[b_dst])

    def transposeH(src3, b_src, dst, b_dst):
        for h in range(H):
            S.op('pe', lambda e, h=h: e.transpose(out=psT[0:64, h * P:(h + 1) * P], in_=src3[:, h, :], identity=ident[:]),
                 reads=[b_src, b_ident], writes=[bT])
        S.op('act', lambda e: e.activation(out=dst[:].rearrange("p h t -> p (h t)"), in_=psT[0:64, :], func=AF.Copy),
             reads=[bT], writes=[b_dst])

    def proj512(wb, b_wb, pst, b_pst):
        for c in range(8):
            S.op('pe', lambda e, c=c: e.matmul(out=pst, lhsT=xnT[:, c, :], rhs=wb[:, c, :], start=(c == 0), stop=(c == 7)),
                 reads=[b_xnT, b_wb], writes=[b_pst])

    def qknorm(pst, b_pst, gain, b_gain, slot, scale_ap=None, b_scale=None, cos=None, sin=None, b_cs=(), out_bf=None, b_out=None):
        S.op('act', lambda e: e.activation(out=qf[:], in_=pst, func=AF.Copy), reads=[b_pst], writes=[b_qf])
        S.op('act', lambda e: e.activation(out=sq_s[:], in_=pst, func=AF.Square), reads=[b_pst], writes=[b_sq])
        S.op('dve', lambda e: e.tensor_reduce(out=st8[:, 0, :], in_=sq_s[:].rearrange("p (h d) -> p h d", d=64), axis=AX.X, op=ALU.add),
             reads=[b_sq], writes=[b_st8])
        S.op('act', lambda e: e.activation(out=st8[:, 1, :], in_=st8[:, 0, :], func=AF.Sqrt, bias=eps_t[:, 0:1], scale=1.0 / 64),
             reads=[b_st8, b_eps], writes=[b_st8])
        S.op('dve', lambda e: e.reciprocal(out=st8[:, 2, :], in_=st8[:, 1, :]), reads=[b_st8], writes=[b_st8])
        rs = st8[:, 2, :]
        if scale_ap is not None:
            S.op('dve', lambda e: e.tensor_tensor(out=st8[:, 3, :], in0=st8[:, 2, :], in1=scale_ap, op=ALU.mult),
                 reads=[b_st8, b_scale], writes=[b_st8])
            rs = st8[:, 3, :]
        S.op('dve', lambda e: e.tensor_tensor(out=qn[:].rearrange("p (h d) -> p h d", d=64), in0=qf[:].rearrange("p (h d) -> p h d", d=64),
                                              in1=rs.unsqueeze(2).to_broadcast([P, H, 64]), op=ALU.mult),
             reads=[b_qf, b_st8], writes=[b_qn])
        if cos is None:
            S.op('dve', lambda e: e.tensor_tensor(out=out_bf[:].rearrange("p h d -> p (h d)"), in0=qn[:], in1=gain[:], op=ALU.mult),
                 reads=[b_qn, b_gain], writes=[b_out])
            return
        S.op('pool', lambda e: e.tensor_tensor(out=qn[:], in0=qn[:], in1=gain[:], op=ALU.mult), reads=[b_qn, b_gain], writes=[b_qn])
        q3 = qn[:].rearrange("p (h d) -> p h d", d=64)
        x1 = q3[:, :, 0:32]; x2 = q3[:, :, 32:64]
        cb = cos.unsqueeze(1).to_broadcast([P, H, 32]); sbb = sin.unsqueeze(1).to_broadcast([P, H, 32])
        r = [rt[:, i, :].rearrange("p (h d) -> p h d", d=32) for i in range(4)]
        S.op('dve', lambda e: e.tensor_tensor(out=r[0], in0=x1, in1=cb, op=ALU.mult), reads=[b_qn] + list(b_cs), writes=[b_rt])
        S.op('pool', lambda e: e.tensor_tensor(out=r[1], in0=x2, in1=sbb, op=ALU.mult), reads=[b_qn] + list(b_cs), writes=[b_rt])
        S.op('dve', lambda e: e.tensor_tensor(out=r[2], in0=x1, in1=sbb, op=ALU.mult), reads=[b_qn] + list(b_cs), writes=[b_rt])
        S.op('pool', lambda e: e.tensor_tensor(out=r[3], in0=x2, in1=cb, op=ALU.mult), reads=[b_qn] + list(b_cs), writes=[b_rt])
        S.op('dve', lambda e: e.tensor_tensor(out=out_bf[:, :, 0:32], in0=r[0], in1=r[1], op=ALU.subtract), reads=[b_rt], writes=[b_out])
        S.op('dve', lambda e: e.tensor_tensor(out=out_bf[:, :, 32:64], in0=r[2], in1=r[3], op=ALU.add), reads=[b_rt], writes=[b_out])

    b_ubf = S.buf("uv_bf", dma=True); b_vbf = b_ubf
    b_wsc = S.buf("wsc", dma=True)
    identF, b_identF = load_const("identF", c_ident, [P, P])
    eps_t = sb("eps_t", [P, 1]); b_eps = S.buf("eps")
    S.op('dve', lambda e: e.memset(eps_t[:], EPS), writes=[b_eps])
    gk_t = sb("gk_t", [P, 1]); b_gk = S.buf("gk")
    S.op('dve', lambda e: e.memset(gk_t[:], 1.5957691216057308), writes=[b_gk])
    one_t = sb("one_t", [P, 1]); b_one = S.buf("one")
    S.op('dve', lambda e: e.memset(one_t[:], 1.0), writes=[b_one])

    state = sb("state", [64, D]); b_state = S.buf("state")
    state_bf = sb("state_bf", [64, D], BF16); b_state_bf = S.buf("state_bf")

    with ExitStack() as es0:
        NBLK = NEXP // 128
        NCB = 4
        cb = [es0.enter_context(nc.sbuf_tensor("cb%d" % i, [P, 1, D], BF16)) for i in range(NCB)]
        b_cb = [S.buf("cb%d" % i, dma=True) for i in range(NCB)]
        b_cin = b_cb; b_cout = []
        pc_jobs = [(tab, dst, bd, blk) for blk in range(NBLK) for (tab, dst, bd) in ((u_tab, uv_bf[:, 0:D], b_ubf), (v_tab, uv_bf[:, D:2 * D], b_vbf))]
        pc_state = [0, 0]

        def _pc_store(k):
            tab, dst, bd, blk = pc_jobs[k]
            i = k % NCB
            S.dma('pool', lambda e: e.dma_start(out=dst[blk * 128:(blk + 1) * 128, :].rearrange("(p r) d -> p r d", r=1), in_=cb[i][:]),
                  bd, reads=[b_cb[i]], writes=[bd])

        def precast(nblocks):
            for _ in range(nblocks):
                k = pc_state[0]
                if k >= len(pc_jobs):
                    break
                pc_state[0] += 1
                tab, dst, bd, blk = pc_jobs[k]
                i = k % NCB
                S.dma('pool', lambda e, tab=tab, blk=blk, i=i: e.dma_start(out=cb[i][:], in_=tab[blk * 128:(blk + 1) * 128, :].rearrange("(p r) d -> p r d", r=1)),
                      b_cb[i], writes=[b_cb[i]])
                if k - 2 >= 0:
                    _pc_store(k - 2)
                    pc_state[1] = k - 1
            if pc_state[0] >= len(pc_jobs):
                while pc_state[1] < len(pc_jobs):
                    _pc_store(pc_state[1])
                    pc_state[1] += 1

        wjobs = [(w_in if blk < 13 else w_q, (blk if blk < 13 else blk - 13) * 512, blk, cc) for blk in range(NWB) for cc in range(4)]

        def _w_store(k):
            src, c0, blk, cc = wjobs[k]
            i = k % NCB
            dstv = wsc[blk * P:(blk + 1) * P, :].rearrange("p (c n) -> p c n", c=8)[:, 2 * cc:2 * cc + 2, :]
            S.dma('pool', lambda e: e.dma_start(out=dstv, in_=cb[i][:, 0, :].rearrange("p (c n) -> p c n", c=2)), b_wsc, reads=[b_cb[i]], writes=[b_wsc])

        for k, (src, c0, blk, cc) in enumerate(wjobs):
            i = k % NCB
            S.dma('pool', lambda e, src=src, c0=c0, cc=cc, i=i: e.dma_start(out=cb[i][:, 0, :].rearrange("p (c n) -> p c n", c=2),
                                                                          in_=wview(src, c0, 512)[:, 2 * cc:2 * cc + 2, :]),
                  b_cb[i], writes=[b_cb[i]])
            if k - 2 >= 0:
                _w_store(k - 2)
        _w_store(len(wjobs) - 2); _w_store(len(wjobs) - 1)
        pc_per_tile = -(-len(pc_jobs) // max(NPRE, 1))
        if NPRE > 0:
            wk = es0.enter_context(nc.sbuf_tensor("wk_pre", [P, 8, 512], BF16)); b_wk = S.buf("wk_pre", dma=True)
            wv = es0.enter_context(nc.sbuf_tensor("wv_pre", [P, 8, 1024], BF16)); b_wv = S.buf("wv_pre", dma=True)
            csp = es0.enter_context(nc.sbuf_tensor("csp", [P, NPRE, 32], F32)); b_csp = S.buf("csp", dma=True)
            snp = es0.enter_context(nc.sbuf_tensor("snp", [P, NPRE, 32], F32)); b_snp = S.buf("snp", dma=True)
            ksp = es0.enter_context(nc.sbuf_tensor("ksp", [P, NPRE, H], F32)); b_ksp = S.buf("ksp", dma=True)
            S.dma('pool', lambda e: e.dma_start(out=wk[:], in_=wview(w_in, 512, 512)), b_wk, writes=[b_wk])
            for hh in range(2):
                S.dma('pool', lambda e, hh=hh: e.dma_start(out=wv[:, :, hh * 512:(hh + 1) * 512], in_=wview(w_in, 1024 + hh * 512, 512)),
                      b_wv, writes=[b_wv])
            S.dma('sp', lambda e: e.dma_start(out=csp[:], in_=cos_pre), b_csp, writes=[b_csp])
            S.dma('sp', lambda e: e.dma_start(out=snp[:], in_=sin_pre), b_snp, writes=[b_snp])
            S.dma('sp', lambda e: e.dma_start(out=ksp[:], in_=ksc_pre), b_ksp, writes=[b_ksp])
            B0 = 4
            def _al(name, shape, dt):
                return es0.enter_context(nc.sbuf_tensor(name, list(shape), dt))
            xnb = _al("xnb", [P, 2, D], BF16); xnTb = _al("xnTb", [P, 2, 8, P], BF16); st4b = _al("st4b", [P, B0, 4], F32)
            b_xnb = [S.buf("xnb%d" % (i % 2)) for i in range(2)] * 2; b_xnTb = [S.buf("xnTb%d" % (i % 2)) for i in range(2)] * 2; b_st4b = [S.buf("st4b%d" % i) for i in range(B0)]
            sets = []
            for si in range(2):
                d_ = dict(kf=TPbig[:, 2 + 4 * si:4 + 4 * si, :].rearrange("p a d -> p (a d)").rearrange("p (b n) -> p b n", b=B0),
                          sq=TPbig[:, 4 + 4 * si:6 + 4 * si, :].rearrange("p a d -> p (a d)").rearrange("p (b n) -> p b n", b=B0),
                          s8=_al("s8b%d" % si, [P, 4, B0 * H], F32),
                          r1=_al("r1b%d" % si, [P, B0 * 256], F32),
                          kd=_al("kdb%d" % si, [P, B0, H, 64], BF16), v=_al("vb%d" % si, [P, B0, D], BF16))
                d_.update(b_kf=S.buf("kfb%d" % si), b_sq=S.buf("sqb%d" % si), b_s8=S.buf("s8b%d" % si), b_r0=S.buf("r0b%d" % si),
                          b_r1=S.buf("r1b%d" % si), b_kd=S.buf("kdb%d" % si), b_v=S.buf("vb%d" % si))
                d_["r0"] = d_["kf"].rearrange("p b n -> p (b n)")[:, 0:B0 * 256]; d_["b_r0"] = d_["b_kf"]
                sets.append(d_)
            all_pre_bufs = b_xnb + b_xnTb + b_st4b + [sets[i][k] for i in range(2) for k in ("b_kf", "b_sq", "b_s8", "b_r0", "b_r1", "b_kd", "b_v")]

            def front_a(m, b, st):
                xb = xt[m % 2]; bx = b_xt[m % 2]
                S.dma('sp', lambda e: e.dma_start(out=xb[:], in_=x_pre[m * P:(m + 1) * P, :]), bx, writes=[bx])
                S.op('act', lambda e: e.activation(out=junk[:], in_=xb[:], func=AF.Square, accum_out=st4b[:, b, 0:1]), reads=[bx], writes=[b_junk, b_st4b[b]])
                S.op('act', lambda e: e.activation(out=st4b[:, b, 1:2], in_=st4b[:, b, 0:1], func=AF.Sqrt, bias=eps_t[:, 0:1], scale=1.0 / D),
                     reads=[b_st4b[b], b_eps], writes=[b_st4b[b]])
                S.op('dve', lambda e: e.reciprocal(out=st4b[:, b, 2:3], in_=st4b[:, b, 1:2]), reads=[b_st4b[b]], writes=[b_st4b[b]])
                S.op('dve', lambda e: e.scalar_tensor_tensor(out=xnb[:, b % 2, :], in0=xb[:], scalar=st4b[:, b, 2:3], in1=gattn[:], op0=ALU.mult, op1=ALU.mult),
                     reads=[bx, b_st4b[b], b_gattn], writes=[b_xnb[b]])

            def front_b(m, b, st):
                precast(pc_per_tile)
                for half in range(2):
                    for k in range(4):
                        c = half * 4 + k
                        S.op('pe', lambda e, c=c, k=k: e.transpose(out=psT[:, k * P:(k + 1) * P], in_=xnb[:, b % 2, c * P:(c + 1) * P], identity=ident[:]),
                             reads=[b_xnb[b], b_ident], writes=[bT])
                    dst = xnTb[:, b % 2, half * 4:half * 4 + 4, :].rearrange("p c t -> p (c t)")
                    if half == 0:
                        S.op('act', lambda e, dst=dst: e.activation(out=dst, in_=psT[:, 0:512], func=AF.Copy), reads=[bT], writes=[b_xnTb[b]])
                    else:
                        S.op('dve', lambda e, dst=dst: e.tensor_copy(out=dst, in_=psT[:, 0:512]), reads=[bT], writes=[b_xnTb[b]])
                for c in range(8):
                    S.op('pe', lambda e, c=c: e.matmul(out=psC[:], lhsT=xnTb[:, b % 2, c, :], rhs=wk[:, c, :], start=(c == 0), stop=(c == 7)),
                         reads=[b_xnTb[b], b_wk], writes=[bC])
                S.op('act', lambda e: e.activation(out=st["kf"][:, b, :], in_=psC[:], func=AF.Copy), reads=[bC], writes=[st["b_kf"]])
                S.op('act', lambda e: e.activation(out=st["sq"][:, b, :], in_=psC[:], func=AF.Square), reads=[bC], writes=[st["b_sq"]])
                psV, bV = (psA, bA) if m % 2 == 0 else (psB, bB)
                for hh in range(2):
                    for c in range(8):
                        S.op('pe', lambda e, c=c, hh=hh: e.matmul(out=psV[:, hh * 512:(hh + 1) * 512], lhsT=xnTb[:, b % 2, c, :], rhs=wv[:, c, hh * 512:(hh + 1) * 512],
                                                                  start=(c == 0), stop=(c == 7)), reads=[b_xnTb[b], b_wv], writes=[bV])
                S.op('act', lambda e: e.activation(out=st["v"][:, b, :], in_=psV[:], func=AF.Copy), reads=[bV], writes=[st["b_v"]])

            def chain_gen(m0, nb, st):
                n8 = nb * H
                kf, sq, s8, r0, r1, kdb = st["kf"], st["sq"], st["s8"], st["r0"], st["r1"], st["kd"]
                S.op('dve', lambda e: e.tensor_reduce(out=s8[:, 0, 0:n8], in_=sq[:, 0:nb, :].rearrange("p b (h d) -> p (b h) d", d=64), axis=AX.X, op=ALU.add),
                     reads=[st["b_sq"]], writes=[st["b_s8"]]); yield
                S.op('act', lambda e: e.activation(out=s8[:, 1, 0:n8], in_=s8[:, 0, 0:n8], func=AF.Sqrt, bias=eps_t[:, 0:1], scale=1.0 / 64),
                     reads=[st["b_s8"], b_eps], writes=[st["b_s8"]]); yield
                S.op('dve', lambda e: e.reciprocal(out=s8[:, 2, 0:n8], in_=s8[:, 1, 0:n8]), reads=[st["b_s8"]], writes=[st["b_s8"]]); yield
                S.op('dve', lambda e: e.tensor_tensor(out=s8[:, 3, 0:n8], in0=s8[:, 2, 0:n8], in1=ksp[:, m0:m0 + nb, :].rearrange("p m h -> p (m h)"), op=ALU.mult),
                     reads=[st["b_s8"], b_ksp], writes=[st["b_s8"]]); yield
                q3 = sq[:, 0:nb, :].rearrange("p b (h d) -> p (b h) d", d=64)
                S.op('dve', lambda e: e.tensor_tensor(out=q3, in0=kf[:, 0:nb, :].rearrange("p b (h d) -> p (b h) d", d=64),
                                                      in1=s8[:, 3, 0:n8].unsqueeze(2).to_broadcast([P, n8, 64]), op=ALU.mult),
                     reads=[st["b_kf"], st["b_s8"]], writes=[st["b_sq"]]); yield
                S.op('pool', lambda e: e.tensor_tensor(out=sq[:, 0:nb, :], in0=sq[:, 0:nb, :], in1=grk[:].unsqueeze(1).to_broadcast([P, nb, 512]), op=ALU.mult),
                     reads=[st["b_sq"], b_grk], writes=[st["b_sq"]]); yield
                q4 = sq[:, 0:nb, :].rearrange("p b (h d) -> p b h d", d=64)
                x1_ = q4[:, :, :, 0:32]; x2_ = q4[:, :, :, 32:64]
                cb = csp[:, m0:m0 + nb, :].unsqueeze(2).to_broadcast([P, nb, H, 32]); sb_ = snp[:, m0:m0 + nb, :].unsqueeze(2).to_broadcast([P, nb, H, 32])
                r0v = r0[:, 0:nb * 256].rearrange("p (b h d) -> p b h d", b=nb, h=H); r1v = r1[:, 0:nb * 256].rearrange("p (b h d) -> p b h d", b=nb, h=H)
                S.op('dve', lambda e: e.tensor_tensor(out=r0v, in0=x1_, in1=cb, op=ALU.mult), reads=[st["b_sq"], b_csp], writes=[st["b_r0"]]); yield
                S.op('pool', lambda e: e.tensor_tensor(out=r1v, in0=x2_, in1=sb_, op=ALU.mult), reads=[st["b_sq"], b_snp], writes=[st["b_r1"]]); yield
                S.op('dve', lambda e: e.tensor_tensor(out=kdb[:, 0:nb, :, 0:32], in0=r0v, in1=r1v, op=ALU.subtract), reads=[st["b_r0"], st["b_r1"]], writes=[st["b_kd"]]); yield
                S.op('dve', lambda e: e.tensor_tensor(out=r0v, in0=x1_, in1=sb_, op=ALU.mult), reads=[st["b_sq"], b_snp], writes=[st["b_r0"]]); yield
                S.op('pool', lambda e: e.tensor_tensor(out=r1v, in0=x2_, in1=cb, op=ALU.mult), reads=[st["b_sq"], b_csp], writes=[st["b_r1"]]); yield
                S.op('dve', lambda e: e.tensor_tensor(out=kdb[:, 0:nb, :, 32:64], in0=r0v, in1=r1v, op=ALU.add), reads=[st["b_r0"], st["b_r1"]], writes=[st["b_kd"]]); yield

            def state_mm(m0, nb, st):
                for b in range(nb):
                    m = m0 + b
                    for h in range(H):
                        acc_ps, b_acc_ps = (psS, bS) if h < 4 else (psD, bD)
                        S.op('pe', lambda e, h=h, m=m, b=b, acc_ps=acc_ps: e.matmul(
                            out=acc_ps[0:64, (h % 4) * P:(h % 4 + 1) * P], lhsT=st["kd"][:, b, h, :], rhs=st["v"][:, b, h * P:(h + 1) * P],
                            start=(m == 0 and h % 4 == 0), stop=(m == NPRE - 1), skip_group_check=True),
                            reads=[st["b_kd"], st["b_v"]], writes=[b_acc_ps])

            batches = [(m0, min(B0, NPRE - m0)) for m0 in range(0, NPRE, B0)]
            tiles = [(m0 + b, b, sets[bi % 2]) for bi, (m0, nb) in enumerate(batches) for b in range(nb)]
            pend = None
            front_a(*tiles[0])
            ti_ = 0
            for bi, (m0, nb) in enumerate(batches):
                st = sets[bi % 2]
                gen = chain_gen(*pend) if pend is not None else None
                for b in range(nb):
                    if ti_ + 1 < len(tiles):
                        front_a(*tiles[ti_ + 1])
                    front_b(m0 + b, b, st)
                    ti_ += 1
                    if gen is not None:
                        for _ in range(4):
                            next(gen, None)
                if gen is not None:
                    for _ in gen:
                        pass
                    state_mm(*pend)
                pend = (m0, nb, st)
            for _ in chain_gen(*pend):
                pass
            state_mm(*pend)
            for e_ in ('pe', 'act', 'dve', 'pool', 'sp'):
                S.wait_all(e_, all_pre_bufs)
            S.op('act', lambda e: e.activation(out=state[:, 0:512], in_=psS[0:64, :], func=AF.Copy), reads=[bS], writes=[b_state])
            S.op('act', lambda e: e.activation(out=state[:, 512:1024], in_=psD[0:64, :], func=AF.Copy), reads=[bD], writes=[b_state])
        else:
            S.op('dve', lambda e: e.memset(state[:], 0.0), writes=[b_state])
        S.op('act', lambda e: e.activation(out=state_bf[:], in_=state[:], func=AF.Copy), reads=[b_state], writes=[b_state_bf])
        precast(len(pc_jobs))
        for e_ in ('pe', 'act', 'dve', 'pool', 'sp'):
            S.wait_all(e_, b_cin + b_cout + [b_ubf, b_wsc])
        if NPRE > 0:
            for e_ in ('pe', 'act', 'dve', 'pool'):
                S.wait_all(e_, [b_wk, b_wv, b_csp, b_snp, b_ksp])

    dump(1, state[:], [b_state], D)
    xn = sb("xn", [P, D], BF16); b_xn = S.buf("xn")
    xnT = sb("xnT", [P, 8, P], BF16); b_xnT = S.buf("xnT")
    qf = sb("qf", [P, 512]); b_qf = S.buf("qf")
    sq_s = sb("sq_s", [P, 512]); b_sq = S.buf("sq_s")
    st8 = sb("st8", [P, 4, H]); b_st8 = S.buf("st8")
    qn = sb("qn", [P, 512]); b_qn = S.buf("qn")
    rt = sb("rt", [P, 4, 256]); b_rt = S.buf("rt")
    kd = sb("kd", [P, H, 64], BF16); b_kd = S.buf("kd")
    v_r = sb("v_r", [P, D], BF16); b_vr = S.buf("v_r")
    wbra = sb("wbra", [P, 8, D], BF16); b_wbra = S.buf("wbra", dma=True)
    wbrb = sb("wbrb", [P, 4, D], BF16); b_wbrb = S.buf("wbrb", dma=True)
    wout = sb("wout", [P, 8, D], BF16); b_wout = S.buf("wout", dma=True)
    k1 = sb("k1", [P, P], BF16); b_k1 = S.buf("k1", dma=True)
    k2 = sb("k2", [P, P], BF16); b_k2 = S.buf("k2", dma=True)
    for hh in range(2):
        S.dma('pool', lambda e, hh=hh: e.dma_start(out=wbra[:, :, hh * 512:(hh + 1) * 512], in_=wview(w_bra, hh * 512, 512)), b_wbra, writes=[b_wbra])
        S.dma('pool', lambda e, hh=hh: e.dma_start(out=wbrb[:, :, hh * 512:(hh + 1) * 512], in_=wview(w_brb, hh * 512, 512)), b_wbrb, writes=[b_wbrb])
        S.dma('pool', lambda e, hh=hh: e.dma_start(out=wout[:, :, hh * 512:(hh + 1) * 512], in_=wview(w_out, hh * 512, 512)), b_wout, writes=[b_wout])
    S.dma('pool', lambda e: e.dma_start(out=k1[:], in_=k1T), b_k1, writes=[b_k1])
    S.dma('pool', lambda e: e.dma_start(out=k2[:], in_=k2T), b_k2, writes=[b_k2])

    wbuf = [sb("wbuf%d" % i, [P, 8, 512], BF16) for i in range(2)]
    b_wbuf = [S.buf("wbuf%d" % i, dma=True) for i in range(2)]
    wcount = [0]

    def stream_w(c0, src=None):
        blk = c0 // 512 if src is None else 13 + c0 // 512
        i = wcount[0] % 2
        wcount[0] += 1
        S.dma('sp', lambda e: e.dma_start(out=wbuf[i][:], in_=wsc[blk * P:(blk + 1) * P, :].rearrange("p (c n) -> p c n", c=8)),
              b_wbuf[i], reads=[b_wsc], writes=[b_wbuf[i]])
        return wbuf[i], b_wbuf[i]

    xres = sb("xres", [P, D]); b_xres = S.buf("xres", dma=True)
    qd = sb("qd", [P, H, 64], BF16); b_qd = S.buf("qd")
    qdT = sb("qdT", [64, H, P], BF16); b_qdT = S.buf("qdT")
    kdT = sb("kdT", [64, H, P], BF16); b_kdT = S.buf("kdT")
    rg = sb("rg", [P, D], BF16); b_rg = S.buf("rg")
    sqn = sb("sqn", [P, H, 64], BF16); b_sqn = S.buf("sqn")
    skn = sb("skn", [P, H, 64], BF16); b_skn = S.buf("skn")
    sqT = sb("sqT", [64, H, P], BF16); b_sqT = S.buf("sqT")
    skT = [sb("skT%d" % i, [64, H, P], BF16) for i in range(2)]; b_skT = [S.buf("skT%d" % i) for i in range(2)]
    sv = [sb("sv%d" % i, [P, 512], BF16) for i in range(2)]; b_sv = [S.buf("sv%d" % i) for i in range(2)]
    siga = TP[7]; b_siga = b_TP[7]
    sigb = TP[8]; b_sigb = b_TP[8]
    pm = sb("pm", [P, D], BF16); b_pm = S.buf("pm")
    yf = TP[3]; b_yf = b_TP[3]
    ysq = TP[4]; b_ysq = b_TP[4]
    gst = sb("gst", [P, 6, H]); b_gst = S.buf("gst")
    ret = sb("ret", [P, D], BF16); b_ret = S.buf("ret")
    retT = sb("retT", [P, 8, P], BF16); b_retT = S.buf("retT")
    e_s = TP[3]; b_es = b_TP[3]
    sp_s = TP[4]; b_sp = b_TP[4]
    spm = sb("spm", [P, D], BF16); b_spm = S.buf("spm")
    u_s = TP[0]; b_us = b_TP[0]
    w_s = sb("w_s", [P, D], BF16); b_ws = S.buf("w_s")
    sbT = sb("sbT", [P, 4, P], BF16); b_sbT = S.buf("sbT")
    m1 = TP[5]; b_m1 = b_TP[5]
    m2 = TP[6]; b_m2 = b_TP[6]
    mixed = ret; b_mixed = b_ret
    mixT = retT; b_mixT = b_retT
    x1 = TP[1]; b_x1 = b_TP[1]
    hn = TP[9]; b_hn = b_TP[9]
    qT = sb("qT", [P, 16, P], BF16); b_qT = S.buf("qT")
    sc = TPbig[:, 3:5, :].rearrange("p a d -> p (a d)").rearrange("p (g n) -> p g n", g=16); b_sc = [b_TP[3], b_TP[4]]
    sc2 = TPbig[:, 5:7, :].rearrange("p a d -> p (a d)").rearrange("p (g n) -> p g n", g=16); b_sc2 = [b_TP[5], b_TP[6]]
    tv = sb("tv", [P, 16, 16]); b_tv = S.buf("tv")
    ti = sb("ti", [P, 16, 16], U32); b_ti = S.buf("ti")
    tif = sb("tif", [P, 16, 16]); b_tif = S.buf("tif")
    cand = TPbig[:, 7:9, :].rearrange("p a d -> p (a d)").rearrange("p (h c) -> p h c", h=H); b_cand = [b_TP[7], b_TP[8]]
    cand2 = sc.rearrange("p g n -> p (g n)").rearrange("p (h c) -> p h c", h=H); b_cand2 = b_sc
    tsv = sb("tsv", [P, H, 16]); b_tsv = S.buf("tsv")
    tpos = sb("tpos", [P, H, 16], U32); b_tpos = S.buf("tpos")
    tposf = sb("tposf", [P, H, 16]); b_tposf = S.buf("tposf")
    ta = sb("ta", [P, H, 16]); b_ta = S.buf("ta")
    tb = sb("tb", [P, H, 16]); b_tb = S.buf("tb")
    iota16 = sb("iota16", [P, 16]); b_iota = S.buf("iota16")
    oh = sc2.rearrange("p g n -> p (g n)").rearrange("p (h a b) -> p h a b", h=H, a=16); b_oh = b_sc2
    idx1 = sb("idx1", [P, H, 16]); b_idx1 = S.buf("idx1")
    idx2 = sb("idx2", [P, H, 16]); b_idx2 = S.buf("idx2")
    eidx = sb("eidx", [P, 128], U32); b_eidx = S.buf("eidx")
    gw = sb("gw", [P, H, 16]); b_gw = S.buf("gw")
    gs = sb("gs", [P, 2, H]); b_gs = S.buf("gs")
    hv = sb("hv", [P, 128]); b_hv = S.buf("hv")
    ga_ = sb("ga_", [P, 6, 128]); b_ga = S.buf("ga_"); b_ga2 = S.buf("ga2"); b_ga3 = S.buf("ga3")
    aw = sb("aw", [P, 128]); b_aw = S.buf("aw")
    NG = 8
    dg = [sb("dg%d" % i, [P, P], BF16) for i in range(4)]; b_dg = [S.buf("dg%d" % i) for i in range(4)]
    gbuf = None; b_gbuf = None
    acc = TP[0]; b_acc = b_TP[0]
    b_yout = S.buf("yout", dma=True)

    _gi = [3, 4, 5, 6, 7, 8, 2, 0]
    gbuf = [TPbig[:, i, :].bitcast(BF16) for i in _gi]
    b_gbuf = [b_TP[i] for i in _gi]
    gsem = [S.buf("gsem%d" % i, dma=True) for i in range(len(_gi))]
    S.op('pool', lambda e: e.iota(iota16[:], pattern=[[1, 16]], base=0, channel_multiplier=0, allow_small_or_imprecise_dtypes=True),
         writes=[b_iota])
    thr16 = sb("thr16", [P, 16])
    S.op('pool', lambda e: e.iota(thr16[:], pattern=[[16, 16]], base=0, channel_multiplier=0, allow_small_or_imprecise_dtypes=True),
         reads=[b_iota], writes=[b_iota])

    def sb_kv(cur):
        wb, bw = stream_w(3584)
        proj512(wb, bw, psC[:], bC)
        qknorm(psC[:], bC, gsk, b_gsk, 0, out_bf=skn, b_out=b_skn)
        transposeH(skn, b_skn, skT[cur], b_skT[cur])
        wb, bw = stream_w(4096)
        proj512(wb, bw, psD[:], bD)
        S.op('act', lambda e: e.activation(out=sv[cur][:], in_=psD[:], func=AF.Copy), reads=[bD], writes=[b_sv[cur]])

    S.dma('sp', lambda e: e.dma_start(out=xt[0][:], in_=x_halo), b_xt[0], writes=[b_xt[0]])
    rmsnorm_T(xt[0][:], b_xt[0], gattn, b_gattn)
    sb_kv(1)
    dump(2, xt[0][:], [b_xt[0], b_sv[1], b_skT[1]])

    def m_head(n):
        cur = n % 2
        S.dma('sp', lambda e, n=n: e.dma_start(out=xres[:], in_=x_own[n * P:(n + 1) * P, :]), b_xres, writes=[b_xres])
        rmsnorm_T(xres[:], b_xres, gattn, b_gattn)
        wb, bw = stream_w(0)
        proj512(wb, bw, psC[:], bC)
        qknorm(psC[:], bC, grq, b_grq, 0, scale_ap=qdec[:], b_scale=b_qdec, cos=cso[:, n, :], sin=sno[:, n, :],
               b_cs=[b_cso, b_sno], out_bf=qd, b_out=b_qd)
        transposeH(qd, b_qd, qdT, b_qdT)
        wb, bw = stream_w(512)
        proj512(wb, bw, psD[:], bD)
        qknorm(psD[:], bD, grk, b_grk, 0, scale_ap=kdec[:], b_scale=b_kdec, cos=cso[:, n, :], sin=sno[:, n, :],
               b_cs=[b_cso, b_sno], out_bf=kd, b_out=b_kd)
        transposeH(kd, b_kd, kdT, b_kdT)
        wb, bw = stream_w(3072)
        proj512(wb, bw, psC[:], bC)
        qknorm(psC[:], bC, gsq, b_gsq, 0, out_bf=sqn, b_out=b_sqn)
        transposeH(sqn, b_sqn, sqT, b_sqT)
        sb_kv(cur)

    m_head(0)
    for n in range(NT):
        cur = n % 2; prv = 1 - cur
        for hh in range(2):
            wb, bw = stream_w(1024 + hh * 512)
            proj512(wb, bw, psA[:, hh * 512:(hh + 1) * 512], bA)
        S.op('act', lambda e: e.activation(out=v_r[:], in_=psA[:], func=AF.Copy), reads=[bA], writes=[b_vr])
        for hh in range(2):
            wb, bw = stream_w(2048 + hh * 512)
            proj512(wb, bw, psB[:, hh * 512:(hh + 1) * 512], bB)
        S.op('act', lambda e: e.activation(out=m1[:], in_=psB[:], func=AF.Silu), reads=[bB], writes=[b_m1])
        S.op('pool', lambda e: e.tensor_tensor(out=rg[:], in0=m1[:], in1=ggn[:], op=ALU.mult), reads=[b_m1, b_ggn], writes=[b_rg])
        for h in range(H):
            S.op('pe', lambda e, h=h: e.matmul(out=psA[:, h * P:(h + 1) * P], lhsT=kdT[:, h, :], rhs=qdT[:, h, :], start=True, stop=True),
                 reads=[b_kdT, b_qdT], writes=[bA])
        S.op('dve', lambda e: e.tensor_tensor(out=pm[:], in0=psA[:], in1=maskT[:], op=ALU.mult), reads=[bA, b_maskT], writes=[b_pm])
        for h in range(H):
            S.op('pe', lambda e, h=h: e.matmul(out=psB[:, h * P:(h + 1) * P], lhsT=pm[:, h * P:(h + 1) * P], rhs=v_r[:, h * P:(h + 1) * P],
                                               start=True, stop=False, skip_group_check=True),
                 reads=[b_pm, b_vr], writes=[bB])
            S.op('pe', lambda e, h=h: e.matmul(out=psB[:, h * P:(h + 1) * P], lhsT=qdT[:, h, :], rhs=state_bf[:, h * P:(h + 1) * P],
                                               start=False, stop=True, skip_group_check=True),
                 reads=[b_qdT, b_state_bf], writes=[bB])
        S.op('dve', lambda e: e.tensor_tensor(out=state[:], in0=state[:], in1=cdec[:], op=ALU.mult), reads=[b_state, b_cdec], writes=[b_state])
        for rnd in range(2):
            for hl in range(4):
                h = rnd * 4 + hl
                S.op('pe', lambda e, h=h, hl=hl: e.matmul(out=psS[0:64, hl * P:(hl + 1) * P], lhsT=kd[:, h, :], rhs=v_r[:, h * P:(h + 1) * P],
                                                   start=True, stop=True, skip_group_check=True),
                     reads=[b_kd, b_vr], writes=[bS])
            S.op('dve', lambda e, rnd=rnd: e.tensor_tensor(out=state[:, rnd * 512:(rnd + 1) * 512], in0=state[:, rnd * 512:(rnd + 1) * 512],
                                                         in1=psS[0:64, :], op=ALU.add), reads=[b_state, bS], writes=[b_state])
        S.op('act', lambda e: e.activation(out=state_bf[:], in_=state[:], func=AF.Copy), reads=[b_state], writes=[b_state_bf])
        S.op('act', lambda e: e.activation(out=yf[:], in_=psB[:], func=AF.Copy), reads=[bB], writes=[b_yf])
        S.op('act', lambda e: e.activation(out=ysq[:], in_=psB[:], func=AF.Square), reads=[bB], writes=[b_ysq])
        S.op('dve', lambda e: e.tensor_reduce(out=gst[:, 0, :], in_=yf[:].rearrange("p (h d) -> p h d", d=P), axis=AX.X, op=ALU.add),
             reads=[b_yf], writes=[b_gst])
        S.op('dve', lambda e: e.tensor_reduce(out=gst[:, 1, :], in_=ysq[:].rearrange("p (h d) -> p h d", d=P), axis=AX.X, op=ALU.add),
             reads=[b_ysq], writes=[b_gst])
        S.op('dve', lambda e: e.tensor_scalar(out=gst[:, 2, :], in0=gst[:, 0, :], scalar1=1.0 / P, scalar2=None, op0=ALU.mult), reads=[b_gst], writes=[b_gst])
        S.op('dve', lambda e: e.tensor_tensor(out=gst[:, 3, :], in0=gst[:, 2, :], in1=gst[:, 2, :], op=ALU.mult), reads=[b_gst], writes=[b_gst])
        S.op('dve', lambda e: e.scalar_tensor_tensor(out=gst[:, 4, :], in0=gst[:, 1, :], scalar=1.0 / P, in1=gst[:, 3, :], op0=ALU.mult, op1=ALU.subtract),
             reads=[b_gst], writes=[b_gst])
        S.op('act', lambda e: e.activation(out=gst[:, 5, :], in_=gst[:, 4, :], func=AF.Sqrt, bias=eps_t[:, 0:1], scale=1.0), reads=[b_gst, b_eps], writes=[b_gst])
        S.op('dve', lambda e: e.reciprocal(out=gst[:, 3, :], in_=gst[:, 5, :]), reads=[b_gst], writes=[b_gst])
        y3 = yf[:].rearrange("p (h d) -> p h d", d=P)
        S.op('dve', lambda e: e.tensor_tensor(out=y3, in0=y3, in1=gst[:, 2, :].unsqueeze(2).to_broadcast([P, H, P]), op=ALU.subtract),
             reads=[b_yf, b_gst], writes=[b_yf])
        S.op('dve', lambda e: e.tensor_tensor(out=y3, in0=y3, in1=gst[:, 3, :].unsqueeze(2).to_broadcast([P, H, P]), op=ALU.mult),
             reads=[b_yf, b_gst], writes=[b_yf])
        S.op('pool', lambda e: e.tensor_tensor(out=ret[:], in0=yf[:], in1=rg[:], op=ALU.mult), reads=[b_yf, b_rg], writes=[b_ret])
        transpose8(ret, b_ret, retT, b_retT)
        dump(3, yf[:], [b_yf, b_retT])
        for half in range(2):
            for blk, kT_ in ((0, skT[prv]), (1, skT[cur])):
                for hl in range(4):
                    h = half * 4 + hl
                    S.op('pe', lambda e, blk=blk, hl=hl, h=h, kT_=kT_: e.matmul(
                        out=psA[:, blk * 512 + hl * P: blk * 512 + (hl + 1) * P], lhsT=kT_[:, h, :],
                        rhs=sqT[:, h, :], start=True, stop=True),
                        reads=[b_skT[prv], b_skT[cur], b_sqT], writes=[bA])
            S.op('act', lambda e: e.activation(out=e_s[:], in_=psA[:], func=AF.Exp, scale=0.125), reads=[bA], writes=[b_es])
            S.op('act', lambda e: e.activation(out=sp_s[:], in_=e_s[:], func=AF.Ln, bias=one_t[:, 0:1], scale=1.0), reads=[b_es, b_one], writes=[b_sp])
            S.op('dve', lambda e: e.tensor_tensor(
                out=spm[:].rearrange("p (b h t) -> p b h t", b=2, h=4), in0=sp_s[:].rearrange("p (b h t) -> p b h t", b=2, h=4),
                in1=mstay[:].rearrange("p (b h t) -> p b h t", b=2, h=4), op=ALU.mult), reads=[b_sp, b_mstay], writes=[b_spm])
            S.op('pe', lambda e: e.matmul(out=psB[:, 0:512], lhsT=tri[:], rhs=spm[:, 0:512], start=True, stop=False), reads=[b_tri, b_spm], writes=[bB])
            S.op('pe', lambda e: e.matmul(out=psB[:, 0:512], lhsT=ones[:], rhs=spm[:, 512:1024], start=False, stop=True), reads=[b_ones, b_spm], writes=[bB])
            S.op('pe', lambda e: e.matmul(out=psB[:, 512:1024], lhsT=tri[:], rhs=spm[:, 512:1024], start=True, stop=False), reads=[b_tri, b_spm], writes=[bB])
            S.op('pe', lambda e: e.matmul(out=psB[:, 512:1024], lhsT=ident[:], rhs=mpos[:], start=False, stop=True), reads=[b_ident, b_mpos], writes=[bB])
            S.op('dve', lambda e: e.scalar_tensor_tensor(out=u_s[:], in0=psA[:], scalar=0.125, in1=sp_s[:], op0=ALU.mult, op1=ALU.subtract),
                 reads=[bA, b_sp], writes=[b_us])
            S.op('dve', lambda e: e.tensor_tensor(out=u_s[:], in0=u_s[:], in1=psB[:], op=ALU.subtract), reads=[b_us, bB], writes=[b_us])
            S.op('act', lambda e: e.activation(out=w_s[:], in_=u_s[:], func=AF.Exp), reads=[b_us], writes=[b_ws])
            for hl in range(4):
                h = half * 4 + hl
                po = (h % 2) * 64
                for blk, svb, bsv in ((0, sv[prv], b_sv[prv]), (1, sv[cur], b_sv[cur])):
                    S.op('pe', lambda e, h=h, hl=hl, po=po, blk=blk, svb=svb: e.matmul(
                        out=psS[po:po + 64, (h // 2) * P:(h // 2 + 1) * P], lhsT=svb[:, h * 64:(h + 1) * 64],
                        rhs=w_s[:, blk * 512 + hl * P: blk * 512 + (hl + 1) * P], start=(blk == 0), stop=(blk == 1), skip_group_check=True),
                        reads=[bsv, b_ws], writes=[bS])
        S.op('act', lambda e: e.activation(out=sbT[:].rearrange("p c t -> p (c t)"), in_=psS[:], func=AF.Copy), reads=[bS], writes=[b_sbT])
        if STAGE == 4:
            S.op('act', lambda e: e.activation(out=m2[:, 0:512], in_=psS[:], func=AF.Copy), reads=[bS], writes=[b_m2])
            dump(4, m2[:, 0:512], [b_m2], 512)
        for hh in range(2):
            for c in range(8):
                S.op('pe', lambda e, c=c, hh=hh: e.matmul(out=psA[:, hh * 512:(hh + 1) * 512], lhsT=retT[:, c, :], rhs=wbra[:, c, hh * 512:(hh + 1) * 512],
                                                          start=(c == 0), stop=(c == 7)), reads=[b_retT, b_wbra], writes=[bA])
            for c in range(4):
                S.op('pe', lambda e, c=c, hh=hh: e.matmul(out=psB[:, hh * 512:(hh + 1) * 512], lhsT=sbT[:, c, :], rhs=wbrb[:, c, hh * 512:(hh + 1) * 512],
                                                          start=(c == 0), stop=(c == 3)), reads=[b_sbT, b_wbrb], writes=[bB])
        for gi, (gt, bg) in enumerate(((siga, b_siga), (sigb, b_sigb))):
            for hh in range(2):
                wb, bw = stream_w(4608 + gi * 1024 + hh * 512)
                pst, bp = (psC, bC) if hh == 0 else (psD, bD)
                proj512(wb, bw, pst[:], bp)
                S.op('act', lambda e, gt=gt, hh=hh, pst=pst: e.activation(out=gt[:, hh * 512:(hh + 1) * 512], in_=pst[:], func=AF.Sigmoid),
                     reads=[bp], writes=[bg])
        S.op('dve', lambda e: e.tensor_tensor(out=m1[:], in0=psA[:], in1=siga[:], op=ALU.mult), reads=[bA, b_siga], writes=[b_m1])
        S.op('dve', lambda e: e.tensor_tensor(out=m2[:], in0=psB[:], in1=sigb[:], op=ALU.mult), reads=[bB, b_sigb], writes=[b_m2])
        S.op('pool', lambda e: e.tensor_tensor(out=mixed[:], in0=m1[:], in1=m2[:], op=ALU.add), reads=[b_m1, b_m2], writes=[b_mixed])
        transpose8(mixed, b_mixed, mixT, b_mixT)
        for hh in range(2):
            for c in range(8):
                S.op('pe', lambda e, c=c, hh=hh: e.matmul(out=psA[:, hh * 512:(hh + 1) * 512], lhsT=mixT[:, c, :], rhs=wout[:, c, hh * 512:(hh + 1) * 512],
                                                          start=(c == 0), stop=(c == 7)), reads=[b_mixT, b_wout], writes=[bA])
        S.op('dve', lambda e: e.tensor_tensor(out=x1[:], in0=psA[:], in1=xres[:], op=ALU.add), reads=[bA, b_xres], writes=[b_x1])
        dump(5, x1[:], [b_x1])
        rmsnorm_T(x1[:], b_x1, gffn, b_gffn, keep_f32=hn[:], b_keep=b_hn)
        for g4 in range(4):
            wqb, b_wq = stream_w(g4 * 512, w_q)
            for gl in range(4):
                g = g4 * 4 + gl
                pst = psA[:, gl * P:(gl + 1) * P] if g4 % 2 == 0 else psB[:, gl * P:(gl + 1) * P]
                bp = bA if g4 % 2 == 0 else bB
                for c in range(8):
                    S.op('pe', lambda e, c=c, gl=gl, pst=pst, wqb=wqb: e.matmul(out=pst, lhsT=wqb[:, c, gl * P:(gl + 1) * P], rhs=xnT[:, c, :],
                                                                     start=(c == 0), stop=(c == 7)), reads=[b_wq, b_xnT], writes=[bp])
            src = psA if g4 % 2 == 0 else psB
            bp = bA if g4 % 2 == 0 else bB
            S.op('act', lambda e, g4=g4, src=src: e.activation(out=qT[:, g4 * 4:(g4 + 1) * 4, :].rearrange("p g t -> p (g t)"), in_=src[:, 0:512], func=AF.Copy),
                 reads=[bp], writes=[b_qT])
        for g4 in range(4):
            pst, bp = (psA, bA) if g4 % 2 == 0 else (psB, bB)
            for gl in range(4):
                g = g4 * 4 + gl
                kk, bk = (k1, b_k1) if g % 2 == 0 else (k2, b_k2)
                S.op('pe', lambda e, g=g, gl=gl, pst=pst, kk=kk: e.matmul(out=pst[:, gl * P:(gl + 1) * P], lhsT=qT[:, g, :], rhs=kk[:], start=True, stop=True),
                     reads=[b_qT, bk], writes=[bp])
            S.op('act', lambda e, g4=g4, pst=pst: e.activation(out=sc[:, g4 * 4:(g4 + 1) * 4, :].rearrange("p g n -> p (g n)"), in_=pst[:, 0:512], func=AF.Copy),
                 reads=[bp], writes=[b_sc])
        cap = []
        if n + 1 < NT:
            S.cap = cap
            m_head(n + 1)
            S.cap = None
        S.after_op = lambda: S.replay(cap, 1)
        bg_tv = [S.buf("tv%d" % g) for g in range(16)]; bg_ti = [S.buf("ti%d" % g) for g in range(16)]; bg_s2 = [S.buf("s2%d" % g) for g in range(16)]
        for g in range(16):
            S.op('dve', lambda e, g=g: e.max(out=tv[:, g, 0:8], in_=sc[:, g, :]), reads=[b_sc], writes=[bg_tv[g]])
        for g in range(16):
            S.op('dve', lambda e, g=g: e.match_replace(out=sc2[:, g, :], in_to_replace=tv[:, g, 0:8], in_values=sc[:, g, :], imm_value=NEG),
                 reads=[b_sc, bg_tv[g]], writes=[bg_s2[g]])
        for g in range(16):
            S.op('dve', lambda e, g=g: e.max_index(out=ti[:, g, 0:8], in_max=tv[:, g, 0:8], in_values=sc[:, g, :]), reads=[b_sc, bg_tv[g]], writes=[bg_ti[g]])
        for g in range(16):
            S.op('dve', lambda e, g=g: e.max(out=tv[:, g, 8:16], in_=sc2[:, g, :]), reads=[bg_s2[g]], writes=[bg_tv[g]])
        for g in range(16):
            S.op('dve', lambda e, g=g: e.max_index(out=ti[:, g, 8:16], in_max=tv[:, g, 8:16], in_values=sc2[:, g, :]), reads=[bg_s2[g], bg_tv[g]], writes=[bg_ti[g]])
        b_tv.w = None; b_ti.w = None
        S.op('dve', lambda e: e.tensor_copy(out=tif[:], in_=ti[:]), reads=bg_ti + bg_tv + bg_s2 + [b_sc2], writes=[b_tif, b_tv, b_ti, b_sc2])
        tv4 = tv[:].rearrange("p (h s) k -> p h s k", s=2)
        tif4 = tif[:].rearrange("p (h s) k -> p h s k", s=2)
        S.op('dve', lambda e: e.tensor_tensor(out=cand.rearrange("p h (a b) -> p h a b", a=16),
                                              in0=tv4[:, :, 0, :].unsqueeze(3).to_broadcast([P, H, 16, 16]),
                                              in1=tv4[:, :, 1, :].unsqueeze(2).to_broadcast([P, H, 16, 16]), op=ALU.add),
             reads=[b_tv], writes=[b_cand])
        bh_ts = [S.buf("ts%d" % h) for h in range(H)]; bh_tp = [S.buf("tp%d" % h) for h in range(H)]; bh_c2 = [S.buf("c2%d" % h) for h in range(H)]
        for h in range(H):
            S.op('dve', lambda e, h=h: e.max(out=tsv[:, h, 0:8], in_=cand[:, h, :]), reads=[b_cand], writes=[bh_ts[h]])
        for h in range(H):
            S.op('dve', lambda e, h=h: e.match_replace(out=cand2[:, h, :], in_to_replace=tsv[:, h, 0:8], in_values=cand[:, h, :], imm_value=NEG),
                 reads=[b_cand, bh_ts[h], b_cand2], writes=[bh_c2[h]])
        for h in range(H):
            S.op('dve', lambda e, h=h: e.max_index(out=tpos[:, h, 0:8], in_max=tsv[:, h, 0:8], in_values=cand[:, h, :]), reads=[b_cand, bh_ts[h]], writes=[bh_tp[h]])
        for h in range(H):
            S.op('dve', lambda e, h=h: e.max(out=tsv[:, h, 8:16], in_=cand2[:, h, :]), reads=[bh_c2[h]], writes=[bh_ts[h]])
        for h in range(H):
            S.op('dve', lambda e, h=h: e.max_index(out=tpos[:, h, 8:16], in_max=tsv[:, h, 8:16], in_values=cand2[:, h, :]), reads=[bh_c2[h], bh_ts[h]], writes=[bh_tp[h]])
        b_tsv.w = None; b_tpos.w = None
        S.op('dve', lambda e: e.tensor_copy(out=tposf[:], in_=tpos[:]), reads=bh_tp + bh_ts + bh_c2, writes=[b_tposf, b_tsv, b_tpos, b_cand2])
        S.op('dve', lambda e: e.tensor_tensor(out=oh, in0=tposf[:].unsqueeze(3).to_broadcast([P, H, 16, 16]),
                                              in1=thr16[:].unsqueeze(1).unsqueeze(1).to_broadcast([P, H, 16, 16]), op=ALU.is_ge),
             reads=[b_tposf, b_iota], writes=[b_oh])
        S.op('dve', lambda e: e.tensor_reduce(out=ta[:], in_=oh, axis=AX.X, op=ALU.add), reads=[b_oh], writes=[b_ta])
        S.op('dve', lambda e: e.tensor_scalar(out=ta[:], in0=ta[:], scalar1=-1.0, scalar2=None, op0=ALU.add), reads=[b_ta], writes=[b_ta])
        S.op('dve', lambda e: e.scalar_tensor_tensor(out=tb[:], in0=ta[:], scalar=-16.0, in1=tposf[:], op0=ALU.mult, op1=ALU.add),
             reads=[b_ta, b_tposf], writes=[b_tb])
        io_b = iota16[:].unsqueeze(1).unsqueeze(1).to_broadcast([P, H, 16, 16])
        for sel, half, dst, bd in ((ta, 0, idx1, b_idx1), (tb, 1, idx2, b_idx2)):
            bsel = b_ta if half == 0 else b_tb
            S.op('dve', lambda e, sel=sel: e.tensor_tensor(out=oh, in0=sel[:].unsqueeze(3).to_broadcast([P, H, 16, 16]), in1=io_b, op=ALU.is_equal),
                 reads=[bsel, b_iota], writes=[b_oh])
            S.op('dve', lambda e, half=half: e.tensor_tensor(out=oh, in0=oh, in1=tif4[:, :, half, :].unsqueeze(2).to_broadcast([P, H, 16, 16]), op=ALU.mult),
                 reads=[b_oh, b_tif], writes=[b_oh])
            S.op('dve', lambda e, dst=dst: e.tensor_reduce(out=dst[:], in_=oh, axis=AX.X, op=ALU.add), reads=[b_oh], writes=[bd])
        S.op('dve', lambda e: e.scalar_tensor_tensor(out=idx1[:], in0=idx1[:], scalar=128.0, in1=idx2[:], op0=ALU.mult, op1=ALU.add),
             reads=[b_idx1, b_idx2], writes=[b_idx1])
        S.op('dve', lambda e: e.tensor_copy(out=eidx[:], in_=idx1[:].rearrange("p h k -> p (h k)")), reads=[b_idx1], writes=[b_eidx])
        S.op('dve', lambda e: e.tensor_tensor(out=gw[:], in0=tsv[:], in1=tsv[:, :, 0:1].to_broadcast([P, H, 16]), op=ALU.subtract), reads=[b_tsv], writes=[b_gw])
        S.op('act', lambda e: e.activation(out=gw[:], in_=gw[:], func=AF.Exp), reads=[b_gw], writes=[b_gw])
        S.op('dve', lambda e: e.tensor_reduce(out=gs[:, 0, :], in_=gw[:], axis=AX.X, op=ALU.add), reads=[b_gw], writes=[b_gs])
        S.op('dve', lambda e: e.reciprocal(out=gs[:, 1, :], in_=gs[:, 0, :]), reads=[b_gs], writes=[b_gs])
        S.op('dve', lambda e: e.tensor_tensor(out=gw[:], in0=gw[:], in1=gs[:, 1, :].unsqueeze(2).to_broadcast([P, H, 16]), op=ALU.mult), reads=[b_gw, b_gs], writes=[b_gw])
        if STAGE == 6:
            S.op('dve', lambda e: e.tensor_copy(out=m2[:, 0:128], in_=idx1[:].rearrange("p h k -> p (h k)")), reads=[b_idx1], writes=[b_m2])
            S.op('dve', lambda e: e.tensor_copy(out=m2[:, 128:256], in_=gw[:].rearrange("p h k -> p (h k)")), reads=[b_gw], writes=[b_m2])
            dump(6, m2[:, 0:256], [b_m2], 256)
        S.after_op = None
        GS = 2
        NGRP = 128 // GS
        gwf = gw[:].rearrange("p h k -> p (h k)")

        def emit_gather(g):
            for k in range(GS):
                j = g * GS + k
                gb_, bgb = gbuf[j % NG], b_gbuf[j % NG]
                S.dma('pool', lambda e, j=j, gb_=gb_: e.indirect_dma_start(out=gb_[:], out_offset=None, in_=uv_bf,
                                                                          in_offset=bass.IndirectOffsetOnAxis(ap=eidx[:, j:j + 1], axis=0)),
                      gsem[j % NG], reads=[b_eidx, b_ubf], writes=[bgb])

        def emit_dots(g):
            for k in range(GS):
                j = g * GS + k
                gb_, bgb = gbuf[j % NG], b_gbuf[j % NG]
                S.op('dve', lambda e, j=j, gb_=gb_: e.scalar_tensor_tensor(out=junk[:], in0=gb_[:, 0:D], scalar=1.0, in1=hn[:],
                                                                          op0=ALU.mult, op1=ALU.mult, accum_out=hv[:, j:j + 1]),
                     reads=[bgb, b_hn], writes=[b_junk, b_hv])

        def emit_pre(g):
            c = slice(g * GS, (g + 1) * GS)
            S.op('act', lambda e: e.activation(out=ga_[:, 0, c], in_=hv[:, c], func=AF.Square), reads=[b_hv], writes=[b_ga])
            S.op('act', lambda e: e.activation(out=ga_[:, 1, c], in_=ga_[:, 0, c], func=AF.Identity, scale=0.0713548162726, bias=gk_t[:, 0:1]),
                 reads=[b_ga, b_gk], writes=[b_ga])
            for k in range(GS):
                j = g * GS + k
                S.op('act', lambda e, j=j: e.activation(out=ga_[:, 3, j:j + 1], in_=ga_[:, 1, j:j + 1], func=AF.Sigmoid, scale=hv[:, j:j + 1]),
                     reads=[b_ga, b_hv], writes=[b_ga2])
            S.op('dve', lambda e: e.tensor_tensor(out=ga_[:, 4, c], in0=hv[:, c], in1=gwf[:, c], op=ALU.mult), reads=[b_hv, b_gw], writes=[b_ga3])

        def emit_post(g):
            for k in range(GS):
                j = g * GS + k
                S.op('act', lambda e, j=j: e.activation(out=aw[:, j:j + 1], in_=ga_[:, 3, j:j + 1], func=AF.Copy, scale=ga_[:, 4, j:j + 1]),
                     reads=[b_ga2, b_ga3], writes=[b_aw])

        def emit_axpy(g):
            for k in range(GS):
                j = g * GS + k
                gb_, bgb = gbuf[j % NG], b_gbuf[j % NG]
                dgj, bdg = dg[j % 4], b_dg[j % 4]
                S.op('act', lambda e, j=j, dgj=dgj: e.activation(out=dgj[:], in_=identF[:], func=AF.Copy, scale=aw[:, j:j + 1]),
                     reads=[b_identF, b_aw], writes=[bdg])
                for hh in range(2):
                    S.op('pe', lambda e, j=j, hh=hh, dgj=dgj, gb_=gb_: e.matmul(out=psA[:, hh * 512:(hh + 1) * 512], lhsT=dgj[:],
                                                                            rhs=gb_[:, D + hh * 512:D + (hh + 1) * 512],
                                                                            start=(j == 0), stop=(j == 127)), reads=[bdg, bgb], writes=[bA])

        for g0 in range(3):
            emit_gather(g0)
        for st_ in range(NGRP + 1):
            S.replay(cap, 3)
            if 0 <= st_ - 1 < NGRP:
                emit_pre(st_ - 1)
            if st_ < NGRP:
                emit_dots(st_)
            if 0 <= st_ - 1 < NGRP:
                emit_post(st_ - 1)
                emit_axpy(st_ - 1)
            if st_ + 3 < NGRP:
                emit_gather(st_ + 3)
        S.replay(cap)
        S.op('dve', lambda e: e.tensor_tensor(out=acc[:], in0=psA[:], in1=x1[:], op=ALU.add), reads=[bA, b_x1], writes=[b_acc])
        S.dma('sp', lambda e, n=n: e.dma_start(out=y_out[n * P:(n + 1) * P, :], in_=acc[:]), b_yout, reads=[b_acc], writes=[b_yout])

    S.wait_all('sp', [b_yout])
    es.close()
    return nc, None


def _consts():
    hs = np.arange(H, dtype=np.float64)
    gam = 1.0 - 2.0 ** (-5.0 - hs)
    i = np.arange(P, dtype=np.float64)
    c = {}
    c["c_ident"] = np.eye(P, dtype=np.float32)
    c["c_tri"] = (i[:, None] > i[None, :]).astype(np.float32)
    c["c_ones"] = np.ones((P, P), np.float32)
    mp = 1.0e4 * (i[:, None] >= i[None, :]).astype(np.float32)
    c["c_mpos"] = np.tile(mp, (1, 4)).astype(np.float32)
    ms = np.ones((P, 2, 4, P), np.float32)
    ms[:, 1, :, :] = (i[:, None] < i[None, :]).astype(np.float32)[:, None, :]
    c["c_mstay"] = ms.reshape(P, 1024)
    mk = np.zeros((P, H, P), np.float64)
    for h in range(H):
        mk[:, h, :] = (i[None, :] >= i[:, None]) * gam[h] ** (-128.0)
    c["c_maskT"] = mk.reshape(P, 1024).astype(np.float32)
    c["c_qdec"] = (gam[None, :] ** (i[:, None] + 1.0)).astype(np.float32)
    c["c_kdec"] = (0.125 * gam[None, :] ** (127.0 - i[:, None])).astype(np.float32)
    cd = np.zeros((64, D), np.float64)
    for h in range(H):
        cd[:, h * P:(h + 1) * P] = gam[h] ** 128.0
    c["c_cdec"] = cd.astype(np.float32)
    return c, gam


def _rope_tabs(pos):
    half = 32
    freqs = (np.float32(10000.0) ** (-np.arange(half, dtype=np.float32) / np.float32(half))).astype(np.float32)
    ang = (pos.astype(np.float32)[:, :, None] * freqs[None, None, :]).astype(np.float32).astype(np.float64)
    return np.cos(ang).astype(np.float32), np.sin(ang).astype(np.float32)


_CACHE = {}


def kernel(x, norm_attn, w_in, ret_q_norm, ret_k_norm, ret_group_norm, sb_q_norm, sb_k_norm,
           w_branch_ret, w_branch_sb, w_out, norm_ffn, peer_w_q, peer_sub_keys_1,
           peer_sub_keys_2, peer_u, peer_v):
    f = np.float32
    x = np.asarray(x, f)
    B, SEQ, _ = x.shape
    assert B == 1
    NT = SEQ // (NCORES * P)
    NPRE = NT * (NCORES - 1) if FORCE_NPRE is None else FORCE_NPRE
    x2 = x[0]
    key = (NT, NPRE)
    if key not in _CACHE:
        try:
            _CACHE[key] = build_program(NT, NPRE)
        except _Stop:
            _H['es'].close()
            _CACHE[key] = (_H['nc'], None)
    nc, _es = _CACHE[key]
    cst, gam = _consts()
    rep = lambda v, n: np.ascontiguousarray(np.broadcast_to(np.tile(np.asarray(v, f).reshape(-1), n)[None, :], (P, np.asarray(v).size * n)))
    shared = dict(cst)
    shared.update({
        "g_attn": rep(norm_attn[0], 1), "g_ffn": rep(norm_ffn[0], 1), "g_gn": rep(ret_group_norm[0], 1),
        "g_rq": rep(ret_q_norm[0], 8), "g_rk": rep(ret_k_norm[0], 8), "g_sq": rep(sb_q_norm[0], 8), "g_sk": rep(sb_k_norm[0], 8),
        "w_in": np.ascontiguousarray(w_in[0], f), "w_bra": np.ascontiguousarray(w_branch_ret[0], f),
        "w_brb": np.ascontiguousarray(w_branch_sb[0], f), "w_out": np.ascontiguousarray(w_out[0], f),
        "w_q": np.ascontiguousarray(peer_w_q[0], f),
        "k1T": np.ascontiguousarray(np.asarray(peer_sub_keys_1[0], f).T), "k2T": np.ascontiguousarray(np.asarray(peer_sub_keys_2[0], f).T),
        "u_tab": np.ascontiguousarray(peer_u[0], f), "v_tab": np.ascontiguousarray(peer_v[0], f),
    })
    in_maps = []
    pp = np.arange(P, dtype=np.float64)
    for c in range(NCORES):
        t0 = c * NT
        m = dict(shared)
        m["x_own"] = np.ascontiguousarray(x2[t0 * P:(t0 + NT) * P])
        m["x_halo"] = np.ascontiguousarray(x2[(t0 - 1) * P:t0 * P]) if c > 0 else np.zeros((P, D), f)
        npre = max(NPRE, 1)
        xp = np.zeros((npre * P, D), f)
        gt = np.arange(npre) + (t0 - NPRE)
        nvalid = min(t0, NPRE)
        if nvalid > 0:
            xp[(NPRE - nvalid) * P:NPRE * P] = x2[(t0 - nvalid) * P:t0 * P]
        m["x_pre"] = xp
        pos_own = (np.arange(NT)[None, :] + t0) * P + pp[:, None]
        m["cos_own"], m["sin_own"] = _rope_tabs(pos_own)
        pos_pre = np.maximum(gt, 0)[None, :] * P + pp[:, None]
        m["cos_pre"], m["sin_pre"] = _rope_tabs(pos_pre)
        ks = np.zeros((P, npre, H), np.float64)
        for mm in range(npre):
            ks[:, mm, :] = 0.125 * gam[None, :] ** (127.0 - pp[:, None]) * gam[None, :] ** (128.0 * (NPRE - 1 - mm))
        m["ksc_pre"] = ks.astype(f)
        in_maps.append(m)
    res = run_bass_kernel_spmd(nc, in_maps, core_ids=list(range(NCORES)), **RUN_KW)
    _H['res'] = res
    out = np.concatenate([np.asarray(r["y_out"], f) for r in res.results], axis=0)
    return out.reshape(1, SEQ, D)
```

```python
import numpy as np
from contextlib import ExitStack
import concourse.bass as bass
import concourse.mybir as mybir
from concourse.bass_utils import run_bass_kernel_spmd

F32 = mybir.dt.float32
BF16 = mybir.dt.bfloat16
U32 = mybir.dt.uint32
I32 = mybir.dt.int32
AF = mybir.ActivationFunctionType
ALU = mybir.AluOpType
AX = mybir.AxisListType

NCORES = 8
D = 1024
P = 128
H = 8
EPS = 1e-6
NEXP = 16384
INW = 6656
NEG = -1.0e30


class Buf:
    def __init__(self, name):
        self.name = name
        self.w = None
        self.r = {}
        self.dsem = None
        self.dcnt = 0


class Sched:
    def __init__(self, nc, es):
        self.nc = nc
        self.es = es
        self.E = {'pe': nc.tensor, 'act': nc.scalar, 'dve': nc.vector, 'pool': nc.gpsimd, 'sp': nc.sync}
        self.sem = {e: es.enter_context(nc.semaphore('sem_' + e)) for e in self.E}
        self.cnt = {e: 0 for e in self.E}
        self.known = {e: {} for e in self.E}
        self.nsem = 0

    def buf(self, name, dma=False):
        b = Buf(name)
        if dma:
            b.dsem = self.es.enter_context(self.nc.semaphore('d_' + name))
        return b

    def _wait(self, e, ev):
        if ev is None:
            return
        sem, val, src = ev
        if src == e and e == 'pe':
            return
        k = self.known[e]
        if k.get(id(sem), 0) >= val:
            return
        self.E[e].wait_ge(sem, val)
        k[id(sem)] = val

    @staticmethod
    def _flat(bs):
        out = []
        for b in bs:
            if isinstance(b, (list, tuple)):
                out.extend(Sched._flat(b))
            else:
                out.append(b)
        return out

    def _deps(self, e, reads, writes):
        reads = self._flat(reads); writes = self._flat(writes)
        for b in reads:
            self._wait(e, b.w)
        for b in writes:
            self._wait(e, b.w)
            for ev in list(b.r.values()):
                self._wait(e, ev)

    def _post(self, ev, reads, writes):
        reads = self._flat(reads); writes = self._flat(writes)
        for b in reads:
            old = b.r.get(id(ev[0]))
            if old is None or old[1] < ev[1]:
                b.r[id(ev[0])] = ev
        for b in writes:
            b.w = ev
            b.r = {}

    cap = None
    after_op = None

    def replay(self, cap, k=None):
        n = len(cap) if k is None else min(k, len(cap))
        for _ in range(n):
            kind, a = cap.pop(0)
            if kind == 'op':
                self.op(*a)
            else:
                self.dma(*a)

    def op(self, e, fn, reads=(), writes=()):
        if self.cap is not None:
            self.cap.append(('op', (e, fn, tuple(reads), tuple(writes))))
            return
        self._deps(e, reads, writes)
        ins = fn(self.E[e])
        self.cnt[e] += 1
        ins.then_inc(self.sem[e], 1)
        self._post((self.sem[e], self.cnt[e], e), reads, writes)
        if self.after_op is not None:
            h, self.after_op = self.after_op, None
            h()
            self.after_op = h

    def dma(self, q, fn, dbuf, reads=(), writes=()):
        if self.cap is not None:
            self.cap.append(('dma', (q, fn, dbuf, tuple(reads), tuple(writes))))
            return
        self._deps(q, reads, writes)
        ins = fn(self.E[q])
        dbuf.dcnt += 16
        ins.then_inc(dbuf.dsem, 16)
        self._post((dbuf.dsem, dbuf.dcnt, 'dma'), reads, writes)

    def wait_all(self, e, bufs):
        for b in self._flat(bufs):
            self._wait(e, b.w)
            for ev in list(b.r.values()):
                self._wait(e, ev)


class _Stop(Exception):
    pass


_H = {}
STAGE = 99
RUN_KW = {}
SKIP_GATHER = False
FORCE_NPRE = None


def build_program(NT, NPRE, dbg=False):
    nc = bass.Bass("TRN2", target_bir_lowering=False)
    es = ExitStack()
    S = Sched(nc, es)
    _H['nc'] = nc; _H['es'] = es

    def din(name, shape, dt=F32):
        return nc.dram_tensor(name, list(shape), dt, kind="ExternalInput").ap()

    x_own = din("x_own", [NT * P, D])
    x_halo = din("x_halo", [P, D])
    x_pre = din("x_pre", [max(NPRE, 1) * P, D])
    cos_own = din("cos_own", [P, NT, 32]); sin_own = din("sin_own", [P, NT, 32])
    cos_pre = din("cos_pre", [P, max(NPRE, 1), 32]); sin_pre = din("sin_pre", [P, max(NPRE, 1), 32])
    ksc_pre = din("ksc_pre", [P, max(NPRE, 1), H])
    g_attn = din("g_attn", [P, D]); g_ffn = din("g_ffn", [P, D]); g_gn = din("g_gn", [P, D])
    g_rq = din("g_rq", [P, 512]); g_rk = din("g_rk", [P, 512]); g_sq = din("g_sq", [P, 512]); g_sk = din("g_sk", [P, 512])
    c_ident = din("c_ident", [P, P]); c_tri = din("c_tri", [P, P]); c_ones = din("c_ones", [P, P])
    c_mpos = din("c_mpos", [P, 512]); c_mstay = din("c_mstay", [P, 1024])
    c_maskT = din("c_maskT", [P, 1024]); c_qdec = din("c_qdec", [P, H]); c_kdec = din("c_kdec", [P, H])
    c_cdec = din("c_cdec", [64, D])
    w_in = din("w_in", [D, INW]); w_bra = din("w_bra", [D, D]); w_brb = din("w_brb", [512, D]); w_out = din("w_out", [D, D])
    w_q = din("w_q", [D, 2048]); k1T = din("k1T", [P, P]); k2T = din("k2T", [P, P])
    u_tab = din("u_tab", [NEXP, D]); v_tab = din("v_tab", [NEXP, D])
    y_out = nc.dram_tensor("y_out", [NT * P, D], F32, kind="ExternalOutput").ap()
    uv_bf = nc.dram_tensor("uv_bf", [NEXP, 2 * D], BF16, kind="Internal").ap()
    NWB = 17
    wsc = nc.dram_tensor("wsc", [NWB * P, 4096], BF16, kind="Internal").ap()
    def dump(stage, src_ap, bsrc, ncols=D):
        if STAGE != stage:
            return
        b_d = S.buf("dump", dma=True)
        npart = src_ap.shape[0]
        S.dma('sp', lambda e: e.dma_start(out=y_out[0:npart, 0:ncols], in_=src_ap), b_d, reads=bsrc, writes=[b_d])
        S.wait_all('sp', [b_d])
        raise _Stop()

    tot = [0]

    def sb(name, shape, dt=F32):
        n = int(np.prod(shape[1:])) * (4 if dt in (F32, U32, I32) else 2)
        tot[0] += n
        if dbg:
            print("SB", name, n, tot[0])
        return es.enter_context(nc.sbuf_tensor(name, list(shape), dt))

    def ps(name, shape, dt=F32):
        return es.enter_context(nc.psum_tensor(name, list(shape), dt))

    psA = ps("psA", [P, 1024]); bA = S.buf("psA")
    psB = ps("psB", [P, 1024]); bB = S.buf("psB")
    psC = ps("psC", [P, 512]); bC = S.buf("psC")
    psD = ps("psD", [P, 512]); bD = S.buf("psD")
    psT = ps("psT", [P, 1024], BF16); bT = S.buf("psT")
    psS = ps("psS", [P, 512]); bS = S.buf("psS")

    consts = []

    def load_const(name, src, shape, dt=F32, q='sp'):
        t = sb(name, shape, dt)
        b = S.buf(name, dma=True)
        S.dma(q, lambda e: e.dma_start(out=t[:], in_=src), b, writes=[b])
        consts.append(b)
        return t, b

    def load_cast(name, src, shape):
        return load_const(name, src, shape, BF16, q='pool')

    ident, b_ident = load_cast("ident", c_ident, [P, P])
    tri, b_tri = load_cast("tri", c_tri, [P, P])
    ones, b_ones = load_cast("ones", c_ones, [P, P])
    mpos, b_mpos = load_cast("mpos", c_mpos, [P, 512])
    mstay, b_mstay = load_cast("mstay", c_mstay, [P, 1024])
    maskT, b_maskT = load_const("maskT", c_maskT, [P, 1024])
    qdec, b_qdec = load_const("qdec", c_qdec, [P, H])
    kdec, b_kdec = load_const("kdec", c_kdec, [P, H])
    cdec, b_cdec = load_const("cdec", c_cdec, [64, D])
    gattn, b_gattn = load_const("gattn", g_attn, [P, D])
    gffn, b_gffn = load_const("gffn", g_ffn, [P, D])
    ggn, b_ggn = load_const("ggn", g_gn, [P, D])
    grq, b_grq = load_const("grq", g_rq, [P, 512])
    grk, b_grk = load_const("grk", g_rk, [P, 512])
    gsq, b_gsq = load_const("gsq", g_sq, [P, 512])
    gsk, b_gsk = load_const("gsk", g_sk, [P, 512])
    cso, b_cso = load_const("cso", cos_own, [P, NT, 32])
    sno, b_sno = load_const("sno", sin_own, [P, NT, 32])

    def wview(w, c0, n):
        return w[:, c0:c0 + n].rearrange("(c p) n -> p c n", p=P)

    TPbig = sb("TPbig", [P, 10, D])
    TP = [TPbig[:, i, :] for i in range(10)]
    b_TP = [S.buf("TP%d" % i, dma=True) for i in range(10)]
    xt = [TP[0], TP[1]]
    b_xt = [b_TP[0], b_TP[1]]
    junk = sb("junk", [P, D], BF16); b_junk = S.buf("junk")
    st4 = sb("st4", [P, 4]); b_st4 = S.buf("st4")

    def rmsnorm_T(src, b_src, gain, b_gain, keep_f32=None, b_keep=None):
        S.op('act', lambda e: e.activation(out=junk[:], in_=src, func=AF.Square, accum_out=st4[:, 0:1]),
             reads=[b_src], writes=[b_junk, b_st4])
        S.op('act', lambda e: e.activation(out=st4[:, 1:2], in_=st4[:, 0:1], func=AF.Sqrt, bias=eps_t[:, 0:1], scale=1.0 / D),
             reads=[b_st4, b_eps], writes=[b_st4])
        S.op('dve', lambda e: e.reciprocal(out=st4[:, 2:3], in_=st4[:, 1:2]), reads=[b_st4], writes=[b_st4])
        if keep_f32 is not None:
            S.op('dve', lambda e: e.scalar_tensor_tensor(out=keep_f32, in0=src, scalar=st4[:, 2:3], in1=gain[:],
                                                         op0=ALU.mult, op1=ALU.mult),
                 reads=[b_src, b_st4, b_gain], writes=[b_keep])
            S.op('act', lambda e: e.activation(out=xn[:], in_=keep_f32, func=AF.Copy), reads=[b_keep], writes=[b_xn])
        else:
            S.op('dve', lambda e: e.scalar_tensor_tensor(out=xn[:], in0=src, scalar=st4[:, 2:3], in1=gain[:],
                                                         op0=ALU.mult, op1=ALU.mult),
                 reads=[b_src, b_st4, b_gain], writes=[b_xn])
        transpose8(xn, b_xn, xnT, b_xnT)

    def transpose8(src, b_src, dst, b_dst, nchunk=8):
        for half in range((nchunk + 3) // 4):
            n = min(4, nchunk - half * 4)
            for k in range(n):
                c = half * 4 + k
                S.op('pe', lambda e, c=c, k=k: e.transpose(out=psT[:, k * P:(k + 1) * P], in_=src[:, c * P:(c + 1) * P],
                                                           identity=ident[:]),
                     reads=[b_src, b_ident], writes=[bT])
            eng = 'act' if half % 2 == 0 else 'dve'
            if eng == 'act':
                S.op('act', lambda e, half=half, n=n: e.activation(
                    out=dst[:, half * 4:half * 4 + n, :].rearrange("p c t -> p (c t)"), in_=psT[:, 0:n * P], func=AF.Copy),
                    reads=[bT], writes=[b_dst])
            else:
                S.op('dve', lambda e, half=half, n=n: e.tensor_copy(
                    out=dst[:, half * 4:half * 4 + n, :].rearrange("p c t -> p (c t)"), in_=psT[:, 0:n * P]),
                    reads=[bT], writes=[b_dst])

    def transposeH(src3, b_src, dst, b_dst):
        for h in range(H):
            S.op('pe', lambda e, h=h: e.transpose(out=psT[0:64, h * P:(h + 1) * P], in_=src3[:, h, :], identity=ident[:]),
                 reads=[b_src, b_ident], writes=[bT])
        S.op('act', lambda e: e.activation(out=dst[:].rearrange("p h t -> p (h t)"), in_=psT[0:64, :], func=AF.Copy),
             reads=[bT], writes=[b_dst])

    def proj512(wb, b_wb, pst, b_pst):
        for c in range(8):
            S.op('pe', lambda e, c=c: e.matmul(out=pst, lhsT=xnT[:, c, :], rhs=wb[:, c, :], start=(c == 0), stop=(c == 7)),
                 reads=[b_xnT, b_wb], writes=[b_pst])

    def qknorm(pst, b_pst, gain, b_gain, slot, scale_ap=None, b_scale=None, cos=None, sin=None, b_cs=(), out_bf=None, b_out=None):
        S.op('act', lambda e: e.activation(out=qf[:], in_=pst, func=AF.Copy), reads=[b_pst], writes=[b_qf])
        S.op('act', lambda e: e.activation(out=sq_s[:], in_=pst, func=AF.Square), reads=[b_pst], writes=[b_sq])
        S.op('dve', lambda e: e.tensor_reduce(out=st8[:, 0, :], in_=sq_s[:].rearrange("p (h d) -> p h d", d=64), axis=AX.X, op=ALU.add),
             reads=[b_sq], writes=[b_st8])
        S.op('act', lambda e: e.activation(out=st8[:, 1, :], in_=st8[:, 0, :], func=AF.Sqrt, bias=eps_t[:, 0:1], scale=1.0 / 64),
             reads=[b_st8, b_eps], writes=[b_st8])
        S.op('dve', lambda e: e.reciprocal(out=st8[:, 2, :], in_=st8[:, 1, :]), reads=[b_st8], writes=[b_st8])
        rs = st8[:, 2, :]
        if scale_ap is not None:
            S.op('dve', lambda e: e.tensor_tensor(out=st8[:, 3, :], in0=st8[:, 2, :], in1=scale_ap, op=ALU.mult),
                 reads=[b_st8, b_scale], writes=[b_st8])
            rs = st8[:, 3, :]
        S.op('dve', lambda e: e.tensor_tensor(out=qn[:].rearrange("p (h d) -> p h d", d=64), in0=qf[:].rearrange("p (h d) -> p h d", d=64),
                                              in1=rs.unsqueeze(2).to_broadcast([P, H, 64]), op=ALU.mult),
             reads=[b_qf, b_st8], writes=[b_qn])
        if cos is None:
            S.op('dve', lambda e: e.tensor_tensor(out=out_bf[:].rearrange("p h d -> p (h d)"), in0=qn[:], in1=gain[:], op=ALU.mult),
                 reads=[b_qn, b_gain], writes=[b_out])
            return
        S.op('pool', lambda e: e.tensor_tensor(out=qn[:], in0=qn[:], in1=gain[:], op=ALU.mult), reads=[b_qn, b_gain], writes=[b_qn])
        q3 = qn[:].rearrange("p (h d) -> p h d", d=64)
        x1 = q3[:, :, 0:32]; x2 = q3[:, :, 32:64]
        cb = cos.unsqueeze(1).to_broadcast([P, H, 32]); sbb = sin.unsqueeze(1).to_broadcast([P, H, 32])
        r = [rt[:, i, :].rearrange("p (h d) -> p h d", d=32) for i in range(4)]
        S.op('dve', lambda e: e.tensor_tensor(out=r[0], in0=x1, in1=cb, op=ALU.mult), reads=[b_qn] + list(b_cs), writes=[b_rt])
        S.op('pool', lambda e: e.tensor_tensor(out=r[1], in0=x2, in1=sbb, op=ALU.mult), reads=[b_qn] + list(b_cs), writes=[b_rt])
        S.op('dve', lambda e: e.tensor_tensor(out=r[2], in0=x1, in1=sbb, op=ALU.mult), reads=[b_qn] + list(b_cs), writes=[b_rt])
        S.op('pool', lambda e: e.tensor_tensor(out=r[3], in0=x2, in1=cb, op=ALU.mult), reads=[b_qn] + list(b_cs), writes=[b_rt])
        S.op('dve', lambda e: e.tensor_tensor(out=out_bf[:, :, 0:32], in0=r[0], in1=r[1], op=ALU.subtract), reads=[b_rt], writes=[b_out])
        S.op('dve', lambda e: e.tensor_tensor(out=out_bf[:, :, 32:64], in0=r[2], in1=r[3], op=ALU.add), reads=[b_rt], writes=[b_out])

    b_ubf = S.buf("uv_bf", dma=True); b_vbf = b_ubf
    b_wsc = S.buf("wsc", dma=True)
    identF, b_identF = load_const("identF", c_ident, [P, P])
    eps_t = sb("eps_t", [P, 1]); b_eps = S.buf("eps")
    S.op('dve', lambda e: e.memset(eps_t[:], EPS), writes=[b_eps])
    gk_t = sb("gk_t", [P, 1]); b_gk = S.buf("gk")
    S.op('dve', lambda e: e.memset(gk_t[:], 1.5957691216057308), writes=[b_gk])
    one_t = sb("one_t", [P, 1]); b_one = S.buf("one")
    S.op('dve', lambda e: e.memset(one_t[:], 1.0), writes=[b_one])

    state = sb("state", [64, D]); b_state = S.buf("state")
    state_bf = sb("state_bf", [64, D], BF16); b_state_bf = S.buf("state_bf")

    with ExitStack() as es0:
        NBLK = NEXP // 128
        NCB = 4
        cb = [es0.enter_context(nc.sbuf_tensor("cb%d" % i, [P, 1, D], BF16)) for i in range(NCB)]
        b_cb = [S.buf("cb%d" % i, dma=True) for i in range(NCB)]
        b_cin = b_cb; b_cout = []
        pc_jobs = [(tab, dst, bd, blk) for blk in range(NBLK) for (tab, dst, bd) in ((u_tab, uv_bf[:, 0:D], b_ubf), (v_tab, uv_bf[:, D:2 * D], b_vbf))]
        pc_state = [0, 0]

        def _pc_store(k):
            tab, dst, bd, blk = pc_jobs[k]
            i = k % NCB
            S.dma('pool', lambda e: e.dma_start(out=dst[blk * 128:(blk + 1) * 128, :].rearrange("(p r) d -> p r d", r=1), in_=cb[i][:]),
                  bd, reads=[b_cb[i]], writes=[bd])

        def precast(nblocks):
            for _ in range(nblocks):
                k = pc_state[0]
                if k >= len(pc_jobs):
                    break
                pc_state[0] += 1
                tab, dst, bd, blk = pc_jobs[k]
                i = k % NCB
                S.dma('pool', lambda e, tab=tab, blk=blk, i=i: e.dma_start(out=cb[i][:], in_=tab[blk * 128:(blk + 1) * 128, :].rearrange("(p r) d -> p r d", r=1)),
                      b_cb[i], writes=[b_cb[i]])
                if k - 2 >= 0:
                    _pc_store(k - 2)
                    pc_state[1] = k - 1
            if pc_state[0] >= len(pc_jobs):
                while pc_state[1] < len(pc_jobs):
                    _pc_store(pc_state[1])
                    pc_state[1] += 1

        wjobs = [(w_in if blk < 13 else w_q, (blk if blk < 13 else blk - 13) * 512, blk, cc) for blk in range(NWB) for cc in range(4)]

        def _w_store(k):
            src, c0, blk, cc = wjobs[k]
            i = k % NCB
            dstv = wsc[blk * P:(blk + 1) * P, :].rearrange("p (c n) -> p c n", c=8)[:, 2 * cc:2 * cc + 2, :]
            S.dma('pool', lambda e: e.dma_start(out=dstv, in_=cb[i][:, 0, :].rearrange("p (c n) -> p c n", c=2)), b_wsc, reads=[b_cb[i]], writes=[b_wsc])

        for k, (src, c0, blk, cc) in enumerate(wjobs):
            i = k % NCB
            S.dma('pool', lambda e, src=src, c0=c0, cc=cc, i=i: e.dma_start(out=cb[i][:, 0, :].rearrange("p (c n) -> p c n", c=2),
                                                                          in_=wview(src, c0, 512)[:, 2 * cc:2 * cc + 2, :]),
                  b_cb[i], writes=[b_cb[i]])
            if k - 2 >= 0:
                _w_store(k - 2)
        _w_store(len(wjobs) - 2); _w_store(len(wjobs) - 1)
        pc_per_tile = -(-len(pc_jobs) // max(NPRE, 1))
        if NPRE > 0:
            wk = es0.enter_context(nc.sbuf_tensor("wk_pre", [P, 8, 512], BF16)); b_wk = S.buf("wk_pre", dma=True)
            wv = es0.enter_context(nc.sbuf_tensor("wv_pre", [P, 8, 1024], BF16)); b_wv = S.buf("wv_pre", dma=True)
            csp = es0.enter_context(nc.sbuf_tensor("csp", [P, NPRE, 32], F32)); b_csp = S.buf("csp", dma=True)
            snp = es0.enter_context(nc.sbuf_tensor("snp", [P, NPRE, 32], F32)); b_snp = S.buf("snp", dma=True)
            ksp = es0.enter_context(nc.sbuf_tensor("ksp", [P, NPRE, H], F32)); b_ksp = S.buf("ksp", dma=True)
            S.dma('pool', lambda e: e.dma_start(out=wk[:], in_=wview(w_in, 512, 512)), b_wk, writes=[b_wk])
            for hh in range(2):
                S.dma('pool', lambda e, hh=hh: e.dma_start(out=wv[:, :, hh * 512:(hh + 1) * 512], in_=wview(w_in, 1024 + hh * 512, 512)),
                      b_wv, writes=[b_wv])
            S.dma('sp', lambda e: e.dma_start(out=csp[:], in_=cos_pre), b_csp, writes=[b_csp])
            S.dma('sp', lambda e: e.dma_start(out=snp[:], in_=sin_pre), b_snp, writes=[b_snp])
            S.dma('sp', lambda e: e.dma_start(out=ksp[:], in_=ksc_pre), b_ksp, writes=[b_ksp])
            B0 = 4
            def _al(name, shape, dt):
                return es0.enter_context(nc.sbuf_tensor(name, list(shape), dt))
            xnb = _al("xnb", [P, 2, D], BF16); xnTb = _al("xnTb", [P, 2, 8, P], BF16); st4b = _al("st4b", [P, B0, 4], F32)
            b_xnb = [S.buf("xnb%d" % (i % 2)) for i in range(2)] * 2; b_xnTb = [S.buf("xnTb%d" % (i % 2)) for i in range(2)] * 2; b_st4b = [S.buf("st4b%d" % i) for i in range(B0)]
            sets = []
            for si in range(2):
                d_ = dict(kf=TPbig[:, 2 + 4 * si:4 + 4 * si, :].rearrange("p a d -> p (a d)").rearrange("p (b n) -> p b n", b=B0),
                          sq=TPbig[:, 4 + 4 * si:6 + 4 * si, :].rearrange("p a d -> p (a d)").rearrange("p (b n) -> p b n", b=B0),
                          s8=_al("s8b%d" % si, [P, 4, B0 * H], F32),
                          r1=_al("r1b%d" % si, [P, B0 * 256], F32),
                          kd=_al("kdb%d" % si, [P, B0, H, 64], BF16), v=_al("vb%d" % si, [P, B0, D], BF16))
                d_.update(b_kf=S.buf("kfb%d" % si), b_sq=S.buf("sqb%d" % si), b_s8=S.buf("s8b%d" % si), b_r0=S.buf("r0b%d" % si),
                          b_r1=S.buf("r1b%d" % si), b_kd=S.buf("kdb%d" % si), b_v=S.buf("vb%d" % si))
                d_["r0"] = d_["kf"].rearrange("p b n -> p (b n)")[:, 0:B0 * 256]; d_["b_r0"] = d_["b_kf"]
                sets.append(d_)
            all_pre_bufs = b_xnb + b_xnTb + b_st4b + [sets[i][k] for i in range(2) for k in ("b_kf", "b_sq", "b_s8", "b_r0", "b_r1", "b_kd", "b_v")]

            def front_a(m, b, st):
                xb = xt[m % 2]; bx = b_xt[m % 2]
                S.dma('sp', lambda e: e.dma_start(out=xb[:], in_=x_pre[m * P:(m + 1) * P, :]), bx, writes=[bx])
                S.op('act', lambda e: e.activation(out=junk[:], in_=xb[:], func=AF.Square, accum_out=st4b[:, b, 0:1]), reads=[bx], writes=[b_junk, b_st4b[b]])
                S.op('act', lambda e: e.activation(out=st4b[:, b, 1:2], in_=st4b[:, b, 0:1], func=AF.Sqrt, bias=eps_t[:, 0:1], scale=1.0 / D),
                     reads=[b_st4b[b], b_eps], writes=[b_st4b[b]])
                S.op('dve', lambda e: e.reciprocal(out=st4b[:, b, 2:3], in_=st4b[:, b, 1:2]), reads=[b_st4b[b]], writes=[b_st4b[b]])
                S.op('dve', lambda e: e.scalar_tensor_tensor(out=xnb[:, b % 2, :], in0=xb[:], scalar=st4b[:, b, 2:3], in1=gattn[:], op0=ALU.mult, op1=ALU.mult),
                     reads=[bx, b_st4b[b], b_gattn], writes=[b_xnb[b]])

            def front_b(m, b, st):
                precast(pc_per_tile)
                for half in range(2):
                    for k in range(4):
                        c = half * 4 + k
                        S.op('pe', lambda e, c=c, k=k: e.transpose(out=psT[:, k * P:(k + 1) * P], in_=xnb[:, b % 2, c * P:(c + 1) * P], identity=ident[:]),
                             reads=[b_xnb[b], b_ident], writes=[bT])
                    dst = xnTb[:, b % 2, half * 4:half * 4 + 4, :].rearrange("p c t -> p (c t)")
                    if half == 0:
                        S.op('act', lambda e, dst=dst: e.activation(out=dst, in_=psT[:, 0:512], func=AF.Copy), reads=[bT], writes=[b_xnTb[b]])
                    else:
                        S.op('dve', lambda e, dst=dst: e.tensor_copy(out=dst, in_=psT[:, 0:512]), reads=[bT], writes=[b_xnTb[b]])
                for c in range(8):
                    S.op('pe', lambda e, c=c: e.matmul(out=psC[:], lhsT=xnTb[:, b % 2, c, :], rhs=wk[:, c, :], start=(c == 0), stop=(c == 7)),
                         reads=[b_xnTb[b], b_wk], writes=[bC])
                S.op('act', lambda e: e.activation(out=st["kf"][:, b, :], in_=psC[:], func=AF.Copy), reads=[bC], writes=[st["b_kf"]])
                S.op('act', lambda e: e.activation(out=st["sq"][:, b, :], in_=psC[:], func=AF.Square), reads=[bC], writes=[st["b_sq"]])
                psV, bV = (psA, bA) if m % 2 == 0 else (psB, bB)
                for hh in range(2):
                    for c in range(8):
                        S.op('pe', lambda e, c=c, hh=hh: e.matmul(out=psV[:, hh * 512:(hh + 1) * 512], lhsT=xnTb[:, b % 2, c, :], rhs=wv[:, c, hh * 512:(hh + 1) * 512],
                                                                  start=(c == 0), stop=(c == 7)), reads=[b_xnTb[b], b_wv], writes=[bV])
                S.op('act', lambda e: e.activation(out=st["v"][:, b, :], in_=psV[:], func=AF.Copy), reads=[bV], writes=[st["b_v"]])

            def chain_gen(m0, nb, st):
                n8 = nb * H
                kf, sq, s8, r0, r1, kdb = st["kf"], st["sq"], st["s8"], st["r0"], st["r1"], st["kd"]
                S.op('dve', lambda e: e.tensor_reduce(out=s8[:, 0, 0:n8], in_=sq[:, 0:nb, :].rearrange("p b (h d) -> p (b h) d", d=64), axis=AX.X, op=ALU.add),
                     reads=[st["b_sq"]], writes=[st["b_s8"]]); yield
                S.op('act', lambda e: e.activation(out=s8[:, 1, 0:n8], in_=s8[:, 0, 0:n8], func=AF.Sqrt, bias=eps_t[:, 0:1], scale=1.0 / 64),
                     reads=[st["b_s8"], b_eps], writes=[st["b_s8"]]); yield
                S.op('dve', lambda e: e.reciprocal(out=s8[:, 2, 0:n8], in_=s8[:, 1, 0:n8]), reads=[st["b_s8"]], writes=[st["b_s8"]]); yield
                S.op('dve', lambda e: e.tensor_tensor(out=s8[:, 3, 0:n8], in0=s8[:, 2, 0:n8], in1=ksp[:, m0:m0 + nb, :].rearrange("p m h -> p (m h)"), op=ALU.mult),
                     reads=[st["b_s8"], b_ksp], writes=[st["b_s8"]]); yield
                q3 = sq[:, 0:nb, :].rearrange("p b (h d) -> p (b h) d", d=64)
                S.op('dve', lambda e: e.tensor_tensor(out=q3, in0=kf[:, 0:nb, :].rearrange("p b (h d) -> p (b h) d", d=64),
                                                      in1=s8[:, 3, 0:n8].unsqueeze(2).to_broadcast([P, n8, 64]), op=ALU.mult),
                     reads=[st["b_kf"], st["b_s8"]], writes=[st["b_sq"]]); yield
                S.op('pool', lambda e: e.tensor_tensor(out=sq[:, 0:nb, :], in0=sq[:, 0:nb, :], in1=grk[:].unsqueeze(1).to_broadcast([P, nb, 512]), op=ALU.mult),
                     reads=[st["b_sq"], b_grk], writes=[st["b_sq"]]); yield
                q4 = sq[:, 0:nb, :].rearrange("p b (h d) -> p b h d", d=64)
                x1_ = q4[:, :, :, 0:32]; x2_ = q4[:, :, :, 32:64]
                cb = csp[:, m0:m0 + nb, :].unsqueeze(2).to_broadcast([P, nb, H, 32]); sb_ = snp[:, m0:m0 + nb, :].unsqueeze(2).to_broadcast([P, nb, H, 32])
                r0v = r0[:, 0:nb * 256].rearrange("p (b h d) -> p b h d", b=nb, h=H); r1v = r1[:, 0:nb * 256].rearrange("p (b h d) -> p b h d", b=nb, h=H)
                S.op('dve', lambda e: e.tensor_tensor(out=r0v, in0=x1_, in1=cb, op=ALU.mult), reads=[st["b_sq"], b_csp], writes=[st["b_r0"]]); yield
                S.op('pool', lambda e: e.tensor_tensor(out=r1v, in0=x2_, in1=sb_, op=ALU.mult), reads=[st["b_sq"], b_snp], writes=[st["b_r1"]]); yield
                S.op('dve', lambda e: e.tensor_tensor(out=kdb[:, 0:nb, :, 0:32], in0=r0v, in1=r1v, op=ALU.subtract), reads=[st["b_r0"], st["b_r1"]], writes=[st["b_kd"]]); yield
                S.op('dve', lambda e: e.tensor_tensor(out=r0v, in0=x1_, in1=sb_, op=ALU.mult), reads=[st["b_sq"], b_snp], writes=[st["b_r0"]]); yield
                S.op('pool', lambda e: e.tensor_tensor(out=r1v, in0=x2_, in1=cb, op=ALU.mult), reads=[st["b_sq"], b_csp], writes=[st["b_r1"]]); yield
                S.op('dve', lambda e: e.tensor_tensor(out=kdb[:, 0:nb, :, 32:64], in0=r0v, in1=r1v, op=ALU.add), reads=[st["b_r0"], st["b_r1"]], writes=[st["b_kd"]]); yield

            def state_mm(m0, nb, st):
                for b in range(nb):
                    m = m0 + b
                    for h in range(H):
                        acc_ps, b_acc_ps = (psS, bS) if h < 4 else (psD, bD)
                        S.op('pe', lambda e, h=h, m=m, b=b, acc_ps=acc_ps: e.matmul(
                            out=acc_ps[0:64, (h % 4) * P:(h % 4 + 1) * P], lhsT=st["kd"][:, b, h, :], rhs=st["v"][:, b, h * P:(h + 1) * P],
                            start=(m == 0 and h % 4 == 0), stop=(m == NPRE - 1), skip_group_check=True),
                            reads=[st["b_kd"], st["b_v"]], writes=[b_acc_ps])

            batches = [(m0, min(B0, NPRE - m0)) for m0 in range(0, NPRE, B0)]
            tiles = [(m0 + b, b, sets[bi % 2]) for bi, (m0, nb) in enumerate(batches) for b in range(nb)]
            pend = None
            front_a(*tiles[0])
            ti_ = 0
            for bi, (m0, nb) in enumerate(batches):
                st = sets[bi % 2]
                gen = chain_gen(*pend) if pend is not None else None
                for b in range(nb):
                    if ti_ + 1 < len(tiles):
                        front_a(*tiles[ti_ + 1])
                    front_b(m0 + b, b, st)
                    ti_ += 1
                    if gen is not None:
                        for _ in range(4):
                            next(gen, None)
                if gen is not None:
                    for _ in gen:
                        pass
                    state_mm(*pend)
                pend = (m0, nb, st)
            for _ in chain_gen(*pend):
                pass
            state_mm(*pend)
            for e_ in ('pe', 'act', 'dve', 'pool', 'sp'):
                S.wait_all(e_, all_pre_bufs)
            S.op('act', lambda e: e.activation(out=state[:, 0:512], in_=psS[0:64, :], func=AF.Copy), reads=[bS], writes=[b_state])
            S.op('act', lambda e: e.activation(out=state[:, 512:1024], in_=psD[0:64, :], func=AF.Copy), reads=[bD], writes=[b_state])
        else:
            S.op('dve', lambda e: e.memset(state[:], 0.0), writes=[b_state])
        S.op('act', lambda e: e.activation(out=state_bf[:], in_=state[:], func=AF.Copy), reads=[b_state], writes=[b_state_bf])
        precast(len(pc_jobs))
        for e_ in ('pe', 'act', 'dve', 'pool', 'sp'):
            S.wait_all(e_, b_cin + b_cout + [b_ubf, b_wsc])
        if NPRE > 0:
            for e_ in ('pe', 'act', 'dve', 'pool'):
                S.wait_all(e_, [b_wk, b_wv, b_csp, b_snp, b_ksp])

    dump(1, state[:], [b_state], D)
    xn = sb("xn", [P, D], BF16); b_xn = S.buf("xn")
    xnT = sb("xnT", [P, 8, P], BF16); b_xnT = S.buf("xnT")
    qf = sb("qf", [P, 512]); b_qf = S.buf("qf")
    sq_s = sb("sq_s", [P, 512]); b_sq = S.buf("sq_s")
    st8 = sb("st8", [P, 4, H]); b_st8 = S.buf("st8")
    qn = sb("qn", [P, 512]); b_qn = S.buf("qn")
    rt = sb("rt", [P, 4, 256]); b_rt = S.buf("rt")
    kd = sb("kd", [P, H, 64], BF16); b_kd = S.buf("kd")
    v_r = sb("v_r", [P, D], BF16); b_vr = S.buf("v_r")
    wbra = sb("wbra", [P, 8, D], BF16); b_wbra = S.buf("wbra", dma=True)
    wbrb = sb("wbrb", [P, 4, D], BF16); b_wbrb = S.buf("wbrb", dma=True)
    wout = sb("wout", [P, 8, D], BF16); b_wout = S.buf("wout", dma=True)
    k1 = sb("k1", [P, P], BF16); b_k1 = S.buf("k1", dma=True)
    k2 = sb("k2", [P, P], BF16); b_k2 = S.buf("k2", dma=True)
    for hh in range(2):
        S.dma('pool', lambda e, hh=hh: e.dma_start(out=wbra[:, :, hh * 512:(hh + 1) * 512], in_=wview(w_bra, hh * 512, 512)), b_wbra, writes=[b_wbra])
        S.dma('pool', lambda e, hh=hh: e.dma_start(out=wbrb[:, :, hh * 512:(hh + 1) * 512], in_=wview(w_brb, hh * 512, 512)), b_wbrb, writes=[b_wbrb])
        S.dma('pool', lambda e, hh=hh: e.dma_start(out=wout[:, :, hh * 512:(hh + 1) * 512], in_=wview(w_out, hh * 512, 512)), b_wout, writes=[b_wout])
    S.dma('pool', lambda e: e.dma_start(out=k1[:], in_=k1T), b_k1, writes=[b_k1])
    S.dma('pool', lambda e: e.dma_start(out=k2[:], in_=k2T), b_k2, writes=[b_k2])

    wbuf = [sb("wbuf%d" % i, [P, 8, 512], BF16) for i in range(2)]
    b_wbuf = [S.buf("wbuf%d" % i, dma=True) for i in range(2)]
    wcount = [0]

    def stream_w(c0, src=None):
        blk = c0 // 512 if src is None else 13 + c0 // 512
        i = wcount[0] % 2
        wcount[0] += 1
        S.dma('sp', lambda e: e.dma_start(out=wbuf[i][:], in_=wsc[blk * P:(blk + 1) * P, :].rearrange("p (c n) -> p c n", c=8)),
              b_wbuf[i], reads=[b_wsc], writes=[b_wbuf[i]])
        return wbuf[i], b_wbuf[i]

    xres = sb("xres", [P, D]); b_xres = S.buf("xres", dma=True)
    qd = sb("qd", [P, H, 64], BF16); b_qd = S.buf("qd")
    qdT = sb("qdT", [64, H, P], BF16); b_qdT = S.buf("qdT")
    kdT = sb("kdT", [64, H, P], BF16); b_kdT = S.buf("kdT")
    rg = sb("rg", [P, D], BF16); b_rg = S.buf("rg")
    sqn = sb("sqn", [P, H, 64], BF16); b_sqn = S.buf("sqn")
    skn = sb("skn", [P, H, 64], BF16); b_skn = S.buf("skn")
    sqT = sb("sqT", [64, H, P], BF16); b_sqT = S.buf("sqT")
    skT = [sb("skT%d" % i, [64, H, P], BF16) for i in range(2)]; b_skT = [S.buf("skT%d" % i) for i in range(2)]
    sv = [sb("sv%d" % i, [P, 512], BF16) for i in range(2)]; b_sv = [S.buf("sv%d" % i) for i in range(2)]
    siga = TP[7]; b_siga = b_TP[7]
    sigb = TP[8]; b_sigb = b_TP[8]
    pm = sb("pm", [P, D], BF16); b_pm = S.buf("pm")
    yf = TP[3]; b_yf = b_TP[3]
    ysq = TP[4]; b_ysq = b_TP[4]
    gst = sb("gst", [P, 6, H]); b_gst = S.buf("gst")
    ret = sb("ret", [P, D], BF16); b_ret = S.buf("ret")
    retT = sb("retT", [P, 8, P], BF16); b_retT = S.buf("retT")
    e_s = TP[3]; b_es = b_TP[3]
    sp_s = TP[4]; b_sp = b_TP[4]
    spm = sb("spm", [P, D], BF16); b_spm = S.buf("spm")
    u_s = TP[0]; b_us = b_TP[0]
    w_s = sb("w_s", [P, D], BF16); b_ws = S.buf("w_s")
    sbT = sb("sbT", [P, 4, P], BF16); b_sbT = S.buf("sbT")
    m1 = TP[5]; b_m1 = b_TP[5]
    m2 = TP[6]; b_m2 = b_TP[6]
    mixed = ret; b_mixed = b_ret
    mixT = retT; b_mixT = b_retT
    x1 = TP[1]; b_x1 = b_TP[1]
    hn = TP[9]; b_hn = b_TP[9]
    qT = sb("qT", [P, 16, P], BF16); b_qT = S.buf("qT")
    sc = TPbig[:, 3:5, :].rearrange("p a d -> p (a d)").rearrange("p (g n) -> p g n", g=16); b_sc = [b_TP[3], b_TP[4]]
    sc2 = TPbig[:, 5:7, :].rearrange("p a d -> p (a d)").rearrange("p (g n) -> p g n", g=16); b_sc2 = [b_TP[5], b_TP[6]]
    tv = sb("tv", [P, 16, 16]); b_tv = S.buf("tv")
    ti = sb("ti", [P, 16, 16], U32); b_ti = S.buf("ti")
    tif = sb("tif", [P, 16, 16]); b_tif = S.buf("tif")
    cand = TPbig[:, 7:9, :].rearrange("p a d -> p (a d)").rearrange("p (h c) -> p h c", h=H); b_cand = [b_TP[7], b_TP[8]]
    cand2 = sc.rearrange("p g n -> p (g n)").rearrange("p (h c) -> p h c", h=H); b_cand2 = b_sc
    tsv = sb("tsv", [P, H, 16]); b_tsv = S.buf("tsv")
    tpos = sb("tpos", [P, H, 16], U32); b_tpos = S.buf("tpos")
    tposf = sb("tposf", [P, H, 16]); b_tposf = S.buf("tposf")
    ta = sb("ta", [P, H, 16]); b_ta = S.buf("ta")
    tb = sb("tb", [P, H, 16]); b_tb = S.buf("tb")
    iota16 = sb("iota16", [P, 16]); b_iota = S.buf("iota16")
    oh = sc2.rearrange("p g n -> p (g n)").rearrange("p (h a b) -> p h a b", h=H, a=16); b_oh = b_sc2
    idx1 = sb("idx1", [P, H, 16]); b_idx1 = S.buf("idx1")
    idx2 = sb("idx2", [P, H, 16]); b_idx2 = S.buf("idx2")
    eidx = sb("eidx", [P, 128], U32); b_eidx = S.buf("eidx")
    gw = sb("gw", [P, H, 16]); b_gw = S.buf("gw")
    gs = sb("gs", [P, 2, H]); b_gs = S.buf("gs")
    hv = sb("hv", [P, 128]); b_hv = S.buf("hv")
    ga_ = sb("ga_", [P, 6, 128]); b_ga = S.buf("ga_"); b_ga2 = S.buf("ga2"); b_ga3 = S.buf("ga3")
    aw = sb("aw", [P, 128]); b_aw = S.buf("aw")
    NG = 8
    dg = [sb("dg%d" % i, [P, P], BF16) for i in range(4)]; b_dg = [S.buf("dg%d" % i) for i in range(4)]
    gbuf = None; b_gbuf = None
    acc = TP[0]; b_acc = b_TP[0]
    b_yout = S.buf("yout", dma=True)

    _gi = [3, 4, 5, 6, 7, 8, 2, 0]
    gbuf = [TPbig[:, i, :].bitcast(BF16) for i in _gi]
    b_gbuf = [b_TP[i] for i in _gi]
    gsem = [S.buf("gsem%d" % i, dma=True) for i in range(len(_gi))]
    S.op('pool', lambda e: e.iota(iota16[:], pattern=[[1, 16]], base=0, channel_multiplier=0, allow_small_or_imprecise_dtypes=True),
         writes=[b_iota])
    thr16 = sb("thr16", [P, 16])
    S.op('pool', lambda e: e.iota(thr16[:], pattern=[[16, 16]], base=0, channel_multiplier=0, allow_small_or_imprecise_dtypes=True),
         reads=[b_iota], writes=[b_iota])

    def sb_kv(cur):
        wb, bw = stream_w(3584)
        proj512(wb, bw, psC[:], bC)
        qknorm(psC[:], bC, gsk, b_gsk, 0, out_bf=skn, b_out=b_skn)
        transposeH(skn, b_skn, skT[cur], b_skT[cur])
        wb, bw = stream_w(4096)
        proj512(wb, bw, psD[:], bD)
        S.op('act', lambda e: e.activation(out=sv[cur][:], in_=psD[:], func=AF.Copy), reads=[bD], writes=[b_sv[cur]])

    S.dma('sp', lambda e: e.dma_start(out=xt[0][:], in_=x_halo), b_xt[0], writes=[b_xt[0]])
    rmsnorm_T(xt[0][:], b_xt[0], gattn, b_gattn)
    sb_kv(1)
    dump(2, xt[0][:], [b_xt[0], b_sv[1], b_skT[1]])

    def m_head(n):
        cur = n % 2
        S.dma('sp', lambda e, n=n: e.dma_start(out=xres[:], in_=x_own[n * P:(n + 1) * P, :]), b_xres, writes=[b_xres])
        rmsnorm_T(xres[:], b_xres, gattn, b_gattn)
        wb, bw = stream_w(0)
        proj512(wb, bw, psC[:], bC)
        qknorm(psC[:], bC, grq, b_grq, 0, scale_ap=qdec[:], b_scale=b_qdec, cos=cso[:, n, :], sin=sno[:, n, :],
               b_cs=[b_cso, b_sno], out_bf=qd, b_out=b_qd)
        transposeH(qd, b_qd, qdT, b_qdT)
        wb, bw = stream_w(512)
        proj512(wb, bw, psD[:], bD)
        qknorm(psD[:], bD, grk, b_grk, 0, scale_ap=kdec[:], b_scale=b_kdec, cos=cso[:, n, :], sin=sno[:, n, :],
               b_cs=[b_cso, b_sno], out_bf=kd, b_out=b_kd)
        transposeH(kd, b_kd, kdT, b_kdT)
        wb, bw = stream_w(3072)
        proj512(wb, bw, psC[:], bC)
        qknorm(psC[:], bC, gsq, b_gsq, 0, out_bf=sqn, b_out=b_sqn)
        transposeH(sqn, b_sqn, sqT, b_sqT)
        sb_kv(cur)
        for hh in range(2):
            wb, bw = stream_w(1024 + hh * 512)
            proj512(wb, bw, psB[:, hh * 512:(hh + 1) * 512], bB)
        S.op('act', lambda e: e.activation(out=v_r[:], in_=psB[:], func=AF.Copy), reads=[bB], writes=[b_vr])

    m_head(0)
    for n in range(NT):
        cur = n % 2; prv = 1 - cur
        for hh in range(2):
            wb, bw = stream_w(2048 + hh * 512)
            proj512(wb, bw, psB[:, hh * 512:(hh + 1) * 512], bB)
        S.op('act', lambda e: e.activation(out=m1[:], in_=psB[:], func=AF.Silu), reads=[bB], writes=[b_m1])
        S.op('pool', lambda e: e.tensor_tensor(out=rg[:], in0=m1[:], in1=ggn[:], op=ALU.mult), reads=[b_m1, b_ggn], writes=[b_rg])
        for h in range(H):
            S.op('pe', lambda e, h=h: e.matmul(out=psA[:, h * P:(h + 1) * P], lhsT=kdT[:, h, :], rhs=qdT[:, h, :], start=True, stop=True),
                 reads=[b_kdT, b_qdT], writes=[bA])
        S.op('dve', lambda e: e.tensor_tensor(out=pm[:], in0=psA[:], in1=maskT[:], op=ALU.mult), reads=[bA, b_maskT], writes=[b_pm])
        for h in range(H):
            S.op('pe', lambda e, h=h: e.matmul(out=psB[:, h * P:(h + 1) * P], lhsT=pm[:, h * P:(h + 1) * P], rhs=v_r[:, h * P:(h + 1) * P],
                                               start=True, stop=False, skip_group_check=True),
                 reads=[b_pm, b_vr], writes=[bB])
            S.op('pe', lambda e, h=h: e.matmul(out=psB[:, h * P:(h + 1) * P], lhsT=qdT[:, h, :], rhs=state_bf[:, h * P:(h + 1) * P],
                                               start=False, stop=True, skip_group_check=True),
                 reads=[b_qdT, b_state_bf], writes=[bB])
        S.op('dve', lambda e: e.tensor_tensor(out=state[:], in0=state[:], in1=cdec[:], op=ALU.mult), reads=[b_state, b_cdec], writes=[b_state])
        for rnd in range(2):
            for hl in range(4):
                h = rnd * 4 + hl
                S.op('pe', lambda e, h=h, hl=hl: e.matmul(out=psS[0:64, hl * P:(hl + 1) * P], lhsT=kd[:, h, :], rhs=v_r[:, h * P:(h + 1) * P],
                                                   start=True, stop=True, skip_group_check=True),
                     reads=[b_kd, b_vr], writes=[bS])
            S.op('dve', lambda e, rnd=rnd: e.tensor_tensor(out=state[:, rnd * 512:(rnd + 1) * 512], in0=state[:, rnd * 512:(rnd + 1) * 512],
                                                         in1=psS[0:64, :], op=ALU.add), reads=[b_state, bS], writes=[b_state])
        S.op('act', lambda e: e.activation(out=state_bf[:], in_=state[:], func=AF.Copy), reads=[b_state], writes=[b_state_bf])
        S.op('act', lambda e: e.activation(out=yf[:], in_=psB[:], func=AF.Copy), reads=[bB], writes=[b_yf])
        S.op('act', lambda e: e.activation(out=ysq[:], in_=psB[:], func=AF.Square), reads=[bB], writes=[b_ysq])
        S.op('dve', lambda e: e.tensor_reduce(out=gst[:, 0, :], in_=yf[:].rearrange("p (h d) -> p h d", d=P), axis=AX.X, op=ALU.add),
             reads=[b_yf], writes=[b_gst])
        S.op('dve', lambda e: e.tensor_reduce(out=gst[:, 1, :], in_=ysq[:].rearrange("p (h d) -> p h d", d=P), axis=AX.X, op=ALU.add),
             reads=[b_ysq], writes=[b_gst])
        S.op('dve', lambda e: e.tensor_scalar(out=gst[:, 2, :], in0=gst[:, 0, :], scalar1=1.0 / P, scalar2=None, op0=ALU.mult), reads=[b_gst], writes=[b_gst])
        S.op('dve', lambda e: e.tensor_tensor(out=gst[:, 3, :], in0=gst[:, 2, :], in1=gst[:, 2, :], op=ALU.mult), reads=[b_gst], writes=[b_gst])
        S.op('dve', lambda e: e.scalar_tensor_tensor(out=gst[:, 4, :], in0=gst[:, 1, :], scalar=1.0 / P, in1=gst[:, 3, :], op0=ALU.mult, op1=ALU.subtract),
             reads=[b_gst], writes=[b_gst])
        S.op('act', lambda e: e.activation(out=gst[:, 5, :], in_=gst[:, 4, :], func=AF.Sqrt, bias=eps_t[:, 0:1], scale=1.0), reads=[b_gst, b_eps], writes=[b_gst])
        S.op('dve', lambda e: e.reciprocal(out=gst[:, 3, :], in_=gst[:, 5, :]), reads=[b_gst], writes=[b_gst])
        y3 = yf[:].rearrange("p (h d) -> p h d", d=P)
        S.op('dve', lambda e: e.tensor_tensor(out=y3, in0=y3, in1=gst[:, 2, :].unsqueeze(2).to_broadcast([P, H, P]), op=ALU.subtract),
             reads=[b_yf, b_gst], writes=[b_yf])
        S.op('dve', lambda e: e.tensor_tensor(out=y3, in0=y3, in1=gst[:, 3, :].unsqueeze(2).to_broadcast([P, H, P]), op=ALU.mult),
             reads=[b_yf, b_gst], writes=[b_yf])
        S.op('pool', lambda e: e.tensor_tensor(out=ret[:], in0=yf[:], in1=rg[:], op=ALU.mult), reads=[b_yf, b_rg], writes=[b_ret])
        transpose8(ret, b_ret, retT, b_retT)
        dump(3, yf[:], [b_yf, b_retT])
        for half in range(2):
            for blk, kT_ in ((0, skT[prv]), (1, skT[cur])):
                for hl in range(4):
                    h = half * 4 + hl
                    S.op('pe', lambda e, blk=blk, hl=hl, h=h, kT_=kT_: e.matmul(
                        out=psA[:, blk * 512 + hl * P: blk * 512 + (hl + 1) * P], lhsT=kT_[:, h, :],
                        rhs=sqT[:, h, :], start=True, stop=True),
                        reads=[b_skT[prv], b_skT[cur], b_sqT], writes=[bA])
            S.op('act', lambda e: e.activation(out=e_s[:], in_=psA[:], func=AF.Exp, scale=0.125), reads=[bA], writes=[b_es])
            S.op('act', lambda e: e.activation(out=sp_s[:], in_=e_s[:], func=AF.Ln, bias=one_t[:, 0:1], scale=1.0), reads=[b_es, b_one], writes=[b_sp])
            S.op('dve', lambda e: e.tensor_tensor(
                out=spm[:].rearrange("p (b h t) -> p b h t", b=2, h=4), in0=sp_s[:].rearrange("p (b h t) -> p b h t", b=2, h=4),
                in1=mstay[:].rearrange("p (b h t) -> p b h t", b=2, h=4), op=ALU.mult), reads=[b_sp, b_mstay], writes=[b_spm])
            S.op('pe', lambda e: e.matmul(out=psB[:, 0:512], lhsT=tri[:], rhs=spm[:, 0:512], start=True, stop=False), reads=[b_tri, b_spm], writes=[bB])
            S.op('pe', lambda e: e.matmul(out=psB[:, 0:512], lhsT=ones[:], rhs=spm[:, 512:1024], start=False, stop=True), reads=[b_ones, b_spm], writes=[bB])
            S.op('pe', lambda e: e.matmul(out=psB[:, 512:1024], lhsT=tri[:], rhs=spm[:, 512:1024], start=True, stop=False), reads=[b_tri, b_spm], writes=[bB])
            S.op('pe', lambda e: e.matmul(out=psB[:, 512:1024], lhsT=ident[:], rhs=mpos[:], start=False, stop=True), reads=[b_ident, b_mpos], writes=[bB])
            S.op('dve', lambda e: e.scalar_tensor_tensor(out=u_s[:], in0=psA[:], scalar=0.125, in1=sp_s[:], op0=ALU.mult, op1=ALU.subtract),
                 reads=[bA, b_sp], writes=[b_us])
            S.op('dve', lambda e: e.tensor_tensor(out=u_s[:], in0=u_s[:], in1=psB[:], op=ALU.subtract), reads=[b_us, bB], writes=[b_us])
            S.op('act', lambda e: e.activation(out=w_s[:], in_=u_s[:], func=AF.Exp), reads=[b_us], writes=[b_ws])
            for hl in range(4):
                h = half * 4 + hl
                po = (h % 2) * 64
                for blk, svb, bsv in ((0, sv[prv], b_sv[prv]), (1, sv[cur], b_sv[cur])):
                    S.op('pe', lambda e, h=h, hl=hl, po=po, blk=blk, svb=svb: e.matmul(
                        out=psS[po:po + 64, (h // 2) * P:(h // 2 + 1) * P], lhsT=svb[:, h * 64:(h + 1) * 64],
                        rhs=w_s[:, blk * 512 + hl * P: blk * 512 + (hl + 1) * P], start=(blk == 0), stop=(blk == 1), skip_group_check=True),
                        reads=[bsv, b_ws], writes=[bS])
        S.op('act', lambda e: e.activation(out=sbT[:].rearrange("p c t -> p (c t)"), in_=psS[:], func=AF.Copy), reads=[bS], writes=[b_sbT])
        if STAGE == 4:
            S.op('act', lambda e: e.activation(out=m2[:, 0:512], in_=psS[:], func=AF.Copy), reads=[bS], writes=[b_m2])
            dump(4, m2[:, 0:512], [b_m2], 512)
        for hh in range(2):
            for c in range(8):
                S.op('pe', lambda e, c=c, hh=hh: e.matmul(out=psA[:, hh * 512:(hh + 1) * 512], lhsT=retT[:, c, :], rhs=wbra[:, c, hh * 512:(hh + 1) * 512],
                                                          start=(c == 0), stop=(c == 7)), reads=[b_retT, b_wbra], writes=[bA])
            for c in range(4):
                S.op('pe', lambda e, c=c, hh=hh: e.matmul(out=psB[:, hh * 512:(hh + 1) * 512], lhsT=sbT[:, c, :], rhs=wbrb[:, c, hh * 512:(hh + 1) * 512],
                                                          start=(c == 0), stop=(c == 3)), reads=[b_sbT, b_wbrb], writes=[bB])
        for gi, (gt, bg) in enumerate(((siga, b_siga), (sigb, b_sigb))):
            for hh in range(2):
                wb, bw = stream_w(4608 + gi * 1024 + hh * 512)
                pst, bp = (psC, bC) if hh == 0 else (psD, bD)
                proj512(wb, bw, pst[:], bp)
                S.op('act', lambda e, gt=gt, hh=hh, pst=pst: e.activation(out=gt[:, hh * 512:(hh + 1) * 512], in_=pst[:], func=AF.Sigmoid),
                     reads=[bp], writes=[bg])
        S.op('dve', lambda e: e.tensor_tensor(out=m1[:], in0=psA[:], in1=siga[:], op=ALU.mult), reads=[bA, b_siga], writes=[b_m1])
        S.op('dve', lambda e: e.tensor_tensor(out=m2[:], in0=psB[:], in1=sigb[:], op=ALU.mult), reads=[bB, b_sigb], writes=[b_m2])
        S.op('pool', lambda e: e.tensor_tensor(out=mixed[:], in0=m1[:], in1=m2[:], op=ALU.add), reads=[b_m1, b_m2], writes=[b_mixed])
        transpose8(mixed, b_mixed, mixT, b_mixT)
        for hh in range(2):
            for c in range(8):
                S.op('pe', lambda e, c=c, hh=hh: e.matmul(out=psA[:, hh * 512:(hh + 1) * 512], lhsT=mixT[:, c, :], rhs=wout[:, c, hh * 512:(hh + 1) * 512],
                                                          start=(c == 0), stop=(c == 7)), reads=[b_mixT, b_wout], writes=[bA])
        S.op('dve', lambda e: e.tensor_tensor(out=x1[:], in0=psA[:], in1=xres[:], op=ALU.add), reads=[bA, b_xres], writes=[b_x1])
        dump(5, x1[:], [b_x1])
        rmsnorm_T(x1[:], b_x1, gffn, b_gffn, keep_f32=hn[:], b_keep=b_hn)
        for g4 in range(4):
            wqb, b_wq = stream_w(g4 * 512, w_q)
            for gl in range(4):
                g = g4 * 4 + gl
                pst = psA[:, gl * P:(gl + 1) * P] if g4 % 2 == 0 else psB[:, gl * P:(gl + 1) * P]
                bp = bA if g4 % 2 == 0 else bB
                for c in range(8):
                    S.op('pe', lambda e, c=c, gl=gl, pst=pst, wqb=wqb: e.matmul(out=pst, lhsT=wqb[:, c, gl * P:(gl + 1) * P], rhs=xnT[:, c, :],
                                                                     start=(c == 0), stop=(c == 7)), reads=[b_wq, b_xnT], writes=[bp])
            src = psA if g4 % 2 == 0 else psB
            bp = bA if g4 % 2 == 0 else bB
            S.op('act', lambda e, g4=g4, src=src: e.activation(out=qT[:, g4 * 4:(g4 + 1) * 4, :].rearrange("p g t -> p (g t)"), in_=src[:, 0:512], func=AF.Copy),
                 reads=[bp], writes=[b_qT])
        for g4 in range(4):
            pst, bp = (psA, bA) if g4 % 2 == 0 else (psB, bB)
            for gl in range(4):
                g = g4 * 4 + gl
                kk, bk = (k1, b_k1) if g % 2 == 0 else (k2, b_k2)
                S.op('pe', lambda e, g=g, gl=gl, pst=pst, kk=kk: e.matmul(out=pst[:, gl * P:(gl + 1) * P], lhsT=qT[:, g, :], rhs=kk[:], start=True, stop=True),
                     reads=[b_qT, bk], writes=[bp])
            S.op('act', lambda e, g4=g4, pst=pst: e.activation(out=sc[:, g4 * 4:(g4 + 1) * 4, :].rearrange("p g n -> p (g n)"), in_=pst[:, 0:512], func=AF.Copy),
                 reads=[bp], writes=[b_sc])
        cap = []
        if n + 1 < NT:
            S.cap = cap
            m_head(n + 1)
            S.cap = None
        S.after_op = lambda: S.replay(cap, 1)
        bg_tv = [S.buf("tv%d" % g) for g in range(16)]; bg_ti = [S.buf("ti%d" % g) for g in range(16)]; bg_s2 = [S.buf("s2%d" % g) for g in range(16)]
        for g in range(16):
            S.op('dve', lambda e, g=g: e.max(out=tv[:, g, 0:8], in_=sc[:, g, :]), reads=[b_sc], writes=[bg_tv[g]])
        for g in range(16):
            S.op('dve', lambda e, g=g: e.match_replace(out=sc2[:, g, :], in_to_replace=tv[:, g, 0:8], in_values=sc[:, g, :], imm_value=NEG),
                 reads=[b_sc, bg_tv[g]], writes=[bg_s2[g]])
        for g in range(16):
            S.op('dve', lambda e, g=g: e.max_index(out=ti[:, g, 0:8], in_max=tv[:, g, 0:8], in_values=sc[:, g, :]), reads=[b_sc, bg_tv[g]], writes=[bg_ti[g]])
        for g in range(16):
            S.op('dve', lambda e, g=g: e.max(out=tv[:, g, 8:16], in_=sc2[:, g, :]), reads=[bg_s2[g]], writes=[bg_tv[g]])
        for g in range(16):
            S.op('dve', lambda e, g=g: e.max_index(out=ti[:, g, 8:16], in_max=tv[:, g, 8:16], in_values=sc2[:, g, :]), reads=[bg_s2[g], bg_tv[g]], writes=[bg_ti[g]])
        b_tv.w = None; b_ti.w = None
        S.op('dve', lambda e: e.tensor_copy(out=tif[:], in_=ti[:]), reads=bg_ti + bg_tv + bg_s2 + [b_sc2], writes=[b_tif, b_tv, b_ti, b_sc2])
        tv4 = tv[:].rearrange("p (h s) k -> p h s k", s=2)
        tif4 = tif[:].rearrange("p (h s) k -> p h s k", s=2)
        S.op('dve', lambda e: e.tensor_tensor(out=cand.rearrange("p h (a b) -> p h a b", a=16),
                                              in0=tv4[:, :, 0, :].unsqueeze(3).to_broadcast([P, H, 16, 16]),
                                              in1=tv4[:, :, 1, :].unsqueeze(2).to_broadcast([P, H, 16, 16]), op=ALU.add),
             reads=[b_tv], writes=[b_cand])
        bh_ts = [S.buf("ts%d" % h) for h in range(H)]; bh_tp = [S.buf("tp%d" % h) for h in range(H)]; bh_c2 = [S.buf("c2%d" % h) for h in range(H)]
        for h in range(H):
            S.op('dve', lambda e, h=h: e.max(out=tsv[:, h, 0:8], in_=cand[:, h, :]), reads=[b_cand], writes=[bh_ts[h]])
        for h in range(H):
            S.op('dve', lambda e, h=h: e.match_replace(out=cand2[:, h, :], in_to_replace=tsv[:, h, 0:8], in_values=cand[:, h, :], imm_value=NEG),
                 reads=[b_cand, bh_ts[h], b_cand2], writes=[bh_c2[h]])
        for h in range(H):
            S.op('dve', lambda e, h=h: e.max_index(out=tpos[:, h, 0:8], in_max=tsv[:, h, 0:8], in_values=cand[:, h, :]), reads=[b_cand, bh_ts[h]], writes=[bh_tp[h]])
        for h in range(H):
            S.op('dve', lambda e, h=h: e.max(out=tsv[:, h, 8:16], in_=cand2[:, h, :]), reads=[bh_c2[h]], writes=[bh_ts[h]])
        for h in range(H):
            S.op('dve', lambda e, h=h: e.max_index(out=tpos[:, h, 8:16], in_max=tsv[:, h, 8:16], in_values=cand2[:, h, :]), reads=[bh_c2[h], bh_ts[h]], writes=[bh_tp[h]])
        b_tsv.w = None; b_tpos.w = None
        S.op('dve', lambda e: e.tensor_copy(out=tposf[:], in_=tpos[:]), reads=bh_tp + bh_ts + bh_c2, writes=[b_tposf, b_tsv, b_tpos, b_cand2])
        S.op('dve', lambda e: e.tensor_tensor(out=oh, in0=tposf[:].unsqueeze(3).to_broadcast([P, H, 16, 16]),
                                              in1=thr16[:].unsqueeze(1).unsqueeze(1).to_broadcast([P, H, 16, 16]), op=ALU.is_ge),
             reads=[b_tposf, b_iota], writes=[b_oh])
        S.op('dve', lambda e: e.tensor_reduce(out=ta[:], in_=oh, axis=AX.X, op=ALU.add), reads=[b_oh], writes=[b_ta])
        S.op('dve', lambda e: e.tensor_scalar(out=ta[:], in0=ta[:], scalar1=-1.0, scalar2=None, op0=ALU.add), reads=[b_ta], writes=[b_ta])
        S.op('dve', lambda e: e.scalar_tensor_tensor(out=tb[:], in0=ta[:], scalar=-16.0, in1=tposf[:], op0=ALU.mult, op1=ALU.add),
             reads=[b_ta, b_tposf], writes=[b_tb])
        io_b = iota16[:].unsqueeze(1).unsqueeze(1).to_broadcast([P, H, 16, 16])
        for sel, half, dst, bd in ((ta, 0, idx1, b_idx1), (tb, 1, idx2, b_idx2)):
            bsel = b_ta if half == 0 else b_tb
            S.op('dve', lambda e, sel=sel: e.tensor_tensor(out=oh, in0=sel[:].unsqueeze(3).to_broadcast([P, H, 16, 16]), in1=io_b, op=ALU.is_equal),
                 reads=[bsel, b_iota], writes=[b_oh])
            S.op('dve', lambda e, half=half: e.tensor_tensor(out=oh, in0=oh, in1=tif4[:, :, half, :].unsqueeze(2).to_broadcast([P, H, 16, 16]), op=ALU.mult),
                 reads=[b_oh, b_tif], writes=[b_oh])
            S.op('dve', lambda e, dst=dst: e.tensor_reduce(out=dst[:], in_=oh, axis=AX.X, op=ALU.add), reads=[b_oh], writes=[bd])
        S.op('dve', lambda e: e.scalar_tensor_tensor(out=idx1[:], in0=idx1[:], scalar=128.0, in1=idx2[:], op0=ALU.mult, op1=ALU.add),
             reads=[b_idx1, b_idx2], writes=[b_idx1])
        S.op('dve', lambda e: e.tensor_copy(out=eidx[:], in_=idx1[:].rearrange("p h k -> p (h k)")), reads=[b_idx1], writes=[b_eidx])
        S.op('dve', lambda e: e.tensor_tensor(out=gw[:], in0=tsv[:], in1=tsv[:, :, 0:1].to_broadcast([P, H, 16]), op=ALU.subtract), reads=[b_tsv], writes=[b_gw])
        S.op('act', lambda e: e.activation(out=gw[:], in_=gw[:], func=AF.Exp), reads=[b_gw], writes=[b_gw])
        S.op('dve', lambda e: e.tensor_reduce(out=gs[:, 0, :], in_=gw[:], axis=AX.X, op=ALU.add), reads=[b_gw], writes=[b_gs])
        S.op('dve', lambda e: e.reciprocal(out=gs[:, 1, :], in_=gs[:, 0, :]), reads=[b_gs], writes=[b_gs])
        S.op('dve', lambda e: e.tensor_tensor(out=gw[:], in0=gw[:], in1=gs[:, 1, :].unsqueeze(2).to_broadcast([P, H, 16]), op=ALU.mult), reads=[b_gw, b_gs], writes=[b_gw])
        if STAGE == 6:
            S.op('dve', lambda e: e.tensor_copy(out=m2[:, 0:128], in_=idx1[:].rearrange("p h k -> p (h k)")), reads=[b_idx1], writes=[b_m2])
            S.op('dve', lambda e: e.tensor_copy(out=m2[:, 128:256], in_=gw[:].rearrange("p h k -> p (h k)")), reads=[b_gw], writes=[b_m2])
            dump(6, m2[:, 0:256], [b_m2], 256)
        S.after_op = None
        GS = 2
        NGRP = 128 // GS
        gwf = gw[:].rearrange("p h k -> p (h k)")

        def emit_gather(g):
            for k in range(GS):
                j = g * GS + k
                gb_, bgb = gbuf[j % NG], b_gbuf[j % NG]
                S.dma('pool', lambda e, j=j, gb_=gb_: e.indirect_dma_start(out=gb_[:], out_offset=None, in_=uv_bf,
                                                                          in_offset=bass.IndirectOffsetOnAxis(ap=eidx[:, j:j + 1], axis=0)),
                      gsem[j % NG], reads=[b_eidx, b_ubf], writes=[bgb])

        def emit_dots(g):
            for k in range(GS):
                j = g * GS + k
                gb_, bgb = gbuf[j % NG], b_gbuf[j % NG]
                S.op('dve', lambda e, j=j, gb_=gb_: e.scalar_tensor_tensor(out=junk[:], in0=gb_[:, 0:D], scalar=1.0, in1=hn[:],
                                                                          op0=ALU.mult, op1=ALU.mult, accum_out=hv[:, j:j + 1]),
                     reads=[bgb, b_hn], writes=[b_junk, b_hv])

        def emit_pre(g):
            c = slice(g * GS, (g + 1) * GS)
            S.op('act', lambda e: e.activation(out=ga_[:, 0, c], in_=hv[:, c], func=AF.Square), reads=[b_hv], writes=[b_ga])
            S.op('act', lambda e: e.activation(out=ga_[:, 1, c], in_=ga_[:, 0, c], func=AF.Identity, scale=0.0713548162726, bias=gk_t[:, 0:1]),
                 reads=[b_ga, b_gk], writes=[b_ga])
            for k in range(GS):
                j = g * GS + k
                S.op('act', lambda e, j=j: e.activation(out=ga_[:, 3, j:j + 1], in_=ga_[:, 1, j:j + 1], func=AF.Sigmoid, scale=hv[:, j:j + 1]),
                     reads=[b_ga, b_hv], writes=[b_ga2])
            S.op('dve', lambda e: e.tensor_tensor(out=ga_[:, 4, c], in0=hv[:, c], in1=gwf[:, c], op=ALU.mult), reads=[b_hv, b_gw], writes=[b_ga3])

        def emit_post(g):
            for k in range(GS):
                j = g * GS + k
                S.op('act', lambda e, j=j: e.activation(out=aw[:, j:j + 1], in_=ga_[:, 3, j:j + 1], func=AF.Copy, scale=ga_[:, 4, j:j + 1]),
                     reads=[b_ga2, b_ga3], writes=[b_aw])

        def emit_axpy(g):
            for k in range(GS):
                j = g * GS + k
                gb_, bgb = gbuf[j % NG], b_gbuf[j % NG]
                dgj, bdg = dg[j % 4], b_dg[j % 4]
                S.op('act', lambda e, j=j, dgj=dgj: e.activation(out=dgj[:], in_=identF[:], func=AF.Copy, scale=aw[:, j:j + 1]),
                     reads=[b_identF, b_aw], writes=[bdg])
                for hh in range(2):
                    S.op('pe', lambda e, j=j, hh=hh, dgj=dgj, gb_=gb_: e.matmul(out=psA[:, hh * 512:(hh + 1) * 512], lhsT=dgj[:],
                                                                            rhs=gb_[:, D + hh * 512:D + (hh + 1) * 512],
                                                                            start=(j == 0), stop=(j == 127)), reads=[bdg, bgb], writes=[bA])

        for g0 in range(3):
            emit_gather(g0)
        for st_ in range(NGRP + 1):
            S.replay(cap, 3)
            if 0 <= st_ - 1 < NGRP:
                emit_pre(st_ - 1)
            if st_ < NGRP:
                emit_dots(st_)
            if 0 <= st_ - 1 < NGRP:
                emit_post(st_ - 1)
                emit_axpy(st_ - 1)
            if st_ + 3 < NGRP:
                emit_gather(st_ + 3)
        S.replay(cap)
        S.op('dve', lambda e: e.tensor_tensor(out=acc[:], in0=psA[:], in1=x1[:], op=ALU.add), reads=[bA, b_x1], writes=[b_acc])
        S.dma('sp', lambda e, n=n: e.dma_start(out=y_out[n * P:(n + 1) * P, :], in_=acc[:]), b_yout, reads=[b_acc], writes=[b_yout])

    S.wait_all('sp', [b_yout])
    es.close()
    return nc, None


def _consts():
    hs = np.arange(H, dtype=np.float64)
    gam = 1.0 - 2.0 ** (-5.0 - hs)
    i = np.arange(P, dtype=np.float64)
    c = {}
    c["c_ident"] = np.eye(P, dtype=np.float32)
    c["c_tri"] = (i[:, None] > i[None, :]).astype(np.float32)
    c["c_ones"] = np.ones((P, P), np.float32)
    mp = 1.0e4 * (i[:, None] >= i[None, :]).astype(np.float32)
    c["c_mpos"] = np.tile(mp, (1, 4)).astype(np.float32)
    ms = np.ones((P, 2, 4, P), np.float32)
    ms[:, 1, :, :] = (i[:, None] < i[None, :]).astype(np.float32)[:, None, :]
    c["c_mstay"] = ms.reshape(P, 1024)
    mk = np.zeros((P, H, P), np.float64)
    for h in range(H):
        mk[:, h, :] = (i[None, :] >= i[:, None]) * gam[h] ** (-128.0)
    c["c_maskT"] = mk.reshape(P, 1024).astype(np.float32)
    c["c_qdec"] = (gam[None, :] ** (i[:, None] + 1.0)).astype(np.float32)
    c["c_kdec"] = (0.125 * gam[None, :] ** (127.0 - i[:, None])).astype(np.float32)
    cd = np.zeros((64, D), np.float64)
    for h in range(H):
        cd[:, h * P:(h + 1) * P] = gam[h] ** 128.0
    c["c_cdec"] = cd.astype(np.float32)
    return c, gam


def _rope_tabs(pos):
    half = 32
    freqs = (np.float32(10000.0) ** (-np.arange(half, dtype=np.float32) / np.float32(half))).astype(np.float32)
    ang = (pos.astype(np.float32)[:, :, None] * freqs[None, None, :]).astype(np.float32).astype(np.float64)
    return np.cos(ang).astype(np.float32), np.sin(ang).astype(np.float32)


_CACHE = {}


def kernel(x, norm_attn, w_in, ret_q_norm, ret_k_norm, ret_group_norm, sb_q_norm, sb_k_norm,
           w_branch_ret, w_branch_sb, w_out, norm_ffn, peer_w_q, peer_sub_keys_1,
           peer_sub_keys_2, peer_u, peer_v):
    f = np.float32
    x = np.asarray(x, f)
    B, SEQ, _ = x.shape
    assert B == 1
    NT = SEQ // (NCORES * P)
    NPRE = NT * (NCORES - 1) if FORCE_NPRE is None else FORCE_NPRE
    x2 = x[0]
    key = (NT, NPRE)
    if key not in _CACHE:
        try:
            _CACHE[key] = build_program(NT, NPRE)
        except _Stop:
            _H['es'].close()
            _CACHE[key] = (_H['nc'], None)
    nc, _es = _CACHE[key]
    cst, gam = _consts()
    rep = lambda v, n: np.ascontiguousarray(np.broadcast_to(np.tile(np.asarray(v, f).reshape(-1), n)[None, :], (P, np.asarray(v).size * n)))
    shared = dict(cst)
    shared.update({
        "g_attn": rep(norm_attn[0], 1), "g_ffn": rep(norm_ffn[0], 1), "g_gn": rep(ret_group_norm[0], 1),
        "g_rq": rep(ret_q_norm[0], 8), "g_rk": rep(ret_k_norm[0], 8), "g_sq": rep(sb_q_norm[0], 8), "g_sk": rep(sb_k_norm[0], 8),
        "w_in": np.ascontiguousarray(w_in[0], f), "w_bra": np.ascontiguousarray(w_branch_ret[0], f),
        "w_brb": np.ascontiguousarray(w_branch_sb[0], f), "w_out": np.ascontiguousarray(w_out[0], f),
        "w_q": np.ascontiguousarray(peer_w_q[0], f),
        "k1T": np.ascontiguousarray(np.asarray(peer_sub_keys_1[0], f).T), "k2T": np.ascontiguousarray(np.asarray(peer_sub_keys_2[0], f).T),
        "u_tab": np.ascontiguousarray(peer_u[0], f), "v_tab": np.ascontiguousarray(peer_v[0], f),
    })
    in_maps = []
    pp = np.arange(P, dtype=np.float64)
    for c in range(NCORES):
        t0 = c * NT
        m = dict(shared)
        m["x_own"] = np.ascontiguousarray(x2[t0 * P:(t0 + NT) * P])
        m["x_halo"] = np.ascontiguousarray(x2[(t0 - 1) * P:t0 * P]) if c > 0 else np.zeros((P, D), f)
        npre = max(NPRE, 1)
        xp = np.zeros((npre * P, D), f)
        gt = np.arange(npre) + (t0 - NPRE)
        nvalid = min(t0, NPRE)
        if nvalid > 0:
            xp[(NPRE - nvalid) * P:NPRE * P] = x2[(t0 - nvalid) * P:t0 * P]
        m["x_pre"] = xp
        pos_own = (np.arange(NT)[None, :] + t0) * P + pp[:, None]
        m["cos_own"], m["sin_own"] = _rope_tabs(pos_own)
        pos_pre = np.maximum(gt, 0)[None, :] * P + pp[:, None]
        m["cos_pre"], m["sin_pre"] = _rope_tabs(pos_pre)
        ks = np.zeros((P, npre, H), np.float64)
        for mm in range(npre):
            ks[:, mm, :] = 0.125 * gam[None, :] ** (127.0 - pp[:, None]) * gam[None, :] ** (128.0 * (NPRE - 1 - mm))
        m["ksc_pre"] = ks.astype(f)
        in_maps.append(m)
    res = run_bass_kernel_spmd(nc, in_maps, core_ids=list(range(NCORES)), **RUN_KW)
    _H['res'] = res
    out = np.concatenate([np.asarray(r["y_out"], f) for r in res.results], axis=0)
    return out.reshape(1, SEQ, D)
```

```python
import numpy as np
from contextlib import ExitStack
import concourse.bass as bass
import concourse.mybir as mybir
from concourse.bass_utils import run_bass_kernel_spmd

F32 = mybir.dt.float32
BF16 = mybir.dt.bfloat16
U32 = mybir.dt.uint32
I32 = mybir.dt.int32
AF = mybir.ActivationFunctionType
ALU = mybir.AluOpType
AX = mybir.AxisListType

NCORES = 8
D = 1024
P = 128
H = 8
EPS = 1e-6
NEXP = 16384
INW = 6656
NEG = -1.0e30


class Buf:
    def __init__(self, name):
        self.name = name
        self.w = None
        self.r = {}
        self.dsem = None
        self.dcnt = 0


class Sched:
    def __init__(self, nc, es):
        self.nc = nc
        self.es = es
        self.E = {'pe': nc.tensor, 'act': nc.scalar, 'dve': nc.vector, 'pool': nc.gpsimd, 'sp': nc.sync}
        self.sem = {e: es.enter_context(nc.semaphore('sem_' + e)) for e in self.E}
        self.cnt = {e: 0 for e in self.E}
        self.known = {e: {} for e in self.E}
        self.nsem = 0

    def buf(self, name, dma=False):
        b = Buf(name)
        if dma:
            b.dsem = self.es.enter_context(self.nc.semaphore('d_' + name))
        return b

    def _wait(self, e, ev):
        if ev is None:
            return
        sem, val, src = ev
        if src == e and e == 'pe':
            return
        k = self.known[e]
        if k.get(id(sem), 0) >= val:
            return
        self.E[e].wait_ge(sem, val)
        k[id(sem)] = val

    @staticmethod
    def _flat(bs):
        out = []
        for b in bs:
            if isinstance(b, (list, tuple)):
                out.extend(Sched._flat(b))
            else:
                out.append(b)
        return out

    def _deps(self, e, reads, writes):
        reads = self._flat(reads); writes = self._flat(writes)
        for b in reads:
            self._wait(e, b.w)
        for b in writes:
            self._wait(e, b.w)
            for ev in list(b.r.values()):
                self._wait(e, ev)

    def _post(self, ev, reads, writes):
        reads = self._flat(reads); writes = self._flat(writes)
        for b in reads:
            old = b.r.get(id(ev[0]))
            if old is None or old[1] < ev[1]:
                b.r[id(ev[0])] = ev
        for b in writes:
            b.w = ev
            b.r = {}

    cap = None
    after_op = None

    def replay(self, cap, k=None):
        n = len(cap) if k is None else min(k, len(cap))
        for _ in range(n):
            kind, a = cap.pop(0)
            if kind == 'op':
                self.op(*a)
            else:
                self.dma(*a)

    def op(self, e, fn, reads=(), writes=()):
        if self.cap is not None:
            self.cap.append(('op', (e, fn, tuple(reads), tuple(writes))))
            return
        self._deps(e, reads, writes)
        ins = fn(self.E[e])
        self.cnt[e] += 1
        ins.then_inc(self.sem[e], 1)
        self._post((self.sem[e], self.cnt[e], e), reads, writes)
        if self.after_op is not None:
            h, self.after_op = self.after_op, None
            h()
            self.after_op = h

    def dma(self, q, fn, dbuf, reads=(), writes=()):
        if self.cap is not None:
            self.cap.append(('dma', (q, fn, dbuf, tuple(reads), tuple(writes))))
            return
        self._deps(q, reads, writes)
        ins = fn(self.E[q])
        dbuf.dcnt += 16
        ins.then_inc(dbuf.dsem, 16)
        self._post((dbuf.dsem, dbuf.dcnt, 'dma'), reads, writes)

    def wait_all(self, e, bufs):
        for b in self._flat(bufs):
            self._wait(e, b.w)
            for ev in list(b.r.values()):
                self._wait(e, ev)


class _Stop(Exception):
    pass


_H = {}
STAGE = 99
RUN_KW = {}
SKIP_GATHER = False
FORCE_NPRE = None


def build_program(NT, NPRE, dbg=False):
    nc = bass.Bass("TRN2", target_bir_lowering=False)
    es = ExitStack()
    S = Sched(nc, es)
    _H['nc'] = nc; _H['es'] = es

    def din(name, shape, dt=F32):
        return nc.dram_tensor(name, list(shape), dt, kind="ExternalInput").ap()

    x_own = din("x_own", [NT * P, D])
    x_halo = din("x_halo", [P, D])
    x_pre = din("x_pre", [max(NPRE, 1) * P, D])
    cos_own = din("cos_own", [P, NT, 32]); sin_own = din("sin_own", [P, NT, 32])
    cos_pre = din("cos_pre", [P, max(NPRE, 1), 32]); sin_pre = din("sin_pre", [P, max(NPRE, 1), 32])
    ksc_pre = din("ksc_pre", [P, max(NPRE, 1), H])
    g_attn = din("g_attn", [P, D]); g_ffn = din("g_ffn", [P, D]); g_gn = din("g_gn", [P, D])
    g_rq = din("g_rq", [P, 512]); g_rk = din("g_rk", [P, 512]); g_sq = din("g_sq", [P, 512]); g_sk = din("g_sk", [P, 512])
    c_ident = din("c_ident", [P, P]); c_tri = din("c_tri", [P, P]); c_ones = din("c_ones", [P, P])
    c_mpos = din("c_mpos", [P, 512]); c_mstay = din("c_mstay", [P, 1024])
    c_maskT = din("c_maskT", [P, 1024]); c_qdec = din("c_qdec", [P, H]); c_kdec = din("c_kdec", [P, H])
    c_cdec = din("c_cdec", [64, D])
    w_in = din("w_in", [D, INW]); w_bra = din("w_bra", [D, D]); w_brb = din("w_brb", [512, D]); w_out = din("w_out", [D, D])
    w_q = din("w_q", [D, 2048]); k1T = din("k1T", [P, P]); k2T = din("k2T", [P, P])
    u_tab = din("u_tab", [NEXP, D]); v_tab = din("v_tab", [NEXP, D])
    y_out = nc.dram_tensor("y_out", [NT * P, D], F32, kind="ExternalOutput").ap()
    uv_bf = nc.dram_tensor("uv_bf", [NEXP, 2 * D], BF16, kind="Internal").ap()
    NWB = 17
    wsc = nc.dram_tensor("wsc", [NWB * P, 4096], BF16, kind="Internal").ap()
    def dump(stage, src_ap, bsrc, ncols=D):
        if STAGE != stage:
            return
        b_d = S.buf("dump", dma=True)
        npart = src_ap.shape[0]
        S.dma('sp', lambda e: e.dma_start(out=y_out[0:npart, 0:ncols], in_=src_ap), b_d, reads=bsrc, writes=[b_d])
        S.wait_all('sp', [b_d])
        raise _Stop()

    tot = [0]

    def sb(name, shape, dt=F32):
        n = int(np.prod(shape[1:])) * (4 if dt in (F32, U32, I32) else 2)
        tot[0] += n
        if dbg:
            print("SB", name, n, tot[0])
        return es.enter_context(nc.sbuf_tensor(name, list(shape), dt))

    def ps(name, shape, dt=F32):
        return es.enter_context(nc.psum_tensor(name, list(shape), dt))

    psA = ps("psA", [P, 1024]); bA = S.buf("psA")
    psB = ps("psB", [P, 1024]); bB = S.buf("psB")
    psC = ps("psC", [P, 512]); bC = S.buf("psC")
    psD = ps("psD", [P, 512]); bD = S.buf("psD")
    psT = ps("psT", [P, 1024], BF16); bT = S.buf("psT")
    psS = ps("psS", [P, 512]); bS = S.buf("psS")

    consts = []

    def load_const(name, src, shape, dt=F32, q='sp'):
        t = sb(name, shape, dt)
        b = S.buf(name, dma=True)
        S.dma(q, lambda e: e.dma_start(out=t[:], in_=src), b, writes=[b])
        consts.append(b)
        return t, b

    def load_cast(name, src, shape):
        return load_const(name, src, shape, BF16, q='pool')

    ident, b_ident = load_cast("ident", c_ident, [P, P])
    tri, b_tri = load_cast("tri", c_tri, [P, P])
    ones, b_ones = load_cast("ones", c_ones, [P, P])
    mpos, b_mpos = load_cast("mpos", c_mpos, [P, 512])
    mstay, b_mstay = load_cast("mstay", c_mstay, [P, 1024])
    maskT, b_maskT = load_const("maskT", c_maskT, [P, 1024])
    qdec, b_qdec = load_const("qdec", c_qdec, [P, H])
    kdec, b_kdec = load_const("kdec", c_kdec, [P, H])
    cdec, b_cdec = load_const("cdec", c_cdec, [64, D])
    gattn, b_gattn = load_const("gattn", g_attn, [P, D])
    gffn, b_gffn = load_const("gffn", g_ffn, [P, D])
    ggn, b_ggn = load_const("ggn", g_gn, [P, D])
    grq, b_grq = load_const("grq", g_rq, [P, 512])
    grk, b_grk = load_const("grk", g_rk, [P, 512])
    gsq, b_gsq = load_const("gsq", g_sq, [P, 512])
    gsk, b_gsk = load_const("gsk", g_sk, [P, 512])
    cso, b_cso = load_const("cso", cos_own, [P, NT, 32])
    sno, b_sno = load_const("sno", sin_own, [P, NT, 32])

    def wview(w, c0, n):
        return w[:, c0:c0 + n].rearrange("(c p) n -> p c n", p=P)

    TPbig = sb("TPbig", [P, 10, D])
    TP = [TPbig[:, i, :] for i in range(10)]
    b_TP = [S.buf("TP%d" % i, dma=True) for i in range(10)]
    xt = [TP[0], TP[1]]
    b_xt = [b_TP[0], b_TP[1]]
    junk = sb("junk", [P, D], BF16); b_junk = S.buf("junk")
    st4 = sb("st4", [P, 4]); b_st4 = S.buf("st4")

    def rmsnorm_T(src, b_src, gain, b_gain, keep_f32=None, b_keep=None):
        S.op('act', lambda e: e.activation(out=junk[:], in_=src, func=AF.Square, accum_out=st4[:, 0:1]),
             reads=[b_src], writes=[b_junk, b_st4])
        S.op('act', lambda e: e.activation(out=st4[:, 1:2], in_=st4[:, 0:1], func=AF.Sqrt, bias=eps_t[:, 0:1], scale=1.0 / D),
             reads=[b_st4, b_eps], writes=[b_st4])
        S.op('dve', lambda e: e.reciprocal(out=st4[:, 2:3], in_=st4[:, 1:2]), reads=[b_st4], writes=[b_st4])
        if keep_f32 is not None:
            S.op('dve', lambda e: e.scalar_tensor_tensor(out=keep_f32, in0=src, scalar=st4[:, 2:3], in1=gain[:],
                                                         op0=ALU.mult, op1=ALU.mult),
                 reads=[b_src, b_st4, b_gain], writes=[b_keep])
            S.op('act', lambda e: e.activation(out=xn[:], in_=keep_f32, func=AF.Copy), reads=[b_keep], writes=[b_xn])
        else:
            S.op('dve', lambda e: e.scalar_tensor_tensor(out=xn[:], in0=src, scalar=st4[:, 2:3], in1=gain[:],
                                                         op0=ALU.mult, op1=ALU.mult),
                 reads=[b_src, b_st4, b_gain], writes=[b_xn])
        transpose8(xn, b_xn, xnT, b_xnT)

    def transpose8(src, b_src, dst, b_dst, nchunk=8):
        for half in range((nchunk + 3) // 4):
            n = min(4, nchunk - half * 4)
            for k in range(n):
                c = half * 4 + k
                S.op('pe', lambda e, c=c, k=k: e.transpose(out=psT[:, k * P:(k + 1) * P], in_=src[:, c * P:(c + 1) * P],
                                                           identity=ident[:]),
                     reads=[b_src, b_ident], writes=[bT])
            eng = 'act' if half % 2 == 0 else 'dve'
            if eng == 'act':
                S.op('act', lambda e, half=half, n=n: e.activation(
                    out=dst[:, half * 4:half * 4 + n, :].rearrange("p c t -> p (c t)"), in_=psT[:, 0:n * P], func=AF.Copy),
                    reads=[bT], writes=[b_dst])
            else:
                S.op('dve', lambda e, half=half, n=n: e.tensor_copy(
                    out=dst[:, half * 4:half * 4 + n, :].rearrange("p c t -> p (c t)"), in_=psT[:, 0:n * P]),
                    reads=[bT], writes=[b_dst])

    def transposeH(src3, b_src, dst, b_dst):
        for h in range(H):
            S.op('pe', lambda e, h=h: e.transpose(out=psT[0:64, h * P:(h + 1) * P], in_=src3[:, h, :], identity=ident[:]),
                 reads=[b_src, b_ident], writes=[bT])
        S.op('act', lambda e: e.activation(out=dst[:].rearrange("p h t -> p (h t)"), in_=psT[0:64, :], func=AF.Copy),
             reads=[bT], writes=[b_dst])

    def proj512(wb, b_wb, pst, b_pst):
        for c in range(8):
            S.op('pe', lambda e, c=c: e.matmul(out=pst, lhsT=xnT[:, c, :], rhs=wb[:, c, :], start=(c == 0), stop=(c == 7)),
                 reads=[b_xnT, b_wb], writes=[b_pst])

    def qknorm(pst, b_pst, gain, b_gain, slot, scale_ap=None, b_scale=None, cos=None, sin=None, b_cs=(), out_bf=None, b_out=None):
        S.op('act', lambda e: e.activation(out=qf[:], in_=pst, func=AF.Copy), reads=[b_pst], writes=[b_qf])
        S.op('act', lambda e: e.activation(out=sq_s[:], in_=pst, func=AF.Square), reads=[b_pst], writes=[b_sq])
        S.op('dve', lambda e: e.tensor_reduce(out=st8[:, 0, :], in_=sq_s[:].rearrange("p (h d) -> p h d", d=64), axis=AX.X, op=ALU.add),
             reads=[b_sq], writes=[b_st8])
        S.op('act', lambda e: e.activation(out=st8[:, 1, :], in_=st8[:, 0, :], func=AF.Sqrt, bias=eps_t[:, 0:1], scale=1.0 / 64),
             reads=[b_st8, b_eps], writes=[b_st8])
        S.op('dve', lambda e: e.reciprocal(out=st8[:, 2, :], in_=st8[:, 1, :]), reads=[b_st8], writes=[b_st8])
        rs = st8[:, 2, :]
        if scale_ap is not None:
            S.op('dve', lambda e: e.tensor_tensor(out=st8[:, 3, :], in0=st8[:, 2, :], in1=scale_ap, op=ALU.mult),
                 reads=[b_st8, b_scale], writes=[b_st8])
            rs = st8[:, 3, :]
        S.op('dve', lambda e: e.tensor_tensor(out=qn[:].rearrange("p (h d) -> p h d", d=64), in0=qf[:].rearrange("p (h d) -> p h d", d=64),
                                              in1=rs.unsqueeze(2).to_broadcast([P, H, 64]), op=ALU.mult),
             reads=[b_qf, b_st8], writes=[b_qn])
        if cos is None:
            S.op('dve', lambda e: e.tensor_tensor(out=out_bf[:].rearrange("p h d -> p (h d)"), in0=qn[:], in1=gain[:], op=ALU.mult),
                 reads=[b_qn, b_gain], writes=[b_out])
            return
        S.op('pool', lambda e: e.tensor_tensor(out=qn[:], in0=qn[:], in1=gain[:], op=ALU.mult), reads=[b_qn, b_gain], writes=[b_qn])
        q3 = qn[:].rearrange("p (h d) -> p h d", d=64)
        x1 = q3[:, :, 0:32]; x2 = q3[:, :, 32:64]
        cb = cos.unsqueeze(1).to_broadcast([P, H, 32]); sbb = sin.unsqueeze(1).to_broadcast([P, H, 32])
        r = [rt[:, i, :].rearrange("p (h d) -> p h d", d=32) for i in range(4)]
        S.op('dve', lambda e: e.tensor_tensor(out=r[0], in0=x1, in1=cb, op=ALU.mult), reads=[b_qn] + list(b_cs), writes=[b_rt])
        S.op('pool', lambda e: e.tensor_tensor(out=r[1], in0=x2, in1=sbb, op=ALU.mult), reads=[b_qn] + list(b_cs), writes=[b_rt])
        S.op('dve', lambda e: e.tensor_tensor(out=r[2], in0=x1, in1=sbb, op=ALU.mult), reads=[b_qn] + list(b_cs), writes=[b_rt])
        S.op('pool', lambda e: e.tensor_tensor(out=r[3], in0=x2, in1=cb, op=ALU.mult), reads=[b_qn] + list(b_cs), writes=[b_rt])
        S.op('dve', lambda e: e.tensor_tensor(out=out_bf[:, :, 0:32], in0=r[0], in1=r[1], op=ALU.subtract), reads=[b_rt], writes=[b_out])
        S.op('dve', lambda e: e.tensor_tensor(out=out_bf[:, :, 32:64], in0=r[2], in1=r[3], op=ALU.add), reads=[b_rt], writes=[b_out])

    b_ubf = S.buf("uv_bf", dma=True); b_vbf = b_ubf
    b_wsc = S.buf("wsc", dma=True)
    identF, b_identF = load_const("identF", c_ident, [P, P])
    eps_t = sb("eps_t", [P, 1]); b_eps = S.buf("eps")
    S.op('dve', lambda e: e.memset(eps_t[:], EPS), writes=[b_eps])
    gk_t = sb("gk_t", [P, 1]); b_gk = S.buf("gk")
    S.op('dve', lambda e: e.memset(gk_t[:], 1.5957691216057308), writes=[b_gk])
    one_t = sb("one_t", [P, 1]); b_one = S.buf("one")
    S.op('dve', lambda e: e.memset(one_t[:], 1.0), writes=[b_one])

    state = sb("state", [64, D]); b_state = S.buf("state")
    state_bf = sb("state_bf", [64, D], BF16); b_state_bf = S.buf("state_bf")

    with ExitStack() as es0:
        RB = 2
        NBLK = NEXP // (128 * RB)
        NCB = 4
        cb = [es0.enter_context(nc.sbuf_tensor("cb%d" % i, [P, RB, D], BF16)) for i in range(NCB)]
        b_cb = [S.buf("cb%d" % i, dma=True) for i in range(NCB)]
        b_cin = b_cb; b_cout = []
        pc_jobs = [(tab, dst, bd, blk) for blk in range(NBLK) for (tab, dst, bd) in ((u_tab, uv_bf[:, 0:D], b_ubf), (v_tab, uv_bf[:, D:2 * D], b_vbf))]
        pc_state = [0, 0]

        def _pc_store(k):
            tab, dst, bd, blk = pc_jobs[k]
            i = k % NCB
            S.dma('pool', lambda e: e.dma_start(out=dst[blk * 128 * RB:(blk + 1) * 128 * RB, :].rearrange("(p r) d -> p r d", r=RB), in_=cb[i][:]),
                  bd, reads=[b_cb[i]], writes=[bd])

        def precast(nblocks):
            for _ in range(nblocks):
                k = pc_state[0]
                if k >= len(pc_jobs):
                    break
                pc_state[0] += 1
                tab, dst, bd, blk = pc_jobs[k]
                i = k % NCB
                S.dma('pool', lambda e, tab=tab, blk=blk, i=i: e.dma_start(out=cb[i][:], in_=tab[blk * 128 * RB:(blk + 1) * 128 * RB, :].rearrange("(p r) d -> p r d", r=RB)),
                      b_cb[i], writes=[b_cb[i]])
                if k - 2 >= 0:
                    _pc_store(k - 2)
                    pc_state[1] = k - 1
            if pc_state[0] >= len(pc_jobs):
                while pc_state[1] < len(pc_jobs):
                    _pc_store(pc_state[1])
                    pc_state[1] += 1

        wjobs = [(w_in if blk < 13 else w_q, (blk if blk < 13 else blk - 13) * 512, blk, cc) for blk in range(NWB) for cc in range(4)]

        def _w_store(k):
            src, c0, blk, cc = wjobs[k]
            i = k % NCB
            dstv = wsc[blk * P:(blk + 1) * P, :].rearrange("p (c n) -> p c n", c=8)[:, 2 * cc:2 * cc + 2, :]
            S.dma('pool', lambda e: e.dma_start(out=dstv, in_=cb[i][:, 0, :].rearrange("p (c n) -> p c n", c=2)), b_wsc, reads=[b_cb[i]], writes=[b_wsc])

        for k, (src, c0, blk, cc) in enumerate(wjobs):
            i = k % NCB
            S.dma('pool', lambda e, src=src, c0=c0, cc=cc, i=i: e.dma_start(out=cb[i][:, 0, :].rearrange("p (c n) -> p c n", c=2),
                                                                          in_=wview(src, c0, 512)[:, 2 * cc:2 * cc + 2, :]),
                  b_cb[i], writes=[b_cb[i]])
            if k - 2 >= 0:
                _w_store(k - 2)
        _w_store(len(wjobs) - 2); _w_store(len(wjobs) - 1)
        pc_per_tile = -(-len(pc_jobs) // max(NPRE, 1))
        if NPRE > 0:
            wk = es0.enter_context(nc.sbuf_tensor("wk_pre", [P, 8, 512], BF16)); b_wk = S.buf("wk_pre", dma=True)
            wv = es0.enter_context(nc.sbuf_tensor("wv_pre", [P, 8, 1024], BF16)); b_wv = S.buf("wv_pre", dma=True)
            csp = es0.enter_context(nc.sbuf_tensor("csp", [P, NPRE, 32], F32)); b_csp = S.buf("csp", dma=True)
            snp = es0.enter_context(nc.sbuf_tensor("snp", [P, NPRE, 32], F32)); b_snp = S.buf("snp", dma=True)
            ksp = es0.enter_context(nc.sbuf_tensor("ksp", [P, NPRE, H], F32)); b_ksp = S.buf("ksp", dma=True)
            S.dma('pool', lambda e: e.dma_start(out=wk[:], in_=wview(w_in, 512, 512)), b_wk, writes=[b_wk])
            for hh in range(2):
                S.dma('pool', lambda e, hh=hh: e.dma_start(out=wv[:, :, hh * 512:(hh + 1) * 512], in_=wview(w_in, 1024 + hh * 512, 512)),
                      b_wv, writes=[b_wv])
            S.dma('sp', lambda e: e.dma_start(out=csp[:], in_=cos_pre), b_csp, writes=[b_csp])
            S.dma('sp', lambda e: e.dma_start(out=snp[:], in_=sin_pre), b_snp, writes=[b_snp])
            S.dma('sp', lambda e: e.dma_start(out=ksp[:], in_=ksc_pre), b_ksp, writes=[b_ksp])
            B0 = 4
            def _al(name, shape, dt):
                return es0.enter_context(nc.sbuf_tensor(name, list(shape), dt))
            xnb = _al("xnb", [P, 2, D], BF16); xnTb = _al("xnTb", [P, 2, 8, P], BF16); st4b = _al("st4b", [P, B0, 4], F32)
            b_xnb = [S.buf("xnb%d" % (i % 2)) for i in range(2)] * 2; b_xnTb = [S.buf("xnTb%d" % (i % 2)) for i in range(2)] * 2; b_st4b = [S.buf("st4b%d" % i) for i in range(B0)]
            sets = []
            for si in range(2):
                d_ = dict(kf=TPbig[:, 2 + 4 * si:4 + 4 * si, :].rearrange("p a d -> p (a d)").rearrange("p (b n) -> p b n", b=B0),
                          sq=TPbig[:, 4 + 4 * si:6 + 4 * si, :].rearrange("p a d -> p (a d)").rearrange("p (b n) -> p b n", b=B0),
                          s8=_al("s8b%d" % si, [P, 4, B0 * H], F32),
                          r1=_al("r1b%d" % si, [P, B0 * 256], F32),
                          kd=_al("kdb%d" % si, [P, B0, H, 64], BF16), v=_al("vb%d" % si, [P, B0, D], BF16))
                d_.update(b_kf=S.buf("kfb%d" % si), b_sq=S.buf("sqb%d" % si), b_s8=S.buf("s8b%d" % si), b_r0=S.buf("r0b%d" % si),
                          b_r1=S.buf("r1b%d" % si), b_kd=S.buf("kdb%d" % si), b_v=S.buf("vb%d" % si))
                d_["r0"] = d_["kf"].rearrange("p b n -> p (b n)")[:, 0:B0 * 256]; d_["b_r0"] = d_["b_kf"]
                sets.append(d_)
            all_pre_bufs = b_xnb + b_xnTb + b_st4b + [sets[i][k] for i in range(2) for k in ("b_kf", "b_sq", "b_s8", "b_r0", "b_r1", "b_kd", "b_v")]

            def front_a(m, b, st):
                xb = xt[m % 2]; bx = b_xt[m % 2]
                S.dma('sp', lambda e: e.dma_start(out=xb[:], in_=x_pre[m * P:(m + 1) * P, :]), bx, writes=[bx])
                S.op('act', lambda e: e.activation(out=junk[:], in_=xb[:], func=AF.Square, accum_out=st4b[:, b, 0:1]), reads=[bx], writes=[b_junk, b_st4b[b]])
                S.op('act', lambda e: e.activation(out=st4b[:, b, 1:2], in_=st4b[:, b, 0:1], func=AF.Sqrt, bias=eps_t[:, 0:1], scale=1.0 / D),
                     reads=[b_st4b[b], b_eps], writes=[b_st4b[b]])
                S.op('dve', lambda e: e.reciprocal(out=st4b[:, b, 2:3], in_=st4b[:, b, 1:2]), reads=[b_st4b[b]], writes=[b_st4b[b]])
                S.op('dve', lambda e: e.scalar_tensor_tensor(out=xnb[:, b % 2, :], in0=xb[:], scalar=st4b[:, b, 2:3], in1=gattn[:], op0=ALU.mult, op1=ALU.mult),
                     reads=[bx, b_st4b[b], b_gattn], writes=[b_xnb[b]])

            def front_b(m, b, st):
                precast(pc_per_tile)
                for half in range(2):
                    for k in range(4):
                        c = half * 4 + k
                        S.op('pe', lambda e, c=c, k=k: e.transpose(out=psT[:, k * P:(k + 1) * P], in_=xnb[:, b % 2, c * P:(c + 1) * P], identity=ident[:]),
                             reads=[b_xnb[b], b_ident], writes=[bT])
                    dst = xnTb[:, b % 2, half * 4:half * 4 + 4, :].rearrange("p c t -> p (c t)")
                    if half == 0:
                        S.op('act', lambda e, dst=dst: e.activation(out=dst, in_=psT[:, 0:512], func=AF.Copy), reads=[bT], writes=[b_xnTb[b]])
                    else:
                        S.op('dve', lambda e, dst=dst: e.tensor_copy(out=dst, in_=psT[:, 0:512]), reads=[bT], writes=[b_xnTb[b]])
                for c in range(8):
                    S.op('pe', lambda e, c=c: e.matmul(out=psC[:], lhsT=xnTb[:, b % 2, c, :], rhs=wk[:, c, :], start=(c == 0), stop=(c == 7)),
                         reads=[b_xnTb[b], b_wk], writes=[bC])
                S.op('act', lambda e: e.activation(out=st["kf"][:, b, :], in_=psC[:], func=AF.Copy), reads=[bC], writes=[st["b_kf"]])
                S.op('act', lambda e: e.activation(out=st["sq"][:, b, :], in_=psC[:], func=AF.Square), reads=[bC], writes=[st["b_sq"]])
                psV, bV = (psA, bA) if m % 2 == 0 else (psB, bB)
                for hh in range(2):
                    for c in range(8):
                        S.op('pe', lambda e, c=c, hh=hh: e.matmul(out=psV[:, hh * 512:(hh + 1) * 512], lhsT=xnTb[:, b % 2, c, :], rhs=wv[:, c, hh * 512:(hh + 1) * 512],
                                                                  start=(c == 0), stop=(c == 7)), reads=[b_xnTb[b], b_wv], writes=[bV])
                S.op('act', lambda e: e.activation(out=st["v"][:, b, :], in_=psV[:], func=AF.Copy), reads=[bV], writes=[st["b_v"]])

            def chain_gen(m0, nb, st):
                n8 = nb * H
                kf, sq, s8, r0, r1, kdb = st["kf"], st["sq"], st["s8"], st["r0"], st["r1"], st["kd"]
                S.op('dve', lambda e: e.tensor_reduce(out=s8[:, 0, 0:n8], in_=sq[:, 0:nb, :].rearrange("p b (h d) -> p (b h) d", d=64), axis=AX.X, op=ALU.add),
                     reads=[st["b_sq"]], writes=[st["b_s8"]]); yield
                S.op('act', lambda e: e.activation(out=s8[:, 1, 0:n8], in_=s8[:, 0, 0:n8], func=AF.Sqrt, bias=eps_t[:, 0:1], scale=1.0 / 64),
                     reads=[st["b_s8"], b_eps], writes=[st["b_s8"]]); yield
                S.op('dve', lambda e: e.reciprocal(out=s8[:, 2, 0:n8], in_=s8[:, 1, 0:n8]), reads=[st["b_s8"]], writes=[st["b_s8"]]); yield
                S.op('dve', lambda e: e.tensor_tensor(out=s8[:, 3, 0:n8], in0=s8[:, 2, 0:n8], in1=ksp[:, m0:m0 + nb, :].rearrange("p m h -> p (m h)"), op=ALU.mult),
                     reads=[st["b_s8"], b_ksp], writes=[st["b_s8"]]); yield
                q3 = sq[:, 0:nb, :].rearrange("p b (h d) -> p (b h) d", d=64)
                S.op('dve', lambda e: e.tensor_tensor(out=q3, in0=kf[:, 0:nb, :].rearrange("p b (h d) -> p (b h) d", d=64),
                                                      in1=s8[:, 3, 0:n8].unsqueeze(2).to_broadcast([P, n8, 64]), op=ALU.mult),
                     reads=[st["b_kf"], st["b_s8"]], writes=[st["b_sq"]]); yield
                S.op('pool', lambda e: e.tensor_tensor(out=sq[:, 0:nb, :], in0=sq[:, 0:nb, :], in1=grk[:].unsqueeze(1).to_broadcast([P, nb, 512]), op=ALU.mult),
                     reads=[st["b_sq"], b_grk], writes=[st["b_sq"]]); yield
                q4 = sq[:, 0:nb, :].rearrange("p b (h d) -> p b h d", d=64)
                x1_ = q4[:, :, :, 0:32]; x2_ = q4[:, :, :, 32:64]
                cb = csp[:, m0:m0 + nb, :].unsqueeze(2).to_broadcast([P, nb, H, 32]); sb_ = snp[:, m0:m0 + nb, :].unsqueeze(2).to_broadcast([P, nb, H, 32])
                r0v = r0[:, 0:nb * 256].rearrange("p (b h d) -> p b h d", b=nb, h=H); r1v = r1[:, 0:nb * 256].rearrange("p (b h d) -> p b h d", b=nb, h=H)
                S.op('dve', lambda e: e.tensor_tensor(out=r0v, in0=x1_, in1=cb, op=ALU.mult), reads=[st["b_sq"], b_csp], writes=[st["b_r0"]]); yield
                S.op('pool', lambda e: e.tensor_tensor(out=r1v, in0=x2_, in1=sb_, op=ALU.mult), reads=[st["b_sq"], b_snp], writes=[st["b_r1"]]); yield
                S.op('dve', lambda e: e.tensor_tensor(out=kdb[:, 0:nb, :, 0:32], in0=r0v, in1=r1v, op=ALU.subtract), reads=[st["b_r0"], st["b_r1"]], writes=[st["b_kd"]]); yield
                S.op('dve', lambda e: e.tensor_tensor(out=r0v, in0=x1_, in1=sb_, op=ALU.mult), reads=[st["b_sq"], b_snp], writes=[st["b_r0"]]); yield
                S.op('pool', lambda e: e.tensor_tensor(out=r1v, in0=x2_, in1=cb, op=ALU.mult), reads=[st["b_sq"], b_csp], writes=[st["b_r1"]]); yield
                S.op('dve', lambda e: e.tensor_tensor(out=kdb[:, 0:nb, :, 32:64], in0=r0v, in1=r1v, op=ALU.add), reads=[st["b_r0"], st["b_r1"]], writes=[st["b_kd"]]); yield

            def state_mm(m0, nb, st):
                for b in range(nb):
                    m = m0 + b
                    for h in range(H):
                        acc_ps, b_acc_ps = (psS, bS) if h < 4 else (psD, bD)
                        S.op('pe', lambda e, h=h, m=m, b=b, acc_ps=acc_ps: e.matmul(
                            out=acc_ps[0:64, (h % 4) * P:(h % 4 + 1) * P], lhsT=st["kd"][:, b, h, :], rhs=st["v"][:, b, h * P:(h + 1) * P],
                            start=(m == 0 and h % 4 == 0), stop=(m == NPRE - 1), skip_group_check=True),
                            reads=[st["b_kd"], st["b_v"]], writes=[b_acc_ps])

            batches = [(m0, min(B0, NPRE - m0)) for m0 in range(0, NPRE, B0)]
            tiles = [(m0 + b, b, sets[bi % 2]) for bi, (m0, nb) in enumerate(batches) for b in range(nb)]
            pend = None
            front_a(*tiles[0])
            ti_ = 0
            for bi, (m0, nb) in enumerate(batches):
                st = sets[bi % 2]
                gen = chain_gen(*pend) if pend is not None else None
                for b in range(nb):
                    if ti_ + 1 < len(tiles):
                        front_a(*tiles[ti_ + 1])
                    front_b(m0 + b, b, st)
                    ti_ += 1
                    if gen is not None:
                        for _ in range(4):
                            next(gen, None)
                if gen is not None:
                    for _ in gen:
                        pass
                    state_mm(*pend)
                pend = (m0, nb, st)
            for _ in chain_gen(*pend):
                pass
            state_mm(*pend)
            for e_ in ('pe', 'act', 'dve', 'pool', 'sp'):
                S.wait_all(e_, all_pre_bufs)
            S.op('act', lambda e: e.activation(out=state[:, 0:512], in_=psS[0:64, :], func=AF.Copy), reads=[bS], writes=[b_state])
            S.op('act', lambda e: e.activation(out=state[:, 512:1024], in_=psD[0:64, :], func=AF.Copy), reads=[bD], writes=[b_state])
        else:
            S.op('dve', lambda e: e.memset(state[:], 0.0), writes=[b_state])
        S.op('act', lambda e: e.activation(out=state_bf[:], in_=state[:], func=AF.Copy), reads=[b_state], writes=[b_state_bf])
        precast(len(pc_jobs))
        for e_ in ('pe', 'act', 'dve', 'pool', 'sp'):
            S.wait_all(e_, b_cin + b_cout + [b_ubf, b_wsc])
        if NPRE > 0:
            for e_ in ('pe', 'act', 'dve', 'pool'):
                S.wait_all(e_, [b_wk, b_wv, b_csp, b_snp, b_ksp])

    dump(1, state[:], [b_state], D)
    xn = sb("xn", [P, D], BF16); b_xn = S.buf("xn")
    xnT = sb("xnT", [P, 8, P], BF16); b_xnT = S.buf("xnT")
    qf = sb("qf", [P, 512]); b_qf = S.buf("qf")
    sq_s = sb("sq_s", [P, 512]); b_sq = S.buf("sq_s")
    st8 = sb("st8", [P, 4, H]); b_st8 = S.buf("st8")
    qn = sb("qn", [P, 512]); b_qn = S.buf("qn")
    rt = sb("rt", [P, 4, 256]); b_rt = S.buf("rt")
    kd = sb("kd", [P, H, 64], BF16); b_kd = S.buf("kd")
    v_r = sb("v_r", [P, D], BF16); b_vr = S.buf("v_r")
    wbra = sb("wbra", [P, 8, D], BF16); b_wbra = S.buf("wbra", dma=True)
    wbrb = sb("wbrb", [P, 4, D], BF16); b_wbrb = S.buf("wbrb", dma=True)
    wout = sb("wout", [P, 8, D], BF16); b_wout = S.buf("wout", dma=True)
    k1 = sb("k1", [P, P], BF16); b_k1 = S.buf("k1", dma=True)
    k2 = sb("k2", [P, P], BF16); b_k2 = S.buf("k2", dma=True)
    for hh in range(2):
        S.dma('pool', lambda e, hh=hh: e.dma_start(out=wbra[:, :, hh * 512:(hh + 1) * 512], in_=wview(w_bra, hh * 512, 512)), b_wbra, writes=[b_wbra])
        S.dma('pool', lambda e, hh=hh: e.dma_start(out=wbrb[:, :, hh * 512:(hh + 1) * 512], in_=wview(w_brb, hh * 512, 512)), b_wbrb, writes=[b_wbrb])
        S.dma('pool', lambda e, hh=hh: e.dma_start(out=wout[:, :, hh * 512:(hh + 1) * 512], in_=wview(w_out, hh * 512, 512)), b_wout, writes=[b_wout])
    S.dma('pool', lambda e: e.dma_start(out=k1[:], in_=k1T), b_k1, writes=[b_k1])
    S.dma('pool', lambda e: e.dma_start(out=k2[:], in_=k2T), b_k2, writes=[b_k2])

    wbuf = [sb("wbuf%d" % i, [P, 8, 512], BF16) for i in range(2)]
    b_wbuf = [S.buf("wbuf%d" % i, dma=True) for i in range(2)]
    wcount = [0]

    def stream_w(c0, src=None):
        blk = c0 // 512 if src is None else 13 + c0 // 512
        i = wcount[0] % 2
        wcount[0] += 1
        S.dma('sp', lambda e: e.dma_start(out=wbuf[i][:], in_=wsc[blk * P:(blk + 1) * P, :].rearrange("p (c n) -> p c n", c=8)),
              b_wbuf[i], reads=[b_wsc], writes=[b_wbuf[i]])
        return wbuf[i], b_wbuf[i]

    xres = sb("xres", [P, D]); b_xres = S.buf("xres", dma=True)
    qd = sb("qd", [P, H, 64], BF16); b_qd = S.buf("qd")
    qdT = sb("qdT", [64, H, P], BF16); b_qdT = S.buf("qdT")
    kdT = sb("kdT", [64, H, P], BF16); b_kdT = S.buf("kdT")
    rg = sb("rg", [P, D], BF16); b_rg = S.buf("rg")
    sqn = sb("sqn", [P, H, 64], BF16); b_sqn = S.buf("sqn")
    skn = sb("skn", [P, H, 64], BF16); b_skn = S.buf("skn")
    sqT = sb("sqT", [64, H, P], BF16); b_sqT = S.buf("sqT")
    skT = [sb("skT%d" % i, [64, H, P], BF16) for i in range(2)]; b_skT = [S.buf("skT%d" % i) for i in range(2)]
    sv = [sb("sv%d" % i, [P, 512], BF16) for i in range(2)]; b_sv = [S.buf("sv%d" % i) for i in range(2)]
    siga = TP[7]; b_siga = b_TP[7]
    sigb = TP[8]; b_sigb = b_TP[8]
    pm = sb("pm", [P, D], BF16); b_pm = S.buf("pm")
    yf = TP[3]; b_yf = b_TP[3]
    ysq = TP[4]; b_ysq = b_TP[4]
    gst = sb("gst", [P, 6, H]); b_gst = S.buf("gst")
    ret = sb("ret", [P, D], BF16); b_ret = S.buf("ret")
    retT = sb("retT", [P, 8, P], BF16); b_retT = S.buf("retT")
    e_s = TP[3]; b_es = b_TP[3]
    sp_s = TP[4]; b_sp = b_TP[4]
    spm = sb("spm", [P, D], BF16); b_spm = S.buf("spm")
    u_s = TP[0]; b_us = b_TP[0]
    w_s = sb("w_s", [P, D], BF16); b_ws = S.buf("w_s")
    sbT = sb("sbT", [P, 4, P], BF16); b_sbT = S.buf("sbT")
    m1 = TP[5]; b_m1 = b_TP[5]
    m2 = TP[6]; b_m2 = b_TP[6]
    mixed = ret; b_mixed = b_ret
    mixT = retT; b_mixT = b_retT
    x1 = TP[1]; b_x1 = b_TP[1]
    hn = TP[9]; b_hn = b_TP[9]
    qT = sb("qT", [P, 16, P], BF16); b_qT = S.buf("qT")
    sc = TPbig[:, 3:5, :].rearrange("p a d -> p (a d)").rearrange("p (g n) -> p g n", g=16); b_sc = [b_TP[3], b_TP[4]]
    sc2 = TPbig[:, 5:7, :].rearrange("p a d -> p (a d)").rearrange("p (g n) -> p g n", g=16); b_sc2 = [b_TP[5], b_TP[6]]
    tv = sb("tv", [P, 16, 16]); b_tv = S.buf("tv")
    ti = sb("ti", [P, 16, 16], U32); b_ti = S.buf("ti")
    tif = sb("tif", [P, 16, 16]); b_tif = S.buf("tif")
    cand = TPbig[:, 7:9, :].rearrange("p a d -> p (a d)").rearrange("p (h c) -> p h c", h=H); b_cand = [b_TP[7], b_TP[8]]
    cand2 = sc.rearrange("p g n -> p (g n)").rearrange("p (h c) -> p h c", h=H); b_cand2 = b_sc
    tsv = sb("tsv", [P, H, 16]); b_tsv = S.buf("tsv")
    tpos = sb("tpos", [P, H, 16], U32); b_tpos = S.buf("tpos")
    tposf = sb("tposf", [P, H, 16]); b_tposf = S.buf("tposf")
    ta = sb("ta", [P, H, 16]); b_ta = S.buf("ta")
    tb = sb("tb", [P, H, 16]); b_tb = S.buf("tb")
    iota16 = sb("iota16", [P, 16]); b_iota = S.buf("iota16")
    oh = sc2.rearrange("p g n -> p (g n)").rearrange("p (h a b) -> p h a b", h=H, a=16); b_oh = b_sc2
    idx1 = sb("idx1", [P, H, 16]); b_idx1 = S.buf("idx1")
    idx2 = sb("idx2", [P, H, 16]); b_idx2 = S.buf("idx2")
    eidx = sb("eidx", [P, 128], U32); b_eidx = S.buf("eidx")
    gw = sb("gw", [P, H, 16]); b_gw = S.buf("gw")
    gs = sb("gs", [P, 2, H]); b_gs = S.buf("gs")
    hv = sb("hv", [P, 128]); b_hv = S.buf("hv")
    ga_ = sb("ga_", [P, 6, 128]); b_ga = S.buf("ga_"); b_ga2 = S.buf("ga2"); b_ga3 = S.buf("ga3")
    aw = sb("aw", [P, 128]); b_aw = S.buf("aw")
    NG = 8
    dg = [sb("dg%d" % i, [P, P], BF16) for i in range(4)]; b_dg = [S.buf("dg%d" % i) for i in range(4)]
    gbuf = None; b_gbuf = None
    acc = TP[0]; b_acc = b_TP[0]
    b_yout = S.buf("yout", dma=True)

    _gi = [3, 4, 5, 6, 7, 8, 2, 0]
    gbuf = [TPbig[:, i, :].bitcast(BF16) for i in _gi]
    b_gbuf = [b_TP[i] for i in _gi]
    gsem = [S.buf("gsem%d" % i, dma=True) for i in range(len(_gi))]
    S.op('pool', lambda e: e.iota(iota16[:], pattern=[[1, 16]], base=0, channel_multiplier=0, allow_small_or_imprecise_dtypes=True),
         writes=[b_iota])
    thr16 = sb("thr16", [P, 16])
    S.op('pool', lambda e: e.iota(thr16[:], pattern=[[16, 16]], base=0, channel_multiplier=0, allow_small_or_imprecise_dtypes=True),
         reads=[b_iota], writes=[b_iota])

    def sb_kv(cur):
        wb, bw = stream_w(3584)
        proj512(wb, bw, psC[:], bC)
        qknorm(psC[:], bC, gsk, b_gsk, 0, out_bf=skn, b_out=b_skn)
        transposeH(skn, b_skn, skT[cur], b_skT[cur])
        wb, bw = stream_w(4096)
        proj512(wb, bw, psD[:], bD)
        S.op('act', lambda e: e.activation(out=sv[cur][:], in_=psD[:], func=AF.Copy), reads=[bD], writes=[b_sv[cur]])

    S.dma('sp', lambda e: e.dma_start(out=xt[0][:], in_=x_halo), b_xt[0], writes=[b_xt[0]])
    rmsnorm_T(xt[0][:], b_xt[0], gattn, b_gattn)
    sb_kv(1)
    dump(2, xt[0][:], [b_xt[0], b_sv[1], b_skT[1]])

    def m_head(n):
        cur = n % 2
        S.dma('sp', lambda e, n=n: e.dma_start(out=xres[:], in_=x_own[n * P:(n + 1) * P, :]), b_xres, writes=[b_xres])
        rmsnorm_T(xres[:], b_xres, gattn, b_gattn)
        wb, bw = stream_w(0)
        proj512(wb, bw, psC[:], bC)
        qknorm(psC[:], bC, grq, b_grq, 0, scale_ap=qdec[:], b_scale=b_qdec, cos=cso[:, n, :], sin=sno[:, n, :],
               b_cs=[b_cso, b_sno], out_bf=qd, b_out=b_qd)
        transposeH(qd, b_qd, qdT, b_qdT)
        wb, bw = stream_w(512)
        proj512(wb, bw, psD[:], bD)
        qknorm(psD[:], bD, grk, b_grk, 0, scale_ap=kdec[:], b_scale=b_kdec, cos=cso[:, n, :], sin=sno[:, n, :],
               b_cs=[b_cso, b_sno], out_bf=kd, b_out=b_kd)
        transposeH(kd, b_kd, kdT, b_kdT)
        wb, bw = stream_w(3072)
        proj512(wb, bw, psC[:], bC)
        qknorm(psC[:], bC, gsq, b_gsq, 0, out_bf=sqn, b_out=b_sqn)
        transposeH(sqn, b_sqn, sqT, b_sqT)
        sb_kv(cur)
        for hh in range(2):
            wb, bw = stream_w(1024 + hh * 512)
            proj512(wb, bw, psB[:, hh * 512:(hh + 1) * 512], bB)
        S.op('act', lambda e: e.activation(out=v_r[:], in_=psB[:], func=AF.Copy), reads=[bB], writes=[b_vr])

    m_head(0)
    for n in range(NT):
        cur = n % 2; prv = 1 - cur
        for hh in range(2):
            wb, bw = stream_w(2048 + hh * 512)
            proj512(wb, bw, psB[:, hh * 512:(hh + 1) * 512], bB)
        S.op('act', lambda e: e.activation(out=m1[:], in_=psB[:], func=AF.Silu), reads=[bB], writes=[b_m1])
        S.op('pool', lambda e: e.tensor_tensor(out=rg[:], in0=m1[:], in1=ggn[:], op=ALU.mult), reads=[b_m1, b_ggn], writes=[b_rg])
        for h in range(H):
            S.op('pe', lambda e, h=h: e.matmul(out=psA[:, h * P:(h + 1) * P], lhsT=kdT[:, h, :], rhs=qdT[:, h, :], start=True, stop=True),
                 reads=[b_kdT, b_qdT], writes=[bA])
        S.op('dve', lambda e: e.tensor_tensor(out=pm[:], in0=psA[:], in1=maskT[:], op=ALU.mult), reads=[bA, b_maskT], writes=[b_pm])
        for h in range(H):
            S.op('pe', lambda e, h=h: e.matmul(out=psB[:, h * P:(h + 1) * P], lhsT=pm[:, h * P:(h + 1) * P], rhs=v_r[:, h * P:(h + 1) * P],
                                               start=True, stop=False, skip_group_check=True),
                 reads=[b_pm, b_vr], writes=[bB])
            S.op('pe', lambda e, h=h: e.matmul(out=psB[:, h * P:(h + 1) * P], lhsT=qdT[:, h, :], rhs=state_bf[:, h * P:(h + 1) * P],
                                               start=False, stop=True, skip_group_check=True),
                 reads=[b_qdT, b_state_bf], writes=[bB])
        S.op('dve', lambda e: e.tensor_tensor(out=state[:], in0=state[:], in1=cdec[:], op=ALU.mult), reads=[b_state, b_cdec], writes=[b_state])
        for rnd in range(2):
            for hl in range(4):
                h = rnd * 4 + hl
                S.op('pe', lambda e, h=h, hl=hl: e.matmul(out=psS[0:64, hl * P:(hl + 1) * P], lhsT=kd[:, h, :], rhs=v_r[:, h * P:(h + 1) * P],
                                                   start=True, stop=True, skip_group_check=True),
                     reads=[b_kd, b_vr], writes=[bS])
            S.op('dve', lambda e, rnd=rnd: e.tensor_tensor(out=state[:, rnd * 512:(rnd + 1) * 512], in0=state[:, rnd * 512:(rnd + 1) * 512],
                                                         in1=psS[0:64, :], op=ALU.add), reads=[b_state, bS], writes=[b_state])
        S.op('act', lambda e: e.activation(out=state_bf[:], in_=state[:], func=AF.Copy), reads=[b_state], writes=[b_state_bf])
        S.op('act', lambda e: e.activation(out=yf[:], in_=psB[:], func=AF.Copy), reads=[bB], writes=[b_yf])
        S.op('act', lambda e: e.activation(out=ysq[:], in_=psB[:], func=AF.Square), reads=[bB], writes=[b_ysq])
        S.op('dve', lambda e: e.tensor_reduce(out=gst[:, 0, :], in_=yf[:].rearrange("p (h d) -> p h d", d=P), axis=AX.X, op=ALU.add),
             reads=[b_yf], writes=[b_gst])
        S.op('dve', lambda e: e.tensor_reduce(out=gst[:, 1, :], in_=ysq[:].rearrange("p (h d) -> p h d", d=P), axis=AX.X, op=ALU.add),
             reads=[b_ysq], writes=[b_gst])
        S.op('dve', lambda e: e.tensor_scalar(out=gst[:, 2, :], in0=gst[:, 0, :], scalar1=1.0 / P, scalar2=None, op0=ALU.mult), reads=[b_gst], writes=[b_gst])
        S.op('dve', lambda e: e.tensor_tensor(out=gst[:, 3, :], in0=gst[:, 2, :], in1=gst[:, 2, :], op=ALU.mult), reads=[b_gst], writes=[b_gst])
        S.op('dve', lambda e: e.scalar_tensor_tensor(out=gst[:, 4, :], in0=gst[:, 1, :], scalar=1.0 / P, in1=gst[:, 3, :], op0=ALU.mult, op1=ALU.subtract),
             reads=[b_gst], writes=[b_gst])
        S.op('act', lambda e: e.activation(out=gst[:, 5, :], in_=gst[:, 4, :], func=AF.Sqrt, bias=eps_t[:, 0:1], scale=1.0), reads=[b_gst, b_eps], writes=[b_gst])
        S.op('dve', lambda e: e.reciprocal(out=gst[:, 3, :], in_=gst[:, 5, :]), reads=[b_gst], writes=[b_gst])
        y3 = yf[:].rearrange("p (h d) -> p h d", d=P)
        S.op('dve', lambda e: e.tensor_tensor(out=y3, in0=y3, in1=gst[:, 2, :].unsqueeze(2).to_broadcast([P, H, P]), op=ALU.subtract),
             reads=[b_yf, b_gst], writes=[b_yf])
        S.op('dve', lambda e: e.tensor_tensor(out=y3, in0=y3, in1=gst[:, 3, :].unsqueeze(2).to_broadcast([P, H, P]), op=ALU.mult),
             reads=[b_yf, b_gst], writes=[b_yf])
        S.op('pool', lambda e: e.tensor_tensor(out=ret[:], in0=yf[:], in1=rg[:], op=ALU.mult), reads=[b_yf, b_rg], writes=[b_ret])
        transpose8(ret, b_ret, retT, b_retT)
        dump(3, yf[:], [b_yf, b_retT])
        for half in range(2):
            for blk, kT_ in ((0, skT[prv]), (1, skT[cur])):
                for hl in range(4):
                    h = half * 4 + hl
                    S.op('pe', lambda e, blk=blk, hl=hl, h=h, kT_=kT_: e.matmul(
                        out=psA[:, blk * 512 + hl * P: blk * 512 + (hl + 1) * P], lhsT=kT_[:, h, :],
                        rhs=sqT[:, h, :], start=True, stop=True),
                        reads=[b_skT[prv], b_skT[cur], b_sqT], writes=[bA])
            S.op('act', lambda e: e.activation(out=e_s[:], in_=psA[:], func=AF.Exp, scale=0.125), reads=[bA], writes=[b_es])
            S.op('act', lambda e: e.activation(out=sp_s[:], in_=e_s[:], func=AF.Ln, bias=one_t[:, 0:1], scale=1.0), reads=[b_es, b_one], writes=[b_sp])
            S.op('dve', lambda e: e.tensor_tensor(
                out=spm[:].rearrange("p (b h t) -> p b h t", b=2, h=4), in0=sp_s[:].rearrange("p (b h t) -> p b h t", b=2, h=4),
                in1=mstay[:].rearrange("p (b h t) -> p b h t", b=2, h=4), op=ALU.mult), reads=[b_sp, b_mstay], writes=[b_spm])
            S.op('pe', lambda e: e.matmul(out=psB[:, 0:512], lhsT=tri[:], rhs=spm[:, 0:512], start=True, stop=False), reads=[b_tri, b_spm], writes=[bB])
            S.op('pe', lambda e: e.matmul(out=psB[:, 0:512], lhsT=ones[:], rhs=spm[:, 512:1024], start=False, stop=True), reads=[b_ones, b_spm], writes=[bB])
            S.op('pe', lambda e: e.matmul(out=psB[:, 512:1024], lhsT=tri[:], rhs=spm[:, 512:1024], start=True, stop=False), reads=[b_tri, b_spm], writes=[bB])
            S.op('pe', lambda e: e.matmul(out=psB[:, 512:1024], lhsT=ident[:], rhs=mpos[:], start=False, stop=True), reads=[b_ident, b_mpos], writes=[bB])
            S.op('dve', lambda e: e.scalar_tensor_tensor(out=u_s[:], in0=psA[:], scalar=0.125, in1=sp_s[:], op0=ALU.mult, op1=ALU.subtract),
                 reads=[bA, b_sp], writes=[b_us])
            S.op('dve', lambda e: e.tensor_tensor(out=u_s[:], in0=u_s[:], in1=psB[:], op=ALU.subtract), reads=[b_us, bB], writes=[b_us])
            S.op('act', lambda e: e.activation(out=w_s[:], in_=u_s[:], func=AF.Exp), reads=[b_us], writes=[b_ws])
            for hl in range(4):
                h = half * 4 + hl
                po = (h % 2) * 64
                for blk, svb, bsv in ((0, sv[prv], b_sv[prv]), (1, sv[cur], b_sv[cur])):
                    S.op('pe', lambda e, h=h, hl=hl, po=po, blk=blk, svb=svb: e.matmul(
                        out=psS[po:po + 64, (h // 2) * P:(h // 2 + 1) * P], lhsT=svb[:, h * 64:(h + 1) * 64],
                        rhs=w_s[:, blk * 512 + hl * P: blk * 512 + (hl + 1) * P], start=(blk == 0), stop=(blk == 1), skip_group_check=True),
                        reads=[bsv, b_ws], writes=[bS])
        S.op('act', lambda e: e.activation(out=sbT[:].rearrange("p c t -> p (c t)"), in_=psS[:], func=AF.Copy), reads=[bS], writes=[b_sbT])
        if STAGE == 4:
            S.op('act', lambda e: e.activation(out=m2[:, 0:512], in_=psS[:], func=AF.Copy), reads=[bS], writes=[b_m2])
            dump(4, m2[:, 0:512], [b_m2], 512)
        for hh in range(2):
            for c in range(8):
                S.op('pe', lambda e, c=c, hh=hh: e.matmul(out=psA[:, hh * 512:(hh + 1) * 512], lhsT=retT[:, c, :], rhs=wbra[:, c, hh * 512:(hh + 1) * 512],
                                                          start=(c == 0), stop=(c == 7)), reads=[b_retT, b_wbra], writes=[bA])
            for c in range(4):
                S.op('pe', lambda e, c=c, hh=hh: e.matmul(out=psB[:, hh * 512:(hh + 1) * 512], lhsT=sbT[:, c, :], rhs=wbrb[:, c, hh * 512:(hh + 1) * 512],
                                                          start=(c == 0), stop=(c == 3)), reads=[b_sbT, b_wbrb], writes=[bB])
        for gi, (gt, bg) in enumerate(((siga, b_siga), (sigb, b_sigb))):
            for hh in range(2):
                wb, bw = stream_w(4608 + gi * 1024 + hh * 512)
                pst, bp = (psC, bC) if hh == 0 else (psD, bD)
                proj512(wb, bw, pst[:], bp)
                S.op('act', lambda e, gt=gt, hh=hh, pst=pst: e.activation(out=gt[:, hh * 512:(hh + 1) * 512], in_=pst[:], func=AF.Sigmoid),
                     reads=[bp], writes=[bg])
        S.op('dve', lambda e: e.tensor_tensor(out=m1[:], in0=psA[:], in1=siga[:], op=ALU.mult), reads=[bA, b_siga], writes=[b_m1])
        S.op('dve', lambda e: e.tensor_tensor(out=m2[:], in0=psB[:], in1=sigb[:], op=ALU.mult), reads=[bB, b_sigb], writes=[b_m2])
        S.op('pool', lambda e: e.tensor_tensor(out=mixed[:], in0=m1[:], in1=m2[:], op=ALU.add), reads=[b_m1, b_m2], writes=[b_mixed])
        transpose8(mixed, b_mixed, mixT, b_mixT)
        for hh in range(2):
            for c in range(8):
                S.op('pe', lambda e, c=c, hh=hh: e.matmul(out=psA[:, hh * 512:(hh + 1) * 512], lhsT=mixT[:, c, :], rhs=wout[:, c, hh * 512:(hh + 1) * 512],
                                                          start=(c == 0), stop=(c == 7)), reads=[b_mixT, b_wout], writes=[bA])
        S.op('dve', lambda e: e.tensor_tensor(out=x1[:], in0=psA[:], in1=xres[:], op=ALU.add), reads=[bA, b_xres], writes=[b_x1])
        dump(5, x1[:], [b_x1])
        rmsnorm_T(x1[:], b_x1, gffn, b_gffn, keep_f32=hn[:], b_keep=b_hn)
        for g4 in range(4):
            wqb, b_wq = stream_w(g4 * 512, w_q)
            for gl in range(4):
                g = g4 * 4 + gl
                pst = psA[:, gl * P:(gl + 1) * P] if g4 % 2 == 0 else psB[:, gl * P:(gl + 1) * P]
                bp = bA if g4 % 2 == 0 else bB
                for c in range(8):
                    S.op('pe', lambda e, c=c, gl=gl, pst=pst, wqb=wqb: e.matmul(out=pst, lhsT=wqb[:, c, gl * P:(gl + 1) * P], rhs=xnT[:, c, :],
                                                                     start=(c == 0), stop=(c == 7)), reads=[b_wq, b_xnT], writes=[bp])
            src = psA if g4 % 2 == 0 else psB
            bp = bA if g4 % 2 == 0 else bB
            S.op('act', lambda e, g4=g4, src=src: e.activation(out=qT[:, g4 * 4:(g4 + 1) * 4, :].rearrange("p g t -> p (g t)"), in_=src[:, 0:512], func=AF.Copy),
                 reads=[bp], writes=[b_qT])
        for g4 in range(4):
            pst, bp = (psA, bA) if g4 % 2 == 0 else (psB, bB)
            for gl in range(4):
                g = g4 * 4 + gl
                kk, bk = (k1, b_k1) if g % 2 == 0 else (k2, b_k2)
                S.op('pe', lambda e, g=g, gl=gl, pst=pst, kk=kk: e.matmul(out=pst[:, gl * P:(gl + 1) * P], lhsT=qT[:, g, :], rhs=kk[:], start=True, stop=True),
                     reads=[b_qT, bk], writes=[bp])
            S.op('act', lambda e, g4=g4, pst=pst: e.activation(out=sc[:, g4 * 4:(g4 + 1) * 4, :].rearrange("p g n -> p (g n)"), in_=pst[:, 0:512], func=AF.Copy),
                 reads=[bp], writes=[b_sc])
        cap = []
        if n + 1 < NT:
            S.cap = cap
            m_head(n + 1)
            S.cap = None
        S.after_op = lambda: S.replay(cap, 1)
        bg_tv = [S.buf("tv%d" % g) for g in range(16)]; bg_ti = [S.buf("ti%d" % g) for g in range(16)]; bg_s2 = [S.buf("s2%d" % g) for g in range(16)]
        for g in range(16):
            S.op('dve', lambda e, g=g: e.max(out=tv[:, g, 0:8], in_=sc[:, g, :]), reads=[b_sc], writes=[bg_tv[g]])
        for g in range(16):
            S.op('dve', lambda e, g=g: e.match_replace(out=sc2[:, g, :], in_to_replace=tv[:, g, 0:8], in_values=sc[:, g, :], imm_value=NEG),
                 reads=[b_sc, bg_tv[g]], writes=[bg_s2[g]])
        for g in range(16):
            S.op('dve', lambda e, g=g: e.max_index(out=ti[:, g, 0:8], in_max=tv[:, g, 0:8], in_values=sc[:, g, :]), reads=[b_sc, bg_tv[g]], writes=[bg_ti[g]])
        for g in range(16):
            S.op('dve', lambda e, g=g: e.max(out=tv[:, g, 8:16], in_=sc2[:, g, :]), reads=[bg_s2[g]], writes=[bg_tv[g]])
        for g in range(16):
            S.op('dve', lambda e, g=g: e.max_index(out=ti[:, g, 8:16], in_max=tv[:, g, 8:16], in_values=sc2[:, g, :]), reads=[bg_s2[g], bg_tv[g]], writes=[bg_ti[g]])
        b_tv.w = None; b_ti.w = None
        S.op('dve', lambda e: e.tensor_copy(out=tif[:], in_=ti[:]), reads=bg_ti + bg_tv + bg_s2 + [b_sc2], writes=[b_tif, b_tv, b_ti, b_sc2])
        tv4 = tv[:].rearrange("p (h s) k -> p h s k", s=2)
        tif4 = tif[:].rearrange("p (h s) k -> p h s k", s=2)
        S.op('dve', lambda e: e.tensor_tensor(out=cand.rearrange("p h (a b) -> p h a b", a=16),
                                              in0=tv4[:, :, 0, :].unsqueeze(3).to_broadcast([P, H, 16, 16]),
                                              in1=tv4[:, :, 1, :].unsqueeze(2).to_broadcast([P, H, 16, 16]), op=ALU.add),
             reads=[b_tv], writes=[b_cand])
        bh_ts = [S.buf("ts%d" % h) for h in range(H)]; bh_tp = [S.buf("tp%d" % h) for h in range(H)]; bh_c2 = [S.buf("c2%d" % h) for h in range(H)]
        for h in range(H):
            S.op('dve', lambda e, h=h: e.max(out=tsv[:, h, 0:8], in_=cand[:, h, :]), reads=[b_cand], writes=[bh_ts[h]])
        for h in range(H):
            S.op('dve', lambda e, h=h: e.match_replace(out=cand2[:, h, :], in_to_replace=tsv[:, h, 0:8], in_values=cand[:, h, :], imm_value=NEG),
                 reads=[b_cand, bh_ts[h], b_cand2], writes=[bh_c2[h]])
        for h in range(H):
            S.op('dve', lambda e, h=h: e.max_index(out=tpos[:, h, 0:8], in_max=tsv[:, h, 0:8], in_values=cand[:, h, :]), reads=[b_cand, bh_ts[h]], writes=[bh_tp[h]])
        for h in range(H):
            S.op('dve', lambda e, h=h: e.max(out=tsv[:, h, 8:16], in_=cand2[:, h, :]), reads=[bh_c2[h]], writes=[bh_ts[h]])
        for h in range(H):
            S.op('dve', lambda e, h=h: e.max_index(out=tpos[:, h, 8:16], in_max=tsv[:, h, 8:16], in_values=cand2[:, h, :]), reads=[bh_c2[h], bh_ts[h]], writes=[bh_tp[h]])
        b_tsv.w = None; b_tpos.w = None
        S.op('dve', lambda e: e.tensor_copy(out=tposf[:], in_=tpos[:]), reads=bh_tp + bh_ts + bh_c2, writes=[b_tposf, b_tsv, b_tpos, b_cand2])
        S.op('dve', lambda e: e.tensor_tensor(out=oh, in0=tposf[:].unsqueeze(3).to_broadcast([P, H, 16, 16]),
                                              in1=thr16[:].unsqueeze(1).unsqueeze(1).to_broadcast([P, H, 16, 16]), op=ALU.is_ge),
             reads=[b_tposf, b_iota], writes=[b_oh])
        S.op('dve', lambda e: e.tensor_reduce(out=ta[:], in_=oh, axis=AX.X, op=ALU.add), reads=[b_oh], writes=[b_ta])
        S.op('dve', lambda e: e.tensor_scalar(out=ta[:], in0=ta[:], scalar1=-1.0, scalar2=None, op0=ALU.add), reads=[b_ta], writes=[b_ta])
        S.op('dve', lambda e: e.scalar_tensor_tensor(out=tb[:], in0=ta[:], scalar=-16.0, in1=tposf[:], op0=ALU.mult, op1=ALU.add),
             reads=[b_ta, b_tposf], writes=[b_tb])
        io_b = iota16[:].unsqueeze(1).unsqueeze(1).to_broadcast([P, H, 16, 16])
        for sel, half, dst, bd in ((ta, 0, idx1, b_idx1), (tb, 1, idx2, b_idx2)):
            bsel = b_ta if half == 0 else b_tb
            S.op('dve', lambda e, sel=sel: e.tensor_tensor(out=oh, in0=sel[:].unsqueeze(3).to_broadcast([P, H, 16, 16]), in1=io_b, op=ALU.is_equal),
                 reads=[bsel, b_iota], writes=[b_oh])
            S.op('dve', lambda e, half=half: e.tensor_tensor(out=oh, in0=oh, in1=tif4[:, :, half, :].unsqueeze(2).to_broadcast([P, H, 16, 16]), op=ALU.mult),
                 reads=[b_oh, b_tif], writes=[b_oh])
            S.op('dve', lambda e, dst=dst: e.tensor_reduce(out=dst[:], in_=oh, axis=AX.X, op=ALU.add), reads=[b_oh], writes=[bd])
        S.op('dve', lambda e: e.scalar_tensor_tensor(out=idx1[:], in0=idx1[:], scalar=128.0, in1=idx2[:], op0=ALU.mult, op1=ALU.add),
             reads=[b_idx1, b_idx2], writes=[b_idx1])
        S.op('dve', lambda e: e.tensor_copy(out=eidx[:], in_=idx1[:].rearrange("p h k -> p (h k)")), reads=[b_idx1], writes=[b_eidx])
        S.op('dve', lambda e: e.tensor_tensor(out=gw[:], in0=tsv[:], in1=tsv[:, :, 0:1].to_broadcast([P, H, 16]), op=ALU.subtract), reads=[b_tsv], writes=[b_gw])
        S.op('act', lambda e: e.activation(out=gw[:], in_=gw[:], func=AF.Exp), reads=[b_gw], writes=[b_gw])
        S.op('dve', lambda e: e.tensor_reduce(out=gs[:, 0, :], in_=gw[:], axis=AX.X, op=ALU.add), reads=[b_gw], writes=[b_gs])
        S.op('dve', lambda e: e.reciprocal(out=gs[:, 1, :], in_=gs[:, 0, :]), reads=[b_gs], writes=[b_gs])
        S.op('dve', lambda e: e.tensor_tensor(out=gw[:], in0=gw[:], in1=gs[:, 1, :].unsqueeze(2).to_broadcast([P, H, 16]), op=ALU.mult), reads=[b_gw, b_gs], writes=[b_gw])
        if STAGE == 6:
            S.op('dve', lambda e: e.tensor_copy(out=m2[:, 0:128], in_=idx1[:].rearrange("p h k -> p (h k)")), reads=[b_idx1], writes=[b_m2])
            S.op('dve', lambda e: e.tensor_copy(out=m2[:, 128:256], in_=gw[:].rearrange("p h k -> p (h k)")), reads=[b_gw], writes=[b_m2])
            dump(6, m2[:, 0:256], [b_m2], 256)
        S.after_op = None
        GS = 2
        NGRP = 128 // GS
        gwf = gw[:].rearrange("p h k -> p (h k)")

        def emit_gather(g):
            for k in range(GS):
                j = g * GS + k
                gb_, bgb = gbuf[j % NG], b_gbuf[j % NG]
                S.dma('pool', lambda e, j=j, gb_=gb_: e.indirect_dma_start(out=gb_[:], out_offset=None, in_=uv_bf,
                                                                          in_offset=bass.IndirectOffsetOnAxis(ap=eidx[:, j:j + 1], axis=0)),
                      gsem[j % NG], reads=[b_eidx, b_ubf], writes=[bgb])

        def emit_dots(g):
            for k in range(GS):
                j = g * GS + k
                gb_, bgb = gbuf[j % NG], b_gbuf[j % NG]
                S.op('dve', lambda e, j=j, gb_=gb_: e.scalar_tensor_tensor(out=junk[:], in0=gb_[:, 0:D], scalar=1.0, in1=hn[:],
                                                                          op0=ALU.mult, op1=ALU.mult, accum_out=hv[:, j:j + 1]),
                     reads=[bgb, b_hn], writes=[b_junk, b_hv])

        def emit_pre(g):
            c = slice(g * GS, (g + 1) * GS)
            S.op('act', lambda e: e.activation(out=ga_[:, 0, c], in_=hv[:, c], func=AF.Square), reads=[b_hv], writes=[b_ga])
            S.op('act', lambda e: e.activation(out=ga_[:, 1, c], in_=ga_[:, 0, c], func=AF.Identity, scale=0.0713548162726, bias=gk_t[:, 0:1]),
                 reads=[b_ga, b_gk], writes=[b_ga])
            for k in range(GS):
                j = g * GS + k
                S.op('act', lambda e, j=j: e.activation(out=ga_[:, 3, j:j + 1], in_=ga_[:, 1, j:j + 1], func=AF.Sigmoid, scale=hv[:, j:j + 1]),
                     reads=[b_ga, b_hv], writes=[b_ga2])
            S.op('dve', lambda e: e.tensor_tensor(out=ga_[:, 4, c], in0=hv[:, c], in1=gwf[:, c], op=ALU.mult), reads=[b_hv, b_gw], writes=[b_ga3])

        def emit_post(g):
            for k in range(GS):
                j = g * GS + k
                S.op('act', lambda e, j=j: e.activation(out=aw[:, j:j + 1], in_=ga_[:, 3, j:j + 1], func=AF.Copy, scale=ga_[:, 4, j:j + 1]),
                     reads=[b_ga2, b_ga3], writes=[b_aw])

        def emit_axpy(g):
            for k in range(GS):
                j = g * GS + k
                gb_, bgb = gbuf[j % NG], b_gbuf[j % NG]
                dgj, bdg = dg[j % 4], b_dg[j % 4]
                S.op('act', lambda e, j=j, dgj=dgj: e.activation(out=dgj[:], in_=identF[:], func=AF.Copy, scale=aw[:, j:j + 1]),
                     reads=[b_identF, b_aw], writes=[bdg])
                for hh in range(2):
                    S.op('pe', lambda e, j=j, hh=hh, dgj=dgj, gb_=gb_: e.matmul(out=psA[:, hh * 512:(hh + 1) * 512], lhsT=dgj[:],
                                                                            rhs=gb_[:, D + hh * 512:D + (hh + 1) * 512],
                                                                            start=(j == 0), stop=(j == 127)), reads=[bdg, bgb], writes=[bA])

        for g0 in range(3):
            emit_gather(g0)
        for st_ in range(NGRP + 1):
            S.replay(cap, 3)
            if 0 <= st_ - 1 < NGRP:
                emit_pre(st_ - 1)
            if st_ < NGRP:
                emit_dots(st_)
            if 0 <= st_ - 1 < NGRP:
                emit_post(st_ - 1)
                emit_axpy(st_ - 1)
            if st_ + 3 < NGRP:
                emit_gather(st_ + 3)
        S.replay(cap)
        S.op('dve', lambda e: e.tensor_tensor(out=acc[:], in0=psA[:], in1=x1[:], op=ALU.add), reads=[bA, b_x1], writes=[b_acc])
        S.dma('sp', lambda e, n=n: e.dma_start(out=y_out[n * P:(n + 1) * P, :], in_=acc[:]), b_yout, reads=[b_acc], writes=[b_yout])

    S.wait_all('sp', [b_yout])
    es.close()
    return nc, None


def _consts():
    hs = np.arange(H, dtype=np.float64)
    gam = 1.0 - 2.0 ** (-5.0 - hs)
    i = np.arange(P, dtype=np.float64)
    c = {}
    c["c_ident"] = np.eye(P, dtype=np.float32)
    c["c_tri"] = (i[:, None] > i[None, :]).astype(np.float32)
    c["c_ones"] = np.ones((P, P), np.float32)
    mp = 1.0e4 * (i[:, None] >= i[None, :]).astype(np.float32)
    c["c_mpos"] = np.tile(mp, (1, 4)).astype(np.float32)
    ms = np.ones((P, 2, 4, P), np.float32)
    ms[:, 1, :, :] = (i[:, None] < i[None, :]).astype(np.float32)[:, None, :]
    c["c_mstay"] = ms.reshape(P, 1024)
    mk = np.zeros((P, H, P), np.float64)
    for h in range(H):
        mk[:, h, :] = (i[None, :] >= i[:, None]) * gam[h] ** (-128.0)
    c["c_maskT"] = mk.reshape(P, 1024).astype(np.float32)
    c["c_qdec"] = (gam[None, :] ** (i[:, None] + 1.0)).astype(np.float32)
    c["c_kdec"] = (0.125 * gam[None, :] ** (127.0 - i[:, None])).astype(np.float32)
    cd = np.zeros((64, D), np.float64)
    for h in range(H):
        cd[:, h * P:(h + 1) * P] = gam[h] ** 128.0
    c["c_cdec"] = cd.astype(np.float32)
    return c, gam


def _rope_tabs(pos):
    half = 32
    freqs = (np.float32(10000.0) ** (-np.arange(half, dtype=np.float32) / np.float32(half))).astype(np.float32)
    ang = (pos.astype(np.float32)[:, :, None] * freqs[None, None, :]).astype(np.float32).astype(np.float64)
    return np.cos(ang).astype(np.float32), np.sin(ang).astype(np.float32)


_CACHE = {}


def kernel(x, norm_attn, w_in, ret_q_norm, ret_k_norm, ret_group_norm, sb_q_norm, sb_k_norm,
           w_branch_ret, w_branch_sb, w_out, norm_ffn, peer_w_q, peer_sub_keys_1,
           peer_sub_keys_2, peer_u, peer_v):
    f = np.float32
    x = np.asarray(x, f)
    B, SEQ, _ = x.shape
    assert B == 1
    NT = SEQ // (NCORES * P)
    NPRE = NT * (NCORES - 1) if FORCE_NPRE is None else FORCE_NPRE
    x2 = x[0]
    key = (NT, NPRE)
    if key not in _CACHE:
        try:
            _CACHE[key] = build_program(NT, NPRE)
        except _Stop:
            _H['es'].close()
            _CACHE[key] = (_H['nc'], None)
    nc, _es = _CACHE[key]
    cst, gam = _consts()
    rep = lambda v, n: np.ascontiguousarray(np.broadcast_to(np.tile(np.asarray(v, f).reshape(-1), n)[None, :], (P, np.asarray(v).size * n)))
    shared = dict(cst)
    shared.update({
        "g_attn": rep(norm_attn[0], 1), "g_ffn": rep(norm_ffn[0], 1), "g_gn": rep(ret_group_norm[0], 1),
        "g_rq": rep(ret_q_norm[0], 8), "g_rk": rep(ret_k_norm[0], 8), "g_sq": rep(sb_q_norm[0], 8), "g_sk": rep(sb_k_norm[0], 8),
        "w_in": np.ascontiguousarray(w_in[0], f), "w_bra": np.ascontiguousarray(w_branch_ret[0], f),
        "w_brb": np.ascontiguousarray(w_branch_sb[0], f), "w_out": np.ascontiguousarray(w_out[0], f),
        "w_q": np.ascontiguousarray(peer_w_q[0], f),
        "k1T": np.ascontiguousarray(np.asarray(peer_sub_keys_1[0], f).T), "k2T": np.ascontiguousarray(np.asarray(peer_sub_keys_2[0], f).T),
        "u_tab": np.ascontiguousarray(peer_u[0], f), "v_tab": np.ascontiguousarray(peer_v[0], f),
    })
    in_maps = []
    pp = np.arange(P, dtype=np.float64)
    for c in range(NCORES):
        t0 = c * NT
        m = dict(shared)
        m["x_own"] = np.ascontiguousarray(x2[t0 * P:(t0 + NT) * P])
        m["x_halo"] = np.ascontiguousarray(x2[(t0 - 1) * P:t0 * P]) if c > 0 else np.zeros((P, D), f)
        npre = max(NPRE, 1)
        xp = np.zeros((npre * P, D), f)
        gt = np.arange(npre) + (t0 - NPRE)
        nvalid = min(t0, NPRE)
        if nvalid > 0:
            xp[(NPRE - nvalid) * P:NPRE * P] = x2[(t0 - nvalid) * P:t0 * P]
        m["x_pre"] = xp
        pos_own = (np.arange(NT)[None, :] + t0) * P + pp[:, None]
        m["cos_own"], m["sin_own"] = _rope_tabs(pos_own)
        pos_pre = np.maximum(gt, 0)[None, :] * P + pp[:, None]
        m["cos_pre"], m["sin_pre"] = _rope_tabs(pos_pre)
        ks = np.zeros((P, npre, H), np.float64)
        for mm in range(npre):
            ks[:, mm, :] = 0.125 * gam[None, :] ** (127.0 - pp[:, None]) * gam[None, :] ** (128.0 * (NPRE - 1 - mm))
        m["ksc_pre"] = ks.astype(f)
        in_maps.append(m)
    res = run_bass_kernel_spmd(nc, in_maps, core_ids=list(range(NCORES)), **RUN_KW)
    _H['res'] = res
    out = np.concatenate([np.asarray(r["y_out"], f) for r in res.results], axis=0)
    return out.reshape(1, SEQ, D)
```

```python
import numpy as np
from contextlib import ExitStack
import concourse.bass as bass
import concourse.mybir as mybir
from concourse.bass_utils import run_bass_kernel_spmd

F32 = mybir.dt.float32
BF16 = mybir.dt.bfloat16
U32 = mybir.dt.uint32
I32 = mybir.dt.int32
AF = mybir.ActivationFunctionType
ALU = mybir.AluOpType
AX = mybir.AxisListType

NCORES = 8
D = 1024
P = 128
H = 8
EPS = 1e-6
NEXP = 16384
INW = 6656
NEG = -1.0e30


class Buf:
    def __init__(self, name):
        self.name = name
        self.w = None
        self.r = {}
        self.dsem = None
        self.dcnt = 0


class Sched:
    def __init__(self, nc, es):
        self.nc = nc
        self.es = es
        self.E = {'pe': nc.tensor, 'act': nc.scalar, 'dve': nc.vector, 'pool': nc.gpsimd, 'sp': nc.sync}
        self.sem = {e: es.enter_context(nc.semaphore('sem_' + e)) for e in self.E}
        self.cnt = {e: 0 for e in self.E}
        self.known = {e: {} for e in self.E}
        self.nsem = 0

    def buf(self, name, dma=False):
        b = Buf(name)
        if dma:
            b.dsem = self.es.enter_context(self.nc.semaphore('d_' + name))
        return b

    def _wait(self, e, ev):
        if ev is None:
            return
        sem, val, src = ev
        if src == e and e == 'pe':
            return
        k = self.known[e]
        if k.get(id(sem), 0) >= val:
            return
        self.E[e].wait_ge(sem, val)
        k[id(sem)] = val

    @staticmethod
    def _flat(bs):
        out = []
        for b in bs:
            if isinstance(b, (list, tuple)):
                out.extend(Sched._flat(b))
            else:
                out.append(b)
        return out

    def _deps(self, e, reads, writes):
        reads = self._flat(reads); writes = self._flat(writes)
        for b in reads:
            self._wait(e, b.w)
        for b in writes:
            self._wait(e, b.w)
            for ev in list(b.r.values()):
                self._wait(e, ev)

    def _post(self, ev, reads, writes):
        reads = self._flat(reads); writes = self._flat(writes)
        for b in reads:
            old = b.r.get(id(ev[0]))
            if old is None or old[1] < ev[1]:
                b.r[id(ev[0])] = ev
        for b in writes:
            b.w = ev
            b.r = {}

    cap = None
    after_op = None

    def replay(self, cap, k=None):
        n = len(cap) if k is None else min(k, len(cap))
        for _ in range(n):
            kind, a = cap.pop(0)
            if kind == 'op':
                self.op(*a)
            else:
                self.dma(*a)

    def op(self, e, fn, reads=(), writes=()):
        if self.cap is not None:
            self.cap.append(('op', (e, fn, tuple(reads), tuple(writes))))
            return
        self._deps(e, reads, writes)
        ins = fn(self.E[e])
        self.cnt[e] += 1
        ins.then_inc(self.sem[e], 1)
        self._post((self.sem[e], self.cnt[e], e), reads, writes)
        if self.after_op is not None:
            h, self.after_op = self.after_op, None
            h()
            self.after_op = h

    def dma(self, q, fn, dbuf, reads=(), writes=()):
        if self.cap is not None:
            self.cap.append(('dma', (q, fn, dbuf, tuple(reads), tuple(writes))))
            return
        self._deps(q, reads, writes)
        ins = fn(self.E[q])
        dbuf.dcnt += 16
        ins.then_inc(dbuf.dsem, 16)
        self._post((dbuf.dsem, dbuf.dcnt, 'dma'), reads, writes)

    def wait_all(self, e, bufs):
        for b in self._flat(bufs):
            self._wait(e, b.w)
            for ev in list(b.r.values()):
                self._wait(e, ev)


class _Stop(Exception):
    pass


_H = {}
STAGE = 99
RUN_KW = {}
SKIP_GATHER = False
FORCE_NPRE = None


def build_program(NT, NPRE, dbg=False):
    nc = bass.Bass("TRN2", target_bir_lowering=False)
    es = ExitStack()
    S = Sched(nc, es)
    _H['nc'] = nc; _H['es'] = es

    def din(name, shape, dt=F32):
        return nc.dram_tensor(name, list(shape), dt, kind="ExternalInput").ap()

    x_own = din("x_own", [NT * P, D])
    x_halo = din("x_halo", [P, D])
    x_pre = din("x_pre", [max(NPRE, 1) * P, D])
    cos_own = din("cos_own", [P, NT, 32]); sin_own = din("sin_own", [P, NT, 32])
    cos_pre = din("cos_pre", [P, max(NPRE, 1), 32]); sin_pre = din("sin_pre", [P, max(NPRE, 1), 32])
    ksc_pre = din("ksc_pre", [P, max(NPRE, 1), H])
    g_attn = din("g_attn", [P, D]); g_ffn = din("g_ffn", [P, D]); g_gn = din("g_gn", [P, D])
    g_rq = din("g_rq", [P, 512]); g_rk = din("g_rk", [P, 512]); g_sq = din("g_sq", [P, 512]); g_sk = din("g_sk", [P, 512])
    c_ident = din("c_ident", [P, P]); c_tri = din("c_tri", [P, P]); c_ones = din("c_ones", [P, P])
    c_mpos = din("c_mpos", [P, 512]); c_mstay = din("c_mstay", [P, 1024])
    c_maskT = din("c_maskT", [P, 1024]); c_qdec = din("c_qdec", [P, H]); c_kdec = din("c_kdec", [P, H])
    c_cdec = din("c_cdec", [64, D])
    w_in = din("w_in", [D, INW]); w_bra = din("w_bra", [D, D]); w_brb = din("w_brb", [512, D]); w_out = din("w_out", [D, D])
    w_q = din("w_q", [D, 2048]); k1T = din("k1T", [P, P]); k2T = din("k2T", [P, P])
    u_tab = din("u_tab", [NEXP, D]); v_tab = din("v_tab", [NEXP, D])
    y_out = nc.dram_tensor("y_out", [NT * P, D], F32, kind="ExternalOutput").ap()
    uv_bf = nc.dram_tensor("uv_bf", [NEXP, 2 * D], BF16, kind="Internal").ap()
    NWB = 17
    wsc = nc.dram_tensor("wsc", [NWB * P, 4096], BF16, kind="Internal").ap()
    def dump(stage, src_ap, bsrc, ncols=D):
        if STAGE != stage:
            return
        b_d = S.buf("dump", dma=True)
        npart = src_ap.shape[0]
        S.dma('sp', lambda e: e.dma_start(out=y_out[0:npart, 0:ncols], in_=src_ap), b_d, reads=bsrc, writes=[b_d])
        S.wait_all('sp', [b_d])
        raise _Stop()

    tot = [0]

    def sb(name, shape, dt=F32):
        n = int(np.prod(shape[1:])) * (4 if dt in (F32, U32, I32) else 2)
        tot[0] += n
        if dbg:
            print("SB", name, n, tot[0])
        return es.enter_context(nc.sbuf_tensor(name, list(shape), dt))

    def ps(name, shape, dt=F32):
        return es.enter_context(nc.psum_tensor(name, list(shape), dt))

    psA = ps("psA", [P, 1024]); bA = S.buf("psA")
    psB = ps("psB", [P, 1024]); bB = S.buf("psB")
    psC = ps("psC", [P, 512]); bC = S.buf("psC")
    psD = ps("psD", [P, 512]); bD = S.buf("psD")
    psT = ps("psT", [P, 1024], BF16); bT = S.buf("psT")
    psS = ps("psS", [P, 512]); bS = S.buf("psS")

    consts = []

    def load_const(name, src, shape, dt=F32, q='sp'):
        t = sb(name, shape, dt)
        b = S.buf(name, dma=True)
        S.dma(q, lambda e: e.dma_start(out=t[:], in_=src), b, writes=[b])
        consts.append(b)
        return t, b

    def load_cast(name, src, shape):
        return load_const(name, src, shape, BF16, q='pool')

    ident, b_ident = load_cast("ident", c_ident, [P, P])
    tri, b_tri = load_cast("tri", c_tri, [P, P])
    ones, b_ones = load_cast("ones", c_ones, [P, P])
    mpos, b_mpos = load_cast("mpos", c_mpos, [P, 512])
    mstay, b_mstay = load_cast("mstay", c_mstay, [P, 1024])
    maskT, b_maskT = load_const("maskT", c_maskT, [P, 1024])
    qdec, b_qdec = load_const("qdec", c_qdec, [P, H])
    kdec, b_kdec = load_const("kdec", c_kdec, [P, H])
    cdec, b_cdec = load_const("cdec", c_cdec, [64, D])
    gattn, b_gattn = load_const("gattn", g_attn, [P, D])
    gffn, b_gffn = load_const("gffn", g_ffn, [P, D])
    ggn, b_ggn = load_const("ggn", g_gn, [P, D])
    grq, b_grq = load_const("grq", g_rq, [P, 512])
    grk, b_grk = load_const("grk", g_rk, [P, 512])
    gsq, b_gsq = load_const("gsq", g_sq, [P, 512])
    gsk, b_gsk = load_const("gsk", g_sk, [P, 512])
    cso, b_cso = load_const("cso", cos_own, [P, NT, 32])
    sno, b_sno = load_const("sno", sin_own, [P, NT, 32])

    def wview(w, c0, n):
        return w[:, c0:c0 + n].rearrange("(c p) n -> p c n", p=P)

    TPbig = sb("TPbig", [P, 10, D])
    TP = [TPbig[:, i, :] for i in range(10)]
    b_TP = [S.buf("TP%d" % i, dma=True) for i in range(10)]
    xt = [TP[0], TP[1]]
    b_xt = [b_TP[0], b_TP[1]]
    junk = sb("junk", [P, D], BF16); b_junk = S.buf("junk")
    st4 = sb("st4", [P, 4]); b_st4 = S.buf("st4")

    def rmsnorm_T(src, b_src, gain, b_gain, keep_f32=None, b_keep=None):
        S.op('act', lambda e: e.activation(out=junk[:], in_=src, func=AF.Square, accum_out=st4[:, 0:1]),
             reads=[b_src], writes=[b_junk, b_st4])
        S.op('act', lambda e: e.activation(out=st4[:, 1:2], in_=st4[:, 0:1], func=AF.Sqrt, bias=eps_t[:, 0:1], scale=1.0 / D),
             reads=[b_st4, b_eps], writes=[b_st4])
        S.op('dve', lambda e: e.reciprocal(out=st4[:, 2:3], in_=st4[:, 1:2]), reads=[b_st4], writes=[b_st4])
        if keep_f32 is not None:
            S.op('dve', lambda e: e.scalar_tensor_tensor(out=keep_f32, in0=src, scalar=st4[:, 2:3], in1=gain[:],
                                                         op0=ALU.mult, op1=ALU.mult),
                 reads=[b_src, b_st4, b_gain], writes=[b_keep])
            S.op('act', lambda e: e.activation(out=xn[:], in_=keep_f32, func=AF.Copy), reads=[b_keep], writes=[b_xn])
        else:
            S.op('dve', lambda e: e.scalar_tensor_tensor(out=xn[:], in0=src, scalar=st4[:, 2:3], in1=gain[:],
                                                         op0=ALU.mult, op1=ALU.mult),
                 reads=[b_src, b_st4, b_gain], writes=[b_xn])
        transpose8(xn, b_xn, xnT, b_xnT)

    def transpose8(src, b_src, dst, b_dst, nchunk=8):
        for half in range((nchunk + 3) // 4):
            n = min(4, nchunk - half * 4)
            for k in range(n):
                c = half * 4 + k
                S.op('pe', lambda e, c=c, k=k: e.transpose(out=psT[:, k * P:(k + 1) * P], in_=src[:, c * P:(c + 1) * P],
                                                           identity=ident[:]),
                     reads=[b_src, b_ident], writes=[bT])
            eng = 'act' if half % 2 == 0 else 'dve'
            if eng == 'act':
                S.op('act', lambda e, half=half, n=n: e.activation(
                    out=dst[:, half * 4:half * 4 + n, :].rearrange("p c t -> p (c t)"), in_=psT[:, 0:n * P], func=AF.Copy),
                    reads=[bT], writes=[b_dst])
            else:
                S.op('dve', lambda e, half=half, n=n: e.tensor_copy(
                    out=dst[:, half * 4:half * 4 + n, :].rearrange("p c t -> p (c t)"), in_=psT[:, 0:n * P]),
                    reads=[bT], writes=[b_dst])

    def transposeH(src3, b_src, dst, b_dst):
        for h in range(H):
            S.op('pe', lambda e, h=h: e.transpose(out=psT[0:64, h * P:(h + 1) * P], in_=src3[:, h, :], identity=ident[:]),
                 reads=[b_src, b_ident], writes=[bT])
        S.op('act', lambda e: e.activation(out=dst[:].rearrange("p h t -> p (h t)"), in_=psT[0:64, :], func=AF.Copy),
             reads=[bT], writes=[b_dst])

    def proj512(wb, b_wb, pst, b_pst):
        for c in range(8):
            S.op('pe', lambda e, c=c: e.matmul(out=pst, lhsT=xnT[:, c, :], rhs=wb[:, c, :], start=(c == 0), stop=(c == 7)),
                 reads=[b_xnT, b_wb], writes=[b_pst])

    def qknorm(pst, b_pst, gain, b_gain, slot, scale_ap=None, b_scale=None, cos=None, sin=None, b_cs=(), out_bf=None, b_out=None):
        S.op('act', lambda e: e.activation(out=qf[:], in_=pst, func=AF.Copy), reads=[b_pst], writes=[b_qf])
        S.op('act', lambda e: e.activation(out=sq_s[:], in_=pst, func=AF.Square), reads=[b_pst], writes=[b_sq])
        S.op('dve', lambda e: e.tensor_reduce(out=st8[:, 0, :], in_=sq_s[:].rearrange("p (h d) -> p h d", d=64), axis=AX.X, op=ALU.add),
             reads=[b_sq], writes=[b_st8])
        S.op('act', lambda e: e.activation(out=st8[:, 1, :], in_=st8[:, 0, :], func=AF.Sqrt, bias=eps_t[:, 0:1], scale=1.0 / 64),
             reads=[b_st8, b_eps], writes=[b_st8])
        S.op('dve', lambda e: e.reciprocal(out=st8[:, 2, :], in_=st8[:, 1, :]), reads=[b_st8], writes=[b_st8])
        rs = st8[:, 2, :]
        if scale_ap is not None:
            S.op('dve', lambda e: e.tensor_tensor(out=st8[:, 3, :], in0=st8[:, 2, :], in1=scale_ap, op=ALU.mult),
                 reads=[b_st8, b_scale], writes=[b_st8])
            rs = st8[:, 3, :]
        S.op('dve', lambda e: e.tensor_tensor(out=qn[:].rearrange("p (h d) -> p h d", d=64), in0=qf[:].rearrange("p (h d) -> p h d", d=64),
                                              in1=rs.unsqueeze(2).to_broadcast([P, H, 64]), op=ALU.mult),
             reads=[b_qf, b_st8], writes=[b_qn])
        if cos is None:
            S.op('dve', lambda e: e.tensor_tensor(out=out_bf[:].rearrange("p h d -> p (h d)"), in0=qn[:], in1=gain[:], op=ALU.mult),
                 reads=[b_qn, b_gain], writes=[b_out])
            return
        S.op('pool', lambda e: e.tensor_tensor(out=qn[:], in0=qn[:], in1=gain[:], op=ALU.mult), reads=[b_qn, b_gain], writes=[b_qn])
        q3 = qn[:].rearrange("p (h d) -> p h d", d=64)
        x1 = q3[:, :, 0:32]; x2 = q3[:, :, 32:64]
        cb = cos.unsqueeze(1).to_broadcast([P, H, 32]); sbb = sin.unsqueeze(1).to_broadcast([P, H, 32])
        r = [rt[:, i, :].rearrange("p (h d) -> p h d", d=32) for i in range(4)]
        S.op('dve', lambda e: e.tensor_tensor(out=r[0], in0=x1, in1=cb, op=ALU.mult), reads=[b_qn] + list(b_cs), writes=[b_rt])
        S.op('pool', lambda e: e.tensor_tensor(out=r[1], in0=x2, in1=sbb, op=ALU.mult), reads=[b_qn] + list(b_cs), writes=[b_rt])
        S.op('dve', lambda e: e.tensor_tensor(out=r[2], in0=x1, in1=sbb, op=ALU.mult), reads=[b_qn] + list(b_cs), writes=[b_rt])
        S.op('pool', lambda e: e.tensor_tensor(out=r[3], in0=x2, in1=cb, op=ALU.mult), reads=[b_qn] + list(b_cs), writes=[b_rt])
        S.op('dve', lambda e: e.tensor_tensor(out=out_bf[:, :, 0:32], in0=r[0], in1=r[1], op=ALU.subtract), reads=[b_rt], writes=[b_out])
        S.op('dve', lambda e: e.tensor_tensor(out=out_bf[:, :, 32:64], in0=r[2], in1=r[3], op=ALU.add), reads=[b_rt], writes=[b_out])

    b_ubf = S.buf("uv_bf", dma=True); b_vbf = b_ubf
    b_wsc = S.buf("wsc", dma=True)
    identF, b_identF = load_const("identF", c_ident, [P, P])
    eps_t = sb("eps_t", [P, 1]); b_eps = S.buf("eps")
    S.op('dve', lambda e: e.memset(eps_t[:], EPS), writes=[b_eps])
    gk_t = sb("gk_t", [P, 1]); b_gk = S.buf("gk")
    S.op('dve', lambda e: e.memset(gk_t[:], 1.5957691216057308), writes=[b_gk])
    one_t = sb("one_t", [P, 1]); b_one = S.buf("one")
    S.op('dve', lambda e: e.memset(one_t[:], 1.0), writes=[b_one])

    state = sb("state", [64, D]); b_state = S.buf("state")
    state_bf = sb("state_bf", [64, D], BF16); b_state_bf = S.buf("state_bf")

    with ExitStack() as es0:
        RB = 2
        NBLK = NEXP // (128 * RB)
        NCB = 4
        cb = [es0.enter_context(nc.sbuf_tensor("cb%d" % i, [P, RB, D], BF16)) for i in range(NCB)]
        b_cb = [S.buf("cb%d" % i, dma=True) for i in range(NCB)]
        b_cin = b_cb; b_cout = []
        pc_jobs = [(tab, dst, bd, blk) for blk in range(NBLK) for (tab, dst, bd) in ((u_tab, uv_bf[:, 0:D], b_ubf), (v_tab, uv_bf[:, D:2 * D], b_vbf))]
        pc_state = [0, 0]

        def _pc_store(k):
            tab, dst, bd, blk = pc_jobs[k]
            i = k % NCB
            S.dma('pool', lambda e: e.dma_start(out=dst[blk * 128 * RB:(blk + 1) * 128 * RB, :].rearrange("(p r) d -> p r d", r=RB), in_=cb[i][:]),
                  bd, reads=[b_cb[i]], writes=[bd])

        def precast(nblocks):
            for _ in range(nblocks):
                k = pc_state[0]
                if k >= len(pc_jobs):
                    break
                pc_state[0] += 1
                tab, dst, bd, blk = pc_jobs[k]
                i = k % NCB
                S.dma('pool', lambda e, tab=tab, blk=blk, i=i: e.dma_start(out=cb[i][:], in_=tab[blk * 128 * RB:(blk + 1) * 128 * RB, :].rearrange("(p r) d -> p r d", r=RB)),
                      b_cb[i], writes=[b_cb[i]])
                if k - 2 >= 0:
                    _pc_store(k - 2)
                    pc_state[1] = k - 1
            if pc_state[0] >= len(pc_jobs):
                while pc_state[1] < len(pc_jobs):
                    _pc_store(pc_state[1])
                    pc_state[1] += 1

        wjobs = [(w_in if blk < 13 else w_q, (blk if blk < 13 else blk - 13) * 512, blk, cc) for blk in range(NWB) for cc in range(4)]

        def _w_store(k):
            src, c0, blk, cc = wjobs[k]
            i = k % NCB
            dstv = wsc[blk * P:(blk + 1) * P, :].rearrange("p (c n) -> p c n", c=8)[:, 2 * cc:2 * cc + 2, :]
            S.dma('pool', lambda e: e.dma_start(out=dstv, in_=cb[i][:, 0, :].rearrange("p (c n) -> p c n", c=2)), b_wsc, reads=[b_cb[i]], writes=[b_wsc])

        for k, (src, c0, blk, cc) in enumerate(wjobs):
            i = k % NCB
            S.dma('pool', lambda e, src=src, c0=c0, cc=cc, i=i: e.dma_start(out=cb[i][:, 0, :].rearrange("p (c n) -> p c n", c=2),
                                                                          in_=wview(src, c0, 512)[:, 2 * cc:2 * cc + 2, :]),
                  b_cb[i], writes=[b_cb[i]])
            if k - 2 >= 0:
                _w_store(k - 2)
        _w_store(len(wjobs) - 2); _w_store(len(wjobs) - 1)
        pc_per_tile = -(-len(pc_jobs) // max(NPRE, 1))
        if NPRE > 0:
            wk = es0.enter_context(nc.sbuf_tensor("wk_pre", [P, 8, 512], BF16)); b_wk = S.buf("wk_pre", dma=True)
            wv = es0.enter_context(nc.sbuf_tensor("wv_pre", [P, 8, 1024], BF16)); b_wv = S.buf("wv_pre", dma=True)
            csp = es0.enter_context(nc.sbuf_tensor("csp", [P, NPRE, 32], F32)); b_csp = S.buf("csp", dma=True)
            snp = es0.enter_context(nc.sbuf_tensor("snp", [P, NPRE, 32], F32)); b_snp = S.buf("snp", dma=True)
            ksp = es0.enter_context(nc.sbuf_tensor("ksp", [P, NPRE, H], F32)); b_ksp = S.buf("ksp", dma=True)
            S.dma('pool', lambda e: e.dma_start(out=wk[:], in_=wview(w_in, 512, 512)), b_wk, writes=[b_wk])
            for hh in range(2):
                S.dma('pool', lambda e, hh=hh: e.dma_start(out=wv[:, :, hh * 512:(hh + 1) * 512], in_=wview(w_in, 1024 + hh * 512, 512)),
                      b_wv, writes=[b_wv])
            S.dma('sp', lambda e: e.dma_start(out=csp[:], in_=cos_pre), b_csp, writes=[b_csp])
            S.dma('sp', lambda e: e.dma_start(out=snp[:], in_=sin_pre), b_snp, writes=[b_snp])
            S.dma('sp', lambda e: e.dma_start(out=ksp[:], in_=ksc_pre), b_ksp, writes=[b_ksp])
            B0 = 4
            def _al(name, shape, dt):
                return es0.enter_context(nc.sbuf_tensor(name, list(shape), dt))
            xnb = _al("xnb", [P, 2, D], BF16); xnTb = _al("xnTb", [P, 2, 8, P], BF16); st4b = _al("st4b", [P, B0, 4], F32)
            b_xnb = [S.buf("xnb%d" % (i % 2)) for i in range(2)] * 2; b_xnTb = [S.buf("xnTb%d" % (i % 2)) for i in range(2)] * 2; b_st4b = [S.buf("st4b%d" % i) for i in range(B0)]
            sets = []
            for si in range(2):
                d_ = dict(kf=TPbig[:, 2 + 4 * si:4 + 4 * si, :].rearrange("p a d -> p (a d)").rearrange("p (b n) -> p b n", b=B0),
                          sq=TPbig[:, 4 + 4 * si:6 + 4 * si, :].rearrange("p a d -> p (a d)").rearrange("p (b n) -> p b n", b=B0),
                          s8=_al("s8b%d" % si, [P, 4, B0 * H], F32),
                          r1=_al("r1b%d" % si, [P, B0 * 256], F32),
                          kd=_al("kdb%d" % si, [P, B0, H, 64], BF16), v=_al("vb%d" % si, [P, B0, D], BF16))
                d_.update(b_kf=S.buf("kfb%d" % si), b_sq=S.buf("sqb%d" % si), b_s8=S.buf("s8b%d" % si), b_r0=S.buf("r0b%d" % si),
                          b_r1=S.buf("r1b%d" % si), b_kd=S.buf("kdb%d" % si), b_v=S.buf("vb%d" % si))
                d_["r0"] = d_["kf"].rearrange("p b n -> p (b n)")[:, 0:B0 * 256]; d_["b_r0"] = d_["b_kf"]
                sets.append(d_)
            all_pre_bufs = b_xnb + b_xnTb + b_st4b + [sets[i][k] for i in range(2) for k in ("b_kf", "b_sq", "b_s8", "b_r0", "b_r1", "b_kd", "b_v")]

            def front_a(m, b, st):
                xb = xt[m % 2]; bx = b_xt[m % 2]
                S.dma('sp', lambda e: e.dma_start(out=xb[:], in_=x_pre[m * P:(m + 1) * P, :]), bx, writes=[bx])
                S.op('act', lambda e: e.activation(out=junk[:], in_=xb[:], func=AF.Square, accum_out=st4b[:, b, 0:1]), reads=[bx], writes=[b_junk, b_st4b[b]])
                S.op('act', lambda e: e.activation(out=st4b[:, b, 1:2], in_=st4b[:, b, 0:1], func=AF.Sqrt, bias=eps_t[:, 0:1], scale=1.0 / D),
                     reads=[b_st4b[b], b_eps], writes=[b_st4b[b]])
                S.op('dve', lambda e: e.reciprocal(out=st4b[:, b, 2:3], in_=st4b[:, b, 1:2]), reads=[b_st4b[b]], writes=[b_st4b[b]])
                S.op('dve', lambda e: e.scalar_tensor_tensor(out=xnb[:, b % 2, :], in0=xb[:], scalar=st4b[:, b, 2:3], in1=gattn[:], op0=ALU.mult, op1=ALU.mult),
                     reads=[bx, b_st4b[b], b_gattn], writes=[b_xnb[b]])

            def front_b(m, b, st):
                precast(pc_per_tile)
                for half in range(2):
                    for k in range(4):
                        c = half * 4 + k
                        S.op('pe', lambda e, c=c, k=k: e.transpose(out=psT[:, k * P:(k + 1) * P], in_=xnb[:, b % 2, c * P:(c + 1) * P], identity=ident[:]),
                             reads=[b_xnb[b], b_ident], writes=[bT])
                    dst = xnTb[:, b % 2, half * 4:half * 4 + 4, :].rearrange("p c t -> p (c t)")
                    if half == 0:
                        S.op('act', lambda e, dst=dst: e.activation(out=dst, in_=psT[:, 0:512], func=AF.Copy), reads=[bT], writes=[b_xnTb[b]])
                    else:
                        S.op('dve', lambda e, dst=dst: e.tensor_copy(out=dst, in_=psT[:, 0:512]), reads=[bT], writes=[b_xnTb[b]])
                for c in range(8):
                    S.op('pe', lambda e, c=c: e.matmul(out=psC[:], lhsT=xnTb[:, b % 2, c, :], rhs=wk[:, c, :], start=(c == 0), stop=(c == 7)),
                         reads=[b_xnTb[b], b_wk], writes=[bC])
                S.op('act', lambda e: e.activation(out=st["kf"][:, b, :], in_=psC[:], func=AF.Copy), reads=[bC], writes=[st["b_kf"]])
                S.op('act', lambda e: e.activation(out=st["sq"][:, b, :], in_=psC[:], func=AF.Square), reads=[bC], writes=[st["b_sq"]])
                psV, bV = (psA, bA) if m % 2 == 0 else (psB, bB)
                for hh in range(2):
                    for c in range(8):
                        S.op('pe', lambda e, c=c, hh=hh: e.matmul(out=psV[:, hh * 512:(hh + 1) * 512], lhsT=xnTb[:, b % 2, c, :], rhs=wv[:, c, hh * 512:(hh + 1) * 512],
                                                                  start=(c == 0), stop=(c == 7)), reads=[b_xnTb[b], b_wv], writes=[bV])
                S.op('act', lambda e: e.activation(out=st["v"][:, b, :], in_=psV[:], func=AF.Copy), reads=[bV], writes=[st["b_v"]])

            def chain_gen(m0, nb, st):
                n8 = nb * H
                kf, sq, s8, r0, r1, kdb = st["kf"], st["sq"], st["s8"], st["r0"], st["r1"], st["kd"]
                S.op('dve', lambda e: e.tensor_reduce(out=s8[:, 0, 0:n8], in_=sq[:, 0:nb, :].rearrange("p b (h d) -> p (b h) d", d=64), axis=AX.X, op=ALU.add),
                     reads=[st["b_sq"]], writes=[st["b_s8"]]); yield
                S.op('act', lambda e: e.activation(out=s8[:, 1, 0:n8], in_=s8[:, 0, 0:n8], func=AF.Sqrt, bias=eps_t[:, 0:1], scale=1.0 / 64),
                     reads=[st["b_s8"], b_eps], writes=[st["b_s8"]]); yield
                S.op('dve', lambda e: e.reciprocal(out=s8[:, 2, 0:n8], in_=s8[:, 1, 0:n8]), reads=[st["b_s8"]], writes=[st["b_s8"]]); yield
                S.op('dve', lambda e: e.tensor_tensor(out=s8[:, 3, 0:n8], in0=s8[:, 2, 0:n8], in1=ksp[:, m0:m0 + nb, :].rearrange("p m h -> p (m h)"), op=ALU.mult),
                     reads=[st["b_s8"], b_ksp], writes=[st["b_s8"]]); yield
                q3 = sq[:, 0:nb, :].rearrange("p b (h d) -> p (b h) d", d=64)
                S.op('dve', lambda e: e.tensor_tensor(out=q3, in0=kf[:, 0:nb, :].rearrange("p b (h d) -> p (b h) d", d=64),
                                                      in1=s8[:, 3, 0:n8].unsqueeze(2).to_broadcast([P, n8, 64]), op=ALU.mult),
                     reads=[st["b_kf"], st["b_s8"]], writes=[st["b_sq"]]); yield
                S.op('pool', lambda e: e.tensor_tensor(out=sq[:, 0:nb, :], in0=sq[:, 0:nb, :], in1=grk[:].unsqueeze(1).to_broadcast([P, nb, 512]), op=ALU.mult),
                     reads=[st["b_sq"], b_grk], writes=[st["b_sq"]]); yield
                q4 = sq[:, 0:nb, :].rearrange("p b (h d) -> p b h d", d=64)
                x1_ = q4[:, :, :, 0:32]; x2_ = q4[:, :, :, 32:64]
                cb = csp[:, m0:m0 + nb, :].unsqueeze(2).to_broadcast([P, nb, H, 32]); sb_ = snp[:, m0:m0 + nb, :].unsqueeze(2).to_broadcast([P, nb, H, 32])
                r0v = r0[:, 0:nb * 256].rearrange("p (b h d) -> p b h d", b=nb, h=H); r1v = r1[:, 0:nb * 256].rearrange("p (b h d) -> p b h d", b=nb, h=H)
                S.op('dve', lambda e: e.tensor_tensor(out=r0v, in0=x1_, in1=cb, op=ALU.mult), reads=[st["b_sq"], b_csp], writes=[st["b_r0"]]); yield
                S.op('pool', lambda e: e.tensor_tensor(out=r1v, in0=x2_, in1=sb_, op=ALU.mult), reads=[st["b_sq"], b_snp], writes=[st["b_r1"]]); yield
                S.op('dve', lambda e: e.tensor_tensor(out=kdb[:, 0:nb, :, 0:32], in0=r0v, in1=r1v, op=ALU.subtract), reads=[st["b_r0"], st["b_r1"]], writes=[st["b_kd"]]); yield
                S.op('dve', lambda e: e.tensor_tensor(out=r0v, in0=x1_, in1=sb_, op=ALU.mult), reads=[st["b_sq"], b_snp], writes=[st["b_r0"]]); yield
                S.op('pool', lambda e: e.tensor_tensor(out=r1v, in0=x2_, in1=cb, op=ALU.mult), reads=[st["b_sq"], b_csp], writes=[st["b_r1"]]); yield
                S.op('dve', lambda e: e.tensor_tensor(out=kdb[:, 0:nb, :, 32:64], in0=r0v, in1=r1v, op=ALU.add), reads=[st["b_r0"], st["b_r1"]], writes=[st["b_kd"]]); yield

            def state_mm(m0, nb, st):
                for b in range(nb):
                    m = m0 + b
                    for h in range(H):
                        acc_ps, b_acc_ps = (psS, bS) if h < 4 else (psD, bD)
                        S.op('pe', lambda e, h=h, m=m, b=b, acc_ps=acc_ps: e.matmul(
                            out=acc_ps[0:64, (h % 4) * P:(h % 4 + 1) * P], lhsT=st["kd"][:, b, h, :], rhs=st["v"][:, b, h * P:(h + 1) * P],
                            start=(m == 0 and h % 4 == 0), stop=(m == NPRE - 1), skip_group_check=True),
                            reads=[st["b_kd"], st["b_v"]], writes=[b_acc_ps])

            batches = [(m0, min(B0, NPRE - m0)) for m0 in range(0, NPRE, B0)]
            tiles = [(m0 + b, b, sets[bi % 2]) for bi, (m0, nb) in enumerate(batches) for b in range(nb)]
            pend = None
            front_a(*tiles[0])
            ti_ = 0
            for bi, (m0, nb) in enumerate(batches):
                st = sets[bi % 2]
                gen = chain_gen(*pend) if pend is not None else None
                for b in range(nb):
                    if ti_ + 1 < len(tiles):
                        front_a(*tiles[ti_ + 1])
                    front_b(m0 + b, b, st)
                    ti_ += 1
                    if gen is not None:
                        for _ in range(4):
                            next(gen, None)
                if gen is not None:
                    for _ in gen:
                        pass
                    state_mm(*pend)
                pend = (m0, nb, st)
            for _ in chain_gen(*pend):
                pass
            state_mm(*pend)
            for e_ in ('pe', 'act', 'dve', 'pool', 'sp'):
                S.wait_all(e_, all_pre_bufs)
            S.op('act', lambda e: e.activation(out=state[:, 0:512], in_=psS[0:64, :], func=AF.Copy), reads=[bS], writes=[b_state])
            S.op('act', lambda e: e.activation(out=state[:, 512:1024], in_=psD[0:64, :], func=AF.Copy), reads=[bD], writes=[b_state])
        else:
            S.op('dve', lambda e: e.memset(state[:], 0.0), writes=[b_state])
        S.op('act', lambda e: e.activation(out=state_bf[:], in_=state[:], func=AF.Copy), reads=[b_state], writes=[b_state_bf])
        precast(len(pc_jobs))
        for e_ in ('pe', 'act', 'dve', 'pool', 'sp'):
            S.wait_all(e_, b_cin + b_cout + [b_ubf, b_wsc])
        if NPRE > 0:
            for e_ in ('pe', 'act', 'dve', 'pool'):
                S.wait_all(e_, [b_wk, b_wv, b_csp, b_snp, b_ksp])

    dump(1, state[:], [b_state], D)
    xn = sb("xn", [P, D], BF16); b_xn = S.buf("xn")
    xnT = sb("xnT", [P, 8, P], BF16); b_xnT = S.buf("xnT")
    qf = sb("qf", [P, 512]); b_qf = S.buf("qf")
    sq_s = sb("sq_s", [P, 512]); b_sq = S.buf("sq_s")
    st8 = sb("st8", [P, 4, H]); b_st8 = S.buf("st8")
    qn = sb("qn", [P, 512]); b_qn = S.buf("qn")
    rt = sb("rt", [P, 4, 256]); b_rt = S.buf("rt")
    kd = sb("kd", [P, H, 64], BF16); b_kd = S.buf("kd")
    v_r = sb("v_r", [P, D], BF16); b_vr = S.buf("v_r")
    wbra = sb("wbra", [P, 8, D], BF16); b_wbra = S.buf("wbra", dma=True)
    wbrb = sb("wbrb", [P, 4, D], BF16); b_wbrb = S.buf("wbrb", dma=True)
    wout = sb("wout", [P, 8, D], BF16); b_wout = S.buf("wout", dma=True)
    k1 = sb("k1", [P, P], BF16); b_k1 = S.buf("k1", dma=True)
    k2 = sb("k2", [P, P], BF16); b_k2 = S.buf("k2", dma=True)
    for hh in range(2):
        S.dma('pool', lambda e, hh=hh: e.dma_start(out=wbra[:, :, hh * 512:(hh + 1) * 512], in_=wview(w_bra, hh * 512, 512)), b_wbra, writes=[b_wbra])
        S.dma('pool', lambda e, hh=hh: e.dma_start(out=wbrb[:, :, hh * 512:(hh + 1) * 512], in_=wview(w_brb, hh * 512, 512)), b_wbrb, writes=[b_wbrb])
        S.dma('pool', lambda e, hh=hh: e.dma_start(out=wout[:, :, hh * 512:(hh + 1) * 512], in_=wview(w_out, hh * 512, 512)), b_wout, writes=[b_wout])
    S.dma('pool', lambda e: e.dma_start(out=k1[:], in_=k1T), b_k1, writes=[b_k1])
    S.dma('pool', lambda e: e.dma_start(out=k2[:], in_=k2T), b_k2, writes=[b_k2])

    wbuf = [sb("wbuf%d" % i, [P, 8, 512], BF16) for i in range(2)]
    b_wbuf = [S.buf("wbuf%d" % i, dma=True) for i in range(2)]
    wcount = [0]

    def stream_w(c0, src=None):
        blk = c0 // 512 if src is None else 13 + c0 // 512
        i = wcount[0] % 2
        wcount[0] += 1
        S.dma('sp', lambda e: e.dma_start(out=wbuf[i][:], in_=wsc[blk * P:(blk + 1) * P, :].rearrange("p (c n) -> p c n", c=8)),
              b_wbuf[i], reads=[b_wsc], writes=[b_wbuf[i]])
        return wbuf[i], b_wbuf[i]

    xres = sb("xres", [P, D]); b_xres = S.buf("xres", dma=True)
    qd = sb("qd", [P, H, 64], BF16); b_qd = S.buf("qd")
    qdT = sb("qdT", [64, H, P], BF16); b_qdT = S.buf("qdT")
    kdT = sb("kdT", [64, H, P], BF16); b_kdT = S.buf("kdT")
    rg = sb("rg", [P, D], BF16); b_rg = S.buf("rg")
    sqn = sb("sqn", [P, H, 64], BF16); b_sqn = S.buf("sqn")
    skn = sb("skn", [P, H, 64], BF16); b_skn = S.buf("skn")
    sqT = sb("sqT", [64, H, P], BF16); b_sqT = S.buf("sqT")
    skT = [sb("skT%d" % i, [64, H, P], BF16) for i in range(2)]; b_skT = [S.buf("skT%d" % i) for i in range(2)]
    sv = [sb("sv%d" % i, [P, 512], BF16) for i in range(2)]; b_sv = [S.buf("sv%d" % i) for i in range(2)]
    siga = TP[7]; b_siga = b_TP[7]
    sigb = TP[8]; b_sigb = b_TP[8]
    pm = sb("pm", [P, D], BF16); b_pm = S.buf("pm")
    yf = TP[3]; b_yf = b_TP[3]
    ysq = TP[4]; b_ysq = b_TP[4]
    gst = sb("gst", [P, 6, H]); b_gst = S.buf("gst")
    ret = sb("ret", [P, D], BF16); b_ret = S.buf("ret")
    retT = sb("retT", [P, 8, P], BF16); b_retT = S.buf("retT")
    e_s = TP[3]; b_es = b_TP[3]
    sp_s = TP[4]; b_sp = b_TP[4]
    spm = sb("spm", [P, D], BF16); b_spm = S.buf("spm")
    u_s = TP[0]; b_us = b_TP[0]
    w_s = sb("w_s", [P, D], BF16); b_ws = S.buf("w_s")
    sbT = sb("sbT", [P, 4, P], BF16); b_sbT = S.buf("sbT")
    m1 = TP[5]; b_m1 = b_TP[5]
    m2 = TP[6]; b_m2 = b_TP[6]
    mixed = ret; b_mixed = b_ret
    mixT = retT; b_mixT = b_retT
    x1 = TP[1]; b_x1 = b_TP[1]
    hn = TP[9]; b_hn = b_TP[9]
    qT = sb("qT", [P, 16, P], BF16); b_qT = S.buf("qT")
    sc = TPbig[:, 3:5, :].rearrange("p a d -> p (a d)").rearrange("p (g n) -> p g n", g=16); b_sc = [b_TP[3], b_TP[4]]
    sc2 = TPbig[:, 5:7, :].rearrange("p a d -> p (a d)").rearrange("p (g n) -> p g n", g=16); b_sc2 = [b_TP[5], b_TP[6]]
    tv = sb("tv", [P, 16, 16]); b_tv = S.buf("tv")
    ti = sb("ti", [P, 16, 16], U32); b_ti = S.buf("ti")
    tif = sb("tif", [P, 16, 16]); b_tif = S.buf("tif")
    cand = TPbig[:, 7:9, :].rearrange("p a d -> p (a d)").rearrange("p (h c) -> p h c", h=H); b_cand = [b_TP[7], b_TP[8]]
    cand2 = sc.rearrange("p g n -> p (g n)").rearrange("p (h c) -> p h c", h=H); b_cand2 = b_sc
    tsv = sb("tsv", [P, H, 16]); b_tsv = S.buf("tsv")
    tpos = sb("tpos", [P, H, 16], U32); b_tpos = S.buf("tpos")
    tposf = sb("tposf", [P, H, 16]); b_tposf = S.buf("tposf")
    ta = sb("ta", [P, H, 16]); b_ta = S.buf("ta")
    tb = sb("tb", [P, H, 16]); b_tb = S.buf("tb")
    iota16 = sb("iota16", [P, 16]); b_iota = S.buf("iota16")
    oh = sc2.rearrange("p g n -> p (g n)").rearrange("p (h a b) -> p h a b", h=H, a=16); b_oh = b_sc2
    idx1 = sb("idx1", [P, H, 16]); b_idx1 = S.buf("idx1")
    idx2 = sb("idx2", [P, H, 16]); b_idx2 = S.buf("idx2")
    eidx = sb("eidx", [P, 128], U32); b_eidx = S.buf("eidx")
    gw = sb("gw", [P, H, 16]); b_gw = S.buf("gw")
    gs = sb("gs", [P, 2, H]); b_gs = S.buf("gs")
    hv = sb("hv", [P, 128]); b_hv = S.buf("hv")
    ga_ = sb("ga_", [P, 6, 128]); b_ga = S.buf("ga_"); b_ga2 = S.buf("ga2"); b_ga3 = S.buf("ga3")
    aw = sb("aw", [P, 128]); b_aw = S.buf("aw")
    NG = 8
    dg = [sb("dg%d" % i, [P, P], BF16) for i in range(4)]; b_dg = [S.buf("dg%d" % i) for i in range(4)]
    gbuf = None; b_gbuf = None
    acc = TP[0]; b_acc = b_TP[0]
    b_yout = S.buf("yout", dma=True)

    _gi = [3, 4, 5, 6, 7, 8, 2, 0]
    gbuf = [TPbig[:, i, :].bitcast(BF16) for i in _gi]
    b_gbuf = [b_TP[i] for i in _gi]
    gsem = [S.buf("gsem%d" % i, dma=True) for i in range(len(_gi))]
    S.op('pool', lambda e: e.iota(iota16[:], pattern=[[1, 16]], base=0, channel_multiplier=0, allow_small_or_imprecise_dtypes=True),
         writes=[b_iota])
    thr16 = sb("thr16", [P, 16])
    S.op('pool', lambda e: e.iota(thr16[:], pattern=[[16, 16]], base=0, channel_multiplier=0, allow_small_or_imprecise_dtypes=True),
         reads=[b_iota], writes=[b_iota])

    def sb_kv(cur):
        wb, bw = stream_w(3584)
        proj512(wb, bw, psC[:], bC)
        qknorm(psC[:], bC, gsk, b_gsk, 0, out_bf=skn, b_out=b_skn)
        transposeH(skn, b_skn, skT[cur], b_skT[cur])
        wb, bw = stream_w(4096)
        proj512(wb, bw, psD[:], bD)
        S.op('act', lambda e: e.activation(out=sv[cur][:], in_=psD[:], func=AF.Copy), reads=[bD], writes=[b_sv[cur]])

    S.dma('sp', lambda e: e.dma_start(out=xt[0][:], in_=x_halo), b_xt[0], writes=[b_xt[0]])
    rmsnorm_T(xt[0][:], b_xt[0], gattn, b_gattn)
    sb_kv(1)
    dump(2, xt[0][:], [b_xt[0], b_sv[1], b_skT[1]])

    def m_head(n):
        cur = n % 2
        S.dma('sp', lambda e, n=n: e.dma_start(out=xres[:], in_=x_own[n * P:(n + 1) * P, :]), b_xres, writes=[b_xres])
        rmsnorm_T(xres[:], b_xres, gattn, b_gattn)
        wb, bw = stream_w(0)
        proj512(wb, bw, psC[:], bC)
        qknorm(psC[:], bC, grq, b_grq, 0, scale_ap=qdec[:], b_scale=b_qdec, cos=cso[:, n, :], sin=sno[:, n, :],
               b_cs=[b_cso, b_sno], out_bf=qd, b_out=b_qd)
        transposeH(qd, b_qd, qdT, b_qdT)
        wb, bw = stream_w(512)
        proj512(wb, bw, psD[:], bD)
        qknorm(psD[:], bD, grk, b_grk, 0, scale_ap=kdec[:], b_scale=b_kdec, cos=cso[:, n, :], sin=sno[:, n, :],
               b_cs=[b_cso, b_sno], out_bf=kd, b_out=b_kd)
        transposeH(kd, b_kd, kdT, b_kdT)
        wb, bw = stream_w(3072)
        proj512(wb, bw, psC[:], bC)
        qknorm(psC[:], bC, gsq, b_gsq, 0, out_bf=sqn, b_out=b_sqn)
        transposeH(sqn, b_sqn, sqT, b_sqT)
        sb_kv(cur)
        for hh in range(2):
            wb, bw = stream_w(1024 + hh * 512)
            proj512(wb, bw, psB[:, hh * 512:(hh + 1) * 512], bB)
        S.op('act', lambda e: e.activation(out=v_r[:], in_=psB[:], func=AF.Copy), reads=[bB], writes=[b_vr])
        for hh in range(2):
            wb, bw = stream_w(2048 + hh * 512)
            proj512(wb, bw, psB[:, hh * 512:(hh + 1) * 512], bB)
        S.op('act', lambda e: e.activation(out=rg[:], in_=psB[:], func=AF.Silu), reads=[bB], writes=[b_rg])
        S.op('pool', lambda e: e.tensor_tensor(out=rg[:], in0=rg[:], in1=ggn[:], op=ALU.mult), reads=[b_rg, b_ggn], writes=[b_rg])

    m_head(0)
    for n in range(NT):
        cur = n % 2; prv = 1 - cur
        for h in range(H):
            S.op('pe', lambda e, h=h: e.matmul(out=psA[:, h * P:(h + 1) * P], lhsT=kdT[:, h, :], rhs=qdT[:, h, :], start=True, stop=True),
                 reads=[b_kdT, b_qdT], writes=[bA])
        S.op('dve', lambda e: e.tensor_tensor(out=pm[:], in0=psA[:], in1=maskT[:], op=ALU.mult), reads=[bA, b_maskT], writes=[b_pm])
        for h in range(H):
            S.op('pe', lambda e, h=h: e.matmul(out=psB[:, h * P:(h + 1) * P], lhsT=pm[:, h * P:(h + 1) * P], rhs=v_r[:, h * P:(h + 1) * P],
                                               start=True, stop=False, skip_group_check=True),
                 reads=[b_pm, b_vr], writes=[bB])
            S.op('pe', lambda e, h=h: e.matmul(out=psB[:, h * P:(h + 1) * P], lhsT=qdT[:, h, :], rhs=state_bf[:, h * P:(h + 1) * P],
                                               start=False, stop=True, skip_group_check=True),
                 reads=[b_qdT, b_state_bf], writes=[bB])
        S.op('dve', lambda e: e.tensor_tensor(out=state[:], in0=state[:], in1=cdec[:], op=ALU.mult), reads=[b_state, b_cdec], writes=[b_state])
        for rnd in range(2):
            for hl in range(4):
                h = rnd * 4 + hl
                S.op('pe', lambda e, h=h, hl=hl: e.matmul(out=psS[0:64, hl * P:(hl + 1) * P], lhsT=kd[:, h, :], rhs=v_r[:, h * P:(h + 1) * P],
                                                   start=True, stop=True, skip_group_check=True),
                     reads=[b_kd, b_vr], writes=[bS])
            S.op('dve', lambda e, rnd=rnd: e.tensor_tensor(out=state[:, rnd * 512:(rnd + 1) * 512], in0=state[:, rnd * 512:(rnd + 1) * 512],
                                                         in1=psS[0:64, :], op=ALU.add), reads=[b_state, bS], writes=[b_state])
        S.op('act', lambda e: e.activation(out=state_bf[:], in_=state[:], func=AF.Copy), reads=[b_state], writes=[b_state_bf])
        S.op('act', lambda e: e.activation(out=yf[:], in_=psB[:], func=AF.Copy), reads=[bB], writes=[b_yf])
        S.op('act', lambda e: e.activation(out=ysq[:], in_=psB[:], func=AF.Square), reads=[bB], writes=[b_ysq])
        S.op('dve', lambda e: e.tensor_reduce(out=gst[:, 0, :], in_=yf[:].rearrange("p (h d) -> p h d", d=P), axis=AX.X, op=ALU.add),
             reads=[b_yf], writes=[b_gst])
        S.op('dve', lambda e: e.tensor_reduce(out=gst[:, 1, :], in_=ysq[:].rearrange("p (h d) -> p h d", d=P), axis=AX.X, op=ALU.add),
             reads=[b_ysq], writes=[b_gst])
        S.op('dve', lambda e: e.tensor_scalar(out=gst[:, 2, :], in0=gst[:, 0, :], scalar1=1.0 / P, scalar2=None, op0=ALU.mult), reads=[b_gst], writes=[b_gst])
        S.op('dve', lambda e: e.tensor_tensor(out=gst[:, 3, :], in0=gst[:, 2, :], in1=gst[:, 2, :], op=ALU.mult), reads=[b_gst], writes=[b_gst])
        S.op('dve', lambda e: e.scalar_tensor_tensor(out=gst[:, 4, :], in0=gst[:, 1, :], scalar=1.0 / P, in1=gst[:, 3, :], op0=ALU.mult, op1=ALU.subtract),
             reads=[b_gst], writes=[b_gst])
        S.op('act', lambda e: e.activation(out=gst[:, 5, :], in_=gst[:, 4, :], func=AF.Sqrt, bias=eps_t[:, 0:1], scale=1.0), reads=[b_gst, b_eps], writes=[b_gst])
        S.op('dve', lambda e: e.reciprocal(out=gst[:, 3, :], in_=gst[:, 5, :]), reads=[b_gst], writes=[b_gst])
        y3 = yf[:].rearrange("p (h d) -> p h d", d=P)
        S.op('dve', lambda e: e.tensor_tensor(out=y3, in0=y3, in1=gst[:, 2, :].unsqueeze(2).to_broadcast([P, H, P]), op=ALU.subtract),
             reads=[b_yf, b_gst], writes=[b_yf])
        S.op('dve', lambda e: e.tensor_tensor(out=y3, in0=y3, in1=gst[:, 3, :].unsqueeze(2).to_broadcast([P, H, P]), op=ALU.mult),
             reads=[b_yf, b_gst], writes=[b_yf])
        S.op('pool', lambda e: e.tensor_tensor(out=ret[:], in0=yf[:], in1=rg[:], op=ALU.mult), reads=[b_yf, b_rg], writes=[b_ret])
        transpose8(ret, b_ret, retT, b_retT)
        dump(3, yf[:], [b_yf, b_retT])
        for half in range(2):
            for blk, kT_ in ((0, skT[prv]), (1, skT[cur])):
                for hl in range(4):
                    h = half * 4 + hl
                    S.op('pe', lambda e, blk=blk, hl=hl, h=h, kT_=kT_: e.matmul(
                        out=psA[:, blk * 512 + hl * P: blk * 512 + (hl + 1) * P], lhsT=kT_[:, h, :],
                        rhs=sqT[:, h, :], start=True, stop=True),
                        reads=[b_skT[prv], b_skT[cur], b_sqT], writes=[bA])
            S.op('act', lambda e: e.activation(out=e_s[:], in_=psA[:], func=AF.Exp, scale=0.125), reads=[bA], writes=[b_es])
            S.op('act', lambda e: e.activation(out=sp_s[:], in_=e_s[:], func=AF.Ln, bias=one_t[:, 0:1], scale=1.0), reads=[b_es, b_one], writes=[b_sp])
            S.op('dve', lambda e: e.tensor_tensor(
                out=spm[:].rearrange("p (b h t) -> p b h t", b=2, h=4), in0=sp_s[:].rearrange("p (b h t) -> p b h t", b=2, h=4),
                in1=mstay[:].rearrange("p (b h t) -> p b h t", b=2, h=4), op=ALU.mult), reads=[b_sp, b_mstay], writes=[b_spm])
            S.op('pe', lambda e: e.matmul(out=psB[:, 0:512], lhsT=tri[:], rhs=spm[:, 0:512], start=True, stop=False), reads=[b_tri, b_spm], writes=[bB])
            S.op('pe', lambda e: e.matmul(out=psB[:, 0:512], lhsT=ones[:], rhs=spm[:, 512:1024], start=False, stop=True), reads=[b_ones, b_spm], writes=[bB])
            S.op('pe', lambda e: e.matmul(out=psB[:, 512:1024], lhsT=tri[:], rhs=spm[:, 512:1024], start=True, stop=False), reads=[b_tri, b_spm], writes=[bB])
            S.op('pe', lambda e: e.matmul(out=psB[:, 512:1024], lhsT=ident[:], rhs=mpos[:], start=False, stop=True), reads=[b_ident, b_mpos], writes=[bB])
            S.op('dve', lambda e: e.scalar_tensor_tensor(out=u_s[:], in0=psA[:], scalar=0.125, in1=sp_s[:], op0=ALU.mult, op1=ALU.subtract),
                 reads=[bA, b_sp], writes=[b_us])
            S.op('dve', lambda e: e.tensor_tensor(out=u_s[:], in0=u_s[:], in1=psB[:], op=ALU.subtract), reads=[b_us, bB], writes=[b_us])
            S.op('act', lambda e: e.activation(out=w_s[:], in_=u_s[:], func=AF.Exp), reads=[b_us], writes=[b_ws])
            for hl in range(4):
                h = half * 4 + hl
                po = (h % 2) * 64
                for blk, svb, bsv in ((0, sv[prv], b_sv[prv]), (1, sv[cur], b_sv[cur])):
                    S.op('pe', lambda e, h=h, hl=hl, po=po, blk=blk, svb=svb: e.matmul(
                        out=psS[po:po + 64, (h // 2) * P:(h // 2 + 1) * P], lhsT=svb[:, h * 64:(h + 1) * 64],
                        rhs=w_s[:, blk * 512 + hl * P: blk * 512 + (hl + 1) * P], start=(blk == 0), stop=(blk == 1), skip_group_check=True),
                        reads=[bsv, b_ws], writes=[bS])
        S.op('act', lambda e: e.activation(out=sbT[:].rearrange("p c t -> p (c t)"), in_=psS[:], func=AF.Copy), reads=[bS], writes=[b_sbT])
        if STAGE == 4:
            S.op('act', lambda e: e.activation(out=m2[:, 0:512], in_=psS[:], func=AF.Copy), reads=[bS], writes=[b_m2])
            dump(4, m2[:, 0:512], [b_m2], 512)
        for hh in range(2):
            for c in range(8):
                S.op('pe', lambda e, c=c, hh=hh: e.matmul(out=psA[:, hh * 512:(hh + 1) * 512], lhsT=retT[:, c, :], rhs=wbra[:, c, hh * 512:(hh + 1) * 512],
                                                          start=(c == 0), stop=(c == 7)), reads=[b_retT, b_wbra], writes=[bA])
            for c in range(4):
                S.op('pe', lambda e, c=c, hh=hh: e.matmul(out=psB[:, hh * 512:(hh + 1) * 512], lhsT=sbT[:, c, :], rhs=wbrb[:, c, hh * 512:(hh + 1) * 512],
                                                          start=(c == 0), stop=(c == 3)), reads=[b_sbT, b_wbrb], writes=[bB])
        for gi, (gt, bg) in enumerate(((siga, b_siga), (sigb, b_sigb))):
            for hh in range(2):
                wb, bw = stream_w(4608 + gi * 1024 + hh * 512)
                pst, bp = (psC, bC) if hh == 0 else (psD, bD)
                proj512(wb, bw, pst[:], bp)
                S.op('act', lambda e, gt=gt, hh=hh, pst=pst: e.activation(out=gt[:, hh * 512:(hh + 1) * 512], in_=pst[:], func=AF.Sigmoid),
                     reads=[bp], writes=[bg])
        S.op('dve', lambda e: e.tensor_tensor(out=m1[:], in0=psA[:], in1=siga[:], op=ALU.mult), reads=[bA, b_siga], writes=[b_m1])
        S.op('dve', lambda e: e.tensor_tensor(out=m2[:], in0=psB[:], in1=sigb[:], op=ALU.mult), reads=[bB, b_sigb], writes=[b_m2])
        S.op('pool', lambda e: e.tensor_tensor(out=mixed[:], in0=m1[:], in1=m2[:], op=ALU.add), reads=[b_m1, b_m2], writes=[b_mixed])
        transpose8(mixed, b_mixed, mixT, b_mixT)
        for hh in range(2):
            for c in range(8):
                S.op('pe', lambda e, c=c, hh=hh: e.matmul(out=psA[:, hh * 512:(hh + 1) * 512], lhsT=mixT[:, c, :], rhs=wout[:, c, hh * 512:(hh + 1) * 512],
                                                          start=(c == 0), stop=(c == 7)), reads=[b_mixT, b_wout], writes=[bA])
        S.op('dve', lambda e: e.tensor_tensor(out=x1[:], in0=psA[:], in1=xres[:], op=ALU.add), reads=[bA, b_xres], writes=[b_x1])
        dump(5, x1[:], [b_x1])
        rmsnorm_T(x1[:], b_x1, gffn, b_gffn, keep_f32=hn[:], b_keep=b_hn)
        for g4 in range(4):
            wqb, b_wq = stream_w(g4 * 512, w_q)
            for gl in range(4):
                g = g4 * 4 + gl
                pst = psA[:, gl * P:(gl + 1) * P] if g4 % 2 == 0 else psB[:, gl * P:(gl + 1) * P]
                bp = bA if g4 % 2 == 0 else bB
                for c in range(8):
                    S.op('pe', lambda e, c=c, gl=gl, pst=pst, wqb=wqb: e.matmul(out=pst, lhsT=wqb[:, c, gl * P:(gl + 1) * P], rhs=xnT[:, c, :],
                                                                     start=(c == 0), stop=(c == 7)), reads=[b_wq, b_xnT], writes=[bp])
            src = psA if g4 % 2 == 0 else psB
            bp = bA if g4 % 2 == 0 else bB
            S.op('act', lambda e, g4=g4, src=src: e.activation(out=qT[:, g4 * 4:(g4 + 1) * 4, :].rearrange("p g t -> p (g t)"), in_=src[:, 0:512], func=AF.Copy),
                 reads=[bp], writes=[b_qT])
        for g4 in range(4):
            pst, bp = (psA, bA) if g4 % 2 == 0 else (psB, bB)
            for gl in range(4):
                g = g4 * 4 + gl
                kk, bk = (k1, b_k1) if g % 2 == 0 else (k2, b_k2)
                S.op('pe', lambda e, g=g, gl=gl, pst=pst, kk=kk: e.matmul(out=pst[:, gl * P:(gl + 1) * P], lhsT=qT[:, g, :], rhs=kk[:], start=True, stop=True),
                     reads=[b_qT, bk], writes=[bp])
            S.op('act', lambda e, g4=g4, pst=pst: e.activation(out=sc[:, g4 * 4:(g4 + 1) * 4, :].rearrange("p g n -> p (g n)"), in_=pst[:, 0:512], func=AF.Copy),
                 reads=[bp], writes=[b_sc])
        cap = []
        if n + 1 < NT:
            S.cap = cap
            m_head(n + 1)
            S.cap = None
        S.after_op = lambda: S.replay(cap, 1)
        bg_tv = [S.buf("tv%d" % g) for g in range(16)]; bg_ti = [S.buf("ti%d" % g) for g in range(16)]; bg_s2 = [S.buf("s2%d" % g) for g in range(16)]
        for g in range(16):
            S.op('dve', lambda e, g=g: e.max(out=tv[:, g, 0:8], in_=sc[:, g, :]), reads=[b_sc], writes=[bg_tv[g]])
        for g in range(16):
            S.op('dve', lambda e, g=g: e.match_replace(out=sc2[:, g, :], in_to_replace=tv[:, g, 0:8], in_values=sc[:, g, :], imm_value=NEG),
                 reads=[b_sc, bg_tv[g]], writes=[bg_s2[g]])
        for g in range(16):
            S.op('dve', lambda e, g=g: e.max_index(out=ti[:, g, 0:8], in_max=tv[:, g, 0:8], in_values=sc[:, g, :]), reads=[b_sc, bg_tv[g]], writes=[bg_ti[g]])
        for g in range(16):
            S.op('dve', lambda e, g=g: e.max(out=tv[:, g, 8:16], in_=sc2[:, g, :]), reads=[bg_s2[g]], writes=[bg_tv[g]])
        for g in range(16):
            S.op('dve', lambda e, g=g: e.max_index(out=ti[:, g, 8:16], in_max=tv[:, g, 8:16], in_values=sc2[:, g, :]), reads=[bg_s2[g], bg_tv[g]], writes=[bg_ti[g]])
        b_tv.w = None; b_ti.w = None
        S.op('dve', lambda e: e.tensor_copy(out=tif[:], in_=ti[:]), reads=bg_ti + bg_tv + bg_s2 + [b_sc2], writes=[b_tif, b_tv, b_ti, b_sc2])
        tv4 = tv[:].rearrange("p (h s) k -> p h s k", s=2)
        tif4 = tif[:].rearrange("p (h s) k -> p h s k", s=2)
        S.op('dve', lambda e: e.tensor_tensor(out=cand.rearrange("p h (a b) -> p h a b", a=16),
                                              in0=tv4[:, :, 0, :].unsqueeze(3).to_broadcast([P, H, 16, 16]),
                                              in1=tv4[:, :, 1, :].unsqueeze(2).to_broadcast([P, H, 16, 16]), op=ALU.add),
             reads=[b_tv], writes=[b_cand])
        bh_ts = [S.buf("ts%d" % h) for h in range(H)]; bh_tp = [S.buf("tp%d" % h) for h in range(H)]; bh_c2 = [S.buf("c2%d" % h) for h in range(H)]
        for h in range(H):
            S.op('dve', lambda e, h=h: e.max(out=tsv[:, h, 0:8], in_=cand[:, h, :]), reads=[b_cand], writes=[bh_ts[h]])
        for h in range(H):
            S.op('dve', lambda e, h=h: e.match_replace(out=cand2[:, h, :], in_to_replace=tsv[:, h, 0:8], in_values=cand[:, h, :], imm_value=NEG),
                 reads=[b_cand, bh_ts[h], b_cand2], writes=[bh_c2[h]])
        for h in range(H):
            S.op('dve', lambda e, h=h: e.max_index(out=tpos[:, h, 0:8], in_max=tsv[:, h, 0:8], in_values=cand[:, h, :]), reads=[b_cand, bh_ts[h]], writes=[bh_tp[h]])
        for h in range(H):
            S.op('dve', lambda e, h=h: e.max(out=tsv[:, h, 8:16], in_=cand2[:, h, :]), reads=[bh_c2[h]], writes=[bh_ts[h]])
        for h in range(H):
            S.op('dve', lambda e, h=h: e.max_index(out=tpos[:, h, 8:16], in_max=tsv[:, h, 8:16], in_values=cand2[:, h, :]), reads=[bh_c2[h], bh_ts[h]], writes=[bh_tp[h]])
        b_tsv.w = None; b_tpos.w = None
        S.op('dve', lambda e: e.tensor_copy(out=tposf[:], in_=tpos[:]), reads=bh_tp + bh_ts + bh_c2, writes=[b_tposf, b_tsv, b_tpos, b_cand2])
        S.op('dve', lambda e: e.tensor_tensor(out=oh, in0=tposf[:].unsqueeze(3).to_broadcast([P, H, 16, 16]),
                                              in1=thr16[:].unsqueeze(1).unsqueeze(1).to_broadcast([P, H, 16, 16]), op=ALU.is_ge),
             reads=[b_tposf, b_iota], writes=[b_oh])
        S.op('dve', lambda e: e.tensor_reduce(out=ta[:], in_=oh, axis=AX.X, op=ALU.add), reads=[b_oh], writes=[b_ta])
        S.op('dve', lambda e: e.tensor_scalar(out=ta[:], in0=ta[:], scalar1=-1.0, scalar2=None, op0=ALU.add), reads=[b_ta], writes=[b_ta])
        S.op('dve', lambda e: e.scalar_tensor_tensor(out=tb[:], in0=ta[:], scalar=-16.0, in1=tposf[:], op0=ALU.mult, op1=ALU.add),
             reads=[b_ta, b_tposf], writes=[b_tb])
        io_b = iota16[:].unsqueeze(1).unsqueeze(1).to_broadcast([P, H, 16, 16])
        for sel, half, dst, bd in ((ta, 0, idx1, b_idx1), (tb, 1, idx2, b_idx2)):
            bsel = b_ta if half == 0 else b_tb
            S.op('dve', lambda e, sel=sel: e.tensor_tensor(out=oh, in0=sel[:].unsqueeze(3).to_broadcast([P, H, 16, 16]), in1=io_b, op=ALU.is_equal),
                 reads=[bsel, b_iota], writes=[b_oh])
            S.op('dve', lambda e, half=half: e.tensor_tensor(out=oh, in0=oh, in1=tif4[:, :, half, :].unsqueeze(2).to_broadcast([P, H, 16, 16]), op=ALU.mult),
                 reads=[b_oh, b_tif], writes=[b_oh])
            S.op('dve', lambda e, dst=dst: e.tensor_reduce(out=dst[:], in_=oh, axis=AX.X, op=ALU.add), reads=[b_oh], writes=[bd])
        S.op('dve', lambda e: e.scalar_tensor_tensor(out=idx1[:], in0=idx1[:], scalar=128.0, in1=idx2[:], op0=ALU.mult, op1=ALU.add),
             reads=[b_idx1, b_idx2], writes=[b_idx1])
        S.op('dve', lambda e: e.tensor_copy(out=eidx[:], in_=idx1[:].rearrange("p h k -> p (h k)")), reads=[b_idx1], writes=[b_eidx])
        S.op('dve', lambda e: e.tensor_tensor(out=gw[:], in0=tsv[:], in1=tsv[:, :, 0:1].to_broadcast([P, H, 16]), op=ALU.subtract), reads=[b_tsv], writes=[b_gw])
        S.op('act', lambda e: e.activation(out=gw[:], in_=gw[:], func=AF.Exp), reads=[b_gw], writes=[b_gw])
        S.op('dve', lambda e: e.tensor_reduce(out=gs[:, 0, :], in_=gw[:], axis=AX.X, op=ALU.add), reads=[b_gw], writes=[b_gs])
        S.op('dve', lambda e: e.reciprocal(out=gs[:, 1, :], in_=gs[:, 0, :]), reads=[b_gs], writes=[b_gs])
        S.op('dve', lambda e: e.tensor_tensor(out=gw[:], in0=gw[:], in1=gs[:, 1, :].unsqueeze(2).to_broadcast([P, H, 16]), op=ALU.mult), reads=[b_gw, b_gs], writes=[b_gw])
        if STAGE == 6:
            S.op('dve', lambda e: e.tensor_copy(out=m2[:, 0:128], in_=idx1[:].rearrange("p h k -> p (h k)")), reads=[b_idx1], writes=[b_m2])
            S.op('dve', lambda e: e.tensor_copy(out=m2[:, 128:256], in_=gw[:].rearrange("p h k -> p (h k)")), reads=[b_gw], writes=[b_m2])
            dump(6, m2[:, 0:256], [b_m2], 256)
        S.after_op = None
        GS = 2
        NGRP = 128 // GS
        gwf = gw[:].rearrange("p h k -> p (h k)")

        def emit_gather(g):
            for k in range(GS):
                j = g * GS + k
                gb_, bgb = gbuf[j % NG], b_gbuf[j % NG]
                S.dma('pool', lambda e, j=j, gb_=gb_: e.indirect_dma_start(out=gb_[:], out_offset=None, in_=uv_bf,
                                                                          in_offset=bass.IndirectOffsetOnAxis(ap=eidx[:, j:j + 1], axis=0)),
                      gsem[j % NG], reads=[b_eidx, b_ubf], writes=[bgb])

        def emit_dots(g):
            for k in range(GS):
                j = g * GS + k
                gb_, bgb = gbuf[j % NG], b_gbuf[j % NG]
                S.op('dve', lambda e, j=j, gb_=gb_: e.scalar_tensor_tensor(out=junk[:], in0=gb_[:, 0:D], scalar=1.0, in1=hn[:],
                                                                          op0=ALU.mult, op1=ALU.mult, accum_out=hv[:, j:j + 1]),
                     reads=[bgb, b_hn], writes=[b_junk, b_hv])

        def emit_pre(g):
            c = slice(g * GS, (g + 1) * GS)
            S.op('act', lambda e: e.activation(out=ga_[:, 0, c], in_=hv[:, c], func=AF.Square), reads=[b_hv], writes=[b_ga])
            S.op('act', lambda e: e.activation(out=ga_[:, 1, c], in_=ga_[:, 0, c], func=AF.Identity, scale=0.0713548162726, bias=gk_t[:, 0:1]),
                 reads=[b_ga, b_gk], writes=[b_ga])
            for k in range(GS):
                j = g * GS + k
                S.op('act', lambda e, j=j: e.activation(out=ga_[:, 3, j:j + 1], in_=ga_[:, 1, j:j + 1], func=AF.Sigmoid, scale=hv[:, j:j + 1]),
                     reads=[b_ga, b_hv], writes=[b_ga2])
            S.op('dve', lambda e: e.tensor_tensor(out=ga_[:, 4, c], in0=hv[:, c], in1=gwf[:, c], op=ALU.mult), reads=[b_hv, b_gw], writes=[b_ga3])

        def emit_post(g):
            for k in range(GS):
                j = g * GS + k
                S.op('act', lambda e, j=j: e.activation(out=aw[:, j:j + 1], in_=ga_[:, 3, j:j + 1], func=AF.Copy, scale=ga_[:, 4, j:j + 1]),
                     reads=[b_ga2, b_ga3], writes=[b_aw])

        def emit_axpy(g):
            for k in range(GS):
                j = g * GS + k
                gb_, bgb = gbuf[j % NG], b_gbuf[j % NG]
                dgj, bdg = dg[j % 4], b_dg[j % 4]
                S.op('act', lambda e, j=j, dgj=dgj: e.activation(out=dgj[:], in_=identF[:], func=AF.Copy, scale=aw[:, j:j + 1]),
                     reads=[b_identF, b_aw], writes=[bdg])
                for hh in range(2):
                    S.op('pe', lambda e, j=j, hh=hh, dgj=dgj, gb_=gb_: e.matmul(out=psA[:, hh * 512:(hh + 1) * 512], lhsT=dgj[:],
                                                                            rhs=gb_[:, D + hh * 512:D + (hh + 1) * 512],
                                                                            start=(j == 0), stop=(j == 127)), reads=[bdg, bgb], writes=[bA])

        for g0 in range(3):
            emit_gather(g0)
        for st_ in range(NGRP + 1):
            S.replay(cap, 3)
            if 0 <= st_ - 1 < NGRP:
                emit_pre(st_ - 1)
            if st_ < NGRP:
                emit_dots(st_)
            if 0 <= st_ - 1 < NGRP:
                emit_post(st_ - 1)
                emit_axpy(st_ - 1)
            if st_ + 3 < NGRP:
                emit_gather(st_ + 3)
        S.replay(cap)
        S.op('dve', lambda e: e.tensor_tensor(out=acc[:], in0=psA[:], in1=x1[:], op=ALU.add), reads=[bA, b_x1], writes=[b_acc])
        S.dma('sp', lambda e, n=n: e.dma_start(out=y_out[n * P:(n + 1) * P, :], in_=acc[:]), b_yout, reads=[b_acc], writes=[b_yout])

    S.wait_all('sp', [b_yout])
    es.close()
    return nc, None


def _consts():
    hs = np.arange(H, dtype=np.float64)
    gam = 1.0 - 2.0 ** (-5.0 - hs)
    i = np.arange(P, dtype=np.float64)
    c = {}
    c["c_ident"] = np.eye(P, dtype=np.float32)
    c["c_tri"] = (i[:, None] > i[None, :]).astype(np.float32)
    c["c_ones"] = np.ones((P, P), np.float32)
    mp = 1.0e4 * (i[:, None] >= i[None, :]).astype(np.float32)
    c["c_mpos"] = np.tile(mp, (1, 4)).astype(np.float32)
    ms = np.ones((P, 2, 4, P), np.float32)
    ms[:, 1, :, :] = (i[:, None] < i[None, :]).astype(np.float32)[:, None, :]
    c["c_mstay"] = ms.reshape(P, 1024)
    mk = np.zeros((P, H, P), np.float64)
    for h in range(H):
        mk[:, h, :] = (i[None, :] >= i[:, None]) * gam[h] ** (-128.0)
    c["c_maskT"] = mk.reshape(P, 1024).astype(np.float32)
    c["c_qdec"] = (gam[None, :] ** (i[:, None] + 1.0)).astype(np.float32)
    c["c_kdec"] = (0.125 * gam[None, :] ** (127.0 - i[:, None])).astype(np.float32)
    cd = np.zeros((64, D), np.float64)
    for h in range(H):
        cd[:, h * P:(h + 1) * P] = gam[h] ** 128.0
    c["c_cdec"] = cd.astype(np.float32)
    return c, gam


def _rope_tabs(pos):
    half = 32
    freqs = (np.float32(10000.0) ** (-np.arange(half, dtype=np.float32) / np.float32(half))).astype(np.float32)
    ang = (pos.astype(np.float32)[:, :, None] * freqs[None, None, :]).astype(np.float32).astype(np.float64)
    return np.cos(ang).astype(np.float32), np.sin(ang).astype(np.float32)


_CACHE = {}


def kernel(x, norm_attn, w_in, ret_q_norm, ret_k_norm, ret_group_norm, sb_q_norm, sb_k_norm,
           w_branch_ret, w_branch_sb, w_out, norm_ffn, peer_w_q, peer_sub_keys_1,
           peer_sub_keys_2, peer_u, peer_v):
    f = np.float32
    x = np.asarray(x, f)
    B, SEQ, _ = x.shape
    assert B == 1
    NT = SEQ // (NCORES * P)
    NPRE = NT * (NCORES - 1) if FORCE_NPRE is None else FORCE_NPRE
    x2 = x[0]
    key = (NT, NPRE)
    if key not in _CACHE:
        try:
            _CACHE[key] = build_program(NT, NPRE)
        except _Stop:
            _H['es'].close()
            _CACHE[key] = (_H['nc'], None)
    nc, _es = _CACHE[key]
    cst, gam = _consts()
    rep = lambda v, n: np.ascontiguousarray(np.broadcast_to(np.tile(np.asarray(v, f).reshape(-1), n)[None, :], (P, np.asarray(v).size * n)))
    shared = dict(cst)
    shared.update({
        "g_attn": rep(norm_attn[0], 1), "g_ffn": rep(norm_ffn[0], 1), "g_gn": rep(ret_group_norm[0], 1),
        "g_rq": rep(ret_q_norm[0], 8), "g_rk": rep(ret_k_norm[0], 8), "g_sq": rep(sb_q_norm[0], 8), "g_sk": rep(sb_k_norm[0], 8),
        "w_in": np.ascontiguousarray(w_in[0], f), "w_bra": np.ascontiguousarray(w_branch_ret[0], f),
        "w_brb": np.ascontiguousarray(w_branch_sb[0], f), "w_out": np.ascontiguousarray(w_out[0], f),
        "w_q": np.ascontiguousarray(peer_w_q[0], f),
        "k1T": np.ascontiguousarray(np.asarray(peer_sub_keys_1[0], f).T), "k2T": np.ascontiguousarray(np.asarray(peer_sub_keys_2[0], f).T),
        "u_tab": np.ascontiguousarray(peer_u[0], f), "v_tab": np.ascontiguousarray(peer_v[0], f),
    })
    in_maps = []
    pp = np.arange(P, dtype=np.float64)
    for c in range(NCORES):
        t0 = c * NT
        m = dict(shared)
        m["x_own"] = np.ascontiguousarray(x2[t0 * P:(t0 + NT) * P])
        m["x_halo"] = np.ascontiguousarray(x2[(t0 - 1) * P:t0 * P]) if c > 0 else np.zeros((P, D), f)
        npre = max(NPRE, 1)
        xp = np.zeros((npre * P, D), f)
        gt = np.arange(npre) + (t0 - NPRE)
        nvalid = min(t0, NPRE)
        if nvalid > 0:
            xp[(NPRE - nvalid) * P:NPRE * P] = x2[(t0 - nvalid) * P:t0 * P]
        m["x_pre"] = xp
        pos_own = (np.arange(NT)[None, :] + t0) * P + pp[:, None]
        m["cos_own"], m["sin_own"] = _rope_tabs(pos_own)
        pos_pre = np.maximum(gt, 0)[None, :] * P + pp[:, None]
        m["cos_pre"], m["sin_pre"] = _rope_tabs(pos_pre)
        ks = np.zeros((P, npre, H), np.float64)
        for mm in range(npre):
            ks[:, mm, :] = 0.125 * gam[None, :] ** (127.0 - pp[:, None]) * gam[None, :] ** (128.0 * (NPRE - 1 - mm))
        m["ksc_pre"] = ks.astype(f)
        in_maps.append(m)
    res = run_bass_kernel_spmd(nc, in_maps, core_ids=list(range(NCORES)), **RUN_KW)
    _H['res'] = res
    out = np.concatenate([np.asarray(r["y_out"], f) for r in res.results], axis=0)
    return out.reshape(1, SEQ, D)
```
